# Optimizing a Trainium2 kernel written in Bass

```python
import math
import jax, jax.numpy as jnp
from jax import lax
import numpy as np


D_MODEL = 1024
BATCH = 8
SEQ = 2048
DEPTH = 4

N_MEM = 256
EPS = 1e-6

A_WIDTH = 512
CONV_WIDTH = 3
POOL_WINDOWS = (2, 4, 8, 16)
POOL_GROUP = 128
B_WIDTH = 512
C_HEADS = 8
C_LATENT = 128
C_HEAD_DIM = 64
IDX_HEADS = 8
IDX_DIM = 64
DSA_TOPK_MAX = 256
Q_BLOCK = 128
REL_BUCKETS = 32
REL_MAX_DIST = 128
D_HEADS = 4
D_KEY = 128
D_VAL = 128
CHUNK = 64
X_HEADS = 4
X_HEAD_DIM = 256
N_KEYS = 128
N_EXPERTS = 16384
PEER_HEADS = 8
PEER_QDIM = 256
PEER_HALF = 128
PEER_TOPK = 16
PEER_BLOCK = 128

EVEN_SPLITS = (512, 512, 512, 512)
ODD_SPLITS = (1024, 128, 512, 64, 8, 512, 512, 512, 512)
EVEN_IN = 2048
ODD_IN = 3784
MIX_WIDTH = 1024

kernel_name = 'hybrid_conv_pool_dsa_hgrn2_peer_trunk'


def _split(a, sizes):
    offs = np.cumsum(sizes)[:-1].tolist()
    return jnp.split(a, offs, axis=-1)


def rmsnorm(x, g):
    xf = x.astype(jnp.float32)
    y = xf * lax.rsqrt(jnp.mean(xf * xf, axis=-1, keepdims=True) + EPS)
    return (y * g.astype(jnp.float32)).astype(x.dtype)


def softmax32(logits, dtype):
    return jax.nn.softmax(logits.astype(jnp.float32), axis=-1).astype(dtype)


def t5_bucket(dist):
    max_exact = REL_BUCKETS // 2
    d = jnp.maximum(dist, 0)
    log_ratio = jnp.log(jnp.maximum(d, 1).astype(jnp.float32) / max_exact) / math.log(REL_MAX_DIST / max_exact)
    large = max_exact + (log_ratio * (REL_BUCKETS - max_exact)).astype(jnp.int32)
    large = jnp.minimum(large, REL_BUCKETS - 1)
    return jnp.where(d < max_exact, d, large)


def short_conv_pool_mixer(h, w_in, conv_w, pool_w, pool_scale, w_out):
    B_, S_, _ = h.shape
    u, gate_c, gate_b, pv = _split(h @ w_in, EVEN_SPLITS)
    z = lax.conv_general_dilated(gate_c * u, conv_w, window_strides=(1,), padding=[(CONV_WIDTH - 1, 0)],
                                 dimension_numbers=('NWC', 'WIO', 'NWC'), feature_group_count=A_WIDTH)
    y_a = gate_b * z
    cs = jnp.cumsum(pv.astype(jnp.float32), axis=1)
    cs = jnp.concatenate([jnp.zeros((B_, 1, B_WIDTH), jnp.float32), cs], axis=1)
    pos = jnp.arange(S_)
    groups = []
    for gi, w in enumerate(POOL_WINDOWS):
        sl = slice(gi * POOL_GROUP, (gi + 1) * POOL_GROUP)
        lo = jnp.maximum(pos + 1 - w, 0)
        cnt = jnp.minimum(pos + 1, w).astype(jnp.float32)
        mean = (cs[:, 1:, sl] - cs[:, lo, sl]) / cnt[None, :, None]
        groups.append(mean.astype(h.dtype) - pv[..., sl])
    pooled = jnp.stack(groups, axis=2)
    y_b = jnp.einsum('bsgc,gcd->bsgd', pooled, pool_w).reshape(B_, S_, B_WIDTH) * pool_scale
    return jnp.concatenate([y_a, y_b], axis=-1) @ w_out


def dsa_attention(q_lat, c_kv, iq, ik, iw, rel_bias):
    B_, S_ = c_kv.shape[:2]
    topk = min(DSA_TOPK_MAX, S_ // 4)
    n_blocks = S_ // Q_BLOCK
    scale = C_LATENT ** -0.5
    idx_scale = IDX_DIM ** -0.5
    key_pos = jnp.arange(S_)

    def block(start):
        qb = lax.dynamic_slice_in_dim(q_lat, start, Q_BLOCK, axis=1)
        iqb = lax.dynamic_slice_in_dim(iq, start, Q_BLOCK, axis=1)
        iwb = lax.dynamic_slice_in_dim(iw, start, Q_BLOCK, axis=1)
        t = start + jnp.arange(Q_BLOCK)
        rel = jax.nn.relu(jnp.einsum('bthd,bsd->bths', iqb, ik) * idx_scale)
        score = jnp.einsum('bths,bth->bts', rel, iwb).astype(jnp.float32)
        causal = key_pos[None, :] <= t[:, None]
        score = jnp.where(causal[None], score, -jnp.inf)
        _, sel = lax.top_k(score, topk)
        c_sel = jax.vmap(lambda c, i: c[i])(c_kv, sel)
        valid = sel <= t[None, :, None]
        bias = rel_bias[t5_bucket(t[None, :, None] - sel)]
        logits = (jnp.einsum('bthr,btkr->bhtk', qb, c_sel).astype(jnp.float32) * scale
                  + jnp.moveaxis(bias, -1, 1).astype(jnp.float32))
        logits = jnp.where(valid[:, None], logits, -jnp.inf)
        p = softmax32(logits, c_kv.dtype)
        return jnp.einsum('bhtk,btkr->bthr', p, c_sel)

    out = lax.map(block, jnp.arange(n_blocks) * Q_BLOCK)
    return jnp.moveaxis(out, 0, 1).reshape(B_, S_, C_HEADS, C_LATENT)


def hgrn2(q, f_logit, inp, gate, lb, norm_g):
    B_, S_, _ = q.shape
    nc = S_ // CHUNK
    f = lb + (1.0 - lb) * jax.nn.sigmoid(f_logit.astype(jnp.float32))
    log_f = jnp.log(f)
    k = 1.0 - f

    def to_chunks(a, d):
        return a.astype(jnp.float32).reshape(B_, nc, CHUNK, D_HEADS, d).transpose(1, 0, 3, 2, 4)

    qc, kc, lfc = to_chunks(q, D_KEY), to_chunks(k, D_KEY), to_chunks(log_f, D_KEY)
    ic = to_chunks(inp, D_VAL)
    causal = jnp.tril(jnp.ones((CHUNK, CHUNK), bool))

    def step(state, xs):
        qj, kj, ij, lfj = xs
        A = jnp.cumsum(lfj, axis=2)
        o_inter = jnp.einsum('bhtk,bhkv->bhtv', qj * jnp.exp(A), state)
        diff = A[:, :, :, None, :] - A[:, :, None, :, :]
        decay = jnp.exp(jnp.where(causal[:, :, None], diff, -jnp.inf))
        scores = jnp.einsum('bhtk,bhsk,bhtsk->bhts', qj, kj, decay)
        o_intra = jnp.einsum('bhts,bhsv->bhtv', scores, ij)
        A_end = A[:, :, -1]
        new_state = (jnp.exp(A_end)[..., None] * state
                     + jnp.einsum('bhsk,bhsv->bhkv', kj * jnp.exp(A_end[:, :, None] - A), ij))
        return new_state, o_inter + o_intra

    state0 = jnp.zeros((B_, D_HEADS, D_KEY, D_VAL), jnp.float32)
    _, o = lax.scan(step, state0, (qc, kc, ic, lfc))
    o = o.transpose(1, 0, 3, 2, 4).reshape(B_, S_, D_HEADS, D_VAL)
    o = o * lax.rsqrt(jnp.mean(o * o, axis=-1, keepdims=True) + EPS) * norm_g.astype(jnp.float32).reshape(D_HEADS, D_VAL)
    return (o.reshape(B_, S_, D_HEADS * D_VAL) * jax.nn.silu(gate.astype(jnp.float32))).astype(q.dtype)


def sparse_attn_hgrn_mixer(h, w_in, kv_norm, w_uv, hg_norm, w_out, rel_bias, lb):
    B_, S_, _ = h.shape
    q_lat, c_kv, iq, ik, iw, hq, hf, hi, hg = _split(h @ w_in, ODD_SPLITS)
    c_kv = rmsnorm(c_kv, kv_norm)
    lat = dsa_attention(q_lat.reshape(B_, S_, C_HEADS, C_LATENT), c_kv,
                        iq.reshape(B_, S_, IDX_HEADS, IDX_DIM), ik, iw * IDX_HEADS ** -0.5, rel_bias)
    y_c = jnp.einsum('bshr,hrd->bshd', lat, w_uv).reshape(B_, S_, C_HEADS * C_HEAD_DIM)
    y_d = hgrn2(hq, hf, hi, hg, lb, hg_norm)
    return jnp.concatenate([y_c, y_d], axis=-1) @ w_out


def memory_cross_attn(h, mem_n, wq, wkv, wo):
    B_, S_, _ = h.shape
    q = (h @ wq).reshape(B_, S_, X_HEADS, X_HEAD_DIM)
    k, v = jnp.split(mem_n @ wkv, 2, axis=-1)
    k = k.reshape(B_, -1, X_HEADS, X_HEAD_DIM)
    v = v.reshape(B_, -1, X_HEADS, X_HEAD_DIM)
    logits = jnp.einsum('bshd,bmhd->bhsm', q, k).astype(jnp.float32) * X_HEAD_DIM ** -0.5
    p = softmax32(logits, h.dtype)
    o = jnp.einsum('bhsm,bmhd->bshd', p, v).reshape(B_, S_, D_MODEL)
    return o @ wo


def peer_ffn(h, wq, sub_keys, u, v):
    B_, S_, D_ = h.shape
    T = B_ * S_
    ht = h.reshape(T, D_)

    def block(start):
        xb = lax.dynamic_slice_in_dim(ht, start, PEER_BLOCK, axis=0)
        q = (xb @ wq).reshape(PEER_BLOCK, PEER_HEADS, 2, PEER_HALF)
        s = jnp.einsum('thpd,hpnd->thpn', q, sub_keys).astype(jnp.float32)
        s1, i1 = lax.top_k(s[:, :, 0], PEER_TOPK)
        s2, i2 = lax.top_k(s[:, :, 1], PEER_TOPK)
        cand = (s1[..., :, None] + s2[..., None, :]).reshape(PEER_BLOCK, PEER_HEADS, PEER_TOPK * PEER_TOPK)
        cand_id = (i1[..., :, None] * N_KEYS + i2[..., None, :]).reshape(PEER_BLOCK, PEER_HEADS, PEER_TOPK * PEER_TOPK)
        top, pos = lax.top_k(cand, PEER_TOPK)
        eid = jnp.take_along_axis(cand_id, pos, axis=-1).reshape(PEER_BLOCK, PEER_HEADS * PEER_TOPK)
        g = jax.nn.softmax(top, axis=-1).reshape(PEER_BLOCK, PEER_HEADS * PEER_TOPK).astype(xb.dtype)
        act = jax.nn.gelu(jnp.einsum('td,ted->te', xb, u[eid]))
        return jnp.einsum('te,ted->td', g * act, v[eid])

    out = lax.map(block, jnp.arange(T // PEER_BLOCK) * PEER_BLOCK)
    return out.reshape(B_, S_, D_)


def setup_inputs(seed: int = 0) -> dict:
    key = jax.random.key(seed)
    ks = iter(jax.random.split(key, 32))
    n_even = (DEPTH + 1) // 2
    n_odd = DEPTH // 2

    def nrm(shape, scale):
        return jax.random.normal(next(ks), shape, jnp.float32) * scale

    def gain(shape):
        return 1.0 + 0.02 * jax.random.normal(next(ks), shape, jnp.float32)

    return {
        'x': nrm((BATCH, SEQ, D_MODEL), 1.0),
        'mem': nrm((BATCH, N_MEM, D_MODEL), 1.0),
        'mix_norm': gain((DEPTH, D_MODEL)),
        'even_w_in': nrm((n_even, D_MODEL, EVEN_IN), D_MODEL ** -0.5),
        'even_conv_w': nrm((n_even, CONV_WIDTH, 1, A_WIDTH), CONV_WIDTH ** -0.5),
        'even_pool_w': nrm((n_even, len(POOL_WINDOWS), POOL_GROUP, POOL_GROUP), POOL_GROUP ** -0.5),
        'even_pool_scale': gain((n_even, B_WIDTH)),
        'even_w_out': nrm((n_even, MIX_WIDTH, D_MODEL), MIX_WIDTH ** -0.5),
        'odd_w_in': nrm((n_odd, D_MODEL, ODD_IN), D_MODEL ** -0.5),
        'odd_kv_norm': gain((n_odd, C_LATENT)),
        'odd_w_uv': nrm((n_odd, C_HEADS, C_LATENT, C_HEAD_DIM), C_LATENT ** -0.5),
        'odd_hg_norm': gain((n_odd, D_HEADS * D_VAL)),
        'odd_w_out': nrm((n_odd, MIX_WIDTH, D_MODEL), MIX_WIDTH ** -0.5),
        'hgrn_gamma': nrm((DEPTH, D_HEADS * D_KEY), 0.5),
        'rel_bias': nrm((REL_BUCKETS, C_HEADS), 0.5),
        'mem_norm': gain((D_MODEL,)),
        'xattn_norm': gain((DEPTH, D_MODEL)),
        'xattn_wq': nrm((DEPTH, D_MODEL, D_MODEL), D_MODEL ** -0.5),
        'xattn_wkv': nrm((DEPTH, D_MODEL, 2 * D_MODEL), D_MODEL ** -0.5),
        'xattn_wo': nrm((DEPTH, D_MODEL, D_MODEL), D_MODEL ** -0.5),
        'peer_norm': gain((DEPTH, D_MODEL)),
        'peer_wq': nrm((DEPTH, D_MODEL, PEER_HEADS * PEER_QDIM), D_MODEL ** -0.5),
        'peer_keys': nrm((DEPTH, PEER_HEADS, 2, N_KEYS, PEER_HALF), PEER_HALF ** -0.5),
        'peer_u': nrm((DEPTH, N_EXPERTS, D_MODEL), D_MODEL ** -0.5),
        'peer_v': nrm((DEPTH, N_EXPERTS, D_MODEL), (PEER_HEADS * PEER_TOPK) ** -0.5),
        'final_norm': gain((D_MODEL,)),
    }


def reference(x, mem, mix_norm, even_w_in, even_conv_w, even_pool_w, even_pool_scale, even_w_out,
              odd_w_in, odd_kv_norm, odd_w_uv, odd_hg_norm, odd_w_out, hgrn_gamma, rel_bias, mem_norm,
              xattn_norm, xattn_wq, xattn_wkv, xattn_wo, peer_norm, peer_wq, peer_keys, peer_u, peer_v,
              final_norm):
    mem_n = rmsnorm(mem, mem_norm)
    p = jax.nn.softmax(hgrn_gamma.astype(jnp.float32), axis=0)
    lower_bounds = jnp.cumsum(p, axis=0) - p
    h = x
    for l in range(DEPTH):
        j = l // 2
        hn = rmsnorm(h, mix_norm[l])
        if l % 2 == 0:
            h = h + short_conv_pool_mixer(hn, even_w_in[j], even_conv_w[j], even_pool_w[j],
                                          even_pool_scale[j], even_w_out[j])
        else:
            h = h + sparse_attn_hgrn_mixer(hn, odd_w_in[j], odd_kv_norm[j], odd_w_uv[j], odd_hg_norm[j],
                                           odd_w_out[j], rel_bias, lower_bounds[l])
        h = h + memory_cross_attn(rmsnorm(h, xattn_norm[l]), mem_n, xattn_wq[l], xattn_wkv[l], xattn_wo[l])
        h = h + peer_ffn(rmsnorm(h, peer_norm[l]), peer_wq[l], peer_keys[l], peer_u[l], peer_v[l])
    return rmsnorm(h, final_norm)
```

```python
import numpy as np
import concourse.bass as bass
import concourse.mybir as mybir
from concourse.bass_utils import run_bass_kernel_spmd
from contextlib import ExitStack

F32 = mybir.dt.float32
I32 = mybir.dt.int32
U32 = mybir.dt.uint32
AF = mybir.ActivationFunctionType
ALU = mybir.AluOpType
AX = mybir.AxisListType

ENGS = ['pe', 'dve', 'act', 'pool', 'sp']
SEM_LIMIT = 30000
DMA_K = 8


class T:
    def __init__(self, h, name):
        self.h = h
        self.name = name
        self.st = {}

    def __getitem__(self, idx):
        return self.h[idx]


def bcast(ap, axis, n):
    l = [list(x) for x in ap.ap]
    l.insert(axis, [0, n])
    return bass.AP(ap.tensor, ap.offset, l)


def rep(ap, axis, n):
    l = [list(x) for x in ap.ap]
    assert l[axis][1] == 1
    l[axis] = [0, n]
    return bass.AP(ap.tensor, ap.offset, l)


class Prog:
    def __init__(self, nc, es):
        self.nc = nc
        self.es = es
        self.pes = es
        self.ops = {e: [] for e in ENGS}
        self.base = {e: 0 for e in ENGS}
        self.waited = {e: {} for e in ENGS}
        self.waited_dma = {e: set() for e in ENGS}
        self.tiles = []
        self.csems = {e: [] for e in ENGS}
        self.ccount = {e: 0 for e in ENGS}
        self.dsems = {}
        self.dcount = {e: 0 for e in ENGS}
        self.dma_hist = {e: [] for e in ENGS}

    def sb(self, name, shape, dt=F32, glob=False):
        es = self.es if glob else self.pes
        self.uid = getattr(self, 'uid', 0) + 1
        name = f"{name}_{self.uid}"
        t = T(es.enter_context(self.nc.sbuf_tensor(name, list(shape), dt)), name)
        self.tiles.append(t)
        return t

    def ps(self, name, shape, dt=F32):
        t = T(self.es.enter_context(self.nc.psum_tensor(name, list(shape), dt)), name)
        self.tiles.append(t)
        return t

    def dram(self, name, shape, dt=F32, kind="Internal"):
        t = T(self.nc.dram_tensor(name, list(shape), dt, kind=kind).ap(), name)
        self.tiles.append(t)
        return t

    @staticmethod
    def _norm(key):
        if isinstance(key, T):
            return key, None
        return key[0], key[1]

    def _entries(self, tile, sub):
        if sub is None:
            return list(tile.st.values())
        out = []
        if sub in tile.st:
            out.append(tile.st[sub])
        if None in tile.st:
            out.append(tile.st[None])
        return out

    def op(self, eng, fn, r=(), w=(), dma=False, extra=()):
        idx = len(self.ops[eng])
        deps = set(extra)
        for key in r:
            tile, sub = self._norm(key)
            for ent in self._entries(tile, sub):
                if ent[0] is not None:
                    deps.add(ent[0])
        for key in w:
            tile, sub = self._norm(key)
            for ent in self._entries(tile, sub):
                if ent[0] is not None:
                    deps.add(ent[0])
                for e2, i2 in ent[1].items():
                    deps.add((e2, i2))
        for key in r:
            tile, sub = self._norm(key)
            ent = tile.st.setdefault(sub, [None, {}])
            ent[1][eng] = idx
        for key in w:
            tile, sub = self._norm(key)
            if sub is None:
                tile.st = {None: [(eng, idx), {}]}
            else:
                tile.st[sub] = [(eng, idx), {}]
        if dma:
            h = self.dma_hist[eng]
            if len(h) >= DMA_K:
                deps.add((eng, h[-DMA_K]))
        final = []
        for (e2, i2) in sorted(deps, reverse=True):
            if e2 == eng and i2 == idx:
                continue
            if self.ops[e2][i2]['dma']:
                if (e2, i2) in self.waited_dma[eng]:
                    continue
                self.waited_dma[eng].add((e2, i2))
                final.append((e2, i2))
            else:
                if e2 == eng and eng == 'pe':
                    continue
                if self.waited[eng].get(e2, -1) >= i2:
                    continue
                self.waited[eng][e2] = i2
                final.append((e2, i2))
        o = dict(fn=fn, deps=final, dma=dma, sig=False)
        if dma:
            o['dma_n'] = self.dcount[eng]
            self.dcount[eng] += 1
            self.dma_hist[eng].append(idx)
        self.ops[eng].append(o)
        return (eng, idx)

    def _sigof(self, e2, i2):
        o = self.ops[e2][i2]
        if o['dma']:
            n = o['dma_n']
            return self.dsems[e2][n % DMA_K], 16 * (n // DMA_K + 1)
        j, v = o['sv']
        return self.csems[e2][j], v

    def flush(self):
        nc = self.nc
        last = {}
        for e in ENGS:
            for i in range(len(self.ops[e]) - 1, self.base[e] - 1, -1):
                if not self.ops[e][i]['dma'] and self.ops[e][i]['fn'] is not None:
                    last[e] = (e, i)
                    break
        dmas = []
        for e in ENGS:
            dmas += [(e, i) for i in self.dma_hist[e][-DMA_K:] if i >= self.base[e]]
        for e in ENGS:
            extra = [v for k, v in last.items() if k != e] + dmas
            self.op(e, None, extra=extra)
        for e in ENGS:
            for o in self.ops[e][self.base[e]:]:
                for (e2, i2) in o['deps']:
                    assert i2 >= self.base[e2], "cross-phase dep"
                    self.ops[e2][i2]['sig'] = True
        for e in ENGS:
            for o in self.ops[e][self.base[e]:]:
                if o['dma']:
                    if e not in self.dsems:
                        self.dsems[e] = [self.es.enter_context(nc.semaphore(f"d_{e}_{j}")) for j in range(DMA_K)]
                    continue
                if o['sig']:
                    c = self.ccount[e]
                    j = c // SEM_LIMIT
                    while len(self.csems[e]) <= j:
                        self.csems[e].append(self.es.enter_context(nc.semaphore(f"c_{e}_{len(self.csems[e])}")))
                    o['sv'] = (j, c % SEM_LIMIT + 1)
                    self.ccount[e] = c + 1

        def run(e, eng):
            for o in self.ops[e][self.base[e]:]:
                for (e2, i2) in o['deps']:
                    s, v = self._sigof(e2, i2)
                    eng.wait_ge(s, v)
                if o['fn'] is None:
                    continue
                ins = o['fn'](eng)
                if o['dma']:
                    n = o['dma_n']
                    ins.then_inc(self.dsems[e][n % DMA_K], 16)
                elif o['sig']:
                    j, v = o['sv']
                    ins.then_inc(self.csems[e][j], 1)

        with nc.Block() as block:
            @block.tensor
            def _(eng):
                run('pe', eng)

            @block.vector
            def _(eng):
                run('dve', eng)

            @block.scalar
            def _(eng):
                run('act', eng)

            @block.gpsimd
            def _(eng):
                run('pool', eng)

            @block.sync
            def _(eng):
                run('sp', eng)
        for e in ENGS:
            self.base[e] = len(self.ops[e])
        for t in self.tiles:
            t.st = {}

D = 1024
S = 2048
NT = 16
NMEM = 256
DEPTH = 4
EPS = 1e-6

IN_SHAPES = {
    'x': [S, D], 'mem': [NMEM, D], 'mix_norm': [4, D],
    'even_w_in': [2, D, 2048], 'even_conv_w': [2, 128, 12], 'even_pool_w': [2, 4, 128, 128],
    'even_pool_scale': [2, 128, 4], 'even_w_out': [2, D, D],
    'odd_w_in': [2, D, 3784], 'odd_kv_norm': [2, 128], 'odd_w_uv': [2, 8, 128, 64],
    'odd_hg_norm': [2, 512], 'odd_w_out': [2, D, D], 'hgrn_gamma': [4, 512],
    'rel_bias': [32, 8], 'biasT': [128, 8, 2, 128], 'mem_norm': [1, D], 'xattn_norm': [4, D],
    'xattn_wq': [4, D, D], 'xattn_wkv': [4, D, 2 * D], 'xattn_wo': [4, D, D],
    'peer_norm': [4, D], 'peer_wq': [4, D, 2048], 'peer_keysT': [4, 128, 16, 128],
    'peer_u': [4, 16384, D], 'peer_v': [4, 16384, D], 'final_norm': [1, D],
}


def make_consts():
    parts = {}
    parts['ident'] = np.eye(128, dtype=np.float32)
    t = np.arange(512)
    invc = np.concatenate([1.0 / np.minimum(t + 1, w) for w in (2, 4, 8, 16)]).astype(np.float32)
    parts['invc'] = np.tile(invc[None, :], (128, 1))
    parts['iota16'] = np.tile(np.arange(16, dtype=np.float32)[None, :], (128, 1))
    ii = np.arange(128)
    parts['U2'] = ((ii[:, None] // 64 == ii[None, :] // 64) & (ii[:, None] <= ii[None, :])).astype(np.float32)
    parts['B2'] = (ii[:, None] // 64 == ii[None, :] // 64).astype(np.float32)
    parts['cmask'] = np.where(ii[None, :] <= ii[:, None], 0.0, -1e30).astype(np.float32)
    parts['ones'] = np.ones((128, 8), dtype=np.float32)
    off = 0
    lay = {}
    arrs = []
    for k, v in parts.items():
        lay[k] = (off, v.shape[1])
        off += v.shape[1]
        arrs.append(v.astype(np.float32))
    return np.ascontiguousarray(np.concatenate(arrs, axis=1)), lay


class Model:
    def __init__(self):
        self.nc = bass.Bass("TRN2", target_bir_lowering=False)
        self.es = ExitStack()
        self.P = Prog(self.nc, self.es)
        P = self.P
        carr, self.clay = make_consts()
        self.carr = carr
        model = self

        class LazyIn(dict):
            def __missing__(self, k):
                shp = list(carr.shape) if k == 'consts' else IN_SHAPES[k]
                t = P.dram(k, shp, F32, kind="ExternalInput")
                self[k] = t
                return t
        self.din = LazyIn()
        self.out = P.dram("out", [S, D], F32, kind="ExternalOutput")
        self.hA = P.dram("hA", [S, D])
        self.hB = P.dram("hB", [S, D])
        self.PS = P.ps("ps", [128, 8, 512])
        self.C = P.sb("consts_sb", [128, carr.shape[1]], glob=True)
        self.memT = P.sb("memT", [128, 8, NMEM], glob=True)
        self.rotc = {}

    def cst(self, name):
        o, w = self.clay[name]
        return self.C[:, o:o + w]

    def psv(self, b0, nb=2):
        ap = self.PS[:, b0:b0 + nb, :].rearrange("p a b -> p (a b)")
        return ap, [(self.PS, b) for b in range(b0, b0 + nb)]

    def mm(self, out, lhsT, rhs, start, stop, r, w):
        self.P.op('pe', lambda e: e.matmul(out, lhsT, rhs, start=start, stop=stop), r=r, w=w)

    def tr(self, out, in_, r, w):
        ident = self.cst('ident')
        self.P.op('pe', lambda e: e.transpose(out, in_, ident), r=list(r) + [self.C], w=w)

    def act(self, out, in_, func, r, w, bias=None, scale=None, accum=None):
        kw = {}
        if bias is not None:
            kw['bias'] = bias
        if scale is not None:
            kw['scale'] = scale
        if accum is not None:
            kw['accum_out'] = accum
        self.P.op('act', lambda e: e.activation(out, in_, func, **kw), r=r, w=w)

    def tt(self, out, a, b, op, r, w, eng='dve'):
        self.P.op(eng, lambda e: e.tensor_tensor(out, a, b, op), r=r, w=w)

    def ts(self, out, a, s1, s2, op0, op1, r, w, eng='dve', accum=None):
        if op1 is None:
            self.P.op(eng, lambda e: e.tensor_scalar(out, a, s1, None, op0), r=r, w=w)
        elif accum is None:
            self.P.op(eng, lambda e: e.tensor_scalar(out, a, s1, s2, op0, op1), r=r, w=w)
        else:
            self.P.op(eng, lambda e: e.tensor_scalar(out, a, s1, s2, op0, op1, accum_out=accum), r=r, w=w)

    def stt(self, out, a, scalar, b, op0, op1, r, w):
        self.P.op('dve', lambda e: e.scalar_tensor_tensor(out, a, scalar, b, op0, op1), r=r, w=w)

    def copy(self, out, in_, r, w, eng='act'):
        if eng == 'act':
            self.P.op('act', lambda e: e.copy(out, in_), r=r, w=w)
        else:
            self.P.op(eng, lambda e: e.tensor_copy(out, in_), r=r, w=w)

    def dma(self, out, in_, r, w, eng='sp'):
        self.P.op(eng, lambda e: e.dma_start(out=out, in_=in_), r=r, w=w, dma=True)

    def recip(self, out, in_, r, w):
        self.P.op('dve', lambda e: e.reciprocal(out, in_), r=r, w=w)

    def rot(self, name, shape, n=2, dt=F32):
        return [self.P.sb(f"{name}{j}", shape, dt) for j in range(n)]

    def wload(self, dst, src_t, src_ap):
        self.dma(dst[:, :, :], src_ap.rearrange("(k p) c -> p k c", p=128), r=[src_t], w=[dst])

    def gload(self, dst, src_t, row_ap):
        n = row_ap.shape[1]
        self.dma(dst[:, :], row_ap.broadcast_to([128, n]), r=[src_t], w=[dst])

    def rms(self, x, g, hn, st, width=D):
        self.act(hn[:, :], x[:, :], AF.Square, r=[x], w=[hn, st], accum=st[:, 0:1])
        self.act(st[:, 1:2], st[:, 0:1], AF.Sqrt, r=[st], w=[st], scale=1.0 / width, bias=self.epsb[:, 0:1])
        self.recip(st[:, 1:2], st[:, 1:2], r=[st], w=[st])
        self.stt(hn[:, :], x[:, :], st[:, 1:2], g[:, :], ALU.mult, ALU.mult, r=[x, st, g], w=[hn])

    def transp8(self, src, dstT, b0, n=8):
        nb = (n * 128 + 511) // 512
        pv, pk = self.psv(b0, nb)
        for k in range(n):
            self.tr(pv[:, k * 128:(k + 1) * 128], src[:, k * 128:(k + 1) * 128], r=[src], w=pk)
        self.copy(dstT[:, :, :].rearrange("p a b -> p (a b)"), pv[:, 0:n * 128], r=pk, w=[dstT])

    def phase_setup(self):
        P = self.P
        self.dma(self.C[:, :], self.din['consts'][:, :], r=[self.din['consts']], w=[self.C])
        self.epsb = P.sb("epsb", [128, 1], glob=True)
        P.op('dve', lambda e: e.memset(self.epsb[:, :], EPS), w=[self.epsb])
        g = P.sb("g_mem", [128, D])
        self.gload(g, self.din['mem_norm'], self.din['mem_norm'][0:1, :])
        xs = self.rot("xm", [128, D])
        hn = self.rot("hnm", [128, D])
        st = self.rot("stm", [128, 4])
        mT = [P.sb(f"mT{j}", [128, 8, 128]) for j in range(2)]
        for m in range(2):
            self.dma(xs[m][:, :], self.din['mem'][m * 128:(m + 1) * 128, :], r=[self.din['mem']], w=[xs[m]])
            self.rms(xs[m], g, hn[m], st[m])
            self.transp8(hn[m], mT[m], 2 * m)
            self.copy(self.memT[:, :, m * 128:(m + 1) * 128], mT[m][:, :, :], r=[mT[m]], w=[(self.memT, m)], eng='dve')

    def phase_xattn(self, l, src, dst):
        P = self.P
        din = self.din
        wq = P.sb("wq", [128, 8, D])
        wo = P.sb("wo", [128, 8, D])
        KT = P.sb("KT", [128, 8, NMEM])
        V = P.sb("V", [128, 2, D])
        g = P.sb("g_x", [128, D])
        wkv = self.rot("wkv", [128, 8, 512])
        self.gload(g, din['xattn_norm'], din['xattn_norm'][l:l + 1, :])
        for c in range(4):
            wc = wkv[c % 2]
            self.wload(wc, din['xattn_wkv'], din['xattn_wkv'][l][:, c * 512:(c + 1) * 512])
            if c < 2:
                for f in range(4):
                    fc = c * 4 + f
                    b = fc % 8
                    pv, pk = self.psv(b, 1)
                    for k in range(8):
                        self.mm(pv[:, 0:NMEM], wc[:, k, f * 128:(f + 1) * 128], self.memT[:, k, :],
                                k == 0, k == 7, r=[wc, self.memT], w=pk)
                    self.act(KT[:, fc, :], pv[:, 0:NMEM], AF.Copy, r=pk, w=[(KT, fc)], scale=1.0 / 16.0)
            else:
                for m in range(2):
                    b = (c - 2) * 2 + m
                    pv, pk = self.psv(b, 1)
                    for k in range(8):
                        self.mm(pv[:, :], self.memT[:, k, m * 128:(m + 1) * 128], wc[:, k, :],
                                k == 0, k == 7, r=[wc, self.memT], w=pk)
                    self.copy(V[:, m, (c - 2) * 512:(c - 1) * 512], pv[:, :], r=pk, w=[(V, (m, c))])
        self.wload(wq, din['xattn_wq'], din['xattn_wq'][l])
        self.wload(wo, din['xattn_wo'], din['xattn_wo'][l])
        xs = self.rot("x", [128, D])
        hns = self.rot("hn", [128, D])
        hnTs = self.rot("hnT", [128, 8, 128])
        sts = self.rot("st", [128, 16])
        qTs = self.rot("qT", [128, 8, 128])
        Pms = self.rot("Pm", [128, 4 * NMEM])
        PTs = self.rot("PT", [128, 8, 128])
        oTs = self.rot("oT", [128, 8, 128])
        for i in range(NT):
            j = i % 2
            x, hn, hnT, st, qT, Pm, PT, oT = xs[j], hns[j], hnTs[j], sts[j], qTs[j], Pms[j], PTs[j], oTs[j]
            self.dma(x[:, :], src[i * 128:(i + 1) * 128, :], r=[(src, i)], w=[x])
            self.rms(x, g, hn, st)
            self.transp8(hn, hnT, 0)
            pv, pk = self.psv(2, 2)
            for f in range(8):
                for k in range(8):
                    self.mm(pv[:, f * 128:(f + 1) * 128], wq[:, k, f * 128:(f + 1) * 128], hnT[:, k, :],
                            k == 0, k == 7, r=[wq, hnT], w=pk)
            self.copy(qT[:, :, :].rearrange("p a b -> p (a b)"), pv[:, :], r=pk, w=[qT])
            lv, lk = self.psv(4, 2)
            for hd in range(4):
                for c in range(2):
                    self.mm(lv[:, hd * 256:(hd + 1) * 256], qT[:, hd * 2 + c, :], KT[:, hd * 2 + c, :],
                            c == 0, c == 1, r=[qT, KT], w=lk)
            lv3 = self.PS[:, 4:6, :].rearrange("p a (h m) -> p (a h) m", m=NMEM)
            P.op('dve', lambda e, lv3=lv3, st=st: e.tensor_reduce(out=st[:, 4:8], in_=lv3, axis=AX.X, op=ALU.max, negate=True),
                 r=lk, w=[st])
            for hd in range(4):
                self.act(Pm[:, hd * 256:(hd + 1) * 256], lv[:, hd * 256:(hd + 1) * 256], AF.Exp, r=lk + [st], w=[Pm, st],
                         bias=st[:, 4 + hd:5 + hd], scale=1.0, accum=st[:, 8 + hd:9 + hd])
            self.recip(st[:, 12:16], st[:, 8:12], r=[st], w=[st])
            for hd in range(4):
                self.ts(Pm[:, hd * 256:(hd + 1) * 256], Pm[:, hd * 256:(hd + 1) * 256], st[:, 12 + hd:13 + hd], None,
                        ALU.mult, None, r=[Pm, st], w=[Pm])
            self.transp8(Pm, PT, 6)
            ov, ok = self.psv(0, 2)
            for jj in range(8):
                for c in range(2):
                    self.mm(ov[:, jj * 128:(jj + 1) * 128], V[:, c, jj * 128:(jj + 1) * 128], PT[:, (jj // 2) * 2 + c, :],
                            c == 0, c == 1, r=[V, PT], w=ok)
            self.copy(oT[:, :, :].rearrange("p a b -> p (a b)"), ov[:, :], r=ok, w=[oT])
            yv, yk = self.psv(2, 2)
            for half in range(2):
                for k in range(8):
                    self.mm(yv[:, half * 512:(half + 1) * 512], oT[:, k, :], wo[:, k, half * 512:(half + 1) * 512],
                            k == 0, k == 7, r=[oT, wo], w=yk)
            self.tt(x[:, :], yv[:, :], x[:, :], ALU.add, r=yk + [x], w=[x])
            self.dma(dst[i * 128:(i + 1) * 128, :], x[:, :], r=[x], w=[(dst, i)])


    def phase_even(self, l, src, dst):
        P = self.P
        din = self.din
        jx = l // 2
        w_in = P.sb("e_win", [128, 8, 2048])
        w_out = P.sb("e_wout", [128, 8, D])
        pw = P.sb("e_pw", [128, 4, 128])
        cw = P.sb("e_cw", [128, 12])
        psc = P.sb("e_psc", [128, 4])
        g = P.sb("e_g", [128, D])
        self.gload(g, din['mix_norm'], din['mix_norm'][l:l + 1, :])
        self.wload(w_in, din['even_w_in'], din['even_w_in'][jx])
        self.wload(w_out, din['even_w_out'], din['even_w_out'][jx])
        self.dma(pw[:, :, :], din['even_pool_w'][jx].rearrange("g c d -> c g d"), r=[din['even_pool_w']], w=[pw])
        self.dma(cw[:, :], din['even_conv_w'][jx], r=[din['even_conv_w']], w=[cw])
        self.dma(psc[:, :], din['even_pool_scale'][jx], r=[din['even_pool_scale']], w=[psc])
        cu_halo = P.sb("e_cuh", [128, 4, 2])
        pv_halo = P.sb("e_pvh", [128, 4, 16])
        P.op('pool', lambda e: e.memset(cu_halo[:, :, :], 0.0), w=[cu_halo])
        P.op('pool', lambda e: e.memset(pv_halo[:, :, :], 0.0), w=[pv_halo])
        hnT = P.sb("e_hnT", [128, 8, 512])
        yT = P.sb("e_yT", [128, 8, 512])
        xs = self.rot("e_x", [128, D])
        hns = self.rot("e_hn", [128, D])
        sts = self.rot("e_st", [128, 4])
        hts = self.rot("e_ht", [128, 8, 128])
        cus = self.rot("e_cu", [128, 514])
        pvs = self.rot("e_pv", [128, 528])
        ut = self.rot("e_ut", [128, 512])
        zt = self.rot("e_z", [128, 512])
        sA = self.rot("e_sA", [128, 528])
        sB = self.rot("e_sB", [128, 528])
        pl = self.rot("e_pl", [128, 512])
        invc = self.cst('invc')
        for b in range(4):
            for t4 in range(4):
                i = b * 4 + t4
                j = i % 2
                self.dma(xs[j][:, :], src[i * 128:(i + 1) * 128, :], r=[(src, i)], w=[xs[j]])
                self.rms(xs[j], g, hns[j], sts[j])
                self.transp8(hns[j], hts[j], 6)
                self.copy(hnT[:, :, t4 * 128:(t4 + 1) * 128], hts[j][:, :, :], r=[hts[j]], w=[(hnT, t4)], eng='pool')
            for c4 in range(4):
                j = c4 % 2
                cu, pv, u_sb, z = cus[j], pvs[j], ut[j], zt[j]

                def proj(cc, bank):
                    pvw, pk = self.psv(bank, 1)
                    for k in range(8):
                        self.mm(pvw[:, :], w_in[:, k, cc * 128:(cc + 1) * 128], hnT[:, k, :], k == 0, k == 7,
                                r=[w_in, hnT], w=pk)
                    return pvw, pk
                pu, ku = proj(c4, 0)
                self.copy(u_sb[:, :], pu[:, :], r=ku, w=[u_sb])
                pg, kg = proj(4 + c4, 1)
                self.copy(cu[:, 0:2], cu_halo[:, c4, :], r=[(cu_halo, c4)], w=[cu], eng='pool')
                self.tt(cu[:, 2:514], pg[:, :], u_sb[:, :], ALU.mult, r=kg + [u_sb, cu], w=[cu])
                self.copy(cu_halo[:, c4, :], cu[:, 512:514], r=[cu], w=[(cu_halo, c4)], eng='pool')
                self.ts(z[:, :], cu[:, 0:512], cw[:, c4 * 3:c4 * 3 + 1], None, ALU.mult, None, r=[cu, cw], w=[z])
                self.stt(z[:, :], cu[:, 1:513], cw[:, c4 * 3 + 1:c4 * 3 + 2], z[:, :], ALU.mult, ALU.add, r=[cu, cw, z], w=[z])
                self.stt(z[:, :], cu[:, 2:514], cw[:, c4 * 3 + 2:c4 * 3 + 3], z[:, :], ALU.mult, ALU.add, r=[cu, cw, z], w=[z])
                pb, kb = proj(8 + c4, 2)
                self.tt(yT[:, c4, :], pb[:, :], z[:, :], ALU.mult, r=kb + [z], w=[(yT, c4)])
                pp, kp = proj(12 + c4, 3)
                self.copy(pv[:, 0:16], pv_halo[:, c4, :], r=[(pv_halo, c4)], w=[pv], eng='pool')
                self.copy(pv[:, 16:528], pp[:, :], r=kp + [pv], w=[pv])
                self.copy(pv_halo[:, c4, :], pv[:, 512:528], r=[pv], w=[(pv_halo, c4)], eng='pool')
                a_, b_ = sA[j], sB[j]
                self.tt(a_[:, 1:528], pv[:, 1:528], pv[:, 0:527], ALU.add, r=[pv], w=[a_])
                cur = a_
                other = b_
                lo = 1
                for st_ in range(c4):
                    sh = 2 ** (st_ + 1)
                    nlo = lo + sh
                    self.tt(other[:, nlo:528], cur[:, nlo:528], cur[:, nlo - sh:528 - sh], ALU.add, r=[cur], w=[other])
                    cur, other = other, cur
                    lo = nlo
                wdw = 2 ** (c4 + 1)
                pool_t = pl[j]
                if b == 0:
                    self.tt(pool_t[:, :], cur[:, 16:528], invc[:, c4 * 512:(c4 + 1) * 512], ALU.mult, r=[cur, self.C], w=[pool_t])
                    self.tt(pool_t[:, :], pool_t[:, :], pv[:, 16:528], ALU.subtract, r=[pool_t, pv], w=[pool_t])
                else:
                    self.stt(pool_t[:, :], cur[:, 16:528], 1.0 / wdw, pv[:, 16:528], ALU.mult, ALU.subtract, r=[cur, pv], w=[pool_t])
                py, ky = self.psv(3, 1)
                self.mm(py[:, :], pw[:, c4, :], pool_t[:, :], True, True, r=[pw, pool_t], w=ky)
                self.act(yT[:, 4 + c4, :], py[:, :], AF.Copy, r=ky + [psc], w=[(yT, 4 + c4)], scale=psc[:, c4:c4 + 1])
            for t4 in range(4):
                i = b * 4 + t4
                j = i % 2
                self.dma(xs[j][:, :], src[i * 128:(i + 1) * 128, :], r=[(src, i)], w=[xs[j]])
                ov, ok = self.psv(4, 2)
                for half in range(2):
                    for k in range(8):
                        self.mm(ov[:, half * 512:(half + 1) * 512], yT[:, k, t4 * 128:(t4 + 1) * 128],
                                w_out[:, k, half * 512:(half + 1) * 512], k == 0, k == 7, r=[yT, w_out], w=ok)
                self.tt(xs[j][:, :], ov[:, :], xs[j][:, :], ALU.add, r=ok + [xs[j]], w=[xs[j]])
                self.dma(dst[i * 128:(i + 1) * 128, :], xs[j][:, :], r=[xs[j]], w=[(dst, i)])


    def top16(self, src_ap, work_ap, tv_ap, ti_ap, r, wk):
        P = self.P
        P.op('dve', lambda e: e.max(out=tv_ap[:, 0:8], in_=src_ap), r=r, w=wk)
        P.op('dve', lambda e: e.max_index(out=ti_ap[:, 0:8], in_max=tv_ap[:, 0:8], in_values=src_ap), r=r + wk, w=wk)
        P.op('dve', lambda e: e.match_replace(out=work_ap, in_to_replace=tv_ap[:, 0:8], in_values=src_ap, imm_value=-1e30),
             r=r + wk, w=wk)
        P.op('dve', lambda e: e.max(out=tv_ap[:, 8:16], in_=work_ap), r=wk, w=wk)
        P.op('dve', lambda e: e.max_index(out=ti_ap[:, 8:16], in_max=tv_ap[:, 8:16], in_values=work_ap), r=wk, w=wk)

    def phase_peer(self, l, src, dst):
        P = self.P
        din = self.din
        wq = P.sb("p_wq", [128, 8, 2048])
        keysT = P.sb("p_keys", [128, 16, 128])
        g = P.sb("p_g", [128, D])
        self.gload(g, din['peer_norm'], din['peer_norm'][l:l + 1, :])
        self.wload(wq, din['peer_wq'], din['peer_wq'][l])
        self.dma(keysT[:, :, :], din['peer_keysT'][l], r=[din['peer_keysT']], w=[keysT])
        u_l = din['peer_u'][:, :, :].rearrange("l n d -> (l n) d")
        v_l = din['peer_v'][:, :, :].rearrange("l n d -> (l n) d")
        xs = self.rot("p_x", [128, D])
        xns = self.rot("p_xn", [128, D])
        xnTs = self.rot("p_xnT", [128, 8, 128])
        sts = self.rot("p_st", [128, 4])
        qT = P.sb("p_qT", [128, 16, 128])
        s_sb = P.sb("p_s", [128, 2048])
        s_wk = P.sb("p_swk", [128, 2048])
        tv = P.sb("p_tv", [128, 16, 16])
        ti = P.sb("p_ti", [128, 16, 16], U32)
        tif = P.sb("p_tif", [128, 16, 16])
        cand = P.sb("p_cand", [128, 8, 256])
        cwk = P.sb("p_cwk", [128, 8, 256])
        cv = P.sb("p_cv", [128, 8, 16])
        cpos = P.sb("p_cpos", [128, 8, 16], U32)
        ab_u = P.sb("p_abu", [128, 2, 128], U32)
        ab_f = P.sb("p_abf", [128, 2, 128])
        eq = P.sb("p_eq", [128, 128, 16])
        isel = P.sb("p_isel", [128, 2, 128])
        eidf = P.sb("p_eidf", [128, 128])
        eid = P.sb("p_eid", [128, 128], I32)
        gg = P.sb("p_gg", [128, 8, 16])
        gz = P.sb("p_gz", [128, 16])
        actv = P.sb("p_act", [128, 128])
        t1 = P.sb("p_t1", [128, 128])
        t2 = P.sb("p_t2", [128, 128])
        wgt = P.sb("p_wgt", [128, 128])
        junk = P.sb("p_junk", [128, D])
        NG = 6
        ugs = self.rot("p_ug", [128, D], NG)
        iota16 = self.cst('iota16')
        for i in range(NT):
            j2 = i % 2
            x, xn, xnT, st = xs[j2], xns[j2], xnTs[j2], sts[j2]
            self.dma(x[:, :], src[i * 128:(i + 1) * 128, :], r=[(src, i)], w=[x])
            self.rms(x, g, xn, st)
            self.transp8(xn, xnT, 0)
            qv, qk = self.psv(4, 4)
            for hp in range(16):
                for k in range(8):
                    self.mm(qv[:, hp * 128:(hp + 1) * 128], wq[:, k, hp * 128:(hp + 1) * 128], xnT[:, k, :],
                            k == 0, k == 7, r=[wq, xnT], w=qk)
            self.copy(qT[:, :, :].rearrange("p a b -> p (a b)"), qv[:, :], r=qk, w=[qT])
            sv, sk = self.psv(0, 4)
            for hp in range(16):
                self.mm(sv[:, hp * 128:(hp + 1) * 128], qT[:, hp, :], keysT[:, hp, :], True, True, r=[qT, keysT], w=sk)
            self.copy(s_sb[:, :], sv[:, :], r=sk, w=[s_sb])
            xpv, xpk = self.psv(4, 2)
            self.copy(xpv[:, :], xn[:, :], r=[xn], w=xpk)
            for hp in range(16):
                self.top16(s_sb[:, hp * 128:(hp + 1) * 128], s_wk[:, hp * 128:(hp + 1) * 128], tv[:, hp, :], ti[:, hp, :],
                           r=[s_sb], wk=[(tv, hp), (ti, hp), (s_wk, hp)])
            self.copy(tif[:, :, :], ti[:, :, :], r=[ti], w=[tif], eng='dve')
            tv4 = tv[:, :, :].rearrange("p (h t) a -> p h t a", t=2)
            tif4 = tif[:, :, :].rearrange("p (h t) a -> p h t a", t=2)
            cand4 = cand[:, :, :].rearrange("p h (a b) -> p h a b", b=16)
            self.tt(cand4, bcast(tv4[:, :, 0, :], 3, 16), bcast(tv4[:, :, 1, :], 2, 16), ALU.add, r=[tv], w=[cand])
            for h in range(8):
                self.top16(cand[:, h, :], cwk[:, h, :], cv[:, h, :], cpos[:, h, :],
                           r=[cand], wk=[(cv, h), (cpos, h), (cwk, h)])
            cposf = cpos[:, :, :].rearrange("p h a -> p (h a)")
            P.op('dve', lambda e, cposf=cposf: e.tensor_single_scalar(ab_u[:, 0, :], cposf, 4, ALU.logical_shift_right), r=[cpos], w=[ab_u])
            P.op('dve', lambda e, cposf=cposf: e.tensor_single_scalar(ab_u[:, 1, :], cposf, 15, ALU.bitwise_and), r=[cpos, ab_u], w=[ab_u])
            self.copy(ab_f[:, :, :], ab_u[:, :, :], r=[ab_u], w=[ab_f], eng='dve')
            for t in range(2):
                self.tt(eq[:, :, :], bcast(ab_f[:, t, :], 2, 16), bcast(iota16, 1, 128), ALU.is_equal, r=[ab_f, self.C], w=[eq])
                eq4 = eq[:, :, :].rearrange("p (h j) a -> p h j a", j=16)
                self.tt(eq4, eq4, bcast(tif4[:, :, t, :], 2, 16), ALU.mult, r=[eq, tif], w=[eq])
                P.op('dve', lambda e, t=t: e.tensor_reduce(out=isel[:, t, :], in_=eq[:, :, :], axis=AX.X, op=ALU.add), r=[eq], w=[isel])
            self.stt(eidf[:, :], isel[:, 0, :], 128.0, isel[:, 1, :], ALU.mult, ALU.add, r=[isel], w=[eidf])
            if l > 0:
                self.ts(eidf[:, :], eidf[:, :], float(l * 16384), None, ALU.add, None, r=[eidf], w=[eidf])
            self.copy(eid[:, :], eidf[:, :], r=[eidf], w=[eid], eng='dve')
            self.tt(gg[:, :, :], cv[:, :, :], bcast(cv[:, :, 0], 2, 16), ALU.subtract, r=[cv], w=[gg])
            self.act(gg[:, :, :], gg[:, :, :], AF.Exp, r=[gg], w=[gg])
            P.op('dve', lambda e: e.tensor_reduce(out=gz[:, 0:8], in_=gg[:, :, :], axis=AX.X, op=ALU.add), r=[gg], w=[gz])
            self.recip(gz[:, 8:16], gz[:, 0:8], r=[gz], w=[gz])
            self.tt(gg[:, :, :], gg[:, :, :], bcast(gz[:, 8:16], 2, 16), ALU.mult, r=[gg, gz], w=[gg])
            for jj in range(128):
                ug = ugs[jj % NG]
                P.op('pool', lambda e, ug=ug, jj=jj: e.indirect_dma_start(
                    out=ug[:, :], out_offset=None, in_=u_l[:, :],
                    in_offset=bass.IndirectOffsetOnAxis(ap=eid[:, jj:jj + 1], axis=0)),
                    r=[eid, din['peer_u']], w=[ug], dma=True)
                P.op('dve', lambda e, ug=ug, jj=jj: e.scalar_tensor_tensor(
                    junk[:, :], ug[:, :], 1.0, xpv[:, :], ALU.mult, ALU.mult, accum_out=actv[:, jj:jj + 1]),
                    r=[ug] + xpk, w=[(actv, jj)])
            self.tt(t1[:, :], actv[:, :], actv[:, :], ALU.mult, r=[actv], w=[t1])
            self.ts(t1[:, :], t1[:, :], 0.044715, 1.0, ALU.mult, ALU.add, r=[t1], w=[t1])
            self.tt(t1[:, :], t1[:, :], actv[:, :], ALU.mult, r=[t1, actv], w=[t1])
            self.act(t2[:, :], t1[:, :], AF.Sigmoid, r=[t1], w=[t2], scale=1.5957691216057308)
            self.tt(t2[:, :], t2[:, :], actv[:, :], ALU.mult, r=[t2, actv], w=[t2])
            self.tt(wgt[:, :], t2[:, :], gg[:, :, :].rearrange("p h a -> p (h a)"), ALU.mult, r=[t2, gg], w=[wgt])
            av, ak = self.psv(6, 2)
            for jj in range(128):
                vg = ugs[jj % NG]
                P.op('pool', lambda e, vg=vg, jj=jj: e.indirect_dma_start(
                    out=vg[:, :], out_offset=None, in_=v_l[:, :],
                    in_offset=bass.IndirectOffsetOnAxis(ap=eid[:, jj:jj + 1], axis=0)),
                    r=[eid, din['peer_v']], w=[vg], dma=True)
                if jj == 0:
                    self.ts(av[:, :], vg[:, :], wgt[:, 0:1], None, ALU.mult, None, r=[vg, wgt], w=ak)
                else:
                    self.stt(av[:, :], vg[:, :], wgt[:, jj:jj + 1], av[:, :], ALU.mult, ALU.add, r=[vg, wgt] + ak, w=ak)
            self.tt(x[:, :], av[:, :], x[:, :], ALU.add, r=ak + [x], w=[x])
            self.dma(dst[i * 128:(i + 1) * 128, :], x[:, :], r=[x], w=[(dst, i)])


    def phase_dsa(self, l, src, dst):
        P = self.P
        din = self.din
        jx = l // 2
        w = P.sb("d_w", [128, 8, 1736])
        w_uv = P.sb("d_wuv", [128, 8, 64])
        w_out = P.sb("d_wout", [128, 4, D])
        g = P.sb("d_g", [128, D])
        gkv = P.sb("d_gkv", [128, 128])
        b31 = P.sb("d_b31", [128, 16])
        corrT = P.sb("d_corr", [128, 8, 2, 128])
        self.gload(g, din['mix_norm'], din['mix_norm'][l:l + 1, :])
        self.gload(gkv, din['odd_kv_norm'], din['odd_kv_norm'][jx:jx + 1, :])
        self.gload(b31, din['rel_bias'], din['rel_bias'][31:32, :]) if False else self.dma(
            b31[:, 0:8], din['rel_bias'][31:32, :].broadcast_to([128, 8]), r=[din['rel_bias']], w=[b31])
        self.ts(b31[:, 8:16], b31[:, 0:8], -1.0, None, ALU.mult, None, r=[b31], w=[b31])
        self.dma(corrT[:, :, :, :], din['biasT'][:, :, :, :], r=[din['biasT']], w=[corrT])
        for h in range(8):
            self.act(corrT[:, h, :, :], corrT[:, h, :, :], AF.Exp, r=[corrT, b31], w=[corrT], bias=b31[:, 8 + h:9 + h], scale=1.0)
        self.dma(w[:, :, :], din['odd_w_in'][jx][:, 0:1736].rearrange("(k p) c -> p k c", p=128), r=[din['odd_w_in']], w=[w])
        self.dma(w_uv[:, :, :], din['odd_w_uv'][jx].rearrange("h r e -> r h e"), r=[din['odd_w_uv']], w=[w_uv])
        self.dma(w_out[:, :, :], din['odd_w_out'][jx][0:512, :].rearrange("(k p) c -> p k c", p=128), r=[din['odd_w_out']], w=[w_out])
        cT = P.sb("d_cT", [128, S])
        c_tm = P.sb("d_ctm", [128, NT, 128])
        ikT = P.sb("d_ikT", [64, S])
        xs = self.rot("d_x", [128, D])
        hn = P.sb("d_hn", [128, D])
        hnT = P.sb("d_hnT", [128, 8, 128])
        st = P.sb("d_st", [128, 8])
        craw = P.sb("d_craw", [128, 128])
        qlT = P.sb("d_qlT", [128, 8, 128])
        iqT = P.sb("d_iqT", [64, 8, 128])
        iw = P.sb("d_iw", [128, 8])
        score = P.sb("d_score", [128, S])
        work = P.sb("d_work", [128, S])
        maskT = P.sb("d_maskT", [128, NT, 128])
        rsb = self.rot("d_r", [128, 512])
        m8 = self.rot("d_m8", [128, 8])
        ET = P.sb("d_ET", [128, NT, 128])
        latT = P.sb("d_latT", [128, 8, 128])
        zz = P.sb("d_zz", [128, 16])
        yc = P.sb("d_yc", [128, 8, 64])
        ycT = P.sb("d_ycT", [128, 4, 128])
        ones = self.cst('ones')
        cmask = self.cst('cmask')
        CK, CQ, CIK, CIW = 1024, 1152, 1664, 1728
        for i in range(NT):
            x = xs[i % 2]
            nk = (i + 1) * 128
            self.dma(x[:, :], src[i * 128:(i + 1) * 128, :], r=[(src, i)], w=[x])
            self.rms(x, g, hn, st)
            self.transp8(hn, hnT, 4)
            pv, pk = self.psv(6, 1)
            for k in range(8):
                self.mm(pv[:, 0:128], hnT[:, k, :], w[:, k, CK:CK + 128], k == 0, k == 7, r=[hnT, w], w=pk)
            self.copy(craw[:, :], pv[:, 0:128], r=pk, w=[craw])
            self.act(work[:, 0:128], craw[:, :], AF.Square, r=[craw], w=[work, st], accum=st[:, 2:3])
            self.act(st[:, 3:4], st[:, 2:3], AF.Sqrt, r=[st], w=[st], scale=1.0 / 128, bias=self.epsb[:, 0:1])
            self.recip(st[:, 3:4], st[:, 3:4], r=[st], w=[st])
            self.stt(c_tm[:, i, :], craw[:, :], st[:, 3:4], gkv[:, :], ALU.mult, ALU.mult, r=[craw, st, gkv], w=[(c_tm, i)])
            pv7, pk7 = self.psv(7, 1)
            self.tr(pv7[:, 0:128], c_tm[:, i, :], r=[(c_tm, i)], w=pk7)
            self.copy(cT[:, i * 128:(i + 1) * 128], pv7[:, 0:128], r=pk7, w=[(cT, i)])
            for k in range(8):
                self.mm(pv[0:64, 0:128], w[:, k, CIK:CIK + 64], hnT[:, k, :], k == 0, k == 7, r=[hnT, w], w=pk)
            self.act(ikT[:, i * 128:(i + 1) * 128], pv[0:64, 0:128], AF.Copy, r=pk, w=[(ikT, i)], scale=0.125)
            for k in range(8):
                self.mm(pv7[:, 0:8], hnT[:, k, :], w[:, k, CIW:CIW + 8], k == 0, k == 7, r=[hnT, w], w=pk7)
            self.act(iw[:, :], pv7[:, 0:8], AF.Copy, r=pk7, w=[iw], scale=8 ** -0.5)
            qv, qk = self.psv(0, 2)
            for h in range(8):
                for k in range(8):
                    self.mm(qv[:, h * 128:(h + 1) * 128], w[:, k, h * 128:(h + 1) * 128], hnT[:, k, :], k == 0, k == 7,
                            r=[hnT, w], w=qk)
            self.act(qlT[:, :, :].rearrange("p a b -> p (a b)"), qv[:, :], AF.Copy, r=qk, w=[qlT], scale=128 ** -0.5)
            iv, ik_ = self.psv(2, 2)
            for h in range(8):
                for k in range(8):
                    self.mm(iv[0:64, h * 128:(h + 1) * 128], w[:, k, CQ + h * 64:CQ + (h + 1) * 64], hnT[:, k, :], k == 0, k == 7,
                            r=[hnT, w], w=ik_)
            self.copy(iqT[:, :, :].rearrange("p a b -> p (a b)"), iv[0:64, :], r=ik_, w=[iqT])
            cnt = 0
            for c0 in range(0, nk, 512):
                cw = min(512, nk - c0)
                for h in range(8):
                    sv, sk = self.psv(4 + cnt % 2, 1)
                    r_ = rsb[cnt % 2]
                    cnt += 1
                    self.mm(sv[:, 0:cw], iqT[:, h, :], ikT[:, c0:c0 + cw], True, True, r=[iqT, ikT], w=sk)
                    self.act(r_[:, 0:cw], sv[:, 0:cw], AF.Relu, r=sk, w=[r_])
                    if h == 0:
                        self.ts(score[:, c0:c0 + cw], r_[:, 0:cw], iw[:, 0:1], None, ALU.mult, None, r=[r_, iw], w=[score])
                    else:
                        self.stt(score[:, c0:c0 + cw], r_[:, 0:cw], iw[:, h:h + 1], score[:, c0:c0 + cw], ALU.mult, ALU.add,
                                 r=[r_, iw, score], w=[score])
            self.tt(score[:, i * 128:nk], score[:, i * 128:nk], cmask, ALU.add, r=[score, self.C], w=[score])
            if i >= 2:
                cur = score
                for rnd in range(32):
                    m = m8[rnd % 2]
                    P.op('dve', lambda e, m=m, cur=cur, nk=nk: e.max(out=m[:, :], in_=cur[:, 0:nk]), r=[cur], w=[m])
                    if rnd < 31:
                        P.op('dve', lambda e, m=m, cur=cur, nk=nk: e.match_replace(
                            out=work[:, 0:nk], in_to_replace=m[:, :], in_values=cur[:, 0:nk], imm_value=-1e30),
                            r=[cur, m], w=[work])
                        cur = work
                self.ts(work[:, 0:nk], score[:, 0:nk], m8[1][:, 7:8], None, ALU.is_ge, None, r=[score, m8[1]], w=[work])
            else:
                self.ts(work[:, 0:nk], score[:, 0:nk], -1e29, None, ALU.is_ge, None, r=[score], w=[work])
            for kt in range(i + 1):
                b = 6 + (kt // 4) % 2
                mv, mk = self.psv(b, 1)
                self.tr(mv[:, (kt % 4) * 128:(kt % 4 + 1) * 128], work[:, kt * 128:(kt + 1) * 128], r=[work], w=mk)
                if kt % 4 == 3 or kt == i:
                    k0 = (kt // 4) * 4
                    n = kt - k0 + 1
                    self.copy(maskT[:, k0:kt + 1, :].rearrange("p a b -> p (a b)"), mv[:, 0:n * 128], r=mk, w=[maskT], eng='pool' if False else 'act')
            lv, lk = self.psv(0, 4)
            av, ak = self.psv(6, 2)
            zv, zk = self.psv(5, 1)
            for h in range(8):
                for kt in range(i + 1):
                    self.mm(lv[:, kt * 128:(kt + 1) * 128], cT[:, kt * 128:(kt + 1) * 128], qlT[:, h, :], True, True,
                            r=[cT, qlT], w=lk)
                ETf = ET[:, :, :].rearrange("p a b -> p (a b)")
                self.act(ETf[:, 0:nk], lv[:, 0:nk], AF.Exp, r=lk + [b31], w=[ET], bias=b31[:, h:h + 1], scale=1.0)
                self.tt(ETf[:, 0:nk], ETf[:, 0:nk], maskT[:, :, :].rearrange("p a b -> p (a b)")[:, 0:nk], ALU.mult,
                        r=[ET, maskT], w=[ET])
                for kt in range(max(0, i - 1), i + 1):
                    self.tt(ET[:, kt, :], ET[:, kt, :], corrT[:, h, i - kt, :], ALU.mult, r=[ET, corrT], w=[ET])
                for kt in range(i + 1):
                    self.mm(av[:, h * 128:(h + 1) * 128], c_tm[:, kt, :], ET[:, kt, :], kt == 0, kt == i, r=[c_tm, ET], w=ak)
                    self.mm(zv[:, h:h + 1], ET[:, kt, :], ones[:, 0:1], kt == 0, kt == i, r=[ET, self.C], w=zk)
            self.copy(latT[:, :, :].rearrange("p a b -> p (a b)"), av[:, :], r=ak, w=[latT])
            self.copy(zz[:, 0:8], zv[:, 0:8], r=zk, w=[zz], eng='dve')
            self.recip(zz[:, 8:16], zz[:, 0:8], r=[zz], w=[zz])
            yv, yk = self.psv(4, 1)
            for h in range(8):
                self.mm(yv[:, h * 64:(h + 1) * 64], latT[:, h, :], w_uv[:, h, :], True, True, r=[latT, w_uv], w=yk)
            self.tt(yc[:, :, :], yv[:, :].rearrange("p (h e) -> p h e", e=64), bcast(zz[:, 8:16], 2, 64), ALU.mult,
                    r=yk + [zz], w=[yc])
            tv_, tk_ = self.psv(5, 1)
            ycf = yc[:, :, :].rearrange("p h e -> p (h e)")
            for k in range(4):
                self.tr(tv_[:, k * 128:(k + 1) * 128], ycf[:, k * 128:(k + 1) * 128], r=[yc], w=tk_)
            self.copy(ycT[:, :, :].rearrange("p a b -> p (a b)"), tv_[:, :], r=tk_, w=[ycT])
            ov, ok = self.psv(0, 2)
            for half in range(2):
                for k in range(4):
                    self.mm(ov[:, half * 512:(half + 1) * 512], ycT[:, k, :], w_out[:, k, half * 512:(half + 1) * 512],
                            k == 0, k == 3, r=[ycT, w_out], w=ok)
            self.tt(x[:, :], ov[:, :], x[:, :], ALU.add, r=ok + [x], w=[x])
            self.dma(dst[i * 128:(i + 1) * 128, :], x[:, :], r=[x], w=[(dst, i)])


    def phase_hgrn(self, l, src, acc):
        P = self.P
        din = self.din
        jx = l // 2
        w = P.sb("h_w", [128, 8, 2048])
        w_out = P.sb("h_wout", [128, 4, D])
        g = P.sb("h_g", [128, D])
        gn = P.sb("h_gn", [128, 512])
        gam = P.sb("h_gam", [128, 4, 512])
        lb = P.sb("h_lb", [128, 512])
        oml = P.sb("h_oml", [128, 512])
        lbT = P.sb("h_lbT", [128, 8])
        tmp = P.sb("h_tmp", [128, 512])
        self.gload(g, din['mix_norm'], din['mix_norm'][l:l + 1, :])
        self.gload(gn, din['odd_hg_norm'], din['odd_hg_norm'][jx:jx + 1, :])
        self.dma(w[:, :, :], din['odd_w_in'][jx][:, 1736:3784].rearrange("(k p) c -> p k c", p=128), r=[din['odd_w_in']], w=[w])
        self.dma(w_out[:, :, :], din['odd_w_out'][jx][512:1024, :].rearrange("(k p) c -> p k c", p=128), r=[din['odd_w_out']], w=[w_out])
        for ll in range(4):
            self.dma(gam[:, ll, :], din['hgrn_gamma'][ll:ll + 1, :].broadcast_to([128, 512]), r=[din['hgrn_gamma']], w=[(gam, ll)])
        self.act(gam[:, :, :], gam[:, :, :], AF.Exp, r=[gam], w=[gam])
        self.tt(tmp[:, :], gam[:, 0, :], gam[:, 1, :], ALU.add, r=[gam], w=[tmp])
        self.tt(tmp[:, :], tmp[:, :], gam[:, 2, :], ALU.add, r=[gam, tmp], w=[tmp])
        self.tt(tmp[:, :], tmp[:, :], gam[:, 3, :], ALU.add, r=[gam, tmp], w=[tmp])
        self.recip(tmp[:, :], tmp[:, :], r=[tmp], w=[tmp])
        P.op('dve', lambda e: e.memset(lb[:, :], 0.0), w=[lb])
        for ll in range(l):
            self.tt(lb[:, :], lb[:, :], gam[:, ll, :], ALU.add, r=[lb, gam], w=[lb])
        self.tt(lb[:, :], lb[:, :], tmp[:, :], ALU.mult, r=[lb, tmp], w=[lb])
        self.ts(oml[:, :], lb[:, :], -1.0, 1.0, ALU.mult, ALU.add, r=[lb], w=[oml])
        pv, pk = self.psv(0, 1)
        for h in range(4):
            self.tr(pv[:, h * 128:(h + 1) * 128], lb[:, h * 128:(h + 1) * 128], r=[lb], w=pk)
        for h in range(4):
            self.copy(lbT[:, h:h + 1], pv[:, h * 128:h * 128 + 1], r=pk, w=[lbT], eng='dve')
        self.ts(lbT[:, 4:8], lbT[:, 0:4], -1.0, 1.0, ALU.mult, ALU.add, r=[lbT], w=[lbT])
        Sst = [P.sb(f"h_S{j}", [128, 4, 128]) for j in range(2)]
        qt0 = P.sb("h_qt0", [128, 4, 128])
        qt1 = P.sb("h_qt1", [128, 4, 128])
        kh0 = P.sb("h_kh0", [128, 512])
        kh1 = P.sb("h_kh1", [128, 512])
        P.op('pool', lambda e: e.memset(Sst[0][:, :, :], 0.0), w=[Sst[0]])
        P.op('pool', lambda e: e.memset(qt0[:, :, :], 0.0), w=[qt0])
        P.op('pool', lambda e: e.memset(qt1[:, :, :], 0.0), w=[qt1])
        P.op('pool', lambda e: e.memset(kh0[:, :], 0.0), w=[kh0])
        P.op('pool', lambda e: e.memset(kh1[:, :], 0.0), w=[kh1])
        xs = self.rot("h_x", [128, D])
        hn = P.sb("h_hn", [128, D])
        hnT = P.sb("h_hnT", [128, 8, 128])
        st = P.sb("h_st", [128, 16])
        sg = P.sb("h_sg", [128, 512])
        f_tm = P.sb("h_f", [128, 512])
        lf = P.sb("h_lf", [128, 512])
        kk = P.sb("h_kk", [128, 512])
        i_sb = P.sb("h_i", [128, 512])
        sil = P.sb("h_sil", [128, 512])
        sgT = P.sb("h_sgT", [128, 4, 128])
        kkT = P.sb("h_kkT", [128, 4, 128])
        eAT = P.sb("h_eAT", [128, 4, 128])
        enAT = P.sb("h_enAT", [128, 4, 128])
        qtT = P.sb("h_qtT", [128, 4, 128])
        ktT = P.sb("h_ktT", [128, 4, 128])
        a_sb = P.sb("h_a", [128, 512])
        d_sb = P.sb("h_d", [128, 512])
        sc = self.rot("h_sc", [128, 128])
        y_sb = P.sb("h_y", [128, 512])
        yT = P.sb("h_yT", [128, 4, 128])
        acc_t = self.rot("h_acc", [128, D])
        U2 = self.cst('U2')
        B2 = self.cst('B2')
        HQ, HF, HI, HG = 0, 512, 1024, 1536

        def proj_tm(c0, bank):
            pvw, pkw = self.psv(bank, 1)
            for k in range(8):
                self.mm(pvw[:, :], hnT[:, k, :], w[:, k, c0:c0 + 512], k == 0, k == 7, r=[hnT, w], w=pkw)
            return pvw, pkw

        def proj_fm(c0, bank):
            pvw, pkw = self.psv(bank, 1)
            for h in range(4):
                for k in range(8):
                    self.mm(pvw[:, h * 128:(h + 1) * 128], w[:, k, c0 + h * 128:c0 + (h + 1) * 128], hnT[:, k, :],
                            k == 0, k == 7, r=[hnT, w], w=pkw)
            return pvw, pkw

        for i in range(NT):
            x = xs[i % 2]
            self.dma(x[:, :], src[i * 128:(i + 1) * 128, :], r=[(src, i)], w=[x])
            self.rms(x, g, hn, st)
            self.transp8(hn, hnT, 0)
            pf, kf = proj_tm(HF, 2)
            self.act(sg[:, :], pf[:, :], AF.Sigmoid, r=kf, w=[sg])
            self.tt(f_tm[:, :], sg[:, :], oml[:, :], ALU.mult, r=[sg, oml], w=[f_tm])
            self.tt(f_tm[:, :], f_tm[:, :], lb[:, :], ALU.add, r=[f_tm, lb], w=[f_tm])
            self.act(lf[:, :], f_tm[:, :], AF.Ln, r=[f_tm], w=[lf])
            self.ts(kk[:, :], f_tm[:, :], -1.0, 1.0, ALU.mult, ALU.add, r=[f_tm], w=[kk])
            pi_, ki_ = proj_tm(HI, 3)
            self.copy(i_sb[:, :], pi_[:, :], r=ki_, w=[i_sb])
            pg_, kg_ = proj_tm(HG, 4)
            self.act(sil[:, :], pg_[:, :], AF.Sigmoid, r=kg_, w=[sil])
            self.tt(sil[:, :], sil[:, :], pg_[:, :], ALU.mult, r=[sil] + kg_, w=[sil])
            pq, kq = proj_fm(HQ, 5)
            pfT, kfT = proj_fm(HF, 6)
            self.act(sgT[:, :, :].rearrange("p a b -> p (a b)"), pfT[:, :], AF.Sigmoid, r=kfT, w=[sgT])
            for h in range(4):
                self.ts(kkT[:, h, :], sgT[:, h, :], lbT[:, 4 + h:5 + h], lbT[:, h:h + 1], ALU.mult, ALU.add, r=[sgT, lbT], w=[kkT])
            self.ts(kkT[:, :, :], kkT[:, :, :], -1.0, 1.0, ALU.mult, ALU.add, r=[kkT], w=[kkT])
            pA, kA = self.psv(2, 1)
            self.mm(pA[:, :], U2, lf[:, :], True, True, r=[self.C, lf], w=kA)
            self.copy(a_sb[:, :], pA[:, :], r=kA, w=[a_sb])
            pE, kE = self.psv(3, 1)
            self.mm(pE[:, :], B2, lf[:, :], True, True, r=[self.C, lf], w=kE)
            pAT, kAT = self.psv(4, 1)
            for h in range(4):
                self.mm(pAT[:, h * 128:(h + 1) * 128], lf[:, h * 128:(h + 1) * 128], U2, True, True, r=[self.C, lf], w=kAT)
            self.act(eAT[:, :, :].rearrange("p a b -> p (a b)"), pAT[:, :], AF.Exp, r=kAT, w=[eAT])
            self.act(enAT[:, :, :].rearrange("p a b -> p (a b)"), pAT[:, :], AF.Exp, r=kAT, w=[enAT], scale=-1.0)
            self.tt(qtT[:, :, :].rearrange("p a b -> p (a b)"), pq[:, :], eAT[:, :, :].rearrange("p a b -> p (a b)"), ALU.mult,
                    r=kq + [eAT], w=[qtT])
            self.tt(ktT[:, :, :], kkT[:, :, :], enAT[:, :, :], ALU.mult, r=[kkT, enAT], w=[ktT])
            self.copy(qt0[:, :, 0:64], qtT[:, :, 0:64], r=[qtT], w=[qt0], eng='pool')
            self.copy(qt1[:, :, 64:128], qtT[:, :, 64:128], r=[qtT], w=[qt1], eng='pool')
            self.tt(d_sb[:, :], pE[:, :], a_sb[:, :], ALU.subtract, r=kE + [a_sb], w=[d_sb])
            self.act(d_sb[:, :], d_sb[:, :], AF.Exp, r=[d_sb], w=[d_sb])
            self.tt(kh0[0:64, :], d_sb[0:64, :], kk[0:64, :], ALU.mult, r=[d_sb, kk], w=[kh0])
            self.tt(kh1[64:128, :], d_sb[64:128, :], kk[64:128, :], ALU.mult, r=[d_sb, kk], w=[kh1])
            po, ko = self.psv(7, 1)
            for h in range(4):
                hs = slice(h * 128, (h + 1) * 128)
                S0, S1 = Sst[0], Sst[1]
                ps_, ks_ = self.psv(0, 1)
                self.mm(ps_[:, 0:128], ktT[:, h, :], qtT[:, h, :], True, True, r=[ktT, qtT], w=ks_)
                scb = sc[h % 2]
                self.tt(scb[:, :], ps_[:, 0:128], U2, ALU.mult, r=ks_ + [self.C], w=[scb])
                self.mm(po[:, hs], scb[:, :], i_sb[:, hs], True, False, r=[scb, i_sb], w=ko)
                self.mm(po[:, hs], qt0[:, h, :], S0[:, h, :], False, False, r=[qt0, (S0, h)], w=ko)
                p1, k1 = self.psv(1, 1)
                self.mm(p1[:, 0:128], kh0[:, hs], i_sb[:, hs], True, True, r=[kh0, i_sb], w=k1)
                self.stt(S1[:, h, :], S0[:, h, :], eAT[:, h, 63:64], p1[:, 0:128], ALU.mult, ALU.add, r=[(S0, h), eAT] + k1, w=[(S1, h)])
                self.mm(po[:, hs], qt1[:, h, :], S1[:, h, :], False, True, r=[qt1, (S1, h)], w=ko)
                p2, k2 = self.psv(2, 1)
                self.mm(p2[:, 0:128], kh1[:, hs], i_sb[:, hs], True, True, r=[kh1, i_sb], w=k2)
                self.stt(S0[:, h, :], S1[:, h, :], eAT[:, h, 127:128], p2[:, 0:128], ALU.mult, ALU.add, r=[(S1, h), eAT] + k2, w=[(S0, h)])
            for h in range(4):
                self.act(y_sb[:, h * 128:(h + 1) * 128], po[:, h * 128:(h + 1) * 128], AF.Square, r=ko, w=[y_sb, st], accum=st[:, 4 + h:5 + h])
            self.act(st[:, 8:12], st[:, 4:8], AF.Sqrt, r=[st], w=[st], scale=1.0 / 128, bias=self.epsb[:, 0:1])
            self.recip(st[:, 8:12], st[:, 8:12], r=[st], w=[st])
            for h in range(4):
                self.ts(y_sb[:, h * 128:(h + 1) * 128], po[:, h * 128:(h + 1) * 128], st[:, 8 + h:9 + h], None, ALU.mult, None,
                        r=ko + [st], w=[y_sb])
            self.tt(y_sb[:, :], y_sb[:, :], gn[:, :], ALU.mult, r=[y_sb, gn], w=[y_sb])
            self.tt(y_sb[:, :], y_sb[:, :], sil[:, :], ALU.mult, r=[y_sb, sil], w=[y_sb])
            tv_, tk_ = self.psv(5, 1)
            for k in range(4):
                self.tr(tv_[:, k * 128:(k + 1) * 128], y_sb[:, k * 128:(k + 1) * 128], r=[y_sb], w=tk_)
            self.copy(yT[:, :, :].rearrange("p a b -> p (a b)"), tv_[:, :], r=tk_, w=[yT])
            at = acc_t[i % 2]
            self.dma(at[:, :], acc[i * 128:(i + 1) * 128, :], r=[(acc, i)], w=[at])
            ov, ok = self.psv(2, 2)
            for half in range(2):
                for k in range(4):
                    self.mm(ov[:, half * 512:(half + 1) * 512], yT[:, k, :], w_out[:, k, half * 512:(half + 1) * 512],
                            k == 0, k == 3, r=[yT, w_out], w=ok)
            self.tt(at[:, :], ov[:, :], at[:, :], ALU.add, r=ok + [at], w=[at])
            self.dma(acc[i * 128:(i + 1) * 128, :], at[:, :], r=[at], w=[(acc, i)])

    def phase_copy(self, src, dst):
        xs = self.rot("c_x", [128, D])
        for i in range(NT):
            self.dma(xs[i % 2][:, :], src[i * 128:(i + 1) * 128, :], r=[(src, i)], w=[xs[i % 2]])
            self.dma(dst[i * 128:(i + 1) * 128, :], xs[i % 2][:, :], r=[xs[i % 2]], w=[(dst, i)])

    def phase_final(self, src):
        P = self.P
        g = P.sb("g_f", [128, D])
        self.gload(g, self.din['final_norm'], self.din['final_norm'][0:1, :])
        xs = self.rot("xf", [128, D])
        hns = self.rot("hnf", [128, D])
        sts = self.rot("stf", [128, 4])
        for i in range(NT):
            j = i % 2
            self.dma(xs[j][:, :], src[i * 128:(i + 1) * 128, :], r=[(src, i)], w=[xs[j]])
            self.rms(xs[j], g, hns[j], sts[j])
            self.dma(self.out[i * 128:(i + 1) * 128, :], hns[j][:, :], r=[hns[j]], w=[(self.out, i)])

    def run_phase(self, fn, *a):
        with ExitStack() as pes:
            self.P.pes = pes
            fn(*a)
            self.P.flush()
        self.P.pes = self.es

    def build(self, plan):
        cur = self.din['x']
        self.run_phase(self.phase_setup)
        for ph in plan:
            if ph[0] == 'xattn':
                self.run_phase(self.phase_xattn, ph[1], cur, self.hA)
                cur = self.hA
            elif ph[0] == 'even':
                self.run_phase(self.phase_even, ph[1], cur, self.hA)
                cur = self.hA
            elif ph[0] == 'peer':
                self.run_phase(self.phase_peer, ph[1], cur, self.hA)
                cur = self.hA
            elif ph[0] == 'dsa':
                other = self.hB if cur is not self.hB else self.hA
                self.run_phase(self.phase_dsa, ph[1], cur, other)
                cur = other
            elif ph[0] == 'hgrn':
                other = self.hB if cur is not self.hB else self.hA
                self.run_phase(self.phase_copy, cur, other)
                self.run_phase(self.phase_hgrn, ph[1], cur, other)
                cur = other
            elif ph[0] == 'odd':
                other = self.hB if cur is not self.hB else self.hA
                self.run_phase(self.phase_dsa, ph[1], cur, other)
                self.run_phase(self.phase_hgrn, ph[1], cur, other)
                cur = other
            elif ph[0] == 'final':
                self.run_phase(self.phase_final, cur)
        return self.nc


FULL_PLAN = []
for _l in range(DEPTH):
    FULL_PLAN.append(('even' if _l % 2 == 0 else 'odd', _l))
    FULL_PLAN.append(('xattn', _l))
    FULL_PLAN.append(('peer', _l))
FULL_PLAN.append(('final',))


def t5_bucket_np(d):
    d = np.maximum(d, 0)
    lr = np.log(np.maximum(d, 1).astype(np.float32) / np.float32(16)) / np.float32(np.log(128 / 16))
    large = 16 + (lr * np.float32(16)).astype(np.int32)
    large = np.minimum(large, 31)
    return np.where(d < 16, d, large)


def prep_inputs(inp):
    f = lambda a: np.ascontiguousarray(np.asarray(a, dtype=np.float32))
    shared = {}
    for k in IN_SHAPES:
        if k in ('x', 'mem'):
            continue
        if k == 'peer_keysT':
            a = np.asarray(inp['peer_keys'], dtype=np.float32)
            shared[k] = f(a.transpose(0, 4, 1, 2, 3).reshape(4, 128, 16, 128))
        elif k == 'even_conv_w':
            a = np.asarray(inp[k]).reshape(2, 3, 4, 128)
            shared[k] = f(a.transpose(0, 3, 2, 1).reshape(2, 128, 12))
        elif k == 'even_pool_scale':
            a = np.asarray(inp[k]).reshape(2, 4, 128)
            shared[k] = f(a.transpose(0, 2, 1))
        elif k == 'biasT':
            rb = np.asarray(inp['rel_bias'], dtype=np.float32)
            ss_, tq_ = np.arange(128)[:, None], np.arange(128)[None, :]
            out = np.zeros((128, 8, 2, 128), dtype=np.float32)
            for dl in (0, 1):
                dist = np.maximum(128 * dl + tq_ - ss_, 0)
                out[:, :, dl, :] = rb[t5_bucket_np(dist)].transpose(0, 2, 1)
            shared[k] = f(out)
        elif k in ('mem_norm', 'final_norm'):
            shared[k] = f(np.asarray(inp[k]).reshape(1, D))
        else:
            shared[k] = f(inp[k])
    return shared


def kernel(plan=None, **inp):
    plan = FULL_PLAN if plan is None else plan
    m = Model()
    nc = m.build(plan)
    shared = prep_inputs(inp)
    shared['consts'] = m.carr
    shared = {k: v for k, v in shared.items() if k in m.din}
    x = np.asarray(inp['x'], dtype=np.float32)
    mem = np.asarray(inp['mem'], dtype=np.float32)
    in_maps = []
    for b in range(8):
        d = dict(shared)
        if 'x' in m.din:
            d['x'] = np.ascontiguousarray(x[b])
        if 'mem' in m.din:
            d['mem'] = np.ascontiguousarray(mem[b])
        in_maps.append(d)
    res = run_bass_kernel_spmd(nc, in_maps, core_ids=list(range(8)))
    m.es.close()
    return np.stack([np.asarray(r["out"], dtype=np.float32) for r in res.results], axis=0)
```

```python
import numpy as np
import concourse.bass as bass
import concourse.mybir as mybir
from concourse.bass_utils import run_bass_kernel_spmd
from contextlib import ExitStack

F32 = mybir.dt.float32
I32 = mybir.dt.int32
U32 = mybir.dt.uint32
AF = mybir.ActivationFunctionType
ALU = mybir.AluOpType
AX = mybir.AxisListType

ENGS = ['pe', 'dve', 'act', 'pool', 'sp']
SEM_LIMIT = 30000
DMA_K = 16


class T:
    def __init__(self, h, name):
        self.h = h
        self.name = name
        self.st = {}

    def __getitem__(self, idx):
        return self.h[idx]


def bcast(ap, axis, n):
    l = [list(x) for x in ap.ap]
    l.insert(axis, [0, n])
    return bass.AP(ap.tensor, ap.offset, l)


def rep(ap, axis, n):
    l = [list(x) for x in ap.ap]
    assert l[axis][1] == 1
    l[axis] = [0, n]
    return bass.AP(ap.tensor, ap.offset, l)


class Prog:
    def __init__(self, nc, es):
        self.nc = nc
        self.es = es
        self.pes = es
        self.ops = {e: [] for e in ENGS}
        self.base = {e: 0 for e in ENGS}
        self.waited = {e: {} for e in ENGS}
        self.waited_dma = {e: set() for e in ENGS}
        self.tiles = []
        self.csems = {e: [] for e in ENGS}
        self.ccount = {e: 0 for e in ENGS}
        self.dsems = {}
        self.dcount = {e: 0 for e in ENGS}
        self.dma_hist = {e: [] for e in ENGS}

    def sb(self, name, shape, dt=F32, glob=False):
        es = self.es if glob else self.pes
        self.uid = getattr(self, 'uid', 0) + 1
        name = f"{name}_{self.uid}"
        t = T(es.enter_context(self.nc.sbuf_tensor(name, list(shape), dt)), name)
        self.tiles.append(t)
        return t

    def ps(self, name, shape, dt=F32):
        t = T(self.es.enter_context(self.nc.psum_tensor(name, list(shape), dt)), name)
        self.tiles.append(t)
        return t

    def dram(self, name, shape, dt=F32, kind="Internal"):
        t = T(self.nc.dram_tensor(name, list(shape), dt, kind=kind).ap(), name)
        self.tiles.append(t)
        return t

    @staticmethod
    def _norm(key):
        if isinstance(key, T):
            return key, None
        return key[0], key[1]

    def _entries(self, tile, sub):
        if sub is None:
            return list(tile.st.values())
        out = []
        if sub in tile.st:
            out.append(tile.st[sub])
        if None in tile.st:
            out.append(tile.st[None])
        return out

    def op(self, eng, fn, r=(), w=(), dma=False, extra=()):
        idx = len(self.ops[eng])
        deps = set(extra)
        for key in r:
            tile, sub = self._norm(key)
            for ent in self._entries(tile, sub):
                if ent[0] is not None:
                    deps.add(ent[0])
        for key in w:
            tile, sub = self._norm(key)
            for ent in self._entries(tile, sub):
                if ent[0] is not None:
                    deps.add(ent[0])
                for e2, i2 in ent[1].items():
                    deps.add((e2, i2))
        for key in r:
            tile, sub = self._norm(key)
            ent = tile.st.setdefault(sub, [None, {}])
            ent[1][eng] = idx
        for key in w:
            tile, sub = self._norm(key)
            if sub is None:
                tile.st = {None: [(eng, idx), {}]}
            else:
                tile.st[sub] = [(eng, idx), {}]
        if dma:
            h = self.dma_hist[eng]
            if len(h) >= DMA_K:
                deps.add((eng, h[-DMA_K]))
        final = []
        for (e2, i2) in sorted(deps, reverse=True):
            if e2 == eng and i2 == idx:
                continue
            if self.ops[e2][i2]['dma']:
                if (e2, i2) in self.waited_dma[eng]:
                    continue
                self.waited_dma[eng].add((e2, i2))
                final.append((e2, i2))
            else:
                if e2 == eng and eng == 'pe':
                    continue
                if self.waited[eng].get(e2, -1) >= i2:
                    continue
                self.waited[eng][e2] = i2
                final.append((e2, i2))
        o = dict(fn=fn, deps=final, dma=dma, sig=False)
        if dma:
            o['dma_n'] = self.dcount[eng]
            self.dcount[eng] += 1
            self.dma_hist[eng].append(idx)
        self.ops[eng].append(o)
        return (eng, idx)

    def _sigof(self, e2, i2):
        o = self.ops[e2][i2]
        if o['dma']:
            n = o['dma_n']
            return self.dsems[e2][n % DMA_K], 16 * (n // DMA_K + 1)
        j, v = o['sv']
        return self.csems[e2][j], v

    def flush(self):
        nc = self.nc
        last = {}
        for e in ENGS:
            for i in range(len(self.ops[e]) - 1, self.base[e] - 1, -1):
                if not self.ops[e][i]['dma'] and self.ops[e][i]['fn'] is not None:
                    last[e] = (e, i)
                    break
        dmas = []
        for e in ENGS:
            dmas += [(e, i) for i in self.dma_hist[e][-DMA_K:] if i >= self.base[e]]
        for e in ENGS:
            extra = [v for k, v in last.items() if k != e] + dmas
            self.op(e, None, extra=extra)
        for e in ENGS:
            for o in self.ops[e][self.base[e]:]:
                for (e2, i2) in o['deps']:
                    assert i2 >= self.base[e2], "cross-phase dep"
                    self.ops[e2][i2]['sig'] = True
        for e in ENGS:
            for o in self.ops[e][self.base[e]:]:
                if o['dma']:
                    if e not in self.dsems:
                        self.dsems[e] = [self.es.enter_context(nc.semaphore(f"d_{e}_{j}")) for j in range(DMA_K)]
                    continue
                if o['sig']:
                    c = self.ccount[e]
                    j = c // SEM_LIMIT
                    while len(self.csems[e]) <= j:
                        self.csems[e].append(self.es.enter_context(nc.semaphore(f"c_{e}_{len(self.csems[e])}")))
                    o['sv'] = (j, c % SEM_LIMIT + 1)
                    self.ccount[e] = c + 1

        def run(e, eng):
            for o in self.ops[e][self.base[e]:]:
                for (e2, i2) in o['deps']:
                    s, v = self._sigof(e2, i2)
                    eng.wait_ge(s, v)
                if o['fn'] is None:
                    continue
                ins = o['fn'](eng)
                if o['dma']:
                    n = o['dma_n']
                    ins.then_inc(self.dsems[e][n % DMA_K], 16)
                elif o['sig']:
                    j, v = o['sv']
                    ins.then_inc(self.csems[e][j], 1)

        with nc.Block() as block:
            @block.tensor
            def _(eng):
                run('pe', eng)

            @block.vector
            def _(eng):
                run('dve', eng)

            @block.scalar
            def _(eng):
                run('act', eng)

            @block.gpsimd
            def _(eng):
                run('pool', eng)

            @block.sync
            def _(eng):
                run('sp', eng)
        for e in ENGS:
            self.base[e] = len(self.ops[e])
        for t in self.tiles:
            t.st = {}

D = 1024
S = 2048
NT = 16
NMEM = 256
DEPTH = 4
EPS = 1e-6
FAST_MM = False
BF16 = mybir.dt.bfloat16
F32R = mybir.dt.float32r

IN_SHAPES = {
    'x': [S, D], 'mem': [NMEM, D], 'mix_norm': [4, D],
    'even_w_in': [2, D, 2048], 'even_conv_w': [2, 128, 12], 'even_pool_w': [2, 4, 128, 128],
    'even_pool_scale': [2, 128, 4], 'even_w_out': [2, D, D],
    'odd_w_in': [2, D, 3784], 'odd_kv_norm': [2, 128], 'odd_w_uv': [2, 8, 128, 64],
    'odd_hg_norm': [2, 512], 'odd_w_out': [2, D, D], 'hgrn_gamma': [4, 512],
    'rel_bias': [32, 8], 'invc': [128, 2048], 'biasT': [128, 8, 2, 128], 'mem_norm': [1, D], 'xattn_norm': [4, D],
    'xattn_wq': [4, D, D], 'xattn_wkv': [4, D, 2 * D], 'xattn_wo': [4, D, D],
    'peer_norm': [4, D], 'peer_wq': [4, D, 2048], 'peer_keysT': [4, 128, 16, 128],
    'peer_u': [4, 16384, D], 'peer_v': [4, 16384, D], 'final_norm': [1, D],
}


INVC = [None]


def make_consts():
    parts = {}
    parts['ident'] = np.eye(128, dtype=np.float32)
    t = np.arange(512)
    invc = np.concatenate([1.0 / np.minimum(t + 1, w) for w in (2, 4, 8, 16)]).astype(np.float32)
    INVC[0] = np.ascontiguousarray(np.tile(invc[None, :], (128, 1)).astype(np.float32))
    parts['iota16'] = np.tile(np.arange(16, dtype=np.float32)[None, :], (128, 1))
    ii = np.arange(128)
    parts['U2'] = ((ii[:, None] // 64 == ii[None, :] // 64) & (ii[:, None] <= ii[None, :])).astype(np.float32)
    parts['B2'] = (ii[:, None] // 64 == ii[None, :] // 64).astype(np.float32)
    parts['cmask'] = np.where(ii[None, :] <= ii[:, None], 0.0, -1e30).astype(np.float32)
    parts['ones'] = np.ones((128, 8), dtype=np.float32)
    off = 0
    lay = {}
    arrs = []
    for k, v in parts.items():
        lay[k] = (off, v.shape[1])
        off += v.shape[1]
        arrs.append(v.astype(np.float32))
    return np.ascontiguousarray(np.concatenate(arrs, axis=1)), lay


class Model:
    def __init__(self):
        self.nc = bass.Bass("TRN2", target_bir_lowering=False)
        self.es = ExitStack()
        self.P = Prog(self.nc, self.es)
        P = self.P
        carr, self.clay = make_consts()
        self.carr = carr
        model = self

        class LazyIn(dict):
            def __missing__(self, k):
                shp = list(carr.shape) if k == 'consts' else IN_SHAPES[k]
                t = P.dram(k, shp, F32, kind="ExternalInput")
                self[k] = t
                return t
        self.din = LazyIn()
        self.out = P.dram("out", [S, D], F32, kind="ExternalOutput")
        self.hA = P.dram("hA", [S, D])
        self.hB = P.dram("hB", [S, D])
        self.PS = P.ps("ps", [128, 8, 512])
        self.C = P.sb("consts_sb", [128, carr.shape[1]], glob=True)
        self.memT = P.sb("memT", [128, 8, NMEM], glob=True)
        self.rotc = {}

    def cst(self, name):
        o, w = self.clay[name]
        return self.C[:, o:o + w]

    def psv(self, b0, nb=2):
        ap = self.PS[:, b0:b0 + nb, :].rearrange("p a b -> p (a b)")
        return ap, [(self.PS, b) for b in range(b0, b0 + nb)]

    def mm(self, out, lhsT, rhs, start, stop, r, w, fast=True):
        if FAST_MM and fast and rhs.shape[-1] % 2 == 0 and out.shape[-1] % 2 == 0:
            lhsT = lhsT.bitcast(F32R)
            rhs = rhs.bitcast(F32R)
        self.P.op('pe', lambda e: e.matmul(out, lhsT, rhs, start=start, stop=stop), r=r, w=w)

    def tr(self, out, in_, r, w):
        ident = self.cst('ident')
        self.P.op('pe', lambda e: e.transpose(out, in_, ident), r=list(r) + [self.C], w=w)

    def act(self, out, in_, func, r, w, bias=None, scale=None, accum=None):
        kw = {}
        if bias is not None:
            kw['bias'] = bias
        if scale is not None:
            kw['scale'] = scale
        if accum is not None:
            kw['accum_out'] = accum
        self.P.op('act', lambda e: e.activation(out, in_, func, **kw), r=r, w=w)

    def tt(self, out, a, b, op, r, w, eng='dve'):
        self.P.op(eng, lambda e: e.tensor_tensor(out, a, b, op), r=r, w=w)

    def ts(self, out, a, s1, s2, op0, op1, r, w, eng='dve', accum=None):
        if op1 is None:
            self.P.op(eng, lambda e: e.tensor_scalar(out, a, s1, None, op0), r=r, w=w)
        elif accum is None:
            self.P.op(eng, lambda e: e.tensor_scalar(out, a, s1, s2, op0, op1), r=r, w=w)
        else:
            self.P.op(eng, lambda e: e.tensor_scalar(out, a, s1, s2, op0, op1, accum_out=accum), r=r, w=w)

    def stt(self, out, a, scalar, b, op0, op1, r, w):
        self.P.op('dve', lambda e: e.scalar_tensor_tensor(out, a, scalar, b, op0, op1), r=r, w=w)

    def copy(self, out, in_, r, w, eng='act'):
        if eng == 'act':
            self.P.op('act', lambda e: e.copy(out, in_), r=r, w=w)
        else:
            self.P.op(eng, lambda e: e.tensor_copy(out, in_), r=r, w=w)

    def dma(self, out, in_, r, w, eng='sp'):
        self.P.op(eng, lambda e: e.dma_start(out=out, in_=in_), r=r, w=w, dma=True)

    def recip(self, out, in_, r, w):
        self.P.op('dve', lambda e: e.reciprocal(out, in_), r=r, w=w)

    def rot(self, name, shape, n=2, dt=F32):
        return [self.P.sb(f"{name}{j}", shape, dt) for j in range(n)]

    def wload(self, dst, src_t, src_ap):
        self.dma(dst[:, :, :], src_ap.rearrange("(k p) c -> p k c", p=128), r=[src_t], w=[dst])

    def gload(self, dst, src_t, row_ap):
        n = row_ap.shape[1]
        self.dma(dst[:, :], row_ap.broadcast_to([128, n]), r=[src_t], w=[dst])

    def rms(self, x, g, hn, st, width=D):
        self.act(hn[:, :], x[:, :], AF.Square, r=[x], w=[hn, st], accum=st[:, 0:1])
        self.act(st[:, 1:2], st[:, 0:1], AF.Sqrt, r=[st], w=[st], scale=1.0 / width, bias=self.epsb[:, 0:1])
        self.recip(st[:, 1:2], st[:, 1:2], r=[st], w=[st])
        self.stt(hn[:, :], x[:, :], st[:, 1:2], g[:, :], ALU.mult, ALU.mult, r=[x, st, g], w=[hn])

    def transp8(self, src, dstT, b0, n=8):
        nb = (n * 128 + 511) // 512
        pv, pk = self.psv(b0, nb)
        for k in range(n):
            self.tr(pv[:, k * 128:(k + 1) * 128], src[:, k * 128:(k + 1) * 128], r=[src], w=pk)
        self.copy(dstT[:, :, :].rearrange("p a b -> p (a b)"), pv[:, 0:n * 128], r=pk, w=[dstT])

    def phase_setup(self):
        P = self.P
        self.dma(self.C[:, :], self.din['consts'][:, :], r=[self.din['consts']], w=[self.C])
        self.epsb = P.sb("epsb", [128, 1], glob=True)
        P.op('dve', lambda e: e.memset(self.epsb[:, :], EPS), w=[self.epsb])
        g = P.sb("g_mem", [128, D])
        self.gload(g, self.din['mem_norm'], self.din['mem_norm'][0:1, :])
        xs = self.rot("xm", [128, D])
        hn = self.rot("hnm", [128, D])
        st = self.rot("stm", [128, 4])
        mT = [P.sb(f"mT{j}", [128, 8, 128]) for j in range(2)]
        for m in range(2):
            self.dma(xs[m][:, :], self.din['mem'][m * 128:(m + 1) * 128, :], r=[self.din['mem']], w=[xs[m]])
            self.rms(xs[m], g, hn[m], st[m])
            self.transp8(hn[m], mT[m], 2 * m)
            self.copy(self.memT[:, :, m * 128:(m + 1) * 128], mT[m][:, :, :], r=[mT[m]], w=[(self.memT, m)], eng='dve')

    def phase_xattn(self, l, src, dst):
        P = self.P
        din = self.din
        wq = P.sb("wq", [128, 8, D])
        wo = P.sb("wo", [128, 8, D])
        KT = P.sb("KT", [128, 8, NMEM])
        V = P.sb("V", [128, 2, D])
        g = P.sb("g_x", [128, D])
        wkv = self.rot("wkv", [128, 8, 512])
        self.gload(g, din['xattn_norm'], din['xattn_norm'][l:l + 1, :])
        for c in range(4):
            wc = wkv[c % 2]
            self.wload(wc, din['xattn_wkv'], din['xattn_wkv'][l][:, c * 512:(c + 1) * 512])
            if c < 2:
                for f in range(4):
                    fc = c * 4 + f
                    b = fc % 8
                    pv, pk = self.psv(b, 1)
                    for k in range(8):
                        self.mm(pv[:, 0:NMEM], wc[:, k, f * 128:(f + 1) * 128], self.memT[:, k, :],
                                k == 0, k == 7, r=[wc, self.memT], w=pk)
                    self.act(KT[:, fc, :], pv[:, 0:NMEM], AF.Copy, r=pk, w=[(KT, fc)], scale=1.0 / 16.0)
            else:
                for m in range(2):
                    b = (c - 2) * 2 + m
                    pv, pk = self.psv(b, 1)
                    for k in range(8):
                        self.mm(pv[:, :], self.memT[:, k, m * 128:(m + 1) * 128], wc[:, k, :],
                                k == 0, k == 7, r=[wc, self.memT], w=pk)
                    self.copy(V[:, m, (c - 2) * 512:(c - 1) * 512], pv[:, :], r=pk, w=[(V, (m, c))])
        self.wload(wq, din['xattn_wq'], din['xattn_wq'][l])
        self.wload(wo, din['xattn_wo'], din['xattn_wo'][l])
        xs = self.rot("x", [128, D])
        hns = self.rot("hn", [128, D])
        hnTs = self.rot("hnT", [128, 8, 128])
        sts = self.rot("st", [128, 16])
        qTs = self.rot("qT", [128, 8, 128])
        Pms = self.rot("Pm", [128, 4 * NMEM])
        PTs = self.rot("PT", [128, 8, 128])
        oTs = self.rot("oT", [128, 8, 128])
        for i in range(NT):
            j = i % 2
            x, hn, hnT, st, qT, Pm, PT, oT = xs[j], hns[j], hnTs[j], sts[j], qTs[j], Pms[j], PTs[j], oTs[j]
            self.dma(x[:, :], src[i * 128:(i + 1) * 128, :], r=[(src, i)], w=[x])
            self.rms(x, g, hn, st)
            self.transp8(hn, hnT, 0)
            pv, pk = self.psv(2, 2)
            for f in range(8):
                for k in range(8):
                    self.mm(pv[:, f * 128:(f + 1) * 128], wq[:, k, f * 128:(f + 1) * 128], hnT[:, k, :],
                            k == 0, k == 7, r=[wq, hnT], w=pk)
            self.copy(qT[:, :, :].rearrange("p a b -> p (a b)"), pv[:, :], r=pk, w=[qT])
            lv, lk = self.psv(4, 2)
            for hd in range(4):
                for c in range(2):
                    self.mm(lv[:, hd * 256:(hd + 1) * 256], qT[:, hd * 2 + c, :], KT[:, hd * 2 + c, :],
                            c == 0, c == 1, r=[qT, KT], w=lk)
            lv3 = self.PS[:, 4:6, :].rearrange("p a (h m) -> p (a h) m", m=NMEM)
            P.op('dve', lambda e, lv3=lv3, st=st: e.tensor_reduce(out=st[:, 4:8], in_=lv3, axis=AX.X, op=ALU.max, negate=True),
                 r=lk, w=[st])
            for hd in range(4):
                self.act(Pm[:, hd * 256:(hd + 1) * 256], lv[:, hd * 256:(hd + 1) * 256], AF.Exp, r=lk + [st], w=[Pm, st],
                         bias=st[:, 4 + hd:5 + hd], scale=1.0, accum=st[:, 8 + hd:9 + hd])
            self.recip(st[:, 12:16], st[:, 8:12], r=[st], w=[st])
            for hd in range(4):
                self.ts(Pm[:, hd * 256:(hd + 1) * 256], Pm[:, hd * 256:(hd + 1) * 256], st[:, 12 + hd:13 + hd], None,
                        ALU.mult, None, r=[Pm, st], w=[Pm])
            self.transp8(Pm, PT, 6)
            ov, ok = self.psv(0, 2)
            for jj in range(8):
                for c in range(2):
                    self.mm(ov[:, jj * 128:(jj + 1) * 128], V[:, c, jj * 128:(jj + 1) * 128], PT[:, (jj // 2) * 2 + c, :],
                            c == 0, c == 1, r=[V, PT], w=ok)
            self.copy(oT[:, :, :].rearrange("p a b -> p (a b)"), ov[:, :], r=ok, w=[oT])
            yv, yk = self.psv(2, 2)
            for half in range(2):
                for k in range(8):
                    self.mm(yv[:, half * 512:(half + 1) * 512], oT[:, k, :], wo[:, k, half * 512:(half + 1) * 512],
                            k == 0, k == 7, r=[oT, wo], w=yk)
            self.tt(x[:, :], yv[:, :], x[:, :], ALU.add, r=yk + [x], w=[x])
            self.dma(dst[i * 128:(i + 1) * 128, :], x[:, :], r=[x], w=[(dst, i)])


    def phase_even(self, l, src, dst):
        P = self.P
        din = self.din
        jx = l // 2
        w_in = P.sb("e_win", [128, 8, 2048])
        w_out = P.sb("e_wout", [128, 8, D])
        pw = P.sb("e_pw", [128, 4, 128])
        cw = P.sb("e_cw", [128, 12])
        psc = P.sb("e_psc", [128, 4])
        g = P.sb("e_g", [128, D])
        self.gload(g, din['mix_norm'], din['mix_norm'][l:l + 1, :])
        self.wload(w_in, din['even_w_in'], din['even_w_in'][jx])
        self.wload(w_out, din['even_w_out'], din['even_w_out'][jx])
        self.dma(pw[:, :, :], din['even_pool_w'][jx].rearrange("g c d -> c g d"), r=[din['even_pool_w']], w=[pw])
        self.dma(cw[:, :], din['even_conv_w'][jx], r=[din['even_conv_w']], w=[cw])
        self.dma(psc[:, :], din['even_pool_scale'][jx], r=[din['even_pool_scale']], w=[psc])
        cu_halo = P.sb("e_cuh", [128, 4, 2])
        pv_halo = P.sb("e_pvh", [128, 4, 16])
        P.op('pool', lambda e: e.memset(cu_halo[:, :, :], 0.0), w=[cu_halo])
        P.op('pool', lambda e: e.memset(pv_halo[:, :, :], 0.0), w=[pv_halo])
        hnT = P.sb("e_hnT", [128, 8, 512])
        yT = P.sb("e_yT", [128, 8, 512])
        xs = self.rot("e_x", [128, D])
        hns = self.rot("e_hn", [128, D])
        sts = self.rot("e_st", [128, 4])
        hts = self.rot("e_ht", [128, 8, 128])
        cus = self.rot("e_cu", [128, 514])
        pvs = self.rot("e_pv", [128, 528])
        ut = self.rot("e_ut", [128, 512])
        zt = self.rot("e_z", [128, 512])
        sA = self.rot("e_sA", [128, 528])
        sB = self.rot("e_sB", [128, 528])
        pl = self.rot("e_pl", [128, 512])
        invc_t = P.sb("e_invc", [128, 2048])
        self.dma(invc_t[:, :], din['invc'][:, :], r=[din['invc']], w=[invc_t])
        invc = invc_t[:, :]
        for b in range(4):
            for t4 in range(4):
                i = b * 4 + t4
                j = i % 2
                self.dma(xs[j][:, :], src[i * 128:(i + 1) * 128, :], r=[(src, i)], w=[xs[j]])
                self.rms(xs[j], g, hns[j], sts[j])
                self.transp8(hns[j], hts[j], 6)
                self.copy(hnT[:, :, t4 * 128:(t4 + 1) * 128], hts[j][:, :, :], r=[hts[j]], w=[(hnT, t4)], eng='pool')
            for c4 in range(4):
                j = c4 % 2
                cu, pv, u_sb, z = cus[j], pvs[j], ut[j], zt[j]

                def proj(cc, bank):
                    pvw, pk = self.psv(bank, 1)
                    for k in range(8):
                        self.mm(pvw[:, :], w_in[:, k, cc * 128:(cc + 1) * 128], hnT[:, k, :], k == 0, k == 7,
                                r=[w_in, hnT], w=pk)
                    return pvw, pk
                pu, ku = proj(c4, 0)
                self.copy(u_sb[:, :], pu[:, :], r=ku, w=[u_sb])
                pg, kg = proj(4 + c4, 1)
                self.copy(cu[:, 0:2], cu_halo[:, c4, :], r=[(cu_halo, c4)], w=[cu], eng='pool')
                self.tt(cu[:, 2:514], pg[:, :], u_sb[:, :], ALU.mult, r=kg + [u_sb, cu], w=[cu])
                self.copy(cu_halo[:, c4, :], cu[:, 512:514], r=[cu], w=[(cu_halo, c4)], eng='pool')
                self.ts(z[:, :], cu[:, 0:512], cw[:, c4 * 3:c4 * 3 + 1], None, ALU.mult, None, r=[cu, cw], w=[z])
                self.stt(z[:, :], cu[:, 1:513], cw[:, c4 * 3 + 1:c4 * 3 + 2], z[:, :], ALU.mult, ALU.add, r=[cu, cw, z], w=[z])
                self.stt(z[:, :], cu[:, 2:514], cw[:, c4 * 3 + 2:c4 * 3 + 3], z[:, :], ALU.mult, ALU.add, r=[cu, cw, z], w=[z])
                pb, kb = proj(8 + c4, 2)
                self.tt(yT[:, c4, :], pb[:, :], z[:, :], ALU.mult, r=kb + [z], w=[(yT, c4)])
                pp, kp = proj(12 + c4, 3)
                self.copy(pv[:, 0:16], pv_halo[:, c4, :], r=[(pv_halo, c4)], w=[pv], eng='pool')
                self.copy(pv[:, 16:528], pp[:, :], r=kp + [pv], w=[pv])
                self.copy(pv_halo[:, c4, :], pv[:, 512:528], r=[pv], w=[(pv_halo, c4)], eng='pool')
                a_, b_ = sA[j], sB[j]
                self.tt(a_[:, 1:528], pv[:, 1:528], pv[:, 0:527], ALU.add, r=[pv], w=[a_])
                cur = a_
                other = b_
                lo = 1
                for st_ in range(c4):
                    sh = 2 ** (st_ + 1)
                    nlo = lo + sh
                    self.tt(other[:, nlo:528], cur[:, nlo:528], cur[:, nlo - sh:528 - sh], ALU.add, r=[cur], w=[other])
                    cur, other = other, cur
                    lo = nlo
                wdw = 2 ** (c4 + 1)
                pool_t = pl[j]
                if b == 0:
                    self.tt(pool_t[:, :], cur[:, 16:528], invc[:, c4 * 512:(c4 + 1) * 512], ALU.mult, r=[cur, invc_t], w=[pool_t])
                    self.tt(pool_t[:, :], pool_t[:, :], pv[:, 16:528], ALU.subtract, r=[pool_t, pv], w=[pool_t])
                else:
                    self.stt(pool_t[:, :], cur[:, 16:528], 1.0 / wdw, pv[:, 16:528], ALU.mult, ALU.subtract, r=[cur, pv], w=[pool_t])
                py, ky = self.psv(3, 1)
                self.mm(py[:, :], pw[:, c4, :], pool_t[:, :], True, True, r=[pw, pool_t], w=ky)
                self.act(yT[:, 4 + c4, :], py[:, :], AF.Copy, r=ky + [psc], w=[(yT, 4 + c4)], scale=psc[:, c4:c4 + 1])
            for t4 in range(4):
                i = b * 4 + t4
                j = i % 2
                self.dma(xs[j][:, :], src[i * 128:(i + 1) * 128, :], r=[(src, i)], w=[xs[j]])
                ov, ok = self.psv(4, 2)
                for half in range(2):
                    for k in range(8):
                        self.mm(ov[:, half * 512:(half + 1) * 512], yT[:, k, t4 * 128:(t4 + 1) * 128],
                                w_out[:, k, half * 512:(half + 1) * 512], k == 0, k == 7, r=[yT, w_out], w=ok)
                self.tt(xs[j][:, :], ov[:, :], xs[j][:, :], ALU.add, r=ok + [xs[j]], w=[xs[j]])
                self.dma(dst[i * 128:(i + 1) * 128, :], xs[j][:, :], r=[xs[j]], w=[(dst, i)])


    def top16(self, src_ap, work_ap, tv_ap, ti_ap, r, wk):
        P = self.P
        P.op('dve', lambda e: e.max(out=tv_ap[:, 0:8], in_=src_ap), r=r, w=wk)
        P.op('dve', lambda e: e.max_index(out=ti_ap[:, 0:8], in_max=tv_ap[:, 0:8], in_values=src_ap), r=r + wk, w=wk)
        P.op('dve', lambda e: e.match_replace(out=work_ap, in_to_replace=tv_ap[:, 0:8], in_values=src_ap, imm_value=-1e30),
             r=r + wk, w=wk)
        P.op('dve', lambda e: e.max(out=tv_ap[:, 8:16], in_=work_ap), r=wk, w=wk)
        P.op('dve', lambda e: e.max_index(out=ti_ap[:, 8:16], in_max=tv_ap[:, 8:16], in_values=work_ap), r=wk, w=wk)

    def phase_peer(self, l, src, dst):
        P = self.P
        din = self.din
        wq = P.sb("p_wq", [128, 8, 2048])
        keysT = P.sb("p_keys", [128, 16, 128])
        g = P.sb("p_g", [128, D])
        self.gload(g, din['peer_norm'], din['peer_norm'][l:l + 1, :])
        self.wload(wq, din['peer_wq'], din['peer_wq'][l])
        self.dma(keysT[:, :, :], din['peer_keysT'][l], r=[din['peer_keysT']], w=[keysT])
        u_l = din['peer_u'][:, :, :].rearrange("l n d -> (l n) d")
        v_l = din['peer_v'][:, :, :].rearrange("l n d -> (l n) d")
        xs = self.rot("p_x", [128, D], 3)
        xns = self.rot("p_xn", [128, D], 2)
        xnT = P.sb("p_xnT", [128, 8, 128])
        sts = self.rot("p_st", [128, 4], 2)
        qT = P.sb("p_qT", [128, 8, 128])
        s_sb = P.sb("p_s", [128, 1024])
        s_wk = P.sb("p_swk", [128, 1024])
        tv = P.sb("p_tv", [128, 16, 16])
        ti = P.sb("p_ti", [128, 16, 16], U32)
        tif = P.sb("p_tif", [128, 16, 16])
        cand = P.sb("p_cand", [128, 8, 256])
        cwk = P.sb("p_cwk", [128, 8, 256])
        cv = P.sb("p_cv", [128, 8, 16])
        cpos = P.sb("p_cpos", [128, 8, 16], U32)
        ab_u = P.sb("p_abu", [128, 2, 128], U32)
        ab_f = P.sb("p_abf", [128, 2, 128])
        eq = P.sb("p_eq", [128, 64, 16])
        isel = P.sb("p_isel", [128, 2, 128])
        eidf = P.sb("p_eidf", [128, 128])
        eids = self.rot("p_eid", [128, 128], 3, dt=I32)
        ggs = self.rot("p_gg", [128, 8, 16], 2)
        gz = P.sb("p_gz", [128, 16])
        actvs = self.rot("p_act", [128, 128], 2)
        t1 = P.sb("p_t1", [128, 128])
        t2 = P.sb("p_t2", [128, 128])
        wgts = self.rot("p_wgt", [128, 128], 2)
        junk = P.sb("p_junk", [128, D], BF16)
        NG = 8
        ugs = self.rot("p_ug", [128, D], NG, dt=BF16)
        NGV = 8
        vgs = self.rot("p_vg", [128, D], NGV, dt=BF16)
        dgs = self.rot("p_dg", [128, 128], 4, dt=BF16)
        iota16 = self.cst('iota16')
        ident = self.cst('ident')

        def front(i):
            x, xn, st = xs[i % 3], xns[i % 2], sts[i % 2]
            eid, gg = eids[i % 3], ggs[i % 2]
            self.dma(x[:, :], src[i * 128:(i + 1) * 128, :], r=[(src, i)], w=[x])
            self.rms(x, g, xn, st)
            yield
            self.transp8(xn, xnT, 0)
            yield
            for hf in range(2):
                qv, qk = self.psv(2, 2)
                for hh in range(8):
                    hp = hf * 8 + hh
                    for k in range(8):
                        self.mm(qv[:, hh * 128:(hh + 1) * 128], wq[:, k, hp * 128:(hp + 1) * 128], xnT[:, k, :],
                                k == 0, k == 7, r=[wq, xnT], w=qk)
                    if hh % 2 == 1:
                        yield
                self.copy(qT[:, :, :].rearrange("p a b -> p (a b)"), qv[:, :], r=qk, w=[qT])
                sv, sk = self.psv(0, 2)
                for hh in range(8):
                    hp = hf * 8 + hh
                    self.mm(sv[:, hh * 128:(hh + 1) * 128], qT[:, hh, :], keysT[:, hp, :], True, True, r=[qT, keysT], w=sk)
                self.copy(s_sb[:, :], sv[:, :], r=sk, w=[s_sb])
                yield
                for hh in range(8):
                    hp = hf * 8 + hh
                    self.top16(s_sb[:, hh * 128:(hh + 1) * 128], s_wk[:, hh * 128:(hh + 1) * 128], tv[:, hp, :], ti[:, hp, :],
                               r=[s_sb], wk=[(tv, hp), (ti, hp), (s_wk, hh)])
                    yield
            self.copy(tif[:, :, :], ti[:, :, :], r=[ti], w=[tif], eng='dve')
            tv4 = tv[:, :, :].rearrange("p (h t) a -> p h t a", t=2)
            tif4 = tif[:, :, :].rearrange("p (h t) a -> p h t a", t=2)
            cand4 = cand[:, :, :].rearrange("p h (a b) -> p h a b", b=16)
            self.tt(cand4, bcast(tv4[:, :, 0, :], 3, 16), bcast(tv4[:, :, 1, :], 2, 16), ALU.add, r=[tv], w=[cand])
            yield
            for h in range(8):
                self.top16(cand[:, h, :], cwk[:, h, :], cv[:, h, :], cpos[:, h, :],
                           r=[cand], wk=[(cv, h), (cpos, h), (cwk, h)])
                yield
            cposf = cpos[:, :, :].rearrange("p h a -> p (h a)")
            P.op('dve', lambda e: e.tensor_single_scalar(ab_u[:, 0, :], cposf, 4, ALU.logical_shift_right), r=[cpos], w=[ab_u])
            P.op('dve', lambda e: e.tensor_single_scalar(ab_u[:, 1, :], cposf, 15, ALU.bitwise_and), r=[cpos, ab_u], w=[ab_u])
            self.copy(ab_f[:, :, :], ab_u[:, :, :], r=[ab_u], w=[ab_f], eng='dve')
            yield
            for t in range(2):
                for hh in range(2):
                    self.tt(eq[:, :, :], bcast(ab_f[:, t, hh * 64:(hh + 1) * 64], 2, 16), bcast(iota16, 1, 64), ALU.is_equal,
                            r=[ab_f, self.C], w=[eq])
                    eq4 = eq[:, :, :].rearrange("p (h j) a -> p h j a", j=16)
                    self.tt(eq4, eq4, bcast(tif4[:, hh * 4:(hh + 1) * 4, t, :], 2, 16), ALU.mult, r=[eq, tif], w=[eq])
                    P.op('dve', lambda e, t=t, hh=hh: e.tensor_reduce(out=isel[:, t, hh * 64:(hh + 1) * 64], in_=eq[:, :, :],
                                                                      axis=AX.X, op=ALU.add), r=[eq], w=[isel])
                    yield
            self.stt(eidf[:, :], isel[:, 0, :], 128.0, isel[:, 1, :], ALU.mult, ALU.add, r=[isel], w=[eidf])
            if l > 0:
                self.ts(eidf[:, :], eidf[:, :], float(l * 16384), None, ALU.add, None, r=[eidf], w=[eidf])
            self.copy(eid[:, :], eidf[:, :], r=[eidf], w=[eid], eng='dve')
            yield
            self.tt(gg[:, :, :], cv[:, :, :], bcast(cv[:, :, 0], 2, 16), ALU.subtract, r=[cv], w=[gg])
            self.act(gg[:, :, :], gg[:, :, :], AF.Exp, r=[gg], w=[gg])
            P.op('dve', lambda e: e.tensor_reduce(out=gz[:, 0:8], in_=gg[:, :, :], axis=AX.X, op=ALU.add), r=[gg], w=[gz])
            self.recip(gz[:, 8:16], gz[:, 0:8], r=[gz], w=[gz])
            self.tt(gg[:, :, :], gg[:, :, :], bcast(gz[:, 8:16], 2, 16), ALU.mult, r=[gg, gz], w=[gg])
            yield

        def ustage(i):
            xn, eid, gg = xns[i % 2], eids[i % 3], ggs[i % 2]
            actv, wgt = actvs[i % 2], wgts[i % 2]
            xpv, xpk = self.psv(4, 2)
            self.copy(xpv[:, :], xn[:, :], r=[xn], w=xpk)
            for jj in range(128):
                ug = ugs[jj % NG]
                P.op('pool', lambda e, ug=ug, jj=jj: e.indirect_dma_start(
                    out=ug[:, :], out_offset=None, in_=u_l[:, :],
                    in_offset=bass.IndirectOffsetOnAxis(ap=eid[:, jj:jj + 1], axis=0)),
                    r=[eid, din['peer_u']], w=[ug], dma=True)
                P.op('dve', lambda e, ug=ug, jj=jj: e.scalar_tensor_tensor(
                    junk[:, :], ug[:, :], 1.0, xpv[:, :], ALU.mult, ALU.mult, accum_out=actv[:, jj:jj + 1]),
                    r=[ug] + xpk, w=[(actv, jj)])
                yield
            self.tt(t1[:, :], actv[:, :], actv[:, :], ALU.mult, r=[actv], w=[t1])
            self.ts(t1[:, :], t1[:, :], 0.044715, 1.0, ALU.mult, ALU.add, r=[t1], w=[t1])
            self.tt(t1[:, :], t1[:, :], actv[:, :], ALU.mult, r=[t1, actv], w=[t1])
            self.act(t2[:, :], t1[:, :], AF.Sigmoid, r=[t1], w=[t2], scale=1.5957691216057308)
            self.tt(t2[:, :], t2[:, :], actv[:, :], ALU.mult, r=[t2, actv], w=[t2])
            self.tt(wgt[:, :], t2[:, :], gg[:, :, :].rearrange("p h a -> p (h a)"), ALU.mult, r=[t2, gg], w=[wgt])
            yield

        def vstage(i):
            x, eid, wgt = xs[i % 3], eids[i % 3], wgts[i % 2]
            av, ak = self.psv(6, 2)
            for jj in range(128):
                vg = vgs[jj % NGV]
                dg = dgs[jj % 4]
                P.op('pool', lambda e, vg=vg, jj=jj: e.indirect_dma_start(
                    out=vg[:, :], out_offset=None, in_=v_l[:, :],
                    in_offset=bass.IndirectOffsetOnAxis(ap=eid[:, jj:jj + 1], axis=0)),
                    r=[eid, din['peer_v']], w=[vg], dma=True)
                self.act(dg[:, :], ident, AF.Copy, r=[self.C, wgt], w=[dg], scale=wgt[:, jj:jj + 1])
                for half in range(2):
                    self.mm(av[:, half * 512:(half + 1) * 512], dg[:, :], vg[:, half * 512:(half + 1) * 512],
                            jj == 0, jj == 127, r=[dg, vg], w=[ak[half]], fast=False)
                yield
            self.tt(x[:, :], av[:, :], x[:, :], ALU.add, r=ak + [x], w=[x])
            self.dma(dst[i * 128:(i + 1) * 128, :], x[:, :], r=[x], w=[(dst, i)])
            yield

        for _ in front(0):
            pass
        for it in range(NT + 1):
            active = []
            if it < NT:
                active.append(ustage(it))
            if it >= 1:
                active.append(vstage(it - 1))
            if it + 1 < NT:
                active.append(front(it + 1))
            while active:
                for gen in list(active):
                    try:
                        next(gen)
                    except StopIteration:
                        active.remove(gen)

    def phase_dsa(self, l, src, dst):
        P = self.P
        din = self.din
        jx = l // 2
        w = P.sb("d_w", [128, 8, 1736])
        w_uv = P.sb("d_wuv", [128, 8, 64])
        w_out = P.sb("d_wout", [128, 4, D])
        g = P.sb("d_g", [128, D])
        gkv = P.sb("d_gkv", [128, 128])
        b31 = P.sb("d_b31", [128, 16])
        corrT = P.sb("d_corr", [128, 8, 2, 128])
        self.gload(g, din['mix_norm'], din['mix_norm'][l:l + 1, :])
        self.gload(gkv, din['odd_kv_norm'], din['odd_kv_norm'][jx:jx + 1, :])
        self.gload(b31, din['rel_bias'], din['rel_bias'][31:32, :]) if False else self.dma(
            b31[:, 0:8], din['rel_bias'][31:32, :].broadcast_to([128, 8]), r=[din['rel_bias']], w=[b31])
        self.ts(b31[:, 8:16], b31[:, 0:8], -1.0, None, ALU.mult, None, r=[b31], w=[b31])
        self.dma(corrT[:, :, :, :], din['biasT'][:, :, :, :], r=[din['biasT']], w=[corrT])
        for h in range(8):
            self.act(corrT[:, h, :, :], corrT[:, h, :, :], AF.Exp, r=[corrT, b31], w=[corrT], bias=b31[:, 8 + h:9 + h], scale=1.0)
        self.dma(w[:, :, :], din['odd_w_in'][jx][:, 0:1736].rearrange("(k p) c -> p k c", p=128), r=[din['odd_w_in']], w=[w])
        self.dma(w_uv[:, :, :], din['odd_w_uv'][jx].rearrange("h r e -> r h e"), r=[din['odd_w_uv']], w=[w_uv])
        self.dma(w_out[:, :, :], din['odd_w_out'][jx][0:512, :].rearrange("(k p) c -> p k c", p=128), r=[din['odd_w_out']], w=[w_out])
        cT = P.sb("d_cT", [128, S])
        c_tm = P.sb("d_ctm", [128, NT, 128])
        ikT = P.sb("d_ikT", [64, S])
        xs = self.rot("d_x", [128, D])
        hn = P.sb("d_hn", [128, D])
        hnT = P.sb("d_hnT", [128, 8, 128])
        st = P.sb("d_st", [128, 8])
        craw = P.sb("d_craw", [128, 128])
        qlT = P.sb("d_qlT", [128, 8, 128])
        iqT = P.sb("d_iqT", [64, 8, 128])
        iw = P.sb("d_iw", [128, 8])
        score = P.sb("d_score", [128, S])
        work = P.sb("d_work", [128, S])
        maskT = P.sb("d_maskT", [128, NT, 128])
        rsb = self.rot("d_r", [128, 512])
        m8 = self.rot("d_m8", [128, 8])
        ET = P.sb("d_ET", [128, NT, 128])
        latT = P.sb("d_latT", [128, 8, 128])
        zz = P.sb("d_zz", [128, 16])
        yc = P.sb("d_yc", [128, 8, 64])
        ycT = P.sb("d_ycT", [128, 4, 128])
        ones = self.cst('ones')
        cmask = self.cst('cmask')
        CK, CQ, CIK, CIW = 1024, 1152, 1664, 1728
        for i in range(NT):
            x = xs[i % 2]
            nk = (i + 1) * 128
            self.dma(x[:, :], src[i * 128:(i + 1) * 128, :], r=[(src, i)], w=[x])
            self.rms(x, g, hn, st)
            self.transp8(hn, hnT, 4)
            pv, pk = self.psv(6, 1)
            for k in range(8):
                self.mm(pv[:, 0:128], hnT[:, k, :], w[:, k, CK:CK + 128], k == 0, k == 7, r=[hnT, w], w=pk)
            self.copy(craw[:, :], pv[:, 0:128], r=pk, w=[craw])
            self.act(work[:, 0:128], craw[:, :], AF.Square, r=[craw], w=[work, st], accum=st[:, 2:3])
            self.act(st[:, 3:4], st[:, 2:3], AF.Sqrt, r=[st], w=[st], scale=1.0 / 128, bias=self.epsb[:, 0:1])
            self.recip(st[:, 3:4], st[:, 3:4], r=[st], w=[st])
            self.stt(c_tm[:, i, :], craw[:, :], st[:, 3:4], gkv[:, :], ALU.mult, ALU.mult, r=[craw, st, gkv], w=[(c_tm, i)])
            pv7, pk7 = self.psv(7, 1)
            self.tr(pv7[:, 0:128], c_tm[:, i, :], r=[(c_tm, i)], w=pk7)
            self.copy(cT[:, i * 128:(i + 1) * 128], pv7[:, 0:128], r=pk7, w=[(cT, i)])
            for k in range(8):
                self.mm(pv[0:64, 0:128], w[:, k, CIK:CIK + 64], hnT[:, k, :], k == 0, k == 7, r=[hnT, w], w=pk)
            self.act(ikT[:, i * 128:(i + 1) * 128], pv[0:64, 0:128], AF.Copy, r=pk, w=[(ikT, i)], scale=0.125)
            for k in range(8):
                self.mm(pv7[:, 0:8], hnT[:, k, :], w[:, k, CIW:CIW + 8], k == 0, k == 7, r=[hnT, w], w=pk7)
            self.act(iw[:, :], pv7[:, 0:8], AF.Copy, r=pk7, w=[iw], scale=8 ** -0.5)
            qv, qk = self.psv(0, 2)
            for h in range(8):
                for k in range(8):
                    self.mm(qv[:, h * 128:(h + 1) * 128], w[:, k, h * 128:(h + 1) * 128], hnT[:, k, :], k == 0, k == 7,
                            r=[hnT, w], w=qk)
            self.act(qlT[:, :, :].rearrange("p a b -> p (a b)"), qv[:, :], AF.Copy, r=qk, w=[qlT], scale=128 ** -0.5)
            iv, ik_ = self.psv(2, 2)
            for h in range(8):
                for k in range(8):
                    self.mm(iv[0:64, h * 128:(h + 1) * 128], w[:, k, CQ + h * 64:CQ + (h + 1) * 64], hnT[:, k, :], k == 0, k == 7,
                            r=[hnT, w], w=ik_)
            self.copy(iqT[:, :, :].rearrange("p a b -> p (a b)"), iv[0:64, :], r=ik_, w=[iqT])
            cnt = 0
            for c0 in range(0, nk, 512):
                cw = min(512, nk - c0)
                for h in range(8):
                    sv, sk = self.psv(4 + cnt % 2, 1)
                    r_ = rsb[cnt % 2]
                    cnt += 1
                    self.mm(sv[:, 0:cw], iqT[:, h, :], ikT[:, c0:c0 + cw], True, True, r=[iqT, ikT], w=sk)
                    self.act(r_[:, 0:cw], sv[:, 0:cw], AF.Relu, r=sk, w=[r_])
                    if h == 0:
                        self.ts(score[:, c0:c0 + cw], r_[:, 0:cw], iw[:, 0:1], None, ALU.mult, None, r=[r_, iw], w=[score])
                    else:
                        self.stt(score[:, c0:c0 + cw], r_[:, 0:cw], iw[:, h:h + 1], score[:, c0:c0 + cw], ALU.mult, ALU.add,
                                 r=[r_, iw, score], w=[score])
            self.tt(score[:, i * 128:nk], score[:, i * 128:nk], cmask, ALU.add, r=[score, self.C], w=[score])
            if i >= 2:
                cur = score
                for rnd in range(32):
                    m = m8[rnd % 2]
                    P.op('dve', lambda e, m=m, cur=cur, nk=nk: e.max(out=m[:, :], in_=cur[:, 0:nk]), r=[cur], w=[m])
                    if rnd < 31:
                        P.op('dve', lambda e, m=m, cur=cur, nk=nk: e.match_replace(
                            out=work[:, 0:nk], in_to_replace=m[:, :], in_values=cur[:, 0:nk], imm_value=-1e30),
                            r=[cur, m], w=[work])
                        cur = work
                self.ts(work[:, 0:nk], score[:, 0:nk], m8[1][:, 7:8], None, ALU.is_ge, None, r=[score, m8[1]], w=[work])
            else:
                self.ts(work[:, 0:nk], score[:, 0:nk], -1e29, None, ALU.is_ge, None, r=[score], w=[work])
            for kt in range(i + 1):
                b = 6 + (kt // 4) % 2
                mv, mk = self.psv(b, 1)
                self.tr(mv[:, (kt % 4) * 128:(kt % 4 + 1) * 128], work[:, kt * 128:(kt + 1) * 128], r=[work], w=mk)
                if kt % 4 == 3 or kt == i:
                    k0 = (kt // 4) * 4
                    n = kt - k0 + 1
                    self.copy(maskT[:, k0:kt + 1, :].rearrange("p a b -> p (a b)"), mv[:, 0:n * 128], r=mk, w=[maskT], eng='pool' if False else 'act')
            lv, lk = self.psv(0, 4)
            av, ak = self.psv(6, 2)
            zv, zk = self.psv(5, 1)
            for h in range(8):
                for kt in range(i + 1):
                    self.mm(lv[:, kt * 128:(kt + 1) * 128], cT[:, kt * 128:(kt + 1) * 128], qlT[:, h, :], True, True,
                            r=[cT, qlT], w=lk)
                ETf = ET[:, :, :].rearrange("p a b -> p (a b)")
                self.act(ETf[:, 0:nk], lv[:, 0:nk], AF.Exp, r=lk + [b31], w=[ET], bias=b31[:, h:h + 1], scale=1.0)
                self.tt(ETf[:, 0:nk], ETf[:, 0:nk], maskT[:, :, :].rearrange("p a b -> p (a b)")[:, 0:nk], ALU.mult,
                        r=[ET, maskT], w=[ET])
                for kt in range(max(0, i - 1), i + 1):
                    self.tt(ET[:, kt, :], ET[:, kt, :], corrT[:, h, i - kt, :], ALU.mult, r=[ET, corrT], w=[ET])
                for kt in range(i + 1):
                    self.mm(av[:, h * 128:(h + 1) * 128], c_tm[:, kt, :], ET[:, kt, :], kt == 0, kt == i, r=[c_tm, ET], w=ak)
                    self.mm(zv[:, h:h + 1], ET[:, kt, :], ones[:, 0:1], kt == 0, kt == i, r=[ET, self.C], w=zk)
            self.copy(latT[:, :, :].rearrange("p a b -> p (a b)"), av[:, :], r=ak, w=[latT])
            self.copy(zz[:, 0:8], zv[:, 0:8], r=zk, w=[zz], eng='dve')
            self.recip(zz[:, 8:16], zz[:, 0:8], r=[zz], w=[zz])
            yv, yk = self.psv(4, 1)
            for h in range(8):
                self.mm(yv[:, h * 64:(h + 1) * 64], latT[:, h, :], w_uv[:, h, :], True, True, r=[latT, w_uv], w=yk)
            self.tt(yc[:, :, :], yv[:, :].rearrange("p (h e) -> p h e", e=64), bcast(zz[:, 8:16], 2, 64), ALU.mult,
                    r=yk + [zz], w=[yc])
            tv_, tk_ = self.psv(5, 1)
            ycf = yc[:, :, :].rearrange("p h e -> p (h e)")
            for k in range(4):
                self.tr(tv_[:, k * 128:(k + 1) * 128], ycf[:, k * 128:(k + 1) * 128], r=[yc], w=tk_)
            self.copy(ycT[:, :, :].rearrange("p a b -> p (a b)"), tv_[:, :], r=tk_, w=[ycT])
            ov, ok = self.psv(0, 2)
            for half in range(2):
                for k in range(4):
                    self.mm(ov[:, half * 512:(half + 1) * 512], ycT[:, k, :], w_out[:, k, half * 512:(half + 1) * 512],
                            k == 0, k == 3, r=[ycT, w_out], w=ok)
            self.tt(x[:, :], ov[:, :], x[:, :], ALU.add, r=ok + [x], w=[x])
            self.dma(dst[i * 128:(i + 1) * 128, :], x[:, :], r=[x], w=[(dst, i)])


    def phase_hgrn(self, l, src, acc):
        P = self.P
        din = self.din
        jx = l // 2
        w = P.sb("h_w", [128, 8, 2048])
        w_out = P.sb("h_wout", [128, 4, D])
        g = P.sb("h_g", [128, D])
        gn = P.sb("h_gn", [128, 512])
        gam = P.sb("h_gam", [128, 4, 512])
        lb = P.sb("h_lb", [128, 512])
        oml = P.sb("h_oml", [128, 512])
        lbT = P.sb("h_lbT", [128, 8])
        tmp = P.sb("h_tmp", [128, 512])
        self.gload(g, din['mix_norm'], din['mix_norm'][l:l + 1, :])
        self.gload(gn, din['odd_hg_norm'], din['odd_hg_norm'][jx:jx + 1, :])
        self.dma(w[:, :, :], din['odd_w_in'][jx][:, 1736:3784].rearrange("(k p) c -> p k c", p=128), r=[din['odd_w_in']], w=[w])
        self.dma(w_out[:, :, :], din['odd_w_out'][jx][512:1024, :].rearrange("(k p) c -> p k c", p=128), r=[din['odd_w_out']], w=[w_out])
        for ll in range(4):
            self.dma(gam[:, ll, :], din['hgrn_gamma'][ll:ll + 1, :].broadcast_to([128, 512]), r=[din['hgrn_gamma']], w=[(gam, ll)])
        self.act(gam[:, :, :], gam[:, :, :], AF.Exp, r=[gam], w=[gam])
        self.tt(tmp[:, :], gam[:, 0, :], gam[:, 1, :], ALU.add, r=[gam], w=[tmp])
        self.tt(tmp[:, :], tmp[:, :], gam[:, 2, :], ALU.add, r=[gam, tmp], w=[tmp])
        self.tt(tmp[:, :], tmp[:, :], gam[:, 3, :], ALU.add, r=[gam, tmp], w=[tmp])
        self.recip(tmp[:, :], tmp[:, :], r=[tmp], w=[tmp])
        P.op('dve', lambda e: e.memset(lb[:, :], 0.0), w=[lb])
        for ll in range(l):
            self.tt(lb[:, :], lb[:, :], gam[:, ll, :], ALU.add, r=[lb, gam], w=[lb])
        self.tt(lb[:, :], lb[:, :], tmp[:, :], ALU.mult, r=[lb, tmp], w=[lb])
        self.ts(oml[:, :], lb[:, :], -1.0, 1.0, ALU.mult, ALU.add, r=[lb], w=[oml])
        pv, pk = self.psv(0, 1)
        for h in range(4):
            self.tr(pv[:, h * 128:(h + 1) * 128], lb[:, h * 128:(h + 1) * 128], r=[lb], w=pk)
        for h in range(4):
            self.copy(lbT[:, h:h + 1], pv[:, h * 128:h * 128 + 1], r=pk, w=[lbT], eng='dve')
        self.ts(lbT[:, 4:8], lbT[:, 0:4], -1.0, 1.0, ALU.mult, ALU.add, r=[lbT], w=[lbT])
        Sst = [P.sb(f"h_S{j}", [128, 4, 128]) for j in range(2)]
        qt0 = P.sb("h_qt0", [128, 4, 128])
        qt1 = P.sb("h_qt1", [128, 4, 128])
        kh0 = P.sb("h_kh0", [128, 512])
        kh1 = P.sb("h_kh1", [128, 512])
        P.op('pool', lambda e: e.memset(Sst[0][:, :, :], 0.0), w=[Sst[0]])
        P.op('pool', lambda e: e.memset(qt0[:, :, :], 0.0), w=[qt0])
        P.op('pool', lambda e: e.memset(qt1[:, :, :], 0.0), w=[qt1])
        P.op('pool', lambda e: e.memset(kh0[:, :], 0.0), w=[kh0])
        P.op('pool', lambda e: e.memset(kh1[:, :], 0.0), w=[kh1])
        xs = self.rot("h_x", [128, D])
        hn = P.sb("h_hn", [128, D])
        hnT = P.sb("h_hnT", [128, 8, 128])
        st = P.sb("h_st", [128, 16])
        sg = P.sb("h_sg", [128, 512])
        f_tm = P.sb("h_f", [128, 512])
        lf = P.sb("h_lf", [128, 512])
        kk = P.sb("h_kk", [128, 512])
        i_sb = P.sb("h_i", [128, 512])
        sil = P.sb("h_sil", [128, 512])
        sgT = P.sb("h_sgT", [128, 4, 128])
        kkT = P.sb("h_kkT", [128, 4, 128])
        eAT = P.sb("h_eAT", [128, 4, 128])
        enAT = P.sb("h_enAT", [128, 4, 128])
        qtT = P.sb("h_qtT", [128, 4, 128])
        ktT = P.sb("h_ktT", [128, 4, 128])
        a_sb = P.sb("h_a", [128, 512])
        d_sb = P.sb("h_d", [128, 512])
        sc = self.rot("h_sc", [128, 128])
        y_sb = P.sb("h_y", [128, 512])
        yT = P.sb("h_yT", [128, 4, 128])
        acc_t = self.rot("h_acc", [128, D])
        U2 = self.cst('U2')
        B2 = self.cst('B2')
        HQ, HF, HI, HG = 0, 512, 1024, 1536

        def proj_tm(c0, bank):
            pvw, pkw = self.psv(bank, 1)
            for k in range(8):
                self.mm(pvw[:, :], hnT[:, k, :], w[:, k, c0:c0 + 512], k == 0, k == 7, r=[hnT, w], w=pkw)
            return pvw, pkw

        def proj_fm(c0, bank):
            pvw, pkw = self.psv(bank, 1)
            for h in range(4):
                for k in range(8):
                    self.mm(pvw[:, h * 128:(h + 1) * 128], w[:, k, c0 + h * 128:c0 + (h + 1) * 128], hnT[:, k, :],
                            k == 0, k == 7, r=[hnT, w], w=pkw)
            return pvw, pkw

        for i in range(NT):
            x = xs[i % 2]
            self.dma(x[:, :], src[i * 128:(i + 1) * 128, :], r=[(src, i)], w=[x])
            self.rms(x, g, hn, st)
            self.transp8(hn, hnT, 0)
            pf, kf = proj_tm(HF, 2)
            self.act(sg[:, :], pf[:, :], AF.Sigmoid, r=kf, w=[sg])
            self.tt(f_tm[:, :], sg[:, :], oml[:, :], ALU.mult, r=[sg, oml], w=[f_tm])
            self.tt(f_tm[:, :], f_tm[:, :], lb[:, :], ALU.add, r=[f_tm, lb], w=[f_tm])
            self.act(lf[:, :], f_tm[:, :], AF.Ln, r=[f_tm], w=[lf])
            self.ts(kk[:, :], f_tm[:, :], -1.0, 1.0, ALU.mult, ALU.add, r=[f_tm], w=[kk])
            pi_, ki_ = proj_tm(HI, 3)
            self.copy(i_sb[:, :], pi_[:, :], r=ki_, w=[i_sb])
            pg_, kg_ = proj_tm(HG, 4)
            self.act(sil[:, :], pg_[:, :], AF.Sigmoid, r=kg_, w=[sil])
            self.tt(sil[:, :], sil[:, :], pg_[:, :], ALU.mult, r=[sil] + kg_, w=[sil])
            pq, kq = proj_fm(HQ, 5)
            pfT, kfT = proj_fm(HF, 6)
            self.act(sgT[:, :, :].rearrange("p a b -> p (a b)"), pfT[:, :], AF.Sigmoid, r=kfT, w=[sgT])
            for h in range(4):
                self.ts(kkT[:, h, :], sgT[:, h, :], lbT[:, 4 + h:5 + h], lbT[:, h:h + 1], ALU.mult, ALU.add, r=[sgT, lbT], w=[kkT])
            self.ts(kkT[:, :, :], kkT[:, :, :], -1.0, 1.0, ALU.mult, ALU.add, r=[kkT], w=[kkT])
            pA, kA = self.psv(2, 1)
            self.mm(pA[:, :], U2, lf[:, :], True, True, r=[self.C, lf], w=kA)
            self.copy(a_sb[:, :], pA[:, :], r=kA, w=[a_sb])
            pE, kE = self.psv(3, 1)
            self.mm(pE[:, :], B2, lf[:, :], True, True, r=[self.C, lf], w=kE)
            pAT, kAT = self.psv(4, 1)
            for h in range(4):
                self.mm(pAT[:, h * 128:(h + 1) * 128], lf[:, h * 128:(h + 1) * 128], U2, True, True, r=[self.C, lf], w=kAT)
            self.act(eAT[:, :, :].rearrange("p a b -> p (a b)"), pAT[:, :], AF.Exp, r=kAT, w=[eAT])
            self.act(enAT[:, :, :].rearrange("p a b -> p (a b)"), pAT[:, :], AF.Exp, r=kAT, w=[enAT], scale=-1.0)
            self.tt(qtT[:, :, :].rearrange("p a b -> p (a b)"), pq[:, :], eAT[:, :, :].rearrange("p a b -> p (a b)"), ALU.mult,
                    r=kq + [eAT], w=[qtT])
            self.tt(ktT[:, :, :], kkT[:, :, :], enAT[:, :, :], ALU.mult, r=[kkT, enAT], w=[ktT])
            self.copy(qt0[:, :, 0:64], qtT[:, :, 0:64], r=[qtT], w=[qt0], eng='pool')
            self.copy(qt1[:, :, 64:128], qtT[:, :, 64:128], r=[qtT], w=[qt1], eng='pool')
            self.tt(d_sb[:, :], pE[:, :], a_sb[:, :], ALU.subtract, r=kE + [a_sb], w=[d_sb])
            self.act(d_sb[:, :], d_sb[:, :], AF.Exp, r=[d_sb], w=[d_sb])
            self.tt(kh0[0:64, :], d_sb[0:64, :], kk[0:64, :], ALU.mult, r=[d_sb, kk], w=[kh0])
            self.tt(kh1[64:128, :], d_sb[64:128, :], kk[64:128, :], ALU.mult, r=[d_sb, kk], w=[kh1])
            po, ko = self.psv(7, 1)
            for h in range(4):
                hs = slice(h * 128, (h + 1) * 128)
                S0, S1 = Sst[0], Sst[1]
                ps_, ks_ = self.psv(0, 1)
                self.mm(ps_[:, 0:128], ktT[:, h, :], qtT[:, h, :], True, True, r=[ktT, qtT], w=ks_)
                scb = sc[h % 2]
                self.tt(scb[:, :], ps_[:, 0:128], U2, ALU.mult, r=ks_ + [self.C], w=[scb])
                self.mm(po[:, hs], scb[:, :], i_sb[:, hs], True, False, r=[scb, i_sb], w=ko)
                self.mm(po[:, hs], qt0[:, h, :], S0[:, h, :], False, False, r=[qt0, (S0, h)], w=ko)
                p1, k1 = self.psv(1, 1)
                self.mm(p1[:, 0:128], kh0[:, hs], i_sb[:, hs], True, True, r=[kh0, i_sb], w=k1)
                self.stt(S1[:, h, :], S0[:, h, :], eAT[:, h, 63:64], p1[:, 0:128], ALU.mult, ALU.add, r=[(S0, h), eAT] + k1, w=[(S1, h)])
                self.mm(po[:, hs], qt1[:, h, :], S1[:, h, :], False, True, r=[qt1, (S1, h)], w=ko)
                p2, k2 = self.psv(2, 1)
                self.mm(p2[:, 0:128], kh1[:, hs], i_sb[:, hs], True, True, r=[kh1, i_sb], w=k2)
                self.stt(S0[:, h, :], S1[:, h, :], eAT[:, h, 127:128], p2[:, 0:128], ALU.mult, ALU.add, r=[(S1, h), eAT] + k2, w=[(S0, h)])
            for h in range(4):
                self.act(y_sb[:, h * 128:(h + 1) * 128], po[:, h * 128:(h + 1) * 128], AF.Square, r=ko, w=[y_sb, st], accum=st[:, 4 + h:5 + h])
            self.act(st[:, 8:12], st[:, 4:8], AF.Sqrt, r=[st], w=[st], scale=1.0 / 128, bias=self.epsb[:, 0:1])
            self.recip(st[:, 8:12], st[:, 8:12], r=[st], w=[st])
            for h in range(4):
                self.ts(y_sb[:, h * 128:(h + 1) * 128], po[:, h * 128:(h + 1) * 128], st[:, 8 + h:9 + h], None, ALU.mult, None,
                        r=ko + [st], w=[y_sb])
            self.tt(y_sb[:, :], y_sb[:, :], gn[:, :], ALU.mult, r=[y_sb, gn], w=[y_sb])
            self.tt(y_sb[:, :], y_sb[:, :], sil[:, :], ALU.mult, r=[y_sb, sil], w=[y_sb])
            tv_, tk_ = self.psv(5, 1)
            for k in range(4):
                self.tr(tv_[:, k * 128:(k + 1) * 128], y_sb[:, k * 128:(k + 1) * 128], r=[y_sb], w=tk_)
            self.copy(yT[:, :, :].rearrange("p a b -> p (a b)"), tv_[:, :], r=tk_, w=[yT])
            at = acc_t[i % 2]
            self.dma(at[:, :], acc[i * 128:(i + 1) * 128, :], r=[(acc, i)], w=[at])
            ov, ok = self.psv(2, 2)
            for half in range(2):
                for k in range(4):
                    self.mm(ov[:, half * 512:(half + 1) * 512], yT[:, k, :], w_out[:, k, half * 512:(half + 1) * 512],
                            k == 0, k == 3, r=[yT, w_out], w=ok)
            self.tt(at[:, :], ov[:, :], at[:, :], ALU.add, r=ok + [at], w=[at])
            self.dma(acc[i * 128:(i + 1) * 128, :], at[:, :], r=[at], w=[(acc, i)])

    def phase_copy(self, src, dst):
        xs = self.rot("c_x", [128, D])
        for i in range(NT):
            self.dma(xs[i % 2][:, :], src[i * 128:(i + 1) * 128, :], r=[(src, i)], w=[xs[i % 2]])
            self.dma(dst[i * 128:(i + 1) * 128, :], xs[i % 2][:, :], r=[xs[i % 2]], w=[(dst, i)])

    def phase_final(self, src):
        P = self.P
        g = P.sb("g_f", [128, D])
        self.gload(g, self.din['final_norm'], self.din['final_norm'][0:1, :])
        xs = self.rot("xf", [128, D])
        hns = self.rot("hnf", [128, D])
        sts = self.rot("stf", [128, 4])
        for i in range(NT):
            j = i % 2
            self.dma(xs[j][:, :], src[i * 128:(i + 1) * 128, :], r=[(src, i)], w=[xs[j]])
            self.rms(xs[j], g, hns[j], sts[j])
            self.dma(self.out[i * 128:(i + 1) * 128, :], hns[j][:, :], r=[hns[j]], w=[(self.out, i)])

    def run_phase(self, fn, *a):
        with ExitStack() as pes:
            self.P.pes = pes
            fn(*a)
            self.P.flush()
        self.P.pes = self.es

    def build(self, plan):
        cur = self.din['x']
        self.run_phase(self.phase_setup)
        for ph in plan:
            if ph[0] == 'xattn':
                self.run_phase(self.phase_xattn, ph[1], cur, self.hA)
                cur = self.hA
            elif ph[0] == 'even':
                self.run_phase(self.phase_even, ph[1], cur, self.hA)
                cur = self.hA
            elif ph[0] == 'peer':
                self.run_phase(self.phase_peer, ph[1], cur, self.hA)
                cur = self.hA
            elif ph[0] == 'dsa':
                other = self.hB if cur is not self.hB else self.hA
                self.run_phase(self.phase_dsa, ph[1], cur, other)
                cur = other
            elif ph[0] == 'hgrn':
                other = self.hB if cur is not self.hB else self.hA
                self.run_phase(self.phase_copy, cur, other)
                self.run_phase(self.phase_hgrn, ph[1], cur, other)
                cur = other
            elif ph[0] == 'odd':
                other = self.hB if cur is not self.hB else self.hA
                self.run_phase(self.phase_dsa, ph[1], cur, other)
                self.run_phase(self.phase_hgrn, ph[1], cur, other)
                cur = other
            elif ph[0] == 'final':
                self.run_phase(self.phase_final, cur)
        return self.nc


FULL_PLAN = []
for _l in range(DEPTH):
    FULL_PLAN.append(('even' if _l % 2 == 0 else 'odd', _l))
    FULL_PLAN.append(('xattn', _l))
    FULL_PLAN.append(('peer', _l))
FULL_PLAN.append(('final',))


def t5_bucket_np(d):
    d = np.maximum(d, 0)
    lr = np.log(np.maximum(d, 1).astype(np.float32) / np.float32(16)) / np.float32(np.log(128 / 16))
    large = 16 + (lr * np.float32(16)).astype(np.int32)
    large = np.minimum(large, 31)
    return np.where(d < 16, d, large)


def prep_inputs(inp):
    f = lambda a: np.ascontiguousarray(np.asarray(a, dtype=np.float32))
    shared = {}
    for k in IN_SHAPES:
        if k in ('x', 'mem'):
            continue
        if k == 'peer_keysT':
            a = np.asarray(inp['peer_keys'], dtype=np.float32)
            shared[k] = f(a.transpose(0, 4, 1, 2, 3).reshape(4, 128, 16, 128))
        elif k == 'even_conv_w':
            a = np.asarray(inp[k]).reshape(2, 3, 4, 128)
            shared[k] = f(a.transpose(0, 3, 2, 1).reshape(2, 128, 12))
        elif k == 'even_pool_scale':
            a = np.asarray(inp[k]).reshape(2, 4, 128)
            shared[k] = f(a.transpose(0, 2, 1))
        elif k == 'invc':
            shared[k] = INVC[0]
        elif k == 'biasT':
            rb = np.asarray(inp['rel_bias'], dtype=np.float32)
            ss_, tq_ = np.arange(128)[:, None], np.arange(128)[None, :]
            out = np.zeros((128, 8, 2, 128), dtype=np.float32)
            for dl in (0, 1):
                dist = np.maximum(128 * dl + tq_ - ss_, 0)
                out[:, :, dl, :] = rb[t5_bucket_np(dist)].transpose(0, 2, 1)
            shared[k] = f(out)
        elif k in ('mem_norm', 'final_norm'):
            shared[k] = f(np.asarray(inp[k]).reshape(1, D))
        else:
            shared[k] = f(inp[k])
    return shared


def kernel(plan=None, **inp):
    plan = FULL_PLAN if plan is None else plan
    m = Model()
    nc = m.build(plan)
    shared = prep_inputs(inp)
    shared['consts'] = m.carr
    shared = {k: v for k, v in shared.items() if k in m.din}
    x = np.asarray(inp['x'], dtype=np.float32)
    mem = np.asarray(inp['mem'], dtype=np.float32)
    in_maps = []
    for b in range(8):
        d = dict(shared)
        if 'x' in m.din:
            d['x'] = np.ascontiguousarray(x[b])
        if 'mem' in m.din:
            d['mem'] = np.ascontiguousarray(mem[b])
        in_maps.append(d)
    res = run_bass_kernel_spmd(nc, in_maps, core_ids=list(range(8)))
    m.es.close()
    return np.stack([np.asarray(r["out"], dtype=np.float32) for r in res.results], axis=0)
```

```python
import numpy as np
import concourse.bass as bass
import concourse.mybir as mybir
from concourse.bass_utils import run_bass_kernel_spmd
from contextlib import ExitStack

F32 = mybir.dt.float32
I32 = mybir.dt.int32
U32 = mybir.dt.uint32
AF = mybir.ActivationFunctionType
ALU = mybir.AluOpType
AX = mybir.AxisListType

ENGS = ['pe', 'dve', 'act', 'pool', 'sp']
SEM_LIMIT = 30000
DMA_K = 16


class T:
    def __init__(self, h, name):
        self.h = h
        self.name = name
        self.st = {}

    def __getitem__(self, idx):
        return self.h[idx]


def bcast(ap, axis, n):
    l = [list(x) for x in ap.ap]
    l.insert(axis, [0, n])
    return bass.AP(ap.tensor, ap.offset, l)


def rep(ap, axis, n):
    l = [list(x) for x in ap.ap]
    assert l[axis][1] == 1
    l[axis] = [0, n]
    return bass.AP(ap.tensor, ap.offset, l)


class Prog:
    def __init__(self, nc, es):
        self.nc = nc
        self.es = es
        self.pes = es
        self.ops = {e: [] for e in ENGS}
        self.base = {e: 0 for e in ENGS}
        self.waited = {e: {} for e in ENGS}
        self.waited_dma = {e: set() for e in ENGS}
        self.tiles = []
        self.csems = {e: [] for e in ENGS}
        self.ccount = {e: 0 for e in ENGS}
        self.dsems = {}
        self.dcount = {e: 0 for e in ENGS}
        self.dma_hist = {e: [] for e in ENGS}

    def sb(self, name, shape, dt=F32, glob=False):
        es = self.es if glob else self.pes
        self.uid = getattr(self, 'uid', 0) + 1
        name = f"{name}_{self.uid}"
        t = T(es.enter_context(self.nc.sbuf_tensor(name, list(shape), dt)), name)
        self.tiles.append(t)
        return t

    def ps(self, name, shape, dt=F32):
        t = T(self.es.enter_context(self.nc.psum_tensor(name, list(shape), dt)), name)
        self.tiles.append(t)
        return t

    def dram(self, name, shape, dt=F32, kind="Internal"):
        t = T(self.nc.dram_tensor(name, list(shape), dt, kind=kind).ap(), name)
        self.tiles.append(t)
        return t

    @staticmethod
    def _norm(key):
        if isinstance(key, T):
            return key, None
        return key[0], key[1]

    def _entries(self, tile, sub):
        if sub is None:
            return list(tile.st.values())
        out = []
        if sub in tile.st:
            out.append(tile.st[sub])
        if None in tile.st:
            out.append(tile.st[None])
        return out

    def op(self, eng, fn, r=(), w=(), dma=False, extra=()):
        idx = len(self.ops[eng])
        deps = set(extra)
        for key in r:
            tile, sub = self._norm(key)
            for ent in self._entries(tile, sub):
                if ent[0] is not None:
                    deps.add(ent[0])
        for key in w:
            tile, sub = self._norm(key)
            for ent in self._entries(tile, sub):
                if ent[0] is not None:
                    deps.add(ent[0])
                for e2, i2 in ent[1].items():
                    deps.add((e2, i2))
        for key in r:
            tile, sub = self._norm(key)
            ent = tile.st.setdefault(sub, [None, {}])
            ent[1][eng] = idx
        for key in w:
            tile, sub = self._norm(key)
            if sub is None:
                tile.st = {None: [(eng, idx), {}]}
            else:
                tile.st[sub] = [(eng, idx), {}]
        if dma:
            h = self.dma_hist[eng]
            if len(h) >= DMA_K:
                deps.add((eng, h[-DMA_K]))
        final = []
        for (e2, i2) in sorted(deps, reverse=True):
            if e2 == eng and i2 == idx:
                continue
            if self.ops[e2][i2]['dma']:
                if (e2, i2) in self.waited_dma[eng]:
                    continue
                self.waited_dma[eng].add((e2, i2))
                final.append((e2, i2))
            else:
                if e2 == eng and eng == 'pe':
                    continue
                if self.waited[eng].get(e2, -1) >= i2:
                    continue
                self.waited[eng][e2] = i2
                final.append((e2, i2))
        o = dict(fn=fn, deps=final, dma=dma, sig=False)
        if dma:
            o['dma_n'] = self.dcount[eng]
            self.dcount[eng] += 1
            self.dma_hist[eng].append(idx)
        self.ops[eng].append(o)
        return (eng, idx)

    def _sigof(self, e2, i2):
        o = self.ops[e2][i2]
        if o['dma']:
            n = o['dma_n']
            return self.dsems[e2][n % DMA_K], 16 * (n // DMA_K + 1)
        j, v = o['sv']
        return self.csems[e2][j], v

    def flush(self):
        nc = self.nc
        last = {}
        for e in ENGS:
            for i in range(len(self.ops[e]) - 1, self.base[e] - 1, -1):
                if not self.ops[e][i]['dma'] and self.ops[e][i]['fn'] is not None:
                    last[e] = (e, i)
                    break
        dmas = []
        for e in ENGS:
            dmas += [(e, i) for i in self.dma_hist[e][-DMA_K:] if i >= self.base[e]]
        for e in ENGS:
            extra = [v for k, v in last.items() if k != e] + dmas
            self.op(e, None, extra=extra)
        for e in ENGS:
            for o in self.ops[e][self.base[e]:]:
                for (e2, i2) in o['deps']:
                    assert i2 >= self.base[e2], "cross-phase dep"
                    self.ops[e2][i2]['sig'] = True
        for e in ENGS:
            for o in self.ops[e][self.base[e]:]:
                if o['dma']:
                    if e not in self.dsems:
                        self.dsems[e] = [self.es.enter_context(nc.semaphore(f"d_{e}_{j}")) for j in range(DMA_K)]
                    continue
                if o['sig']:
                    c = self.ccount[e]
                    j = c // SEM_LIMIT
                    while len(self.csems[e]) <= j:
                        self.csems[e].append(self.es.enter_context(nc.semaphore(f"c_{e}_{len(self.csems[e])}")))
                    o['sv'] = (j, c % SEM_LIMIT + 1)
                    self.ccount[e] = c + 1

        def run(e, eng):
            for o in self.ops[e][self.base[e]:]:
                for (e2, i2) in o['deps']:
                    s, v = self._sigof(e2, i2)
                    eng.wait_ge(s, v)
                if o['fn'] is None:
                    continue
                ins = o['fn'](eng)
                if o['dma']:
                    n = o['dma_n']
                    ins.then_inc(self.dsems[e][n % DMA_K], 16)
                elif o['sig']:
                    j, v = o['sv']
                    ins.then_inc(self.csems[e][j], 1)

        with nc.Block() as block:
            @block.tensor
            def _(eng):
                run('pe', eng)

            @block.vector
            def _(eng):
                run('dve', eng)

            @block.scalar
            def _(eng):
                run('act', eng)

            @block.gpsimd
            def _(eng):
                run('pool', eng)

            @block.sync
            def _(eng):
                run('sp', eng)
        for e in ENGS:
            self.base[e] = len(self.ops[e])
        for t in self.tiles:
            t.st = {}

D = 1024
S = 2048
NT = 16
NMEM = 256
DEPTH = 4
EPS = 1e-6
FAST_MM = False
BF16 = mybir.dt.bfloat16
F32R = mybir.dt.float32r

IN_SHAPES = {
    'x': [S, D], 'mem': [NMEM, D], 'mix_norm': [4, D],
    'even_w_in': [2, D, 2048], 'even_conv_w': [2, 128, 12], 'even_pool_w': [2, 4, 128, 128],
    'even_pool_scale': [2, 128, 4], 'even_w_out': [2, D, D],
    'odd_w_in': [2, D, 3784], 'odd_kv_norm': [2, 128], 'odd_w_uv': [2, 8, 128, 64],
    'odd_hg_norm': [2, 512], 'odd_w_out': [2, D, D], 'hgrn_gamma': [4, 512],
    'rel_bias': [32, 8], 'invc': [128, 2048], 'biasT': [128, 8, 2, 128], 'mem_norm': [1, D], 'xattn_norm': [4, D],
    'xattn_wq': [4, D, D], 'xattn_wkv': [4, D, 2 * D], 'xattn_wo': [4, D, D],
    'peer_norm': [4, D], 'peer_wq': [4, D, 2048], 'peer_keysT': [4, 128, 16, 128],
    'peer_uv': [4, 16384, 2 * D], 'final_norm': [1, D],
}


INVC = [None]


def make_consts():
    parts = {}
    parts['ident'] = np.eye(128, dtype=np.float32)
    t = np.arange(512)
    invc = np.concatenate([1.0 / np.minimum(t + 1, w) for w in (2, 4, 8, 16)]).astype(np.float32)
    INVC[0] = np.ascontiguousarray(np.tile(invc[None, :], (128, 1)).astype(np.float32))
    parts['iota16'] = np.tile(np.arange(16, dtype=np.float32)[None, :], (128, 1))
    ii = np.arange(128)
    parts['U2'] = ((ii[:, None] // 64 == ii[None, :] // 64) & (ii[:, None] <= ii[None, :])).astype(np.float32)
    parts['B2'] = (ii[:, None] // 64 == ii[None, :] // 64).astype(np.float32)
    parts['cmask'] = np.where(ii[None, :] <= ii[:, None], 0.0, -1e30).astype(np.float32)
    parts['ones'] = np.ones((128, 8), dtype=np.float32)
    off = 0
    lay = {}
    arrs = []
    for k, v in parts.items():
        lay[k] = (off, v.shape[1])
        off += v.shape[1]
        arrs.append(v.astype(np.float32))
    return np.ascontiguousarray(np.concatenate(arrs, axis=1)), lay


class Model:
    def __init__(self):
        self.nc = bass.Bass("TRN2", target_bir_lowering=False)
        self.es = ExitStack()
        self.P = Prog(self.nc, self.es)
        P = self.P
        carr, self.clay = make_consts()
        self.carr = carr
        model = self

        class LazyIn(dict):
            def __missing__(self, k):
                shp = list(carr.shape) if k == 'consts' else IN_SHAPES[k]
                t = P.dram(k, shp, F32, kind="ExternalInput")
                self[k] = t
                return t
        self.din = LazyIn()
        self.out = P.dram("out", [S, D], F32, kind="ExternalOutput")
        self.hA = P.dram("hA", [S, D])
        self.hB = P.dram("hB", [S, D])
        self.uvb = P.dram("uvb", [16384, 2 * D], BF16)
        self.PS = P.ps("ps", [128, 8, 512])
        self.C = P.sb("consts_sb", [128, carr.shape[1]], glob=True)
        self.memT = P.sb("memT", [128, 8, NMEM], glob=True)
        self.rotc = {}

    def cst(self, name):
        o, w = self.clay[name]
        return self.C[:, o:o + w]

    def psv(self, b0, nb=2):
        ap = self.PS[:, b0:b0 + nb, :].rearrange("p a b -> p (a b)")
        return ap, [(self.PS, b) for b in range(b0, b0 + nb)]

    def mm(self, out, lhsT, rhs, start, stop, r, w, fast=True):
        if FAST_MM and fast and rhs.shape[-1] % 2 == 0 and out.shape[-1] % 2 == 0:
            lhsT = lhsT.bitcast(F32R)
            rhs = rhs.bitcast(F32R)
        self.P.op('pe', lambda e: e.matmul(out, lhsT, rhs, start=start, stop=stop), r=r, w=w)

    def tr(self, out, in_, r, w):
        ident = self.cst('ident')
        self.P.op('pe', lambda e: e.transpose(out, in_, ident), r=list(r) + [self.C], w=w)

    def act(self, out, in_, func, r, w, bias=None, scale=None, accum=None):
        kw = {}
        if bias is not None:
            kw['bias'] = bias
        if scale is not None:
            kw['scale'] = scale
        if accum is not None:
            kw['accum_out'] = accum
        self.P.op('act', lambda e: e.activation(out, in_, func, **kw), r=r, w=w)

    def tt(self, out, a, b, op, r, w, eng='dve'):
        self.P.op(eng, lambda e: e.tensor_tensor(out, a, b, op), r=r, w=w)

    def ts(self, out, a, s1, s2, op0, op1, r, w, eng='dve', accum=None):
        if op1 is None:
            self.P.op(eng, lambda e: e.tensor_scalar(out, a, s1, None, op0), r=r, w=w)
        elif accum is None:
            self.P.op(eng, lambda e: e.tensor_scalar(out, a, s1, s2, op0, op1), r=r, w=w)
        else:
            self.P.op(eng, lambda e: e.tensor_scalar(out, a, s1, s2, op0, op1, accum_out=accum), r=r, w=w)

    def stt(self, out, a, scalar, b, op0, op1, r, w):
        self.P.op('dve', lambda e: e.scalar_tensor_tensor(out, a, scalar, b, op0, op1), r=r, w=w)

    def copy(self, out, in_, r, w, eng='act'):
        if eng == 'act':
            self.P.op('act', lambda e: e.copy(out, in_), r=r, w=w)
        else:
            self.P.op(eng, lambda e: e.tensor_copy(out, in_), r=r, w=w)

    def dma(self, out, in_, r, w, eng='sp'):
        self.P.op(eng, lambda e: e.dma_start(out=out, in_=in_), r=r, w=w, dma=True)

    def recip(self, out, in_, r, w):
        self.P.op('dve', lambda e: e.reciprocal(out, in_), r=r, w=w)

    def rot(self, name, shape, n=2, dt=F32):
        return [self.P.sb(f"{name}{j}", shape, dt) for j in range(n)]

    def wload(self, dst, src_t, src_ap):
        self.dma(dst[:, :, :], src_ap.rearrange("(k p) c -> p k c", p=128), r=[src_t], w=[dst])

    def gload(self, dst, src_t, row_ap):
        n = row_ap.shape[1]
        self.dma(dst[:, :], row_ap.broadcast_to([128, n]), r=[src_t], w=[dst])

    def rms(self, x, g, hn, st, width=D):
        self.act(hn[:, :], x[:, :], AF.Square, r=[x], w=[hn, st], accum=st[:, 0:1])
        self.act(st[:, 1:2], st[:, 0:1], AF.Sqrt, r=[st], w=[st], scale=1.0 / width, bias=self.epsb[:, 0:1])
        self.recip(st[:, 1:2], st[:, 1:2], r=[st], w=[st])
        self.stt(hn[:, :], x[:, :], st[:, 1:2], g[:, :], ALU.mult, ALU.mult, r=[x, st, g], w=[hn])

    def transp8(self, src, dstT, b0, n=8):
        nb = (n * 128 + 511) // 512
        pv, pk = self.psv(b0, nb)
        for k in range(n):
            self.tr(pv[:, k * 128:(k + 1) * 128], src[:, k * 128:(k + 1) * 128], r=[src], w=pk)
        self.copy(dstT[:, :, :].rearrange("p a b -> p (a b)"), pv[:, 0:n * 128], r=pk, w=[dstT])

    def phase_setup(self):
        P = self.P
        self.dma(self.C[:, :], self.din['consts'][:, :], r=[self.din['consts']], w=[self.C])
        self.epsb = P.sb("epsb", [128, 1], glob=True)
        P.op('dve', lambda e: e.memset(self.epsb[:, :], EPS), w=[self.epsb])
        g = P.sb("g_mem", [128, D])
        self.gload(g, self.din['mem_norm'], self.din['mem_norm'][0:1, :])
        xs = self.rot("xm", [128, D])
        hn = self.rot("hnm", [128, D])
        st = self.rot("stm", [128, 4])
        mT = [P.sb(f"mT{j}", [128, 8, 128]) for j in range(2)]
        for m in range(2):
            self.dma(xs[m][:, :], self.din['mem'][m * 128:(m + 1) * 128, :], r=[self.din['mem']], w=[xs[m]])
            self.rms(xs[m], g, hn[m], st[m])
            self.transp8(hn[m], mT[m], 2 * m)
            self.copy(self.memT[:, :, m * 128:(m + 1) * 128], mT[m][:, :, :], r=[mT[m]], w=[(self.memT, m)], eng='dve')

    def phase_xattn(self, l, src, dst):
        P = self.P
        din = self.din
        wq = P.sb("wq", [128, 8, D])
        wo = P.sb("wo", [128, 8, D])
        KT = P.sb("KT", [128, 8, NMEM])
        V = P.sb("V", [128, 2, D])
        g = P.sb("g_x", [128, D])
        wkv = self.rot("wkv", [128, 8, 512])
        self.gload(g, din['xattn_norm'], din['xattn_norm'][l:l + 1, :])
        for c in range(4):
            wc = wkv[c % 2]
            self.wload(wc, din['xattn_wkv'], din['xattn_wkv'][l][:, c * 512:(c + 1) * 512])
            if c < 2:
                for f in range(4):
                    fc = c * 4 + f
                    b = fc % 8
                    pv, pk = self.psv(b, 1)
                    for k in range(8):
                        self.mm(pv[:, 0:NMEM], wc[:, k, f * 128:(f + 1) * 128], self.memT[:, k, :],
                                k == 0, k == 7, r=[wc, self.memT], w=pk)
                    self.act(KT[:, fc, :], pv[:, 0:NMEM], AF.Copy, r=pk, w=[(KT, fc)], scale=1.0 / 16.0)
            else:
                for m in range(2):
                    b = (c - 2) * 2 + m
                    pv, pk = self.psv(b, 1)
                    for k in range(8):
                        self.mm(pv[:, :], self.memT[:, k, m * 128:(m + 1) * 128], wc[:, k, :],
                                k == 0, k == 7, r=[wc, self.memT], w=pk)
                    self.copy(V[:, m, (c - 2) * 512:(c - 1) * 512], pv[:, :], r=pk, w=[(V, (m, c))])
        self.wload(wq, din['xattn_wq'], din['xattn_wq'][l])
        self.wload(wo, din['xattn_wo'], din['xattn_wo'][l])
        xs = self.rot("x", [128, D])
        hns = self.rot("hn", [128, D])
        hnTs = self.rot("hnT", [128, 8, 128])
        sts = self.rot("st", [128, 16])
        qTs = self.rot("qT", [128, 8, 128])
        Pms = self.rot("Pm", [128, 4 * NMEM])
        PTs = self.rot("PT", [128, 8, 128])
        oTs = self.rot("oT", [128, 8, 128])
        for i in range(NT):
            j = i % 2
            x, hn, hnT, st, qT, Pm, PT, oT = xs[j], hns[j], hnTs[j], sts[j], qTs[j], Pms[j], PTs[j], oTs[j]
            self.dma(x[:, :], src[i * 128:(i + 1) * 128, :], r=[(src, i)], w=[x])
            self.rms(x, g, hn, st)
            self.transp8(hn, hnT, 0)
            pv, pk = self.psv(2, 2)
            for f in range(8):
                for k in range(8):
                    self.mm(pv[:, f * 128:(f + 1) * 128], wq[:, k, f * 128:(f + 1) * 128], hnT[:, k, :],
                            k == 0, k == 7, r=[wq, hnT], w=pk)
            self.copy(qT[:, :, :].rearrange("p a b -> p (a b)"), pv[:, :], r=pk, w=[qT])
            lv, lk = self.psv(4, 2)
            for hd in range(4):
                for c in range(2):
                    self.mm(lv[:, hd * 256:(hd + 1) * 256], qT[:, hd * 2 + c, :], KT[:, hd * 2 + c, :],
                            c == 0, c == 1, r=[qT, KT], w=lk)
            lv3 = self.PS[:, 4:6, :].rearrange("p a (h m) -> p (a h) m", m=NMEM)
            P.op('dve', lambda e, lv3=lv3, st=st: e.tensor_reduce(out=st[:, 4:8], in_=lv3, axis=AX.X, op=ALU.max, negate=True),
                 r=lk, w=[st])
            for hd in range(4):
                self.act(Pm[:, hd * 256:(hd + 1) * 256], lv[:, hd * 256:(hd + 1) * 256], AF.Exp, r=lk + [st], w=[Pm, st],
                         bias=st[:, 4 + hd:5 + hd], scale=1.0, accum=st[:, 8 + hd:9 + hd])
            self.recip(st[:, 12:16], st[:, 8:12], r=[st], w=[st])
            for hd in range(4):
                self.ts(Pm[:, hd * 256:(hd + 1) * 256], Pm[:, hd * 256:(hd + 1) * 256], st[:, 12 + hd:13 + hd], None,
                        ALU.mult, None, r=[Pm, st], w=[Pm])
            self.transp8(Pm, PT, 6)
            ov, ok = self.psv(0, 2)
            for jj in range(8):
                for c in range(2):
                    self.mm(ov[:, jj * 128:(jj + 1) * 128], V[:, c, jj * 128:(jj + 1) * 128], PT[:, (jj // 2) * 2 + c, :],
                            c == 0, c == 1, r=[V, PT], w=ok)
            self.copy(oT[:, :, :].rearrange("p a b -> p (a b)"), ov[:, :], r=ok, w=[oT])
            yv, yk = self.psv(2, 2)
            for half in range(2):
                for k in range(8):
                    self.mm(yv[:, half * 512:(half + 1) * 512], oT[:, k, :], wo[:, k, half * 512:(half + 1) * 512],
                            k == 0, k == 7, r=[oT, wo], w=yk)
            self.tt(x[:, :], yv[:, :], x[:, :], ALU.add, r=yk + [x], w=[x])
            self.dma(dst[i * 128:(i + 1) * 128, :], x[:, :], r=[x], w=[(dst, i)])


    def phase_even(self, l, src, dst):
        P = self.P
        din = self.din
        jx = l // 2
        w_in = P.sb("e_win", [128, 8, 2048])
        w_out = P.sb("e_wout", [128, 8, D])
        pw = P.sb("e_pw", [128, 4, 128])
        cw = P.sb("e_cw", [128, 12])
        psc = P.sb("e_psc", [128, 4])
        g = P.sb("e_g", [128, D])
        self.gload(g, din['mix_norm'], din['mix_norm'][l:l + 1, :])
        self.wload(w_in, din['even_w_in'], din['even_w_in'][jx])
        self.wload(w_out, din['even_w_out'], din['even_w_out'][jx])
        self.dma(pw[:, :, :], din['even_pool_w'][jx].rearrange("g c d -> c g d"), r=[din['even_pool_w']], w=[pw])
        self.dma(cw[:, :], din['even_conv_w'][jx], r=[din['even_conv_w']], w=[cw])
        self.dma(psc[:, :], din['even_pool_scale'][jx], r=[din['even_pool_scale']], w=[psc])
        cu_halo = P.sb("e_cuh", [128, 4, 2])
        pv_halo = P.sb("e_pvh", [128, 4, 16])
        P.op('pool', lambda e: e.memset(cu_halo[:, :, :], 0.0), w=[cu_halo])
        P.op('pool', lambda e: e.memset(pv_halo[:, :, :], 0.0), w=[pv_halo])
        hnT = P.sb("e_hnT", [128, 8, 512])
        yT = P.sb("e_yT", [128, 8, 512])
        xs = self.rot("e_x", [128, D])
        hns = self.rot("e_hn", [128, D])
        sts = self.rot("e_st", [128, 4])
        hts = self.rot("e_ht", [128, 8, 128])
        cus = self.rot("e_cu", [128, 514])
        pvs = self.rot("e_pv", [128, 528])
        ut = self.rot("e_ut", [128, 512])
        zt = self.rot("e_z", [128, 512])
        sA = self.rot("e_sA", [128, 528])
        sB = self.rot("e_sB", [128, 528])
        pl = self.rot("e_pl", [128, 512])
        invc_t = P.sb("e_invc", [128, 2048])
        self.dma(invc_t[:, :], din['invc'][:, :], r=[din['invc']], w=[invc_t])
        invc = invc_t[:, :]
        for b in range(4):
            for t4 in range(4):
                i = b * 4 + t4
                j = i % 2
                self.dma(xs[j][:, :], src[i * 128:(i + 1) * 128, :], r=[(src, i)], w=[xs[j]])
                self.rms(xs[j], g, hns[j], sts[j])
                self.transp8(hns[j], hts[j], 6)
                self.copy(hnT[:, :, t4 * 128:(t4 + 1) * 128], hts[j][:, :, :], r=[hts[j]], w=[(hnT, t4)], eng='pool')
            for c4 in range(4):
                j = c4 % 2
                cu, pv, u_sb, z = cus[j], pvs[j], ut[j], zt[j]

                def proj(cc, bank):
                    pvw, pk = self.psv(bank, 1)
                    for k in range(8):
                        self.mm(pvw[:, :], w_in[:, k, cc * 128:(cc + 1) * 128], hnT[:, k, :], k == 0, k == 7,
                                r=[w_in, hnT], w=pk)
                    return pvw, pk
                pu, ku = proj(c4, 0)
                self.copy(u_sb[:, :], pu[:, :], r=ku, w=[u_sb])
                pg, kg = proj(4 + c4, 1)
                self.copy(cu[:, 0:2], cu_halo[:, c4, :], r=[(cu_halo, c4)], w=[cu], eng='pool')
                self.tt(cu[:, 2:514], pg[:, :], u_sb[:, :], ALU.mult, r=kg + [u_sb, cu], w=[cu])
                self.copy(cu_halo[:, c4, :], cu[:, 512:514], r=[cu], w=[(cu_halo, c4)], eng='pool')
                self.ts(z[:, :], cu[:, 0:512], cw[:, c4 * 3:c4 * 3 + 1], None, ALU.mult, None, r=[cu, cw], w=[z])
                self.stt(z[:, :], cu[:, 1:513], cw[:, c4 * 3 + 1:c4 * 3 + 2], z[:, :], ALU.mult, ALU.add, r=[cu, cw, z], w=[z])
                self.stt(z[:, :], cu[:, 2:514], cw[:, c4 * 3 + 2:c4 * 3 + 3], z[:, :], ALU.mult, ALU.add, r=[cu, cw, z], w=[z])
                pb, kb = proj(8 + c4, 2)
                self.tt(yT[:, c4, :], pb[:, :], z[:, :], ALU.mult, r=kb + [z], w=[(yT, c4)])
                pp, kp = proj(12 + c4, 3)
                self.copy(pv[:, 0:16], pv_halo[:, c4, :], r=[(pv_halo, c4)], w=[pv], eng='pool')
                self.copy(pv[:, 16:528], pp[:, :], r=kp + [pv], w=[pv])
                self.copy(pv_halo[:, c4, :], pv[:, 512:528], r=[pv], w=[(pv_halo, c4)], eng='pool')
                a_, b_ = sA[j], sB[j]
                self.tt(a_[:, 1:528], pv[:, 1:528], pv[:, 0:527], ALU.add, r=[pv], w=[a_])
                cur = a_
                other = b_
                lo = 1
                for st_ in range(c4):
                    sh = 2 ** (st_ + 1)
                    nlo = lo + sh
                    self.tt(other[:, nlo:528], cur[:, nlo:528], cur[:, nlo - sh:528 - sh], ALU.add, r=[cur], w=[other])
                    cur, other = other, cur
                    lo = nlo
                wdw = 2 ** (c4 + 1)
                pool_t = pl[j]
                if b == 0:
                    self.tt(pool_t[:, :], cur[:, 16:528], invc[:, c4 * 512:(c4 + 1) * 512], ALU.mult, r=[cur, invc_t], w=[pool_t])
                    self.tt(pool_t[:, :], pool_t[:, :], pv[:, 16:528], ALU.subtract, r=[pool_t, pv], w=[pool_t])
                else:
                    self.stt(pool_t[:, :], cur[:, 16:528], 1.0 / wdw, pv[:, 16:528], ALU.mult, ALU.subtract, r=[cur, pv], w=[pool_t])
                py, ky = self.psv(3, 1)
                self.mm(py[:, :], pw[:, c4, :], pool_t[:, :], True, True, r=[pw, pool_t], w=ky)
                self.act(yT[:, 4 + c4, :], py[:, :], AF.Copy, r=ky + [psc], w=[(yT, 4 + c4)], scale=psc[:, c4:c4 + 1])
            for t4 in range(4):
                i = b * 4 + t4
                j = i % 2
                self.dma(xs[j][:, :], src[i * 128:(i + 1) * 128, :], r=[(src, i)], w=[xs[j]])
                ov, ok = self.psv(4, 2)
                for half in range(2):
                    for k in range(8):
                        self.mm(ov[:, half * 512:(half + 1) * 512], yT[:, k, t4 * 128:(t4 + 1) * 128],
                                w_out[:, k, half * 512:(half + 1) * 512], k == 0, k == 7, r=[yT, w_out], w=ok)
                self.tt(xs[j][:, :], ov[:, :], xs[j][:, :], ALU.add, r=ok + [xs[j]], w=[xs[j]])
                self.dma(dst[i * 128:(i + 1) * 128, :], xs[j][:, :], r=[xs[j]], w=[(dst, i)])


    def top16(self, src_ap, work_ap, tv_ap, ti_ap, r, wk):
        P = self.P
        P.op('dve', lambda e: e.max(out=tv_ap[:, 0:8], in_=src_ap), r=r, w=wk)
        P.op('dve', lambda e: e.max_index(out=ti_ap[:, 0:8], in_max=tv_ap[:, 0:8], in_values=src_ap), r=r + wk, w=wk)
        P.op('dve', lambda e: e.match_replace(out=work_ap, in_to_replace=tv_ap[:, 0:8], in_values=src_ap, imm_value=-1e30),
             r=r + wk, w=wk)
        P.op('dve', lambda e: e.max(out=tv_ap[:, 8:16], in_=work_ap), r=wk, w=wk)
        P.op('dve', lambda e: e.max_index(out=ti_ap[:, 8:16], in_max=tv_ap[:, 8:16], in_values=work_ap), r=wk, w=wk)

    def phase_peer(self, l, src, dst):
        P = self.P
        din = self.din
        wq = P.sb("p_wq", [128, 8, 2048], BF16)
        keysT = P.sb("p_keys", [128, 16, 128])
        g = P.sb("p_g", [128, D])
        self.gload(g, din['peer_norm'], din['peer_norm'][l:l + 1, :])
        wst = self.rot("p_wst", [128, 8, 256], 2)
        for c in range(8):
            ws_ = wst[c % 2]
            self.dma(ws_[:, :, :], din['peer_wq'][l][:, c * 256:(c + 1) * 256].rearrange("(k p) c -> p k c", p=128),
                     r=[din['peer_wq']], w=[ws_])
            self.copy(wq[:, :, c * 256:(c + 1) * 256], ws_[:, :, :], r=[ws_], w=[(wq, c)], eng='act' if c % 2 == 0 else 'dve')
        self.dma(keysT[:, :, :], din['peer_keysT'][l], r=[din['peer_keysT']], w=[keysT])
        uv_l = self.uvb
        uvb2 = self.uvb[:, :].rearrange("n (a d) -> (n a) d", a=2)
        uvf2 = din['peer_uv'][l].rearrange("n (a d) -> (n a) d", a=2)
        for c in range(16):
            P.op('pool', lambda e, c=c: e.dma_start(out=uvb2[c * 2048:(c + 1) * 2048, :],
                                                    in_=uvf2[c * 2048:(c + 1) * 2048, :]),
                 r=[din['peer_uv']], w=[self.uvb], dma=True)
        xs = self.rot("p_x", [128, D], 2)
        xns = self.rot("p_xn", [128, D], 2)
        xnT = P.sb("p_xnT", [128, 8, 128], BF16)
        sts = self.rot("p_st", [128, 4], 2)
        qT = P.sb("p_qT", [128, 8, 128])
        s_sb = P.sb("p_s", [128, 1024])
        tv = P.sb("p_tv", [128, 16, 16])
        ti = P.sb("p_ti", [128, 16, 16], U32)
        tif = P.sb("p_tif", [128, 16, 16])
        cand = P.sb("p_cand", [128, 8, 256])
        cwk = P.sb("p_cwk", [128, 8, 256])
        cv = P.sb("p_cv", [128, 8, 16])
        cpos = P.sb("p_cpos", [128, 8, 16], U32)
        ab_u = P.sb("p_abu", [128, 2, 128], U32)
        ab_f = P.sb("p_abf", [128, 2, 128])
        isel = P.sb("p_isel", [128, 2, 128])
        eidf = P.sb("p_eidf", [128, 128])
        eids = self.rot("p_eid", [128, 128], 2, dt=I32)
        ggs = self.rot("p_gg", [128, 8, 16], 2)
        gz = P.sb("p_gz", [128, 16])
        actvs = self.rot("p_act", [128, 128], 2)
        wgts = self.rot("p_wgt", [128, 128], 2)
        GS = 4
        t1s = self.rot("p_t1", [128, GS], 4)
        t2s = self.rot("p_t2", [128, GS], 4)
        junk = P.sb("p_junk", [128, D], BF16)
        NUV = 16
        uvs = self.rot("p_uv", [128, 2 * D], NUV, dt=BF16)
        dgs = self.rot("p_dg", [128, 128], 4, dt=BF16)
        iota16 = self.cst('iota16')
        ident = self.cst('ident')
        cwkf = cwk[:, :, :].rearrange("p a b -> p (a b)")
        candf = cand[:, :, :].rearrange("p a b -> p (a b)")

        def front(i):
            x, xn, st = xs[i % 2], xns[i % 2], sts[i % 2]
            eid, gg = eids[i % 2], ggs[i % 2]
            self.dma(x[:, :], src[i * 128:(i + 1) * 128, :], r=[(src, i)], w=[x])
            self.rms(x, g, xn, st)
            yield
            self.transp8(xn, xnT, 0)
            yield
            for hf in range(2):
                qv, qk = self.psv(2, 2)
                for hh in range(8):
                    hp = hf * 8 + hh
                    for k in range(8):
                        self.mm(qv[:, hh * 128:(hh + 1) * 128], wq[:, k, hp * 128:(hp + 1) * 128], xnT[:, k, :],
                                k == 0, k == 7, r=[wq, xnT], w=qk, fast=False)
                    if hh % 2 == 1:
                        yield
                self.copy(qT[:, :, :].rearrange("p a b -> p (a b)"), qv[:, :], r=qk, w=[qT])
                sv, sk = self.psv(0, 2)
                for hh in range(8):
                    hp = hf * 8 + hh
                    self.mm(sv[:, hh * 128:(hh + 1) * 128], qT[:, hh, :], keysT[:, hp, :], True, True, r=[qT, keysT], w=sk)
                self.copy(s_sb[:, :], sv[:, :], r=sk, w=[s_sb])
                yield
                for hh in range(8):
                    hp = hf * 8 + hh
                    self.top16(s_sb[:, hh * 128:(hh + 1) * 128], cwkf[:, hh * 128:(hh + 1) * 128], tv[:, hp, :], ti[:, hp, :],
                               r=[s_sb], wk=[(tv, hp), (ti, hp), (cwk, hh)])
                    yield
            self.copy(tif[:, :, :], ti[:, :, :], r=[ti], w=[tif], eng='dve')
            tv4 = tv[:, :, :].rearrange("p (h t) a -> p h t a", t=2)
            tif4 = tif[:, :, :].rearrange("p (h t) a -> p h t a", t=2)
            cand4 = cand[:, :, :].rearrange("p h (a b) -> p h a b", b=16)
            self.tt(cand4, bcast(tv4[:, :, 0, :], 3, 16), bcast(tv4[:, :, 1, :], 2, 16), ALU.add, r=[tv], w=[cand])
            yield
            for h in range(8):
                self.top16(cand[:, h, :], cwk[:, h, :], cv[:, h, :], cpos[:, h, :],
                           r=[cand], wk=[(cv, h), (cpos, h), (cwk, h)])
                yield
            cposf = cpos[:, :, :].rearrange("p h a -> p (h a)")
            P.op('dve', lambda e: e.tensor_single_scalar(ab_u[:, 0, :], cposf, 4, ALU.logical_shift_right), r=[cpos], w=[ab_u])
            P.op('dve', lambda e: e.tensor_single_scalar(ab_u[:, 1, :], cposf, 15, ALU.bitwise_and), r=[cpos, ab_u], w=[ab_u])
            self.copy(ab_f[:, :, :], ab_u[:, :, :], r=[ab_u], w=[ab_f], eng='dve')
            yield
            eqv = candf[:, 0:1024].rearrange("p (m a) -> p m a", a=16)
            for t in range(2):
                for hh in range(2):
                    self.tt(eqv, bcast(ab_f[:, t, hh * 64:(hh + 1) * 64], 2, 16), bcast(iota16, 1, 64), ALU.is_equal,
                            r=[ab_f, self.C], w=[cand])
                    eq4 = candf[:, 0:1024].rearrange("p (h j a) -> p h j a", j=16, a=16)
                    self.tt(eq4, eq4, bcast(tif4[:, hh * 4:(hh + 1) * 4, t, :], 2, 16), ALU.mult, r=[cand, tif], w=[cand])
                    P.op('dve', lambda e, t=t, hh=hh: e.tensor_reduce(out=isel[:, t, hh * 64:(hh + 1) * 64], in_=eqv,
                                                                      axis=AX.X, op=ALU.add), r=[cand], w=[isel])
                    yield
            self.stt(eidf[:, :], isel[:, 0, :], 128.0, isel[:, 1, :], ALU.mult, ALU.add, r=[isel], w=[eidf])
            self.copy(eid[:, :], eidf[:, :], r=[eidf], w=[eid], eng='dve')
            yield
            self.tt(gg[:, :, :], cv[:, :, :], bcast(cv[:, :, 0], 2, 16), ALU.subtract, r=[cv], w=[gg])
            self.act(gg[:, :, :], gg[:, :, :], AF.Exp, r=[gg], w=[gg])
            P.op('dve', lambda e: e.tensor_reduce(out=gz[:, 0:8], in_=gg[:, :, :], axis=AX.X, op=ALU.add), r=[gg], w=[gz])
            self.recip(gz[:, 8:16], gz[:, 0:8], r=[gz], w=[gz])
            self.tt(gg[:, :, :], gg[:, :, :], bcast(gz[:, 8:16], 2, 16), ALU.mult, r=[gg, gz], w=[gg])
            yield

        def uvstage(i):
            x, xn, eid, gg = xs[i % 2], xns[i % 2], eids[i % 2], ggs[i % 2]
            actv, wgt = actvs[i % 2], wgts[i % 2]
            ggf = gg[:, :, :].rearrange("p h a -> p (h a)")
            xpv, xpk = self.psv(4, 2)
            av, ak = self.psv(6, 2)
            self.copy(xpv[:, :], xn[:, :], r=[xn], w=xpk)
            NGRP = 128 // GS

            def stA(gi):
                g0 = gi * GS
                for jj in range(g0, g0 + GS):
                    uv = uvs[jj % NUV]
                    P.op('pool', lambda e, uv=uv, jj=jj: e.indirect_dma_start(
                        out=uv[:, :], out_offset=None, in_=uv_l[:, :],
                        in_offset=bass.IndirectOffsetOnAxis(ap=eid[:, jj:jj + 1], axis=0)),
                        r=[eid, self.uvb], w=[uv], dma=True)
                    P.op('dve', lambda e, uv=uv, jj=jj: e.scalar_tensor_tensor(
                        junk[:, :], uv[:, 0:D], 1.0, xpv[:, :], ALU.mult, ALU.mult, accum_out=actv[:, jj:jj + 1]),
                        r=[uv] + xpk, w=[(actv, jj)])
                a_ = actv[:, g0:g0 + GS]
                ak_ = [(actv, jj) for jj in range(g0, g0 + GS)]
                t1, t2 = t1s[gi % 4], t2s[gi % 4]
                self.tt(t1[:, :], a_, a_, ALU.mult, r=ak_, w=[t1])
                self.ts(t1[:, :], t1[:, :], 0.044715, 1.0, ALU.mult, ALU.add, r=[t1], w=[t1])
                self.tt(t1[:, :], t1[:, :], a_, ALU.mult, r=[t1] + ak_, w=[t1])
                self.act(t2[:, :], t1[:, :], AF.Sigmoid, r=[t1], w=[t2], scale=1.5957691216057308)

            def stC(gi):
                g0 = gi * GS
                a_ = actv[:, g0:g0 + GS]
                ak_ = [(actv, jj) for jj in range(g0, g0 + GS)]
                t2 = t2s[gi % 4]
                self.tt(t2[:, :], t2[:, :], a_, ALU.mult, r=[t2] + ak_, w=[t2])
                self.tt(wgt[:, g0:g0 + GS], t2[:, :], ggf[:, g0:g0 + GS], ALU.mult, r=[t2, gg], w=[(wgt, gi)])

            def stD(gi):
                g0 = gi * GS
                for jj in range(g0, g0 + GS):
                    uv = uvs[jj % NUV]
                    dg = dgs[jj % 4]
                    self.ts(dg[:, :], ident, wgt[:, jj:jj + 1], 1.0, ALU.mult, ALU.mult, r=[self.C, (wgt, gi)], w=[dg], eng='pool')
                    for half in range(2):
                        self.mm(av[:, half * 512:(half + 1) * 512], dg[:, :], uv[:, D + half * 512:D + (half + 1) * 512],
                                jj == 0, jj == 127, r=[dg, uv], w=[ak[half]], fast=False)

            for gi in range(NGRP + 2):
                if gi < NGRP:
                    stA(gi)
                if 1 <= gi <= NGRP:
                    stC(gi - 1)
                if gi >= 2:
                    stD(gi - 2)
                yield
            self.tt(x[:, :], av[:, :], x[:, :], ALU.add, r=ak + [x], w=[x])
            self.dma(dst[i * 128:(i + 1) * 128, :], x[:, :], r=[x], w=[(dst, i)])
            yield

        for _ in front(0):
            pass
        for it in range(NT):
            active = [uvstage(it)]
            if it + 1 < NT:
                active.append(front(it + 1))
            while active:
                for gen in list(active):
                    try:
                        next(gen)
                    except StopIteration:
                        active.remove(gen)

    def phase_dsa(self, l, src, dst):
        P = self.P
        din = self.din
        jx = l // 2
        w = P.sb("d_w", [128, 8, 1736])
        w_uv = P.sb("d_wuv", [128, 8, 64])
        w_out = P.sb("d_wout", [128, 4, D])
        g = P.sb("d_g", [128, D])
        gkv = P.sb("d_gkv", [128, 128])
        b31 = P.sb("d_b31", [128, 16])
        corrT = P.sb("d_corr", [128, 8, 2, 128])
        self.gload(g, din['mix_norm'], din['mix_norm'][l:l + 1, :])
        self.gload(gkv, din['odd_kv_norm'], din['odd_kv_norm'][jx:jx + 1, :])
        self.gload(b31, din['rel_bias'], din['rel_bias'][31:32, :]) if False else self.dma(
            b31[:, 0:8], din['rel_bias'][31:32, :].broadcast_to([128, 8]), r=[din['rel_bias']], w=[b31])
        self.ts(b31[:, 8:16], b31[:, 0:8], -1.0, None, ALU.mult, None, r=[b31], w=[b31])
        self.dma(corrT[:, :, :, :], din['biasT'][:, :, :, :], r=[din['biasT']], w=[corrT])
        for h in range(8):
            self.act(corrT[:, h, :, :], corrT[:, h, :, :], AF.Exp, r=[corrT, b31], w=[corrT], bias=b31[:, 8 + h:9 + h], scale=1.0)
        self.dma(w[:, :, :], din['odd_w_in'][jx][:, 0:1736].rearrange("(k p) c -> p k c", p=128), r=[din['odd_w_in']], w=[w])
        self.dma(w_uv[:, :, :], din['odd_w_uv'][jx].rearrange("h r e -> r h e"), r=[din['odd_w_uv']], w=[w_uv])
        self.dma(w_out[:, :, :], din['odd_w_out'][jx][0:512, :].rearrange("(k p) c -> p k c", p=128), r=[din['odd_w_out']], w=[w_out])
        cT = P.sb("d_cT", [128, S])
        c_tm = P.sb("d_ctm", [128, NT, 128])
        ikT = P.sb("d_ikT", [64, S])
        xs = self.rot("d_x", [128, D])
        hn = P.sb("d_hn", [128, D])
        hnT = P.sb("d_hnT", [128, 8, 128])
        st = P.sb("d_st", [128, 8])
        craw = P.sb("d_craw", [128, 128])
        qlT = P.sb("d_qlT", [128, 8, 128])
        iqT = P.sb("d_iqT", [64, 8, 128])
        iw = P.sb("d_iw", [128, 8])
        score = P.sb("d_score", [128, S])
        work = P.sb("d_work", [128, S])
        maskT = P.sb("d_maskT", [128, NT, 128])
        rsb = self.rot("d_r", [128, 512])
        m8 = self.rot("d_m8", [128, 8])
        ET = P.sb("d_ET", [128, NT, 128])
        latT = P.sb("d_latT", [128, 8, 128])
        zz = P.sb("d_zz", [128, 16])
        yc = P.sb("d_yc", [128, 8, 64])
        ycT = P.sb("d_ycT", [128, 4, 128])
        ones = self.cst('ones')
        cmask = self.cst('cmask')
        CK, CQ, CIK, CIW = 1024, 1152, 1664, 1728
        for i in range(NT):
            x = xs[i % 2]
            nk = (i + 1) * 128
            self.dma(x[:, :], src[i * 128:(i + 1) * 128, :], r=[(src, i)], w=[x])
            self.rms(x, g, hn, st)
            self.transp8(hn, hnT, 4)
            pv, pk = self.psv(6, 1)
            for k in range(8):
                self.mm(pv[:, 0:128], hnT[:, k, :], w[:, k, CK:CK + 128], k == 0, k == 7, r=[hnT, w], w=pk)
            self.copy(craw[:, :], pv[:, 0:128], r=pk, w=[craw])
            self.act(work[:, 0:128], craw[:, :], AF.Square, r=[craw], w=[work, st], accum=st[:, 2:3])
            self.act(st[:, 3:4], st[:, 2:3], AF.Sqrt, r=[st], w=[st], scale=1.0 / 128, bias=self.epsb[:, 0:1])
            self.recip(st[:, 3:4], st[:, 3:4], r=[st], w=[st])
            self.stt(c_tm[:, i, :], craw[:, :], st[:, 3:4], gkv[:, :], ALU.mult, ALU.mult, r=[craw, st, gkv], w=[(c_tm, i)])
            pv7, pk7 = self.psv(7, 1)
            self.tr(pv7[:, 0:128], c_tm[:, i, :], r=[(c_tm, i)], w=pk7)
            self.copy(cT[:, i * 128:(i + 1) * 128], pv7[:, 0:128], r=pk7, w=[(cT, i)])
            for k in range(8):
                self.mm(pv[0:64, 0:128], w[:, k, CIK:CIK + 64], hnT[:, k, :], k == 0, k == 7, r=[hnT, w], w=pk)
            self.act(ikT[:, i * 128:(i + 1) * 128], pv[0:64, 0:128], AF.Copy, r=pk, w=[(ikT, i)], scale=0.125)
            for k in range(8):
                self.mm(pv7[:, 0:8], hnT[:, k, :], w[:, k, CIW:CIW + 8], k == 0, k == 7, r=[hnT, w], w=pk7)
            self.act(iw[:, :], pv7[:, 0:8], AF.Copy, r=pk7, w=[iw], scale=8 ** -0.5)
            qv, qk = self.psv(0, 2)
            for h in range(8):
                for k in range(8):
                    self.mm(qv[:, h * 128:(h + 1) * 128], w[:, k, h * 128:(h + 1) * 128], hnT[:, k, :], k == 0, k == 7,
                            r=[hnT, w], w=qk)
            self.act(qlT[:, :, :].rearrange("p a b -> p (a b)"), qv[:, :], AF.Copy, r=qk, w=[qlT], scale=128 ** -0.5)
            iv, ik_ = self.psv(2, 2)
            for h in range(8):
                for k in range(8):
                    self.mm(iv[0:64, h * 128:(h + 1) * 128], w[:, k, CQ + h * 64:CQ + (h + 1) * 64], hnT[:, k, :], k == 0, k == 7,
                            r=[hnT, w], w=ik_)
            self.copy(iqT[:, :, :].rearrange("p a b -> p (a b)"), iv[0:64, :], r=ik_, w=[iqT])
            cnt = 0
            for c0 in range(0, nk, 512):
                cw = min(512, nk - c0)
                for h in range(8):
                    sv, sk = self.psv(4 + cnt % 2, 1)
                    r_ = rsb[cnt % 2]
                    cnt += 1
                    self.mm(sv[:, 0:cw], iqT[:, h, :], ikT[:, c0:c0 + cw], True, True, r=[iqT, ikT], w=sk)
                    self.act(r_[:, 0:cw], sv[:, 0:cw], AF.Relu, r=sk, w=[r_])
                    if h == 0:
                        self.ts(score[:, c0:c0 + cw], r_[:, 0:cw], iw[:, 0:1], None, ALU.mult, None, r=[r_, iw], w=[score])
                    else:
                        self.stt(score[:, c0:c0 + cw], r_[:, 0:cw], iw[:, h:h + 1], score[:, c0:c0 + cw], ALU.mult, ALU.add,
                                 r=[r_, iw, score], w=[score])
            self.tt(score[:, i * 128:nk], score[:, i * 128:nk], cmask, ALU.add, r=[score, self.C], w=[score])
            if i >= 2:
                cur = score
                for rnd in range(32):
                    m = m8[rnd % 2]
                    P.op('dve', lambda e, m=m, cur=cur, nk=nk: e.max(out=m[:, :], in_=cur[:, 0:nk]), r=[cur], w=[m])
                    if rnd < 31:
                        P.op('dve', lambda e, m=m, cur=cur, nk=nk: e.match_replace(
                            out=work[:, 0:nk], in_to_replace=m[:, :], in_values=cur[:, 0:nk], imm_value=-1e30),
                            r=[cur, m], w=[work])
                        cur = work
                self.ts(work[:, 0:nk], score[:, 0:nk], m8[1][:, 7:8], None, ALU.is_ge, None, r=[score, m8[1]], w=[work])
            else:
                self.ts(work[:, 0:nk], score[:, 0:nk], -1e29, None, ALU.is_ge, None, r=[score], w=[work])
            for kt in range(i + 1):
                b = 6 + (kt // 4) % 2
                mv, mk = self.psv(b, 1)
                self.tr(mv[:, (kt % 4) * 128:(kt % 4 + 1) * 128], work[:, kt * 128:(kt + 1) * 128], r=[work], w=mk)
                if kt % 4 == 3 or kt == i:
                    k0 = (kt // 4) * 4
                    n = kt - k0 + 1
                    self.copy(maskT[:, k0:kt + 1, :].rearrange("p a b -> p (a b)"), mv[:, 0:n * 128], r=mk, w=[maskT], eng='pool' if False else 'act')
            lv, lk = self.psv(0, 4)
            av, ak = self.psv(6, 2)
            zv, zk = self.psv(5, 1)
            for h in range(8):
                for kt in range(i + 1):
                    self.mm(lv[:, kt * 128:(kt + 1) * 128], cT[:, kt * 128:(kt + 1) * 128], qlT[:, h, :], True, True,
                            r=[cT, qlT], w=lk)
                ETf = ET[:, :, :].rearrange("p a b -> p (a b)")
                self.act(ETf[:, 0:nk], lv[:, 0:nk], AF.Exp, r=lk + [b31], w=[ET], bias=b31[:, h:h + 1], scale=1.0)
                self.tt(ETf[:, 0:nk], ETf[:, 0:nk], maskT[:, :, :].rearrange("p a b -> p (a b)")[:, 0:nk], ALU.mult,
                        r=[ET, maskT], w=[ET])
                for kt in range(max(0, i - 1), i + 1):
                    self.tt(ET[:, kt, :], ET[:, kt, :], corrT[:, h, i - kt, :], ALU.mult, r=[ET, corrT], w=[ET])
                for kt in range(i + 1):
                    self.mm(av[:, h * 128:(h + 1) * 128], c_tm[:, kt, :], ET[:, kt, :], kt == 0, kt == i, r=[c_tm, ET], w=ak)
                    self.mm(zv[:, h:h + 1], ET[:, kt, :], ones[:, 0:1], kt == 0, kt == i, r=[ET, self.C], w=zk)
            self.copy(latT[:, :, :].rearrange("p a b -> p (a b)"), av[:, :], r=ak, w=[latT])
            self.copy(zz[:, 0:8], zv[:, 0:8], r=zk, w=[zz], eng='dve')
            self.recip(zz[:, 8:16], zz[:, 0:8], r=[zz], w=[zz])
            yv, yk = self.psv(4, 1)
            for h in range(8):
                self.mm(yv[:, h * 64:(h + 1) * 64], latT[:, h, :], w_uv[:, h, :], True, True, r=[latT, w_uv], w=yk)
            self.tt(yc[:, :, :], yv[:, :].rearrange("p (h e) -> p h e", e=64), bcast(zz[:, 8:16], 2, 64), ALU.mult,
                    r=yk + [zz], w=[yc])
            tv_, tk_ = self.psv(5, 1)
            ycf = yc[:, :, :].rearrange("p h e -> p (h e)")
            for k in range(4):
                self.tr(tv_[:, k * 128:(k + 1) * 128], ycf[:, k * 128:(k + 1) * 128], r=[yc], w=tk_)
            self.copy(ycT[:, :, :].rearrange("p a b -> p (a b)"), tv_[:, :], r=tk_, w=[ycT])
            ov, ok = self.psv(0, 2)
            for half in range(2):
                for k in range(4):
                    self.mm(ov[:, half * 512:(half + 1) * 512], ycT[:, k, :], w_out[:, k, half * 512:(half + 1) * 512],
                            k == 0, k == 3, r=[ycT, w_out], w=ok)
            self.tt(x[:, :], ov[:, :], x[:, :], ALU.add, r=ok + [x], w=[x])
            self.dma(dst[i * 128:(i + 1) * 128, :], x[:, :], r=[x], w=[(dst, i)])


    def phase_hgrn(self, l, src, acc):
        P = self.P
        din = self.din
        jx = l // 2
        w = P.sb("h_w", [128, 8, 2048])
        w_out = P.sb("h_wout", [128, 4, D])
        g = P.sb("h_g", [128, D])
        gn = P.sb("h_gn", [128, 512])
        gam = P.sb("h_gam", [128, 4, 512])
        lb = P.sb("h_lb", [128, 512])
        oml = P.sb("h_oml", [128, 512])
        lbT = P.sb("h_lbT", [128, 8])
        tmp = P.sb("h_tmp", [128, 512])
        self.gload(g, din['mix_norm'], din['mix_norm'][l:l + 1, :])
        self.gload(gn, din['odd_hg_norm'], din['odd_hg_norm'][jx:jx + 1, :])
        self.dma(w[:, :, :], din['odd_w_in'][jx][:, 1736:3784].rearrange("(k p) c -> p k c", p=128), r=[din['odd_w_in']], w=[w])
        self.dma(w_out[:, :, :], din['odd_w_out'][jx][512:1024, :].rearrange("(k p) c -> p k c", p=128), r=[din['odd_w_out']], w=[w_out])
        for ll in range(4):
            self.dma(gam[:, ll, :], din['hgrn_gamma'][ll:ll + 1, :].broadcast_to([128, 512]), r=[din['hgrn_gamma']], w=[(gam, ll)])
        self.act(gam[:, :, :], gam[:, :, :], AF.Exp, r=[gam], w=[gam])
        self.tt(tmp[:, :], gam[:, 0, :], gam[:, 1, :], ALU.add, r=[gam], w=[tmp])
        self.tt(tmp[:, :], tmp[:, :], gam[:, 2, :], ALU.add, r=[gam, tmp], w=[tmp])
        self.tt(tmp[:, :], tmp[:, :], gam[:, 3, :], ALU.add, r=[gam, tmp], w=[tmp])
        self.recip(tmp[:, :], tmp[:, :], r=[tmp], w=[tmp])
        P.op('dve', lambda e: e.memset(lb[:, :], 0.0), w=[lb])
        for ll in range(l):
            self.tt(lb[:, :], lb[:, :], gam[:, ll, :], ALU.add, r=[lb, gam], w=[lb])
        self.tt(lb[:, :], lb[:, :], tmp[:, :], ALU.mult, r=[lb, tmp], w=[lb])
        self.ts(oml[:, :], lb[:, :], -1.0, 1.0, ALU.mult, ALU.add, r=[lb], w=[oml])
        pv, pk = self.psv(0, 1)
        for h in range(4):
            self.tr(pv[:, h * 128:(h + 1) * 128], lb[:, h * 128:(h + 1) * 128], r=[lb], w=pk)
        for h in range(4):
            self.copy(lbT[:, h:h + 1], pv[:, h * 128:h * 128 + 1], r=pk, w=[lbT], eng='dve')
        self.ts(lbT[:, 4:8], lbT[:, 0:4], -1.0, 1.0, ALU.mult, ALU.add, r=[lbT], w=[lbT])
        Sst = [P.sb(f"h_S{j}", [128, 4, 128]) for j in range(2)]
        qt0 = P.sb("h_qt0", [128, 4, 128])
        qt1 = P.sb("h_qt1", [128, 4, 128])
        kh0 = P.sb("h_kh0", [128, 512])
        kh1 = P.sb("h_kh1", [128, 512])
        P.op('pool', lambda e: e.memset(Sst[0][:, :, :], 0.0), w=[Sst[0]])
        P.op('pool', lambda e: e.memset(qt0[:, :, :], 0.0), w=[qt0])
        P.op('pool', lambda e: e.memset(qt1[:, :, :], 0.0), w=[qt1])
        P.op('pool', lambda e: e.memset(kh0[:, :], 0.0), w=[kh0])
        P.op('pool', lambda e: e.memset(kh1[:, :], 0.0), w=[kh1])
        xs = self.rot("h_x", [128, D])
        hn = P.sb("h_hn", [128, D])
        hnT = P.sb("h_hnT", [128, 8, 128])
        st = P.sb("h_st", [128, 16])
        sg = P.sb("h_sg", [128, 512])
        f_tm = P.sb("h_f", [128, 512])
        lf = P.sb("h_lf", [128, 512])
        kk = P.sb("h_kk", [128, 512])
        i_sb = P.sb("h_i", [128, 512])
        sil = P.sb("h_sil", [128, 512])
        sgT = P.sb("h_sgT", [128, 4, 128])
        kkT = P.sb("h_kkT", [128, 4, 128])
        eAT = P.sb("h_eAT", [128, 4, 128])
        enAT = P.sb("h_enAT", [128, 4, 128])
        qtT = P.sb("h_qtT", [128, 4, 128])
        ktT = P.sb("h_ktT", [128, 4, 128])
        a_sb = P.sb("h_a", [128, 512])
        d_sb = P.sb("h_d", [128, 512])
        sc = self.rot("h_sc", [128, 128])
        y_sb = P.sb("h_y", [128, 512])
        yT = P.sb("h_yT", [128, 4, 128])
        acc_t = self.rot("h_acc", [128, D])
        U2 = self.cst('U2')
        B2 = self.cst('B2')
        HQ, HF, HI, HG = 0, 512, 1024, 1536

        def proj_tm(c0, bank):
            pvw, pkw = self.psv(bank, 1)
            for k in range(8):
                self.mm(pvw[:, :], hnT[:, k, :], w[:, k, c0:c0 + 512], k == 0, k == 7, r=[hnT, w], w=pkw)
            return pvw, pkw

        def proj_fm(c0, bank):
            pvw, pkw = self.psv(bank, 1)
            for h in range(4):
                for k in range(8):
                    self.mm(pvw[:, h * 128:(h + 1) * 128], w[:, k, c0 + h * 128:c0 + (h + 1) * 128], hnT[:, k, :],
                            k == 0, k == 7, r=[hnT, w], w=pkw)
            return pvw, pkw

        for i in range(NT):
            x = xs[i % 2]
            self.dma(x[:, :], src[i * 128:(i + 1) * 128, :], r=[(src, i)], w=[x])
            self.rms(x, g, hn, st)
            self.transp8(hn, hnT, 0)
            pf, kf = proj_tm(HF, 2)
            self.act(sg[:, :], pf[:, :], AF.Sigmoid, r=kf, w=[sg])
            self.tt(f_tm[:, :], sg[:, :], oml[:, :], ALU.mult, r=[sg, oml], w=[f_tm])
            self.tt(f_tm[:, :], f_tm[:, :], lb[:, :], ALU.add, r=[f_tm, lb], w=[f_tm])
            self.act(lf[:, :], f_tm[:, :], AF.Ln, r=[f_tm], w=[lf])
            self.ts(kk[:, :], f_tm[:, :], -1.0, 1.0, ALU.mult, ALU.add, r=[f_tm], w=[kk])
            pi_, ki_ = proj_tm(HI, 3)
            self.copy(i_sb[:, :], pi_[:, :], r=ki_, w=[i_sb])
            pg_, kg_ = proj_tm(HG, 4)
            self.act(sil[:, :], pg_[:, :], AF.Sigmoid, r=kg_, w=[sil])
            self.tt(sil[:, :], sil[:, :], pg_[:, :], ALU.mult, r=[sil] + kg_, w=[sil])
            pq, kq = proj_fm(HQ, 5)
            pfT, kfT = proj_fm(HF, 6)
            self.act(sgT[:, :, :].rearrange("p a b -> p (a b)"), pfT[:, :], AF.Sigmoid, r=kfT, w=[sgT])
            for h in range(4):
                self.ts(kkT[:, h, :], sgT[:, h, :], lbT[:, 4 + h:5 + h], lbT[:, h:h + 1], ALU.mult, ALU.add, r=[sgT, lbT], w=[kkT])
            self.ts(kkT[:, :, :], kkT[:, :, :], -1.0, 1.0, ALU.mult, ALU.add, r=[kkT], w=[kkT])
            pA, kA = self.psv(2, 1)
            self.mm(pA[:, :], U2, lf[:, :], True, True, r=[self.C, lf], w=kA)
            self.copy(a_sb[:, :], pA[:, :], r=kA, w=[a_sb])
            pE, kE = self.psv(3, 1)
            self.mm(pE[:, :], B2, lf[:, :], True, True, r=[self.C, lf], w=kE)
            pAT, kAT = self.psv(4, 1)
            for h in range(4):
                self.mm(pAT[:, h * 128:(h + 1) * 128], lf[:, h * 128:(h + 1) * 128], U2, True, True, r=[self.C, lf], w=kAT)
            self.act(eAT[:, :, :].rearrange("p a b -> p (a b)"), pAT[:, :], AF.Exp, r=kAT, w=[eAT])
            self.act(enAT[:, :, :].rearrange("p a b -> p (a b)"), pAT[:, :], AF.Exp, r=kAT, w=[enAT], scale=-1.0)
            self.tt(qtT[:, :, :].rearrange("p a b -> p (a b)"), pq[:, :], eAT[:, :, :].rearrange("p a b -> p (a b)"), ALU.mult,
                    r=kq + [eAT], w=[qtT])
            self.tt(ktT[:, :, :], kkT[:, :, :], enAT[:, :, :], ALU.mult, r=[kkT, enAT], w=[ktT])
            self.copy(qt0[:, :, 0:64], qtT[:, :, 0:64], r=[qtT], w=[qt0], eng='pool')
            self.copy(qt1[:, :, 64:128], qtT[:, :, 64:128], r=[qtT], w=[qt1], eng='pool')
            self.tt(d_sb[:, :], pE[:, :], a_sb[:, :], ALU.subtract, r=kE + [a_sb], w=[d_sb])
            self.act(d_sb[:, :], d_sb[:, :], AF.Exp, r=[d_sb], w=[d_sb])
            self.tt(kh0[0:64, :], d_sb[0:64, :], kk[0:64, :], ALU.mult, r=[d_sb, kk], w=[kh0])
            self.tt(kh1[64:128, :], d_sb[64:128, :], kk[64:128, :], ALU.mult, r=[d_sb, kk], w=[kh1])
            po, ko = self.psv(7, 1)
            for h in range(4):
                hs = slice(h * 128, (h + 1) * 128)
                S0, S1 = Sst[0], Sst[1]
                ps_, ks_ = self.psv(0, 1)
                self.mm(ps_[:, 0:128], ktT[:, h, :], qtT[:, h, :], True, True, r=[ktT, qtT], w=ks_)
                scb = sc[h % 2]
                self.tt(scb[:, :], ps_[:, 0:128], U2, ALU.mult, r=ks_ + [self.C], w=[scb])
                self.mm(po[:, hs], scb[:, :], i_sb[:, hs], True, False, r=[scb, i_sb], w=ko)
                self.mm(po[:, hs], qt0[:, h, :], S0[:, h, :], False, False, r=[qt0, (S0, h)], w=ko)
                p1, k1 = self.psv(1, 1)
                self.mm(p1[:, 0:128], kh0[:, hs], i_sb[:, hs], True, True, r=[kh0, i_sb], w=k1)
                self.stt(S1[:, h, :], S0[:, h, :], eAT[:, h, 63:64], p1[:, 0:128], ALU.mult, ALU.add, r=[(S0, h), eAT] + k1, w=[(S1, h)])
                self.mm(po[:, hs], qt1[:, h, :], S1[:, h, :], False, True, r=[qt1, (S1, h)], w=ko)
                p2, k2 = self.psv(2, 1)
                self.mm(p2[:, 0:128], kh1[:, hs], i_sb[:, hs], True, True, r=[kh1, i_sb], w=k2)
                self.stt(S0[:, h, :], S1[:, h, :], eAT[:, h, 127:128], p2[:, 0:128], ALU.mult, ALU.add, r=[(S1, h), eAT] + k2, w=[(S0, h)])
            for h in range(4):
                self.act(y_sb[:, h * 128:(h + 1) * 128], po[:, h * 128:(h + 1) * 128], AF.Square, r=ko, w=[y_sb, st], accum=st[:, 4 + h:5 + h])
            self.act(st[:, 8:12], st[:, 4:8], AF.Sqrt, r=[st], w=[st], scale=1.0 / 128, bias=self.epsb[:, 0:1])
            self.recip(st[:, 8:12], st[:, 8:12], r=[st], w=[st])
            for h in range(4):
                self.ts(y_sb[:, h * 128:(h + 1) * 128], po[:, h * 128:(h + 1) * 128], st[:, 8 + h:9 + h], None, ALU.mult, None,
                        r=ko + [st], w=[y_sb])
            self.tt(y_sb[:, :], y_sb[:, :], gn[:, :], ALU.mult, r=[y_sb, gn], w=[y_sb])
            self.tt(y_sb[:, :], y_sb[:, :], sil[:, :], ALU.mult, r=[y_sb, sil], w=[y_sb])
            tv_, tk_ = self.psv(5, 1)
            for k in range(4):
                self.tr(tv_[:, k * 128:(k + 1) * 128], y_sb[:, k * 128:(k + 1) * 128], r=[y_sb], w=tk_)
            self.copy(yT[:, :, :].rearrange("p a b -> p (a b)"), tv_[:, :], r=tk_, w=[yT])
            at = acc_t[i % 2]
            self.dma(at[:, :], acc[i * 128:(i + 1) * 128, :], r=[(acc, i)], w=[at])
            ov, ok = self.psv(2, 2)
            for half in range(2):
                for k in range(4):
                    self.mm(ov[:, half * 512:(half + 1) * 512], yT[:, k, :], w_out[:, k, half * 512:(half + 1) * 512],
                            k == 0, k == 3, r=[yT, w_out], w=ok)
            self.tt(at[:, :], ov[:, :], at[:, :], ALU.add, r=ok + [at], w=[at])
            self.dma(acc[i * 128:(i + 1) * 128, :], at[:, :], r=[at], w=[(acc, i)])

    def phase_copy(self, src, dst):
        xs = self.rot("c_x", [128, D])
        for i in range(NT):
            self.dma(xs[i % 2][:, :], src[i * 128:(i + 1) * 128, :], r=[(src, i)], w=[xs[i % 2]])
            self.dma(dst[i * 128:(i + 1) * 128, :], xs[i % 2][:, :], r=[xs[i % 2]], w=[(dst, i)])

    def phase_final(self, src):
        P = self.P
        g = P.sb("g_f", [128, D])
        self.gload(g, self.din['final_norm'], self.din['final_norm'][0:1, :])
        xs = self.rot("xf", [128, D])
        hns = self.rot("hnf", [128, D])
        sts = self.rot("stf", [128, 4])
        for i in range(NT):
            j = i % 2
            self.dma(xs[j][:, :], src[i * 128:(i + 1) * 128, :], r=[(src, i)], w=[xs[j]])
            self.rms(xs[j], g, hns[j], sts[j])
            self.dma(self.out[i * 128:(i + 1) * 128, :], hns[j][:, :], r=[hns[j]], w=[(self.out, i)])

    def run_phase(self, fn, *a):
        with ExitStack() as pes:
            self.P.pes = pes
            fn(*a)
            self.P.flush()
        self.P.pes = self.es

    def build(self, plan):
        cur = self.din['x']
        self.run_phase(self.phase_setup)
        for ph in plan:
            if ph[0] == 'xattn':
                self.run_phase(self.phase_xattn, ph[1], cur, self.hA)
                cur = self.hA
            elif ph[0] == 'even':
                self.run_phase(self.phase_even, ph[1], cur, self.hA)
                cur = self.hA
            elif ph[0] == 'peer':
                self.run_phase(self.phase_peer, ph[1], cur, self.hA)
                cur = self.hA
            elif ph[0] == 'dsa':
                other = self.hB if cur is not self.hB else self.hA
                self.run_phase(self.phase_dsa, ph[1], cur, other)
                cur = other
            elif ph[0] == 'hgrn':
                other = self.hB if cur is not self.hB else self.hA
                self.run_phase(self.phase_copy, cur, other)
                self.run_phase(self.phase_hgrn, ph[1], cur, other)
                cur = other
            elif ph[0] == 'odd':
                other = self.hB if cur is not self.hB else self.hA
                self.run_phase(self.phase_dsa, ph[1], cur, other)
                self.run_phase(self.phase_hgrn, ph[1], cur, other)
                cur = other
            elif ph[0] == 'final':
                self.run_phase(self.phase_final, cur)
        return self.nc


FULL_PLAN = []
for _l in range(DEPTH):
    FULL_PLAN.append(('even' if _l % 2 == 0 else 'odd', _l))
    FULL_PLAN.append(('xattn', _l))
    FULL_PLAN.append(('peer', _l))
FULL_PLAN.append(('final',))


def t5_bucket_np(d):
    d = np.maximum(d, 0)
    lr = np.log(np.maximum(d, 1).astype(np.float32) / np.float32(16)) / np.float32(np.log(128 / 16))
    large = 16 + (lr * np.float32(16)).astype(np.int32)
    large = np.minimum(large, 31)
    return np.where(d < 16, d, large)


def prep_inputs(inp):
    f = lambda a: np.ascontiguousarray(np.asarray(a, dtype=np.float32))
    shared = {}
    for k in IN_SHAPES:
        if k in ('x', 'mem'):
            continue
        if k == 'peer_keysT':
            a = np.asarray(inp['peer_keys'], dtype=np.float32)
            shared[k] = f(a.transpose(0, 4, 1, 2, 3).reshape(4, 128, 16, 128))
        elif k == 'even_conv_w':
            a = np.asarray(inp[k]).reshape(2, 3, 4, 128)
            shared[k] = f(a.transpose(0, 3, 2, 1).reshape(2, 128, 12))
        elif k == 'even_pool_scale':
            a = np.asarray(inp[k]).reshape(2, 4, 128)
            shared[k] = f(a.transpose(0, 2, 1))
        elif k == 'peer_uv':
            shared[k] = np.ascontiguousarray(np.concatenate(
                [np.asarray(inp['peer_u'], dtype=np.float32), np.asarray(inp['peer_v'], dtype=np.float32)], axis=-1))
        elif k == 'invc':
            shared[k] = INVC[0]
        elif k == 'biasT':
            rb = np.asarray(inp['rel_bias'], dtype=np.float32)
            ss_, tq_ = np.arange(128)[:, None], np.arange(128)[None, :]
            out = np.zeros((128, 8, 2, 128), dtype=np.float32)
            for dl in (0, 1):
                dist = np.maximum(128 * dl + tq_ - ss_, 0)
                out[:, :, dl, :] = rb[t5_bucket_np(dist)].transpose(0, 2, 1)
            shared[k] = f(out)
        elif k in ('mem_norm', 'final_norm'):
            shared[k] = f(np.asarray(inp[k]).reshape(1, D))
        else:
            shared[k] = f(inp[k])
    return shared


def kernel(plan=None, **inp):
    plan = FULL_PLAN if plan is None else plan
    m = Model()
    nc = m.build(plan)
    shared = prep_inputs(inp)
    shared['consts'] = m.carr
    shared = {k: v for k, v in shared.items() if k in m.din}
    x = np.asarray(inp['x'], dtype=np.float32)
    mem = np.asarray(inp['mem'], dtype=np.float32)
    in_maps = []
    for b in range(8):
        d = dict(shared)
        if 'x' in m.din:
            d['x'] = np.ascontiguousarray(x[b])
        if 'mem' in m.din:
            d['mem'] = np.ascontiguousarray(mem[b])
        in_maps.append(d)
    res = run_bass_kernel_spmd(nc, in_maps, core_ids=list(range(8)))
    m.es.close()
    return np.stack([np.asarray(r["out"], dtype=np.float32) for r in res.results], axis=0)
```

```python
import numpy as np
import concourse.bass as bass
import concourse.mybir as mybir
from concourse.bass_utils import run_bass_kernel_spmd
from contextlib import ExitStack

F32 = mybir.dt.float32
I32 = mybir.dt.int32
U32 = mybir.dt.uint32
AF = mybir.ActivationFunctionType
ALU = mybir.AluOpType
AX = mybir.AxisListType

ENGS = ['pe', 'dve', 'act', 'pool', 'sp']
SEM_LIMIT = 30000
DMA_K = 16


class T:
    def __init__(self, h, name):
        self.h = h
        self.name = name
        self.st = {}

    def __getitem__(self, idx):
        return self.h[idx]


def bcast(ap, axis, n):
    l = [list(x) for x in ap.ap]
    l.insert(axis, [0, n])
    return bass.AP(ap.tensor, ap.offset, l)


def rep(ap, axis, n):
    l = [list(x) for x in ap.ap]
    assert l[axis][1] == 1
    l[axis] = [0, n]
    return bass.AP(ap.tensor, ap.offset, l)


class Prog:
    def __init__(self, nc, es):
        self.nc = nc
        self.es = es
        self.pes = es
        self.ops = {e: [] for e in ENGS}
        self.base = {e: 0 for e in ENGS}
        self.waited = {e: {} for e in ENGS}
        self.waited_dma = {e: set() for e in ENGS}
        self.tiles = []
        self.csems = {e: [] for e in ENGS}
        self.ccount = {e: 0 for e in ENGS}
        self.dsems = {}
        self.dcount = {e: 0 for e in ENGS}
        self.dma_hist = {e: [] for e in ENGS}

    def sb(self, name, shape, dt=F32, glob=False):
        es = self.es if glob else self.pes
        self.uid = getattr(self, 'uid', 0) + 1
        name = f"{name}_{self.uid}"
        t = T(es.enter_context(self.nc.sbuf_tensor(name, list(shape), dt)), name)
        self.tiles.append(t)
        return t

    def ps(self, name, shape, dt=F32):
        t = T(self.es.enter_context(self.nc.psum_tensor(name, list(shape), dt)), name)
        self.tiles.append(t)
        return t

    def dram(self, name, shape, dt=F32, kind="Internal"):
        t = T(self.nc.dram_tensor(name, list(shape), dt, kind=kind).ap(), name)
        self.tiles.append(t)
        return t

    @staticmethod
    def _norm(key):
        if isinstance(key, T):
            return key, None
        return key[0], key[1]

    def _entries(self, tile, sub):
        if sub is None:
            return list(tile.st.values())
        out = []
        if sub in tile.st:
            out.append(tile.st[sub])
        if None in tile.st:
            out.append(tile.st[None])
        return out

    def op(self, eng, fn, r=(), w=(), dma=False, extra=()):
        idx = len(self.ops[eng])
        deps = set(extra)
        for key in r:
            tile, sub = self._norm(key)
            for ent in self._entries(tile, sub):
                if ent[0] is not None:
                    deps.add(ent[0])
        for key in w:
            tile, sub = self._norm(key)
            for ent in self._entries(tile, sub):
                if ent[0] is not None:
                    deps.add(ent[0])
                for e2, i2 in ent[1].items():
                    deps.add((e2, i2))
        for key in r:
            tile, sub = self._norm(key)
            ent = tile.st.setdefault(sub, [None, {}])
            ent[1][eng] = idx
        for key in w:
            tile, sub = self._norm(key)
            if sub is None:
                tile.st = {None: [(eng, idx), {}]}
            else:
                tile.st[sub] = [(eng, idx), {}]
        if dma:
            h = self.dma_hist[eng]
            if len(h) >= DMA_K:
                deps.add((eng, h[-DMA_K]))
        final = []
        for (e2, i2) in sorted(deps, reverse=True):
            if e2 == eng and i2 == idx:
                continue
            if self.ops[e2][i2]['dma']:
                if (e2, i2) in self.waited_dma[eng]:
                    continue
                self.waited_dma[eng].add((e2, i2))
                final.append((e2, i2))
            else:
                if e2 == eng and eng == 'pe':
                    continue
                if self.waited[eng].get(e2, -1) >= i2:
                    continue
                self.waited[eng][e2] = i2
                final.append((e2, i2))
        o = dict(fn=fn, deps=final, dma=dma, sig=False)
        if dma:
            o['dma_n'] = self.dcount[eng]
            self.dcount[eng] += 1
            self.dma_hist[eng].append(idx)
        self.ops[eng].append(o)
        return (eng, idx)

    def _sigof(self, e2, i2):
        o = self.ops[e2][i2]
        if o['dma']:
            n = o['dma_n']
            return self.dsems[e2][n % DMA_K], 16 * (n // DMA_K + 1)
        j, v = o['sv']
        return self.csems[e2][j], v

    def flush(self):
        nc = self.nc
        last = {}
        for e in ENGS:
            for i in range(len(self.ops[e]) - 1, self.base[e] - 1, -1):
                if not self.ops[e][i]['dma'] and self.ops[e][i]['fn'] is not None:
                    last[e] = (e, i)
                    break
        dmas = []
        for e in ENGS:
            dmas += [(e, i) for i in self.dma_hist[e][-DMA_K:] if i >= self.base[e]]
        for e in ENGS:
            extra = [v for k, v in last.items() if k != e] + dmas
            self.op(e, None, extra=extra)
        for e in ENGS:
            for o in self.ops[e][self.base[e]:]:
                for (e2, i2) in o['deps']:
                    assert i2 >= self.base[e2], "cross-phase dep"
                    self.ops[e2][i2]['sig'] = True
        for e in ENGS:
            for o in self.ops[e][self.base[e]:]:
                if o['dma']:
                    if e not in self.dsems:
                        self.dsems[e] = [self.es.enter_context(nc.semaphore(f"d_{e}_{j}")) for j in range(DMA_K)]
                    continue
                if o['sig']:
                    c = self.ccount[e]
                    j = c // SEM_LIMIT
                    while len(self.csems[e]) <= j:
                        self.csems[e].append(self.es.enter_context(nc.semaphore(f"c_{e}_{len(self.csems[e])}")))
                    o['sv'] = (j, c % SEM_LIMIT + 1)
                    self.ccount[e] = c + 1

        def run(e, eng):
            for o in self.ops[e][self.base[e]:]:
                for (e2, i2) in o['deps']:
                    s, v = self._sigof(e2, i2)
                    eng.wait_ge(s, v)
                if o['fn'] is None:
                    continue
                ins = o['fn'](eng)
                if o['dma']:
                    n = o['dma_n']
                    ins.then_inc(self.dsems[e][n % DMA_K], 16)
                elif o['sig']:
                    j, v = o['sv']
                    ins.then_inc(self.csems[e][j], 1)

        with nc.Block() as block:
            @block.tensor
            def _(eng):
                run('pe', eng)

            @block.vector
            def _(eng):
                run('dve', eng)

            @block.scalar
            def _(eng):
                run('act', eng)

            @block.gpsimd
            def _(eng):
                run('pool', eng)

            @block.sync
            def _(eng):
                run('sp', eng)
        for e in ENGS:
            self.base[e] = len(self.ops[e])
        for t in self.tiles:
            t.st = {}

D = 1024
S = 2048
NT = 16
NMEM = 256
DEPTH = 4
EPS = 1e-6
FAST_MM = False
BF16 = mybir.dt.bfloat16
F32R = mybir.dt.float32r

IN_SHAPES = {
    'x': [S, D], 'mem': [NMEM, D], 'mix_norm': [4, D],
    'even_w_in': [2, D, 2048], 'even_conv_w': [2, 128, 12], 'even_pool_w': [2, 4, 128, 128],
    'even_pool_scale': [2, 128, 4], 'even_w_out': [2, D, D],
    'odd_w_in': [2, D, 3784], 'odd_kv_norm': [2, 128], 'odd_w_uv': [2, 8, 128, 64],
    'odd_hg_norm': [2, 512], 'odd_w_out': [2, D, D], 'hgrn_gamma': [4, 512],
    'rel_bias': [32, 8], 'invc': [128, 2048], 'biasT': [128, 8, 2, 128], 'mem_norm': [1, D], 'xattn_norm': [4, D],
    'xattn_wq': [4, D, D], 'xattn_wkv': [4, D, 2 * D], 'xattn_wo': [4, D, D],
    'peer_norm': [4, D], 'peer_wq': [4, D, 2048], 'peer_keysT': [4, 128, 16, 128],
    'peer_uv': [4, 16384, 2 * D], 'final_norm': [1, D],
}


INVC = [None]


def make_consts():
    parts = {}
    parts['ident'] = np.eye(128, dtype=np.float32)
    t = np.arange(512)
    invc = np.concatenate([1.0 / np.minimum(t + 1, w) for w in (2, 4, 8, 16)]).astype(np.float32)
    INVC[0] = np.ascontiguousarray(np.tile(invc[None, :], (128, 1)).astype(np.float32))
    parts['iota16'] = np.tile(np.arange(16, dtype=np.float32)[None, :], (128, 1))
    ii = np.arange(128)
    parts['U2'] = ((ii[:, None] // 64 == ii[None, :] // 64) & (ii[:, None] <= ii[None, :])).astype(np.float32)
    parts['B2'] = (ii[:, None] // 64 == ii[None, :] // 64).astype(np.float32)
    parts['cmask'] = np.where(ii[None, :] <= ii[:, None], 0.0, -1e30).astype(np.float32)
    parts['ones'] = np.ones((128, 8), dtype=np.float32)
    off = 0
    lay = {}
    arrs = []
    for k, v in parts.items():
        lay[k] = (off, v.shape[1])
        off += v.shape[1]
        arrs.append(v.astype(np.float32))
    return np.ascontiguousarray(np.concatenate(arrs, axis=1)), lay


class Model:
    def __init__(self):
        self.nc = bass.Bass("TRN2", target_bir_lowering=False)
        self.es = ExitStack()
        self.P = Prog(self.nc, self.es)
        P = self.P
        carr, self.clay = make_consts()
        self.carr = carr
        model = self

        class LazyIn(dict):
            def __missing__(self, k):
                shp = list(carr.shape) if k == 'consts' else IN_SHAPES[k]
                t = P.dram(k, shp, F32, kind="ExternalInput")
                self[k] = t
                return t
        self.din = LazyIn()
        self.out = P.dram("out", [S, D], F32, kind="ExternalOutput")
        self.hA = P.dram("hA", [S, D])
        self.hB = P.dram("hB", [S, D])
        self.uvb = P.dram("uvb", [16384, 2 * D], BF16)
        self.PS = P.ps("ps", [128, 8, 512])
        self.C = P.sb("consts_sb", [128, carr.shape[1]], glob=True)
        self.memT = P.sb("memT", [128, 8, NMEM], glob=True)
        self.rotc = {}
        self.uvb_ready = {}
        self.want_peer = {}

    def cst(self, name):
        o, w = self.clay[name]
        return self.C[:, o:o + w]

    def psv(self, b0, nb=2):
        ap = self.PS[:, b0:b0 + nb, :].rearrange("p a b -> p (a b)")
        return ap, [(self.PS, b) for b in range(b0, b0 + nb)]

    def mm(self, out, lhsT, rhs, start, stop, r, w, fast=True):
        if FAST_MM and fast and rhs.shape[-1] % 2 == 0 and out.shape[-1] % 2 == 0:
            lhsT = lhsT.bitcast(F32R)
            rhs = rhs.bitcast(F32R)
        self.P.op('pe', lambda e: e.matmul(out, lhsT, rhs, start=start, stop=stop), r=r, w=w)

    def tr(self, out, in_, r, w):
        ident = self.cst('ident')
        self.P.op('pe', lambda e: e.transpose(out, in_, ident), r=list(r) + [self.C], w=w)

    def act(self, out, in_, func, r, w, bias=None, scale=None, accum=None):
        kw = {}
        if bias is not None:
            kw['bias'] = bias
        if scale is not None:
            kw['scale'] = scale
        if accum is not None:
            kw['accum_out'] = accum
        self.P.op('act', lambda e: e.activation(out, in_, func, **kw), r=r, w=w)

    def tt(self, out, a, b, op, r, w, eng='dve'):
        self.P.op(eng, lambda e: e.tensor_tensor(out, a, b, op), r=r, w=w)

    def ts(self, out, a, s1, s2, op0, op1, r, w, eng='dve', accum=None):
        if op1 is None:
            self.P.op(eng, lambda e: e.tensor_scalar(out, a, s1, None, op0), r=r, w=w)
        elif accum is None:
            self.P.op(eng, lambda e: e.tensor_scalar(out, a, s1, s2, op0, op1), r=r, w=w)
        else:
            self.P.op(eng, lambda e: e.tensor_scalar(out, a, s1, s2, op0, op1, accum_out=accum), r=r, w=w)

    def stt(self, out, a, scalar, b, op0, op1, r, w):
        self.P.op('dve', lambda e: e.scalar_tensor_tensor(out, a, scalar, b, op0, op1), r=r, w=w)

    def copy(self, out, in_, r, w, eng='act'):
        if eng == 'act':
            self.P.op('act', lambda e: e.copy(out, in_), r=r, w=w)
        else:
            self.P.op(eng, lambda e: e.tensor_copy(out, in_), r=r, w=w)

    def dma(self, out, in_, r, w, eng='sp'):
        self.P.op(eng, lambda e: e.dma_start(out=out, in_=in_), r=r, w=w, dma=True)

    def recip(self, out, in_, r, w):
        self.P.op('dve', lambda e: e.reciprocal(out, in_), r=r, w=w)

    def rot(self, name, shape, n=2, dt=F32):
        return [self.P.sb(f"{name}{j}", shape, dt) for j in range(n)]

    def wload(self, dst, src_t, src_ap):
        self.dma(dst[:, :, :], src_ap.rearrange("(k p) c -> p k c", p=128), r=[src_t], w=[dst])

    def gload(self, dst, src_t, row_ap):
        n = row_ap.shape[1]
        self.dma(dst[:, :], row_ap.broadcast_to([128, n]), r=[src_t], w=[dst])

    def rms(self, x, g, hn, st, width=D):
        self.act(hn[:, :], x[:, :], AF.Square, r=[x], w=[hn, st], accum=st[:, 0:1])
        self.act(st[:, 1:2], st[:, 0:1], AF.Sqrt, r=[st], w=[st], scale=1.0 / width, bias=self.epsb[:, 0:1])
        self.recip(st[:, 1:2], st[:, 1:2], r=[st], w=[st])
        self.stt(hn[:, :], x[:, :], st[:, 1:2], g[:, :], ALU.mult, ALU.mult, r=[x, st, g], w=[hn])

    def transp8(self, src, dstT, b0, n=8):
        nb = (n * 128 + 511) // 512
        pv, pk = self.psv(b0, nb)
        for k in range(n):
            self.tr(pv[:, k * 128:(k + 1) * 128], src[:, k * 128:(k + 1) * 128], r=[src], w=pk)
        self.copy(dstT[:, :, :].rearrange("p a b -> p (a b)"), pv[:, 0:n * 128], r=pk, w=[dstT])

    def phase_setup(self):
        P = self.P
        self.dma(self.C[:, :], self.din['consts'][:, :], r=[self.din['consts']], w=[self.C])
        self.epsb = P.sb("epsb", [128, 1], glob=True)
        P.op('dve', lambda e: e.memset(self.epsb[:, :], EPS), w=[self.epsb])
        g = P.sb("g_mem", [128, D])
        self.gload(g, self.din['mem_norm'], self.din['mem_norm'][0:1, :])
        xs = self.rot("xm", [128, D])
        hn = self.rot("hnm", [128, D])
        st = self.rot("stm", [128, 4])
        mT = [P.sb(f"mT{j}", [128, 8, 128]) for j in range(2)]
        for m in range(2):
            self.dma(xs[m][:, :], self.din['mem'][m * 128:(m + 1) * 128, :], r=[self.din['mem']], w=[xs[m]])
            self.rms(xs[m], g, hn[m], st[m])
            self.transp8(hn[m], mT[m], 2 * m)
            self.copy(self.memT[:, :, m * 128:(m + 1) * 128], mT[m][:, :, :], r=[mT[m]], w=[(self.memT, m)], eng='dve')

    def convert_uv(self, l):
        P = self.P
        din = self.din
        uvb2 = self.uvb[:, :].rearrange("n (a d) -> (n a) d", a=2)
        uvf2 = din['peer_uv'][l].rearrange("n (a d) -> (n a) d", a=2)
        for c in range(16):
            P.op('pool', lambda e, c=c: e.dma_start(out=uvb2[c * 2048:(c + 1) * 2048, :],
                                                    in_=uvf2[c * 2048:(c + 1) * 2048, :]),
                 r=[din['peer_uv']], w=[self.uvb], dma=True)
        self.uvb_ready[l] = True

    def phase_xattn(self, l, src, dst):
        P = self.P
        din = self.din
        if self.want_peer.get(l):
            self.convert_uv(l)
        wq = P.sb("wq", [128, 8, D])
        wo = P.sb("wo", [128, 8, D])
        KT = P.sb("KT", [128, 8, NMEM])
        V = P.sb("V", [128, 2, D])
        g = P.sb("g_x", [128, D])
        wkv = self.rot("wkv", [128, 8, 512])
        self.gload(g, din['xattn_norm'], din['xattn_norm'][l:l + 1, :])
        for c in range(4):
            wc = wkv[c % 2]
            self.wload(wc, din['xattn_wkv'], din['xattn_wkv'][l][:, c * 512:(c + 1) * 512])
            if c < 2:
                for f in range(4):
                    fc = c * 4 + f
                    b = fc % 8
                    pv, pk = self.psv(b, 1)
                    for k in range(8):
                        self.mm(pv[:, 0:NMEM], wc[:, k, f * 128:(f + 1) * 128], self.memT[:, k, :],
                                k == 0, k == 7, r=[wc, self.memT], w=pk)
                    self.act(KT[:, fc, :], pv[:, 0:NMEM], AF.Copy, r=pk, w=[(KT, fc)], scale=1.0 / 16.0)
            else:
                for m in range(2):
                    b = (c - 2) * 2 + m
                    pv, pk = self.psv(b, 1)
                    for k in range(8):
                        self.mm(pv[:, :], self.memT[:, k, m * 128:(m + 1) * 128], wc[:, k, :],
                                k == 0, k == 7, r=[wc, self.memT], w=pk)
                    self.copy(V[:, m, (c - 2) * 512:(c - 1) * 512], pv[:, :], r=pk, w=[(V, (m, c))])
        self.wload(wq, din['xattn_wq'], din['xattn_wq'][l])
        self.wload(wo, din['xattn_wo'], din['xattn_wo'][l])
        xs = self.rot("x", [128, D])
        hns = self.rot("hn", [128, D])
        hnTs = self.rot("hnT", [128, 8, 128])
        sts = self.rot("st", [128, 16])
        qTs = self.rot("qT", [128, 8, 128])
        Pms = self.rot("Pm", [128, 4 * NMEM])
        PTs = self.rot("PT", [128, 8, 128])
        oTs = self.rot("oT", [128, 8, 128])
        def xfront(i):
                j = i % 2
                x, hn, hnT, st, qT, Pm, PT, oT = xs[j], hns[j], hnTs[j], sts[j], qTs[j], Pms[j], PTs[j], oTs[j]
                self.dma(x[:, :], src[i * 128:(i + 1) * 128, :], r=[(src, i)], w=[x])
                self.rms(x, g, hn, st)
                self.transp8(hn, hnT, 0)
                pv, pk = self.psv(2, 2)
                for f in range(8):
                    for k in range(8):
                        self.mm(pv[:, f * 128:(f + 1) * 128], wq[:, k, f * 128:(f + 1) * 128], hnT[:, k, :],
                                k == 0, k == 7, r=[wq, hnT], w=pk)
                self.copy(qT[:, :, :].rearrange("p a b -> p (a b)"), pv[:, :], r=pk, w=[qT])

        def xmid(i):
                j = i % 2
                x, hn, hnT, st, qT, Pm, PT, oT = xs[j], hns[j], hnTs[j], sts[j], qTs[j], Pms[j], PTs[j], oTs[j]
                lv, lk = self.psv(4, 2)
                for hd in range(4):
                    for c in range(2):
                        self.mm(lv[:, hd * 256:(hd + 1) * 256], qT[:, hd * 2 + c, :], KT[:, hd * 2 + c, :],
                                c == 0, c == 1, r=[qT, KT], w=lk)
                lv3 = self.PS[:, 4:6, :].rearrange("p a (h m) -> p (a h) m", m=NMEM)
                P.op('dve', lambda e, lv3=lv3, st=st: e.tensor_reduce(out=st[:, 4:8], in_=lv3, axis=AX.X, op=ALU.max, negate=True),
                     r=lk, w=[st])
                for hd in range(4):
                    self.act(Pm[:, hd * 256:(hd + 1) * 256], lv[:, hd * 256:(hd + 1) * 256], AF.Exp, r=lk + [st], w=[Pm, st],
                             bias=st[:, 4 + hd:5 + hd], scale=1.0, accum=st[:, 8 + hd:9 + hd])
                self.recip(st[:, 12:16], st[:, 8:12], r=[st], w=[st])
                for hd in range(4):
                    self.ts(Pm[:, hd * 256:(hd + 1) * 256], Pm[:, hd * 256:(hd + 1) * 256], st[:, 12 + hd:13 + hd], None,
                            ALU.mult, None, r=[Pm, st], w=[Pm])

        def xback(i):
                j = i % 2
                x, hn, hnT, st, qT, Pm, PT, oT = xs[j], hns[j], hnTs[j], sts[j], qTs[j], Pms[j], PTs[j], oTs[j]
                self.transp8(Pm, PT, 6)
                ov, ok = self.psv(0, 2)
                for jj in range(8):
                    for c in range(2):
                        self.mm(ov[:, jj * 128:(jj + 1) * 128], V[:, c, jj * 128:(jj + 1) * 128], PT[:, (jj // 2) * 2 + c, :],
                                c == 0, c == 1, r=[V, PT], w=ok)
                self.copy(oT[:, :, :].rearrange("p a b -> p (a b)"), ov[:, :], r=ok, w=[oT])
                yv, yk = self.psv(2, 2)
                for half in range(2):
                    for k in range(8):
                        self.mm(yv[:, half * 512:(half + 1) * 512], oT[:, k, :], wo[:, k, half * 512:(half + 1) * 512],
                                k == 0, k == 7, r=[oT, wo], w=yk)
                self.tt(x[:, :], yv[:, :], x[:, :], ALU.add, r=yk + [x], w=[x])
                self.dma(dst[i * 128:(i + 1) * 128, :], x[:, :], r=[x], w=[(dst, i)])


        xfront(0)
        for i in range(NT):
            xmid(i)
            if i + 1 < NT:
                xfront(i + 1)
            xback(i)

    def phase_even(self, l, src, dst):
        P = self.P
        din = self.din
        jx = l // 2
        w_in = P.sb("e_win", [128, 8, 2048])
        w_out = P.sb("e_wout", [128, 8, D])
        pw = P.sb("e_pw", [128, 4, 128])
        cw = P.sb("e_cw", [128, 12])
        psc = P.sb("e_psc", [128, 4])
        g = P.sb("e_g", [128, D])
        self.gload(g, din['mix_norm'], din['mix_norm'][l:l + 1, :])
        self.wload(w_in, din['even_w_in'], din['even_w_in'][jx])
        self.wload(w_out, din['even_w_out'], din['even_w_out'][jx])
        self.dma(pw[:, :, :], din['even_pool_w'][jx].rearrange("g c d -> c g d"), r=[din['even_pool_w']], w=[pw])
        self.dma(cw[:, :], din['even_conv_w'][jx], r=[din['even_conv_w']], w=[cw])
        self.dma(psc[:, :], din['even_pool_scale'][jx], r=[din['even_pool_scale']], w=[psc])
        cu_halo = P.sb("e_cuh", [128, 4, 2])
        pv_halo = P.sb("e_pvh", [128, 4, 16])
        P.op('pool', lambda e: e.memset(cu_halo[:, :, :], 0.0), w=[cu_halo])
        P.op('pool', lambda e: e.memset(pv_halo[:, :, :], 0.0), w=[pv_halo])
        hnT = P.sb("e_hnT", [128, 8, 512])
        yT = P.sb("e_yT", [128, 8, 512])
        xs = self.rot("e_x", [128, D])
        hns = self.rot("e_hn", [128, D])
        sts = self.rot("e_st", [128, 4])
        hts = self.rot("e_ht", [128, 8, 128])
        cus = self.rot("e_cu", [128, 514])
        pvs = self.rot("e_pv", [128, 528])
        ut = self.rot("e_ut", [128, 512])
        zt = self.rot("e_z", [128, 512])
        sA = self.rot("e_sA", [128, 528])
        sB = self.rot("e_sB", [128, 528])
        pl = self.rot("e_pl", [128, 512])
        invc_t = P.sb("e_invc", [128, 2048])
        self.dma(invc_t[:, :], din['invc'][:, :], r=[din['invc']], w=[invc_t])
        invc = invc_t[:, :]
        for b in range(4):
            for t4 in range(4):
                i = b * 4 + t4
                j = i % 2
                self.dma(xs[j][:, :], src[i * 128:(i + 1) * 128, :], r=[(src, i)], w=[xs[j]])
                self.rms(xs[j], g, hns[j], sts[j])
                self.transp8(hns[j], hts[j], 6)
                self.copy(hnT[:, :, t4 * 128:(t4 + 1) * 128], hts[j][:, :, :], r=[hts[j]], w=[(hnT, t4)], eng='pool')
            for c4 in range(4):
                j = c4 % 2
                cu, pv, u_sb, z = cus[j], pvs[j], ut[j], zt[j]

                def proj(cc, bank):
                    pvw, pk = self.psv(bank, 1)
                    for k in range(8):
                        self.mm(pvw[:, :], w_in[:, k, cc * 128:(cc + 1) * 128], hnT[:, k, :], k == 0, k == 7,
                                r=[w_in, hnT], w=pk)
                    return pvw, pk
                pu, ku = proj(c4, 0)
                self.copy(u_sb[:, :], pu[:, :], r=ku, w=[u_sb])
                pg, kg = proj(4 + c4, 1)
                self.copy(cu[:, 0:2], cu_halo[:, c4, :], r=[(cu_halo, c4)], w=[cu], eng='pool')
                self.tt(cu[:, 2:514], pg[:, :], u_sb[:, :], ALU.mult, r=kg + [u_sb, cu], w=[cu])
                self.copy(cu_halo[:, c4, :], cu[:, 512:514], r=[cu], w=[(cu_halo, c4)], eng='pool')
                self.ts(z[:, :], cu[:, 0:512], cw[:, c4 * 3:c4 * 3 + 1], None, ALU.mult, None, r=[cu, cw], w=[z])
                self.stt(z[:, :], cu[:, 1:513], cw[:, c4 * 3 + 1:c4 * 3 + 2], z[:, :], ALU.mult, ALU.add, r=[cu, cw, z], w=[z])
                self.stt(z[:, :], cu[:, 2:514], cw[:, c4 * 3 + 2:c4 * 3 + 3], z[:, :], ALU.mult, ALU.add, r=[cu, cw, z], w=[z])
                pb, kb = proj(8 + c4, 2)
                self.tt(yT[:, c4, :], pb[:, :], z[:, :], ALU.mult, r=kb + [z], w=[(yT, c4)])
                pp, kp = proj(12 + c4, 3)
                self.copy(pv[:, 0:16], pv_halo[:, c4, :], r=[(pv_halo, c4)], w=[pv], eng='pool')
                self.copy(pv[:, 16:528], pp[:, :], r=kp + [pv], w=[pv])
                self.copy(pv_halo[:, c4, :], pv[:, 512:528], r=[pv], w=[(pv_halo, c4)], eng='pool')
                a_, b_ = sA[j], sB[j]
                self.tt(a_[:, 1:528], pv[:, 1:528], pv[:, 0:527], ALU.add, r=[pv], w=[a_])
                cur = a_
                other = b_
                lo = 1
                for st_ in range(c4):
                    sh = 2 ** (st_ + 1)
                    nlo = lo + sh
                    self.tt(other[:, nlo:528], cur[:, nlo:528], cur[:, nlo - sh:528 - sh], ALU.add, r=[cur], w=[other])
                    cur, other = other, cur
                    lo = nlo
                wdw = 2 ** (c4 + 1)
                pool_t = pl[j]
                if b == 0:
                    self.tt(pool_t[:, :], cur[:, 16:528], invc[:, c4 * 512:(c4 + 1) * 512], ALU.mult, r=[cur, invc_t], w=[pool_t])
                    self.tt(pool_t[:, :], pool_t[:, :], pv[:, 16:528], ALU.subtract, r=[pool_t, pv], w=[pool_t])
                else:
                    self.stt(pool_t[:, :], cur[:, 16:528], 1.0 / wdw, pv[:, 16:528], ALU.mult, ALU.subtract, r=[cur, pv], w=[pool_t])
                py, ky = self.psv(3, 1)
                self.mm(py[:, :], pw[:, c4, :], pool_t[:, :], True, True, r=[pw, pool_t], w=ky)
                self.act(yT[:, 4 + c4, :], py[:, :], AF.Copy, r=ky + [psc], w=[(yT, 4 + c4)], scale=psc[:, c4:c4 + 1])
            for t4 in range(4):
                i = b * 4 + t4
                j = i % 2
                self.dma(xs[j][:, :], src[i * 128:(i + 1) * 128, :], r=[(src, i)], w=[xs[j]])
                ov, ok = self.psv(4, 2)
                for half in range(2):
                    for k in range(8):
                        self.mm(ov[:, half * 512:(half + 1) * 512], yT[:, k, t4 * 128:(t4 + 1) * 128],
                                w_out[:, k, half * 512:(half + 1) * 512], k == 0, k == 7, r=[yT, w_out], w=ok)
                self.tt(xs[j][:, :], ov[:, :], xs[j][:, :], ALU.add, r=ok + [xs[j]], w=[xs[j]])
                self.dma(dst[i * 128:(i + 1) * 128, :], xs[j][:, :], r=[xs[j]], w=[(dst, i)])


    def top16(self, src_ap, work_ap, tv_ap, ti_ap, r, wk):
        P = self.P
        P.op('dve', lambda e: e.max(out=tv_ap[:, 0:8], in_=src_ap), r=r, w=wk)
        P.op('dve', lambda e: e.max_index(out=ti_ap[:, 0:8], in_max=tv_ap[:, 0:8], in_values=src_ap), r=r + wk, w=wk)
        P.op('dve', lambda e: e.match_replace(out=work_ap, in_to_replace=tv_ap[:, 0:8], in_values=src_ap, imm_value=-1e30),
             r=r + wk, w=wk)
        P.op('dve', lambda e: e.max(out=tv_ap[:, 8:16], in_=work_ap), r=wk, w=wk)
        P.op('dve', lambda e: e.max_index(out=ti_ap[:, 8:16], in_max=tv_ap[:, 8:16], in_values=work_ap), r=wk, w=wk)

    def phase_peer(self, l, src, dst):
        P = self.P
        din = self.din
        wq = P.sb("p_wq", [128, 8, 2048], BF16)
        keysT = P.sb("p_keys", [128, 16, 128])
        g = P.sb("p_g", [128, D])
        self.gload(g, din['peer_norm'], din['peer_norm'][l:l + 1, :])
        wst = self.rot("p_wst", [128, 8, 256], 2)
        for c in range(8):
            ws_ = wst[c % 2]
            self.dma(ws_[:, :, :], din['peer_wq'][l][:, c * 256:(c + 1) * 256].rearrange("(k p) c -> p k c", p=128),
                     r=[din['peer_wq']], w=[ws_])
            self.copy(wq[:, :, c * 256:(c + 1) * 256], ws_[:, :, :], r=[ws_], w=[(wq, c)], eng='act' if c % 2 == 0 else 'dve')
        self.dma(keysT[:, :, :], din['peer_keysT'][l], r=[din['peer_keysT']], w=[keysT])
        uv_l = self.uvb
        if not self.uvb_ready.get(l):
            self.convert_uv(l)
        xs = self.rot("p_x", [128, D], 2)
        xns = self.rot("p_xn", [128, D], 2)
        xnT = P.sb("p_xnT", [128, 8, 128], BF16)
        sts = self.rot("p_st", [128, 4], 2)
        qT = P.sb("p_qT", [128, 8, 128])
        s_sb = P.sb("p_s", [128, 1024])
        tv = P.sb("p_tv", [128, 16, 16])
        ti = P.sb("p_ti", [128, 16, 16], U32)
        tif = P.sb("p_tif", [128, 16, 16])
        cand = P.sb("p_cand", [128, 8, 256])
        cwk = P.sb("p_cwk", [128, 8, 256])
        cv = P.sb("p_cv", [128, 8, 16])
        cpos = P.sb("p_cpos", [128, 8, 16], U32)
        ab_u = P.sb("p_abu", [128, 2, 128], U32)
        ab_f = P.sb("p_abf", [128, 2, 128])
        isel = P.sb("p_isel", [128, 2, 128])
        eidf = P.sb("p_eidf", [128, 128])
        eids = self.rot("p_eid", [128, 128], 2, dt=I32)
        ggs = self.rot("p_gg", [128, 8, 16], 2)
        gz = P.sb("p_gz", [128, 16])
        actvs = self.rot("p_act", [128, 128], 2)
        wgts = self.rot("p_wgt", [128, 128], 2)
        GS = 4
        t1s = self.rot("p_t1", [128, GS], 4)
        t2s = self.rot("p_t2", [128, GS], 4)
        junk = P.sb("p_junk", [128, D], BF16)
        NUV = 16
        uvs = self.rot("p_uv", [128, 2 * D], NUV, dt=BF16)
        dgs = self.rot("p_dg", [128, 128], 4, dt=BF16)
        iota16 = self.cst('iota16')
        ident = self.cst('ident')
        cwkf = cwk[:, :, :].rearrange("p a b -> p (a b)")
        candf = cand[:, :, :].rearrange("p a b -> p (a b)")

        def front(i):
            x, xn, st = xs[i % 2], xns[i % 2], sts[i % 2]
            eid, gg = eids[i % 2], ggs[i % 2]
            self.dma(x[:, :], src[i * 128:(i + 1) * 128, :], r=[(src, i)], w=[x])
            self.rms(x, g, xn, st)
            yield
            self.transp8(xn, xnT, 0)
            yield
            for hf in range(2):
                qv, qk = self.psv(2, 2)
                for hh in range(8):
                    hp = hf * 8 + hh
                    for k in range(8):
                        self.mm(qv[:, hh * 128:(hh + 1) * 128], wq[:, k, hp * 128:(hp + 1) * 128], xnT[:, k, :],
                                k == 0, k == 7, r=[wq, xnT], w=qk, fast=False)
                    if hh % 2 == 1:
                        yield
                self.copy(qT[:, :, :].rearrange("p a b -> p (a b)"), qv[:, :], r=qk, w=[qT])
                sv, sk = self.psv(0, 2)
                for hh in range(8):
                    hp = hf * 8 + hh
                    self.mm(sv[:, hh * 128:(hh + 1) * 128], qT[:, hh, :], keysT[:, hp, :], True, True, r=[qT, keysT], w=sk)
                self.copy(s_sb[:, :], sv[:, :], r=sk, w=[s_sb])
                yield
                for hh in range(8):
                    hp = hf * 8 + hh
                    self.top16(s_sb[:, hh * 128:(hh + 1) * 128], cwkf[:, hh * 128:(hh + 1) * 128], tv[:, hp, :], ti[:, hp, :],
                               r=[s_sb], wk=[(tv, hp), (ti, hp), (cwk, hh)])
                    yield
            self.copy(tif[:, :, :], ti[:, :, :], r=[ti], w=[tif], eng='dve')
            tv4 = tv[:, :, :].rearrange("p (h t) a -> p h t a", t=2)
            tif4 = tif[:, :, :].rearrange("p (h t) a -> p h t a", t=2)
            cand4 = cand[:, :, :].rearrange("p h (a b) -> p h a b", b=16)
            self.tt(cand4, bcast(tv4[:, :, 0, :], 3, 16), bcast(tv4[:, :, 1, :], 2, 16), ALU.add, r=[tv], w=[cand])
            yield
            for h in range(8):
                self.top16(cand[:, h, :], cwk[:, h, :], cv[:, h, :], cpos[:, h, :],
                           r=[cand], wk=[(cv, h), (cpos, h), (cwk, h)])
                yield
            cposf = cpos[:, :, :].rearrange("p h a -> p (h a)")
            P.op('dve', lambda e: e.tensor_single_scalar(ab_u[:, 0, :], cposf, 4, ALU.logical_shift_right), r=[cpos], w=[ab_u])
            P.op('dve', lambda e: e.tensor_single_scalar(ab_u[:, 1, :], cposf, 15, ALU.bitwise_and), r=[cpos, ab_u], w=[ab_u])
            self.copy(ab_f[:, :, :], ab_u[:, :, :], r=[ab_u], w=[ab_f], eng='dve')
            yield
            eqv = candf[:, 0:1024].rearrange("p (m a) -> p m a", a=16)
            for t in range(2):
                for hh in range(2):
                    self.tt(eqv, bcast(ab_f[:, t, hh * 64:(hh + 1) * 64], 2, 16), bcast(iota16, 1, 64), ALU.is_equal,
                            r=[ab_f, self.C], w=[cand])
                    eq4 = candf[:, 0:1024].rearrange("p (h j a) -> p h j a", j=16, a=16)
                    self.tt(eq4, eq4, bcast(tif4[:, hh * 4:(hh + 1) * 4, t, :], 2, 16), ALU.mult, r=[cand, tif], w=[cand])
                    P.op('dve', lambda e, t=t, hh=hh: e.tensor_reduce(out=isel[:, t, hh * 64:(hh + 1) * 64], in_=eqv,
                                                                      axis=AX.X, op=ALU.add), r=[cand], w=[isel])
                    yield
            self.stt(eidf[:, :], isel[:, 0, :], 128.0, isel[:, 1, :], ALU.mult, ALU.add, r=[isel], w=[eidf])
            self.copy(eid[:, :], eidf[:, :], r=[eidf], w=[eid], eng='dve')
            yield
            self.tt(gg[:, :, :], cv[:, :, :], bcast(cv[:, :, 0], 2, 16), ALU.subtract, r=[cv], w=[gg])
            self.act(gg[:, :, :], gg[:, :, :], AF.Exp, r=[gg], w=[gg])
            P.op('dve', lambda e: e.tensor_reduce(out=gz[:, 0:8], in_=gg[:, :, :], axis=AX.X, op=ALU.add), r=[gg], w=[gz])
            self.recip(gz[:, 8:16], gz[:, 0:8], r=[gz], w=[gz])
            self.tt(gg[:, :, :], gg[:, :, :], bcast(gz[:, 8:16], 2, 16), ALU.mult, r=[gg, gz], w=[gg])
            yield

        def uvstage(i):
            x, xn, eid, gg = xs[i % 2], xns[i % 2], eids[i % 2], ggs[i % 2]
            actv, wgt = actvs[i % 2], wgts[i % 2]
            ggf = gg[:, :, :].rearrange("p h a -> p (h a)")
            xpv, xpk = self.psv(4, 2)
            av, ak = self.psv(6, 2)
            self.copy(xpv[:, :], xn[:, :], r=[xn], w=xpk)
            NGRP = 128 // GS

            def stA(gi):
                g0 = gi * GS
                for jj in range(g0, g0 + GS):
                    uv = uvs[jj % NUV]
                    P.op('pool', lambda e, uv=uv, jj=jj: e.indirect_dma_start(
                        out=uv[:, :], out_offset=None, in_=uv_l[:, :],
                        in_offset=bass.IndirectOffsetOnAxis(ap=eid[:, jj:jj + 1], axis=0)),
                        r=[eid, self.uvb], w=[uv], dma=True)
                    P.op('dve', lambda e, uv=uv, jj=jj: e.scalar_tensor_tensor(
                        junk[:, :], uv[:, 0:D], 1.0, xpv[:, :], ALU.mult, ALU.mult, accum_out=actv[:, jj:jj + 1]),
                        r=[uv] + xpk, w=[(actv, jj)])
                a_ = actv[:, g0:g0 + GS]
                ak_ = [(actv, jj) for jj in range(g0, g0 + GS)]
                t1, t2 = t1s[gi % 4], t2s[gi % 4]
                self.tt(t1[:, :], a_, a_, ALU.mult, r=ak_, w=[t1])
                self.ts(t1[:, :], t1[:, :], 0.044715, 1.0, ALU.mult, ALU.add, r=[t1], w=[t1])
                self.tt(t1[:, :], t1[:, :], a_, ALU.mult, r=[t1] + ak_, w=[t1])
                self.act(t2[:, :], t1[:, :], AF.Sigmoid, r=[t1], w=[t2], scale=1.5957691216057308)

            def stC(gi):
                g0 = gi * GS
                a_ = actv[:, g0:g0 + GS]
                ak_ = [(actv, jj) for jj in range(g0, g0 + GS)]
                t2 = t2s[gi % 4]
                self.tt(t2[:, :], t2[:, :], a_, ALU.mult, r=[t2] + ak_, w=[t2])
                self.tt(wgt[:, g0:g0 + GS], t2[:, :], ggf[:, g0:g0 + GS], ALU.mult, r=[t2, gg], w=[(wgt, gi)])

            def stD(gi):
                g0 = gi * GS
                for jj in range(g0, g0 + GS):
                    uv = uvs[jj % NUV]
                    dg = dgs[jj % 4]
                    self.ts(dg[:, :], ident, wgt[:, jj:jj + 1], 1.0, ALU.mult, ALU.mult, r=[self.C, (wgt, gi)], w=[dg], eng='pool')
                    for half in range(2):
                        self.mm(av[:, half * 512:(half + 1) * 512], dg[:, :], uv[:, D + half * 512:D + (half + 1) * 512],
                                jj == 0, jj == 127, r=[dg, uv], w=[ak[half]], fast=False)

            for gi in range(NGRP + 2):
                if gi < NGRP:
                    stA(gi)
                if 1 <= gi <= NGRP:
                    stC(gi - 1)
                if gi >= 2:
                    stD(gi - 2)
                yield
            self.tt(x[:, :], av[:, :], x[:, :], ALU.add, r=ak + [x], w=[x])
            self.dma(dst[i * 128:(i + 1) * 128, :], x[:, :], r=[x], w=[(dst, i)])
            yield

        for _ in front(0):
            pass
        for it in range(NT):
            active = [uvstage(it)]
            if it + 1 < NT:
                active.append(front(it + 1))
            while active:
                for gen in list(active):
                    try:
                        next(gen)
                    except StopIteration:
                        active.remove(gen)

    def phase_dsa(self, l, src, dst):
        P = self.P
        din = self.din
        jx = l // 2
        w = P.sb("d_w", [128, 8, 1736])
        w_uv = P.sb("d_wuv", [128, 8, 64])
        w_out = P.sb("d_wout", [128, 4, D])
        g = P.sb("d_g", [128, D])
        gkv = P.sb("d_gkv", [128, 128])
        b31 = P.sb("d_b31", [128, 16])
        corrT = P.sb("d_corr", [128, 8, 2, 128])
        self.gload(g, din['mix_norm'], din['mix_norm'][l:l + 1, :])
        self.gload(gkv, din['odd_kv_norm'], din['odd_kv_norm'][jx:jx + 1, :])
        self.gload(b31, din['rel_bias'], din['rel_bias'][31:32, :]) if False else self.dma(
            b31[:, 0:8], din['rel_bias'][31:32, :].broadcast_to([128, 8]), r=[din['rel_bias']], w=[b31])
        self.ts(b31[:, 8:16], b31[:, 0:8], -1.0, None, ALU.mult, None, r=[b31], w=[b31])
        self.dma(corrT[:, :, :, :], din['biasT'][:, :, :, :], r=[din['biasT']], w=[corrT])
        for h in range(8):
            self.act(corrT[:, h, :, :], corrT[:, h, :, :], AF.Exp, r=[corrT, b31], w=[corrT], bias=b31[:, 8 + h:9 + h], scale=1.0)
        self.dma(w[:, :, :], din['odd_w_in'][jx][:, 0:1736].rearrange("(k p) c -> p k c", p=128), r=[din['odd_w_in']], w=[w])
        self.dma(w_uv[:, :, :], din['odd_w_uv'][jx].rearrange("h r e -> r h e"), r=[din['odd_w_uv']], w=[w_uv])
        self.dma(w_out[:, :, :], din['odd_w_out'][jx][0:512, :].rearrange("(k p) c -> p k c", p=128), r=[din['odd_w_out']], w=[w_out])
        cT = P.sb("d_cT", [128, S])
        c_tm = P.sb("d_ctm", [128, NT, 128])
        ikT = P.sb("d_ikT", [64, S])
        xs = self.rot("d_x", [128, D])
        hn = P.sb("d_hn", [128, D])
        hnT = P.sb("d_hnT", [128, 8, 128])
        st = P.sb("d_st", [128, 8])
        craw = P.sb("d_craw", [128, 128])
        qlTs = self.rot("d_qlT", [128, 8, 128], 2)
        iqT = P.sb("d_iqT", [64, 8, 128])
        iw = P.sb("d_iw", [128, 8])
        score = P.sb("d_score", [128, S])
        work = P.sb("d_work", [128, S])
        maskT = P.sb("d_maskT", [128, NT, 128])
        rsb = self.rot("d_r", [128, 512])
        m8 = self.rot("d_m8", [128, 8])
        ETs = self.rot("d_ET", [128, NT, 128], 2)
        latT = P.sb("d_latT", [128, 8, 128])
        zz = P.sb("d_zz", [128, 16])
        yc = P.sb("d_yc", [128, 8, 64])
        ycT = P.sb("d_ycT", [128, 4, 128])
        ones = self.cst('ones')
        cmask = self.cst('cmask')
        CK, CQ, CIK, CIW = 1024, 1152, 1664, 1728
        def projpart(i):
            x = xs[i % 2]
            qlT = qlTs[i % 2]
            nk = (i + 1) * 128
            self.dma(x[:, :], src[i * 128:(i + 1) * 128, :], r=[(src, i)], w=[x])
            self.rms(x, g, hn, st)
            self.transp8(hn, hnT, 4)
            pv, pk = self.psv(6, 1)
            for k in range(8):
                self.mm(pv[:, 0:128], hnT[:, k, :], w[:, k, CK:CK + 128], k == 0, k == 7, r=[hnT, w], w=pk)
            self.copy(craw[:, :], pv[:, 0:128], r=pk, w=[craw])
            self.act(work[:, 0:128], craw[:, :], AF.Square, r=[craw], w=[work, st], accum=st[:, 2:3])
            self.act(st[:, 3:4], st[:, 2:3], AF.Sqrt, r=[st], w=[st], scale=1.0 / 128, bias=self.epsb[:, 0:1])
            self.recip(st[:, 3:4], st[:, 3:4], r=[st], w=[st])
            self.stt(c_tm[:, i, :], craw[:, :], st[:, 3:4], gkv[:, :], ALU.mult, ALU.mult, r=[craw, st, gkv], w=[(c_tm, i)])
            pv7, pk7 = self.psv(7, 1)
            self.tr(pv7[:, 0:128], c_tm[:, i, :], r=[(c_tm, i)], w=pk7)
            self.copy(cT[:, i * 128:(i + 1) * 128], pv7[:, 0:128], r=pk7, w=[(cT, i)])
            for k in range(8):
                self.mm(pv[0:64, 0:128], w[:, k, CIK:CIK + 64], hnT[:, k, :], k == 0, k == 7, r=[hnT, w], w=pk)
            self.act(ikT[:, i * 128:(i + 1) * 128], pv[0:64, 0:128], AF.Copy, r=pk, w=[(ikT, i)], scale=0.125)
            for k in range(8):
                self.mm(pv7[:, 0:8], hnT[:, k, :], w[:, k, CIW:CIW + 8], k == 0, k == 7, r=[hnT, w], w=pk7)
            self.act(iw[:, :], pv7[:, 0:8], AF.Copy, r=pk7, w=[iw], scale=8 ** -0.5)
            qv, qk = self.psv(0, 2)
            for h in range(8):
                for k in range(8):
                    self.mm(qv[:, h * 128:(h + 1) * 128], w[:, k, h * 128:(h + 1) * 128], hnT[:, k, :], k == 0, k == 7,
                            r=[hnT, w], w=qk)
            self.act(qlT[:, :, :].rearrange("p a b -> p (a b)"), qv[:, :], AF.Copy, r=qk, w=[qlT], scale=128 ** -0.5)
            iv, ik_ = self.psv(2, 2)
            for h in range(8):
                for k in range(8):
                    self.mm(iv[0:64, h * 128:(h + 1) * 128], w[:, k, CQ + h * 64:CQ + (h + 1) * 64], hnT[:, k, :], k == 0, k == 7,
                            r=[hnT, w], w=ik_)
            self.copy(iqT[:, :, :].rearrange("p a b -> p (a b)"), iv[0:64, :], r=ik_, w=[iqT])
        projpart(0)
        for i in range(NT):
            x = xs[i % 2]
            qlT = qlTs[i % 2]
            nk = (i + 1) * 128
            cnt = 0
            for c0 in range(0, nk, 512):
                cw = min(512, nk - c0)
                for h in range(8):
                    sv, sk = self.psv(4 + cnt % 2, 1)
                    r_ = rsb[cnt % 2]
                    cnt += 1
                    self.mm(sv[:, 0:cw], iqT[:, h, :], ikT[:, c0:c0 + cw], True, True, r=[iqT, ikT], w=sk)
                    self.act(r_[:, 0:cw], sv[:, 0:cw], AF.Relu, r=sk, w=[r_])
                    if h == 0:
                        self.ts(score[:, c0:c0 + cw], r_[:, 0:cw], iw[:, 0:1], None, ALU.mult, None, r=[r_, iw], w=[score])
                    else:
                        self.stt(score[:, c0:c0 + cw], r_[:, 0:cw], iw[:, h:h + 1], score[:, c0:c0 + cw], ALU.mult, ALU.add,
                                 r=[r_, iw, score], w=[score])
            if i + 1 < NT:
                projpart(i + 1)
            self.tt(score[:, i * 128:nk], score[:, i * 128:nk], cmask, ALU.add, r=[score, self.C], w=[score])
            if i >= 2:
                cur = score
                for rnd in range(32):
                    m = m8[rnd % 2]
                    P.op('dve', lambda e, m=m, cur=cur, nk=nk: e.max(out=m[:, :], in_=cur[:, 0:nk]), r=[cur], w=[m])
                    if rnd < 31:
                        P.op('dve', lambda e, m=m, cur=cur, nk=nk: e.match_replace(
                            out=work[:, 0:nk], in_to_replace=m[:, :], in_values=cur[:, 0:nk], imm_value=-1e30),
                            r=[cur, m], w=[work])
                        cur = work
                self.ts(work[:, 0:nk], score[:, 0:nk], m8[1][:, 7:8], None, ALU.is_ge, None, r=[score, m8[1]], w=[work])
            else:
                self.ts(work[:, 0:nk], score[:, 0:nk], -1e29, None, ALU.is_ge, None, r=[score], w=[work])
            for kt in range(i + 1):
                b = 6 + (kt // 4) % 2
                mv, mk = self.psv(b, 1)
                self.tr(mv[:, (kt % 4) * 128:(kt % 4 + 1) * 128], work[:, kt * 128:(kt + 1) * 128], r=[work], w=mk)
                if kt % 4 == 3 or kt == i:
                    k0 = (kt // 4) * 4
                    n = kt - k0 + 1
                    self.copy(maskT[:, k0:kt + 1, :].rearrange("p a b -> p (a b)"), mv[:, 0:n * 128], r=mk, w=[maskT], eng='pool' if False else 'act')
            lv, lk = self.psv(0, 4)
            av, ak = self.psv(6, 2)
            zv, zk = self.psv(5, 1)
            def lg(h):
                ET = ETs[h % 2]
                for kt in range(i + 1):
                    self.mm(lv[:, kt * 128:(kt + 1) * 128], cT[:, kt * 128:(kt + 1) * 128], qlT[:, h, :], True, True,
                            r=[cT, qlT], w=lk)
                ETf = ET[:, :, :].rearrange("p a b -> p (a b)")
                self.act(ETf[:, 0:nk], lv[:, 0:nk], AF.Exp, r=lk + [b31], w=[ET], bias=b31[:, h:h + 1], scale=1.0)

            def pvh(h):
                ET = ETs[h % 2]
                ETf = ET[:, :, :].rearrange("p a b -> p (a b)")
                self.tt(ETf[:, 0:nk], ETf[:, 0:nk], maskT[:, :, :].rearrange("p a b -> p (a b)")[:, 0:nk], ALU.mult,
                        r=[ET, maskT], w=[ET])
                for kt in range(max(0, i - 1), i + 1):
                    self.tt(ET[:, kt, :], ET[:, kt, :], corrT[:, h, i - kt, :], ALU.mult, r=[ET, corrT], w=[ET])
                for kt in range(i + 1):
                    self.mm(av[:, h * 128:(h + 1) * 128], c_tm[:, kt, :], ET[:, kt, :], kt == 0, kt == i, r=[c_tm, ET], w=ak)
                    self.mm(zv[:, h:h + 1], ET[:, kt, :], ones[:, 0:1], kt == 0, kt == i, r=[ET, self.C], w=zk)

            lg(0)
            for h in range(8):
                if h + 1 < 8:
                    lg(h + 1)
                pvh(h)
            self.copy(latT[:, :, :].rearrange("p a b -> p (a b)"), av[:, :], r=ak, w=[latT])
            self.copy(zz[:, 0:8], zv[:, 0:8], r=zk, w=[zz], eng='dve')
            self.recip(zz[:, 8:16], zz[:, 0:8], r=[zz], w=[zz])
            yv, yk = self.psv(4, 1)
            for h in range(8):
                self.mm(yv[:, h * 64:(h + 1) * 64], latT[:, h, :], w_uv[:, h, :], True, True, r=[latT, w_uv], w=yk)
            self.tt(yc[:, :, :], yv[:, :].rearrange("p (h e) -> p h e", e=64), bcast(zz[:, 8:16], 2, 64), ALU.mult,
                    r=yk + [zz], w=[yc])
            tv_, tk_ = self.psv(5, 1)
            ycf = yc[:, :, :].rearrange("p h e -> p (h e)")
            for k in range(4):
                self.tr(tv_[:, k * 128:(k + 1) * 128], ycf[:, k * 128:(k + 1) * 128], r=[yc], w=tk_)
            self.copy(ycT[:, :, :].rearrange("p a b -> p (a b)"), tv_[:, :], r=tk_, w=[ycT])
            ov, ok = self.psv(0, 2)
            for half in range(2):
                for k in range(4):
                    self.mm(ov[:, half * 512:(half + 1) * 512], ycT[:, k, :], w_out[:, k, half * 512:(half + 1) * 512],
                            k == 0, k == 3, r=[ycT, w_out], w=ok)
            self.tt(x[:, :], ov[:, :], x[:, :], ALU.add, r=ok + [x], w=[x])
            self.dma(dst[i * 128:(i + 1) * 128, :], x[:, :], r=[x], w=[(dst, i)])


    def phase_hgrn(self, l, src, acc):
        P = self.P
        din = self.din
        jx = l // 2
        w = P.sb("h_w", [128, 8, 2048])
        w_out = P.sb("h_wout", [128, 4, D])
        g = P.sb("h_g", [128, D])
        gn = P.sb("h_gn", [128, 512])
        gam = P.sb("h_gam", [128, 4, 512])
        lb = P.sb("h_lb", [128, 512])
        oml = P.sb("h_oml", [128, 512])
        lbT = P.sb("h_lbT", [128, 8])
        tmp = P.sb("h_tmp", [128, 512])
        self.gload(g, din['mix_norm'], din['mix_norm'][l:l + 1, :])
        self.gload(gn, din['odd_hg_norm'], din['odd_hg_norm'][jx:jx + 1, :])
        self.dma(w[:, :, :], din['odd_w_in'][jx][:, 1736:3784].rearrange("(k p) c -> p k c", p=128), r=[din['odd_w_in']], w=[w])
        self.dma(w_out[:, :, :], din['odd_w_out'][jx][512:1024, :].rearrange("(k p) c -> p k c", p=128), r=[din['odd_w_out']], w=[w_out])
        for ll in range(4):
            self.dma(gam[:, ll, :], din['hgrn_gamma'][ll:ll + 1, :].broadcast_to([128, 512]), r=[din['hgrn_gamma']], w=[(gam, ll)])
        self.act(gam[:, :, :], gam[:, :, :], AF.Exp, r=[gam], w=[gam])
        self.tt(tmp[:, :], gam[:, 0, :], gam[:, 1, :], ALU.add, r=[gam], w=[tmp])
        self.tt(tmp[:, :], tmp[:, :], gam[:, 2, :], ALU.add, r=[gam, tmp], w=[tmp])
        self.tt(tmp[:, :], tmp[:, :], gam[:, 3, :], ALU.add, r=[gam, tmp], w=[tmp])
        self.recip(tmp[:, :], tmp[:, :], r=[tmp], w=[tmp])
        P.op('dve', lambda e: e.memset(lb[:, :], 0.0), w=[lb])
        for ll in range(l):
            self.tt(lb[:, :], lb[:, :], gam[:, ll, :], ALU.add, r=[lb, gam], w=[lb])
        self.tt(lb[:, :], lb[:, :], tmp[:, :], ALU.mult, r=[lb, tmp], w=[lb])
        self.ts(oml[:, :], lb[:, :], -1.0, 1.0, ALU.mult, ALU.add, r=[lb], w=[oml])
        pv, pk = self.psv(0, 1)
        for h in range(4):
            self.tr(pv[:, h * 128:(h + 1) * 128], lb[:, h * 128:(h + 1) * 128], r=[lb], w=pk)
        for h in range(4):
            self.copy(lbT[:, h:h + 1], pv[:, h * 128:h * 128 + 1], r=pk, w=[lbT], eng='dve')
        self.ts(lbT[:, 4:8], lbT[:, 0:4], -1.0, 1.0, ALU.mult, ALU.add, r=[lbT], w=[lbT])
        Sst = [P.sb(f"h_S{j}", [128, 4, 128]) for j in range(2)]
        qt0 = P.sb("h_qt0", [128, 4, 128])
        qt1 = P.sb("h_qt1", [128, 4, 128])
        kh0 = P.sb("h_kh0", [128, 512])
        kh1 = P.sb("h_kh1", [128, 512])
        P.op('pool', lambda e: e.memset(Sst[0][:, :, :], 0.0), w=[Sst[0]])
        P.op('pool', lambda e: e.memset(qt0[:, :, :], 0.0), w=[qt0])
        P.op('pool', lambda e: e.memset(qt1[:, :, :], 0.0), w=[qt1])
        P.op('pool', lambda e: e.memset(kh0[:, :], 0.0), w=[kh0])
        P.op('pool', lambda e: e.memset(kh1[:, :], 0.0), w=[kh1])
        xs = self.rot("h_x", [128, D])
        hn = P.sb("h_hn", [128, D])
        hnT = P.sb("h_hnT", [128, 8, 128])
        st = P.sb("h_st", [128, 16])
        sg = P.sb("h_sg", [128, 512])
        f_tm = P.sb("h_f", [128, 512])
        lf = P.sb("h_lf", [128, 512])
        kk = P.sb("h_kk", [128, 512])
        i_sb = P.sb("h_i", [128, 512])
        sil = P.sb("h_sil", [128, 512])
        sgT = P.sb("h_sgT", [128, 4, 128])
        kkT = P.sb("h_kkT", [128, 4, 128])
        eAT = P.sb("h_eAT", [128, 4, 128])
        enAT = P.sb("h_enAT", [128, 4, 128])
        qtT = P.sb("h_qtT", [128, 4, 128])
        ktT = P.sb("h_ktT", [128, 4, 128])
        a_sb = P.sb("h_a", [128, 512])
        d_sb = P.sb("h_d", [128, 512])
        sc = self.rot("h_sc", [128, 128])
        y_sb = P.sb("h_y", [128, 512])
        yT = P.sb("h_yT", [128, 4, 128])
        acc_t = self.rot("h_acc", [128, D])
        U2 = self.cst('U2')
        B2 = self.cst('B2')
        HQ, HF, HI, HG = 0, 512, 1024, 1536

        def proj_tm(c0, bank):
            pvw, pkw = self.psv(bank, 1)
            for k in range(8):
                self.mm(pvw[:, :], hnT[:, k, :], w[:, k, c0:c0 + 512], k == 0, k == 7, r=[hnT, w], w=pkw)
            return pvw, pkw

        def proj_fm(c0, bank):
            pvw, pkw = self.psv(bank, 1)
            for h in range(4):
                for k in range(8):
                    self.mm(pvw[:, h * 128:(h + 1) * 128], w[:, k, c0 + h * 128:c0 + (h + 1) * 128], hnT[:, k, :],
                            k == 0, k == 7, r=[hnT, w], w=pkw)
            return pvw, pkw

        for i in range(NT):
            x = xs[i % 2]
            self.dma(x[:, :], src[i * 128:(i + 1) * 128, :], r=[(src, i)], w=[x])
            self.rms(x, g, hn, st)
            self.transp8(hn, hnT, 0)
            pf, kf = proj_tm(HF, 2)
            self.act(sg[:, :], pf[:, :], AF.Sigmoid, r=kf, w=[sg])
            self.tt(f_tm[:, :], sg[:, :], oml[:, :], ALU.mult, r=[sg, oml], w=[f_tm])
            self.tt(f_tm[:, :], f_tm[:, :], lb[:, :], ALU.add, r=[f_tm, lb], w=[f_tm])
            self.act(lf[:, :], f_tm[:, :], AF.Ln, r=[f_tm], w=[lf])
            self.ts(kk[:, :], f_tm[:, :], -1.0, 1.0, ALU.mult, ALU.add, r=[f_tm], w=[kk])
            pi_, ki_ = proj_tm(HI, 3)
            self.copy(i_sb[:, :], pi_[:, :], r=ki_, w=[i_sb])
            pg_, kg_ = proj_tm(HG, 4)
            self.act(sil[:, :], pg_[:, :], AF.Sigmoid, r=kg_, w=[sil])
            self.tt(sil[:, :], sil[:, :], pg_[:, :], ALU.mult, r=[sil] + kg_, w=[sil])
            pq, kq = proj_fm(HQ, 5)
            pfT, kfT = proj_fm(HF, 6)
            self.act(sgT[:, :, :].rearrange("p a b -> p (a b)"), pfT[:, :], AF.Sigmoid, r=kfT, w=[sgT])
            for h in range(4):
                self.ts(kkT[:, h, :], sgT[:, h, :], lbT[:, 4 + h:5 + h], lbT[:, h:h + 1], ALU.mult, ALU.add, r=[sgT, lbT], w=[kkT])
            self.ts(kkT[:, :, :], kkT[:, :, :], -1.0, 1.0, ALU.mult, ALU.add, r=[kkT], w=[kkT])
            pA, kA = self.psv(2, 1)
            self.mm(pA[:, :], U2, lf[:, :], True, True, r=[self.C, lf], w=kA)
            self.copy(a_sb[:, :], pA[:, :], r=kA, w=[a_sb])
            pE, kE = self.psv(3, 1)
            self.mm(pE[:, :], B2, lf[:, :], True, True, r=[self.C, lf], w=kE)
            pAT, kAT = self.psv(4, 1)
            for h in range(4):
                self.mm(pAT[:, h * 128:(h + 1) * 128], lf[:, h * 128:(h + 1) * 128], U2, True, True, r=[self.C, lf], w=kAT)
            self.act(eAT[:, :, :].rearrange("p a b -> p (a b)"), pAT[:, :], AF.Exp, r=kAT, w=[eAT])
            self.act(enAT[:, :, :].rearrange("p a b -> p (a b)"), pAT[:, :], AF.Exp, r=kAT, w=[enAT], scale=-1.0)
            self.tt(qtT[:, :, :].rearrange("p a b -> p (a b)"), pq[:, :], eAT[:, :, :].rearrange("p a b -> p (a b)"), ALU.mult,
                    r=kq + [eAT], w=[qtT])
            self.tt(ktT[:, :, :], kkT[:, :, :], enAT[:, :, :], ALU.mult, r=[kkT, enAT], w=[ktT])
            self.copy(qt0[:, :, 0:64], qtT[:, :, 0:64], r=[qtT], w=[qt0], eng='pool')
            self.copy(qt1[:, :, 64:128], qtT[:, :, 64:128], r=[qtT], w=[qt1], eng='pool')
            self.tt(d_sb[:, :], pE[:, :], a_sb[:, :], ALU.subtract, r=kE + [a_sb], w=[d_sb])
            self.act(d_sb[:, :], d_sb[:, :], AF.Exp, r=[d_sb], w=[d_sb])
            self.tt(kh0[0:64, :], d_sb[0:64, :], kk[0:64, :], ALU.mult, r=[d_sb, kk], w=[kh0])
            self.tt(kh1[64:128, :], d_sb[64:128, :], kk[64:128, :], ALU.mult, r=[d_sb, kk], w=[kh1])
            po, ko = self.psv(7, 1)
            for h in range(4):
                hs = slice(h * 128, (h + 1) * 128)
                S0, S1 = Sst[0], Sst[1]
                ps_, ks_ = self.psv(0, 1)
                self.mm(ps_[:, 0:128], ktT[:, h, :], qtT[:, h, :], True, True, r=[ktT, qtT], w=ks_)
                scb = sc[h % 2]
                self.tt(scb[:, :], ps_[:, 0:128], U2, ALU.mult, r=ks_ + [self.C], w=[scb])
                self.mm(po[:, hs], scb[:, :], i_sb[:, hs], True, False, r=[scb, i_sb], w=ko)
                self.mm(po[:, hs], qt0[:, h, :], S0[:, h, :], False, False, r=[qt0, (S0, h)], w=ko)
                p1, k1 = self.psv(1, 1)
                self.mm(p1[:, 0:128], kh0[:, hs], i_sb[:, hs], True, True, r=[kh0, i_sb], w=k1)
                self.stt(S1[:, h, :], S0[:, h, :], eAT[:, h, 63:64], p1[:, 0:128], ALU.mult, ALU.add, r=[(S0, h), eAT] + k1, w=[(S1, h)])
                self.mm(po[:, hs], qt1[:, h, :], S1[:, h, :], False, True, r=[qt1, (S1, h)], w=ko)
                p2, k2 = self.psv(2, 1)
                self.mm(p2[:, 0:128], kh1[:, hs], i_sb[:, hs], True, True, r=[kh1, i_sb], w=k2)
                self.stt(S0[:, h, :], S1[:, h, :], eAT[:, h, 127:128], p2[:, 0:128], ALU.mult, ALU.add, r=[(S1, h), eAT] + k2, w=[(S0, h)])
            for h in range(4):
                self.act(y_sb[:, h * 128:(h + 1) * 128], po[:, h * 128:(h + 1) * 128], AF.Square, r=ko, w=[y_sb, st], accum=st[:, 4 + h:5 + h])
            self.act(st[:, 8:12], st[:, 4:8], AF.Sqrt, r=[st], w=[st], scale=1.0 / 128, bias=self.epsb[:, 0:1])
            self.recip(st[:, 8:12], st[:, 8:12], r=[st], w=[st])
            for h in range(4):
                self.ts(y_sb[:, h * 128:(h + 1) * 128], po[:, h * 128:(h + 1) * 128], st[:, 8 + h:9 + h], None, ALU.mult, None,
                        r=ko + [st], w=[y_sb])
            self.tt(y_sb[:, :], y_sb[:, :], gn[:, :], ALU.mult, r=[y_sb, gn], w=[y_sb])
            self.tt(y_sb[:, :], y_sb[:, :], sil[:, :], ALU.mult, r=[y_sb, sil], w=[y_sb])
            tv_, tk_ = self.psv(5, 1)
            for k in range(4):
                self.tr(tv_[:, k * 128:(k + 1) * 128], y_sb[:, k * 128:(k + 1) * 128], r=[y_sb], w=tk_)
            self.copy(yT[:, :, :].rearrange("p a b -> p (a b)"), tv_[:, :], r=tk_, w=[yT])
            at = acc_t[i % 2]
            self.dma(at[:, :], acc[i * 128:(i + 1) * 128, :], r=[(acc, i)], w=[at])
            ov, ok = self.psv(2, 2)
            for half in range(2):
                for k in range(4):
                    self.mm(ov[:, half * 512:(half + 1) * 512], yT[:, k, :], w_out[:, k, half * 512:(half + 1) * 512],
                            k == 0, k == 3, r=[yT, w_out], w=ok)
            self.tt(at[:, :], ov[:, :], at[:, :], ALU.add, r=ok + [at], w=[at])
            self.dma(acc[i * 128:(i + 1) * 128, :], at[:, :], r=[at], w=[(acc, i)])

    def phase_copy(self, src, dst):
        xs = self.rot("c_x", [128, D])
        for i in range(NT):
            self.dma(xs[i % 2][:, :], src[i * 128:(i + 1) * 128, :], r=[(src, i)], w=[xs[i % 2]])
            self.dma(dst[i * 128:(i + 1) * 128, :], xs[i % 2][:, :], r=[xs[i % 2]], w=[(dst, i)])

    def phase_final(self, src):
        P = self.P
        g = P.sb("g_f", [128, D])
        self.gload(g, self.din['final_norm'], self.din['final_norm'][0:1, :])
        xs = self.rot("xf", [128, D])
        hns = self.rot("hnf", [128, D])
        sts = self.rot("stf", [128, 4])
        for i in range(NT):
            j = i % 2
            self.dma(xs[j][:, :], src[i * 128:(i + 1) * 128, :], r=[(src, i)], w=[xs[j]])
            self.rms(xs[j], g, hns[j], sts[j])
            self.dma(self.out[i * 128:(i + 1) * 128, :], hns[j][:, :], r=[hns[j]], w=[(self.out, i)])

    def run_phase(self, fn, *a):
        with ExitStack() as pes:
            self.P.pes = pes
            fn(*a)
            self.P.flush()
        self.P.pes = self.es

    def build(self, plan):
        for ph in plan:
            if ph[0] == 'peer':
                self.want_peer[ph[1]] = True
        cur = self.din['x']
        self.run_phase(self.phase_setup)
        for ph in plan:
            if ph[0] == 'xattn':
                self.run_phase(self.phase_xattn, ph[1], cur, self.hA)
                cur = self.hA
            elif ph[0] == 'even':
                self.run_phase(self.phase_even, ph[1], cur, self.hA)
                cur = self.hA
            elif ph[0] == 'peer':
                self.run_phase(self.phase_peer, ph[1], cur, self.hA)
                cur = self.hA
            elif ph[0] == 'dsa':
                other = self.hB if cur is not self.hB else self.hA
                self.run_phase(self.phase_dsa, ph[1], cur, other)
                cur = other
            elif ph[0] == 'hgrn':
                other = self.hB if cur is not self.hB else self.hA
                self.run_phase(self.phase_copy, cur, other)
                self.run_phase(self.phase_hgrn, ph[1], cur, other)
                cur = other
            elif ph[0] == 'odd':
                other = self.hB if cur is not self.hB else self.hA
                self.run_phase(self.phase_dsa, ph[1], cur, other)
                self.run_phase(self.phase_hgrn, ph[1], cur, other)
                cur = other
            elif ph[0] == 'final':
                self.run_phase(self.phase_final, cur)
        return self.nc


FULL_PLAN = []
for _l in range(DEPTH):
    FULL_PLAN.append(('even' if _l % 2 == 0 else 'odd', _l))
    FULL_PLAN.append(('xattn', _l))
    FULL_PLAN.append(('peer', _l))
FULL_PLAN.append(('final',))


def t5_bucket_np(d):
    d = np.maximum(d, 0)
    lr = np.log(np.maximum(d, 1).astype(np.float32) / np.float32(16)) / np.float32(np.log(128 / 16))
    large = 16 + (lr * np.float32(16)).astype(np.int32)
    large = np.minimum(large, 31)
    return np.where(d < 16, d, large)


def prep_inputs(inp):
    f = lambda a: np.ascontiguousarray(np.asarray(a, dtype=np.float32))
    shared = {}
    for k in IN_SHAPES:
        if k in ('x', 'mem'):
            continue
        if k == 'peer_keysT':
            a = np.asarray(inp['peer_keys'], dtype=np.float32)
            shared[k] = f(a.transpose(0, 4, 1, 2, 3).reshape(4, 128, 16, 128))
        elif k == 'even_conv_w':
            a = np.asarray(inp[k]).reshape(2, 3, 4, 128)
            shared[k] = f(a.transpose(0, 3, 2, 1).reshape(2, 128, 12))
        elif k == 'even_pool_scale':
            a = np.asarray(inp[k]).reshape(2, 4, 128)
            shared[k] = f(a.transpose(0, 2, 1))
        elif k == 'peer_uv':
            shared[k] = np.ascontiguousarray(np.concatenate(
                [np.asarray(inp['peer_u'], dtype=np.float32), np.asarray(inp['peer_v'], dtype=np.float32)], axis=-1))
        elif k == 'invc':
            shared[k] = INVC[0]
        elif k == 'biasT':
            rb = np.asarray(inp['rel_bias'], dtype=np.float32)
            ss_, tq_ = np.arange(128)[:, None], np.arange(128)[None, :]
            out = np.zeros((128, 8, 2, 128), dtype=np.float32)
            for dl in (0, 1):
                dist = np.maximum(128 * dl + tq_ - ss_, 0)
                out[:, :, dl, :] = rb[t5_bucket_np(dist)].transpose(0, 2, 1)
            shared[k] = f(out)
        elif k in ('mem_norm', 'final_norm'):
            shared[k] = f(np.asarray(inp[k]).reshape(1, D))
        else:
            shared[k] = f(inp[k])
    return shared


def kernel(plan=None, **inp):
    plan = FULL_PLAN if plan is None else plan
    m = Model()
    nc = m.build(plan)
    shared = prep_inputs(inp)
    shared['consts'] = m.carr
    shared = {k: v for k, v in shared.items() if k in m.din}
    x = np.asarray(inp['x'], dtype=np.float32)
    mem = np.asarray(inp['mem'], dtype=np.float32)
    in_maps = []
    for b in range(8):
        d = dict(shared)
        if 'x' in m.din:
            d['x'] = np.ascontiguousarray(x[b])
        if 'mem' in m.din:
            d['mem'] = np.ascontiguousarray(mem[b])
        in_maps.append(d)
    res = run_bass_kernel_spmd(nc, in_maps, core_ids=list(range(8)))
    m.es.close()
    return np.stack([np.asarray(r["out"], dtype=np.float32) for r in res.results], axis=0)
```

```python
import numpy as np
import concourse.bass as bass
import concourse.mybir as mybir
from concourse.bass_utils import run_bass_kernel_spmd
from contextlib import ExitStack

F32 = mybir.dt.float32
I32 = mybir.dt.int32
U32 = mybir.dt.uint32
AF = mybir.ActivationFunctionType
ALU = mybir.AluOpType
AX = mybir.AxisListType

ENGS = ['pe', 'dve', 'act', 'pool', 'sp']
SEM_LIMIT = 30000
DMA_K = 16


class T:
    def __init__(self, h, name):
        self.h = h
        self.name = name
        self.st = {}

    def __getitem__(self, idx):
        return self.h[idx]


def bcast(ap, axis, n):
    l = [list(x) for x in ap.ap]
    l.insert(axis, [0, n])
    return bass.AP(ap.tensor, ap.offset, l)


def rep(ap, axis, n):
    l = [list(x) for x in ap.ap]
    assert l[axis][1] == 1
    l[axis] = [0, n]
    return bass.AP(ap.tensor, ap.offset, l)


class Prog:
    def __init__(self, nc, es):
        self.nc = nc
        self.es = es
        self.pes = es
        self.ops = {e: [] for e in ENGS}
        self.base = {e: 0 for e in ENGS}
        self.waited = {e: {} for e in ENGS}
        self.waited_dma = {e: set() for e in ENGS}
        self.tiles = []
        self.csems = {e: [] for e in ENGS}
        self.ccount = {e: 0 for e in ENGS}
        self.dsems = {}
        self.dcount = {e: 0 for e in ENGS}
        self.dma_hist = {e: [] for e in ENGS}

    def sb(self, name, shape, dt=F32, glob=False):
        es = self.es if glob else self.pes
        self.uid = getattr(self, 'uid', 0) + 1
        name = f"{name}_{self.uid}"
        t = T(es.enter_context(self.nc.sbuf_tensor(name, list(shape), dt)), name)
        self.tiles.append(t)
        return t

    def ps(self, name, shape, dt=F32):
        t = T(self.es.enter_context(self.nc.psum_tensor(name, list(shape), dt)), name)
        self.tiles.append(t)
        return t

    def dram(self, name, shape, dt=F32, kind="Internal"):
        t = T(self.nc.dram_tensor(name, list(shape), dt, kind=kind).ap(), name)
        self.tiles.append(t)
        return t

    @staticmethod
    def _norm(key):
        if isinstance(key, T):
            return key, None
        return key[0], key[1]

    def _entries(self, tile, sub):
        if sub is None:
            return list(tile.st.values())
        out = []
        if sub in tile.st:
            out.append(tile.st[sub])
        if None in tile.st:
            out.append(tile.st[None])
        return out

    def op(self, eng, fn, r=(), w=(), dma=False, extra=()):
        idx = len(self.ops[eng])
        deps = set(extra)
        for key in r:
            tile, sub = self._norm(key)
            for ent in self._entries(tile, sub):
                if ent[0] is not None:
                    deps.add(ent[0])
        for key in w:
            tile, sub = self._norm(key)
            for ent in self._entries(tile, sub):
                if ent[0] is not None:
                    deps.add(ent[0])
                for e2, i2 in ent[1].items():
                    deps.add((e2, i2))
        for key in r:
            tile, sub = self._norm(key)
            ent = tile.st.setdefault(sub, [None, {}])
            ent[1][eng] = idx
        for key in w:
            tile, sub = self._norm(key)
            if sub is None:
                tile.st = {None: [(eng, idx), {}]}
            else:
                tile.st[sub] = [(eng, idx), {}]
        if dma:
            h = self.dma_hist[eng]
            if len(h) >= DMA_K:
                deps.add((eng, h[-DMA_K]))
        final = []
        for (e2, i2) in sorted(deps, reverse=True):
            if e2 == eng and i2 == idx:
                continue
            if self.ops[e2][i2]['dma']:
                if (e2, i2) in self.waited_dma[eng]:
                    continue
                self.waited_dma[eng].add((e2, i2))
                final.append((e2, i2))
            else:
                if e2 == eng and eng == 'pe':
                    continue
                if self.waited[eng].get(e2, -1) >= i2:
                    continue
                self.waited[eng][e2] = i2
                final.append((e2, i2))
        o = dict(fn=fn, deps=final, dma=dma, sig=False)
        if dma:
            o['dma_n'] = self.dcount[eng]
            self.dcount[eng] += 1
            self.dma_hist[eng].append(idx)
        self.ops[eng].append(o)
        return (eng, idx)

    def _sigof(self, e2, i2):
        o = self.ops[e2][i2]
        if o['dma']:
            n = o['dma_n']
            return self.dsems[e2][n % DMA_K], 16 * (n // DMA_K + 1)
        j, v = o['sv']
        return self.csems[e2][j], v

    def flush(self):
        nc = self.nc
        last = {}
        for e in ENGS:
            for i in range(len(self.ops[e]) - 1, self.base[e] - 1, -1):
                if not self.ops[e][i]['dma'] and self.ops[e][i]['fn'] is not None:
                    last[e] = (e, i)
                    break
        dmas = []
        for e in ENGS:
            dmas += [(e, i) for i in self.dma_hist[e][-DMA_K:] if i >= self.base[e]]
        for e in ENGS:
            extra = [v for k, v in last.items() if k != e] + dmas
            self.op(e, None, extra=extra)
        for e in ENGS:
            for o in self.ops[e][self.base[e]:]:
                for (e2, i2) in o['deps']:
                    assert i2 >= self.base[e2], "cross-phase dep"
                    self.ops[e2][i2]['sig'] = True
        for e in ENGS:
            for o in self.ops[e][self.base[e]:]:
                if o['dma']:
                    if e not in self.dsems:
                        self.dsems[e] = [self.es.enter_context(nc.semaphore(f"d_{e}_{j}")) for j in range(DMA_K)]
                    continue
                if o['sig']:
                    c = self.ccount[e]
                    j = c // SEM_LIMIT
                    while len(self.csems[e]) <= j:
                        self.csems[e].append(self.es.enter_context(nc.semaphore(f"c_{e}_{len(self.csems[e])}")))
                    o['sv'] = (j, c % SEM_LIMIT + 1)
                    self.ccount[e] = c + 1

        def run(e, eng):
            for o in self.ops[e][self.base[e]:]:
                for (e2, i2) in o['deps']:
                    s, v = self._sigof(e2, i2)
                    eng.wait_ge(s, v)
                if o['fn'] is None:
                    continue
                ins = o['fn'](eng)
                if o['dma']:
                    n = o['dma_n']
                    ins.then_inc(self.dsems[e][n % DMA_K], 16)
                elif o['sig']:
                    j, v = o['sv']
                    ins.then_inc(self.csems[e][j], 1)

        with nc.Block() as block:
            @block.tensor
            def _(eng):
                run('pe', eng)

            @block.vector
            def _(eng):
                run('dve', eng)

            @block.scalar
            def _(eng):
                run('act', eng)

            @block.gpsimd
            def _(eng):
                run('pool', eng)

            @block.sync
            def _(eng):
                run('sp', eng)
        for e in ENGS:
            self.base[e] = len(self.ops[e])
        for t in self.tiles:
            t.st = {}

D = 1024
S = 2048
NT = 16
NMEM = 256
DEPTH = 4
EPS = 1e-6
FAST_MM = True
BF16 = mybir.dt.bfloat16
F32R = mybir.dt.float32r
R32 = F32R

IN_SHAPES = {
    'x': [S, D], 'mem': [NMEM, D], 'mix_norm': [4, D],
    'even_w_in': [2, D, 2048], 'even_conv_w': [2, 128, 12], 'even_pool_w': [2, 4, 128, 128],
    'even_pool_scale': [2, 128, 4], 'even_w_out': [2, D, D],
    'odd_w_in': [2, D, 3784], 'odd_kv_norm': [2, 128], 'odd_w_uv': [2, 8, 128, 64],
    'odd_hg_norm': [2, 512], 'odd_w_out': [2, D, D], 'hgrn_gamma': [4, 512],
    'rel_bias': [32, 8], 'invc': [128, 2048], 'biasT': [128, 8, 2, 128], 'mem_norm': [1, D], 'xattn_norm': [4, D],
    'xattn_wq': [4, D, D], 'xattn_wkv': [4, D, 2 * D], 'xattn_wo': [4, D, D],
    'peer_norm': [4, D], 'peer_wq': [4, D, 2048], 'peer_keysT': [4, 128, 16, 128],
    'peer_uv': [4, 16384, 2 * D], 'final_norm': [1, D],
}


INVC = [None]


def make_consts():
    parts = {}
    parts['ident'] = np.eye(128, dtype=np.float32)
    t = np.arange(512)
    invc = np.concatenate([1.0 / np.minimum(t + 1, w) for w in (2, 4, 8, 16)]).astype(np.float32)
    INVC[0] = np.ascontiguousarray(np.tile(invc[None, :], (128, 1)).astype(np.float32))
    invc16 = np.concatenate([1.0 / np.minimum(np.arange(16) + 1, w) for w in (2, 4, 8, 16)]).astype(np.float32)
    parts['invc16'] = np.tile(invc16[None, :], (128, 1))
    parts['iota16'] = np.tile(np.arange(16, dtype=np.float32)[None, :], (128, 1))
    ii = np.arange(128)
    parts['U2'] = ((ii[:, None] // 64 == ii[None, :] // 64) & (ii[:, None] <= ii[None, :])).astype(np.float32)
    parts['B2'] = (ii[:, None] // 64 == ii[None, :] // 64).astype(np.float32)
    parts['cmask'] = np.where(ii[None, :] <= ii[:, None], 0.0, -1e30).astype(np.float32)
    parts['ones'] = np.ones((128, 8), dtype=np.float32)
    off = 0
    lay = {}
    arrs = []
    for k, v in parts.items():
        lay[k] = (off, v.shape[1])
        off += v.shape[1]
        arrs.append(v.astype(np.float32))
    return np.ascontiguousarray(np.concatenate(arrs, axis=1)), lay


class Model:
    def __init__(self):
        self.nc = bass.Bass("TRN2", target_bir_lowering=False)
        self.es = ExitStack()
        self.P = Prog(self.nc, self.es)
        P = self.P
        carr, self.clay = make_consts()
        self.carr = carr
        model = self

        class LazyIn(dict):
            def __missing__(self, k):
                shp = list(carr.shape) if k == 'consts' else IN_SHAPES[k]
                t = P.dram(k, shp, F32, kind="ExternalInput")
                self[k] = t
                return t
        self.din = LazyIn()
        self.out = P.dram("out", [S, D], F32, kind="ExternalOutput")
        self.hA = P.dram("hA", [S, D])
        self.hB = P.dram("hB", [S, D])
        self.uvb = P.dram("uvb", [16384, 2 * D], BF16)
        self.PS = P.ps("ps", [128, 8, 512])
        self.C = P.sb("consts_sb", [128, carr.shape[1]], glob=True)
        self.memT = P.sb("memT", [128, 8, NMEM], R32, glob=True)
        self.rotc = {}
        self.uvb_ready = {}
        self.want_peer = {}

    def cst(self, name):
        o, w = self.clay[name]
        return self.C[:, o:o + w]

    def psv(self, b0, nb=2):
        ap = self.PS[:, b0:b0 + nb, :].rearrange("p a b -> p (a b)")
        return ap, [(self.PS, b) for b in range(b0, b0 + nb)]

    def mm(self, out, lhsT, rhs, start, stop, r, w, fast=False):
        if FAST_MM and fast and rhs.shape[-1] % 2 == 0 and out.shape[-1] % 2 == 0:
            lhsT = lhsT.bitcast(F32R)
            rhs = rhs.bitcast(F32R)
        self.P.op('pe', lambda e: e.matmul(out, lhsT, rhs, start=start, stop=stop), r=r, w=w)

    def tr(self, out, in_, r, w):
        ident = self.cst('ident')
        self.P.op('pe', lambda e: e.transpose(out, in_, ident), r=list(r) + [self.C], w=w)

    def act(self, out, in_, func, r, w, bias=None, scale=None, accum=None):
        kw = {}
        if bias is not None:
            kw['bias'] = bias
        if scale is not None:
            kw['scale'] = scale
        if accum is not None:
            kw['accum_out'] = accum
        self.P.op('act', lambda e: e.activation(out, in_, func, **kw), r=r, w=w)

    def tt(self, out, a, b, op, r, w, eng='dve'):
        self.P.op(eng, lambda e: e.tensor_tensor(out, a, b, op), r=r, w=w)

    def ts(self, out, a, s1, s2, op0, op1, r, w, eng='dve', accum=None):
        if op1 is None:
            self.P.op(eng, lambda e: e.tensor_scalar(out, a, s1, None, op0), r=r, w=w)
        elif accum is None:
            self.P.op(eng, lambda e: e.tensor_scalar(out, a, s1, s2, op0, op1), r=r, w=w)
        else:
            self.P.op(eng, lambda e: e.tensor_scalar(out, a, s1, s2, op0, op1, accum_out=accum), r=r, w=w)

    def stt(self, out, a, scalar, b, op0, op1, r, w):
        self.P.op('dve', lambda e: e.scalar_tensor_tensor(out, a, scalar, b, op0, op1), r=r, w=w)

    def copy(self, out, in_, r, w, eng='act'):
        if eng == 'act':
            self.P.op('act', lambda e: e.copy(out, in_), r=r, w=w)
        else:
            self.P.op(eng, lambda e: e.tensor_copy(out, in_), r=r, w=w)

    def dma(self, out, in_, r, w, eng='sp'):
        self.P.op(eng, lambda e: e.dma_start(out=out, in_=in_), r=r, w=w, dma=True)

    def recip(self, out, in_, r, w):
        self.P.op('dve', lambda e: e.reciprocal(out, in_), r=r, w=w)

    def rot(self, name, shape, n=2, dt=F32):
        return [self.P.sb(f"{name}{j}", shape, dt) for j in range(n)]

    def wload(self, dst, src_t, src_ap):
        self.dma(dst[:, :, :], src_ap.rearrange("(k p) c -> p k c", p=128), r=[src_t], w=[dst])

    def wload_r(self, dst, src_t, src_ap, nk=8, cw=128):
        C = src_ap.shape[1]
        if not hasattr(self, '_wst') or self._wst_phase is not self.P.pes:
            self._wst = self.rot("wst", [128, 8, cw], 2)
            self._wst_phase = self.P.pes
            self._wst_n = 0
        for c0 in range(0, C, cw):
            c1 = min(C, c0 + cw)
            st_ = self._wst[self._wst_n % 2]
            eng = ('act', 'dve', 'pool')[self._wst_n % 3]
            self._wst_n += 1
            self.dma(st_[:, 0:nk, 0:c1 - c0], src_ap[:, c0:c1].rearrange("(k p) c -> p k c", p=128), r=[src_t], w=[st_])
            self.copy(dst[:, :, c0:c1], st_[:, 0:nk, 0:c1 - c0], r=[st_], w=[dst], eng=eng)

    def gload(self, dst, src_t, row_ap):
        n = row_ap.shape[1]
        self.dma(dst[:, :], row_ap.broadcast_to([128, n]), r=[src_t], w=[dst])

    def rms(self, x, g, hn, st, width=D):
        self.act(hn[:, :], x[:, :], AF.Square, r=[x], w=[hn, st], accum=st[:, 0:1])
        self.act(st[:, 1:2], st[:, 0:1], AF.Sqrt, r=[st], w=[st], scale=1.0 / width, bias=self.epsb[:, 0:1])
        self.recip(st[:, 1:2], st[:, 1:2], r=[st], w=[st])
        self.stt(hn[:, :], x[:, :], st[:, 1:2], g[:, :], ALU.mult, ALU.mult, r=[x, st, g], w=[hn])

    def transp8(self, src, dstT, b0, n=8):
        nb = (n * 128 + 511) // 512
        pv, pk = self.psv(b0, nb)
        for k in range(n):
            self.tr(pv[:, k * 128:(k + 1) * 128], src[:, k * 128:(k + 1) * 128], r=[src], w=pk)
        self.copy(dstT[:, :, :].rearrange("p a b -> p (a b)"), pv[:, 0:n * 128], r=pk, w=[dstT])

    def phase_setup(self):
        P = self.P
        self.dma(self.C[:, :], self.din['consts'][:, :], r=[self.din['consts']], w=[self.C])
        self.epsb = P.sb("epsb", [128, 1], glob=True)
        P.op('dve', lambda e: e.memset(self.epsb[:, :], EPS), w=[self.epsb])
        g = P.sb("g_mem", [128, D])
        self.gload(g, self.din['mem_norm'], self.din['mem_norm'][0:1, :])
        xs = self.rot("xm", [128, D])
        hn = self.rot("hnm", [128, D])
        st = self.rot("stm", [128, 4])
        mT = [P.sb(f"mT{j}", [128, 8, 128]) for j in range(2)]
        for m in range(2):
            self.dma(xs[m][:, :], self.din['mem'][m * 128:(m + 1) * 128, :], r=[self.din['mem']], w=[xs[m]])
            self.rms(xs[m], g, hn[m], st[m])
            self.transp8(hn[m], mT[m], 2 * m)
            self.copy(self.memT[:, :, m * 128:(m + 1) * 128], mT[m][:, :, :], r=[mT[m]], w=[(self.memT, m)], eng='dve')

    def convert_uv(self, l):
        P = self.P
        din = self.din
        uvb2 = self.uvb[:, :].rearrange("n (a d) -> (n a) d", a=2)
        uvf2 = din['peer_uv'][l].rearrange("n (a d) -> (n a) d", a=2)
        for c in range(16):
            P.op('pool', lambda e, c=c: e.dma_start(out=uvb2[c * 2048:(c + 1) * 2048, :],
                                                    in_=uvf2[c * 2048:(c + 1) * 2048, :]),
                 r=[din['peer_uv']], w=[self.uvb], dma=True)
        self.uvb_ready[l] = True

    def phase_xattn(self, l, src, dst):
        P = self.P
        din = self.din
        if self.want_peer.get(l):
            self.convert_uv(l)
        wq = P.sb("wq", [128, 8, D], R32)
        wo = P.sb("wo", [128, 8, D], R32)
        KT = P.sb("KT", [128, 8, NMEM], R32)
        V = P.sb("V", [128, 2, D], R32)
        g = P.sb("g_x", [128, D])
        wkv = self.rot("wkv", [128, 8, 512], dt=R32)
        self.gload(g, din['xattn_norm'], din['xattn_norm'][l:l + 1, :])
        for c in range(4):
            wc = wkv[c % 2]
            self.wload_r(wc, din['xattn_wkv'], din['xattn_wkv'][l][:, c * 512:(c + 1) * 512])
            if c < 2:
                for f in range(4):
                    fc = c * 4 + f
                    b = fc % 8
                    pv, pk = self.psv(b, 1)
                    for k in range(8):
                        self.mm(pv[:, 0:NMEM], wc[:, k, f * 128:(f + 1) * 128], self.memT[:, k, :],
                                k == 0, k == 7, r=[wc, self.memT], w=pk, fast=True)
                    self.act(KT[:, fc, :], pv[:, 0:NMEM], AF.Copy, r=pk, w=[(KT, fc)], scale=1.0 / 16.0)
            else:
                for m in range(2):
                    b = (c - 2) * 2 + m
                    pv, pk = self.psv(b, 1)
                    for k in range(8):
                        self.mm(pv[:, :], self.memT[:, k, m * 128:(m + 1) * 128], wc[:, k, :],
                                k == 0, k == 7, r=[wc, self.memT], w=pk, fast=True)
                    self.copy(V[:, m, (c - 2) * 512:(c - 1) * 512], pv[:, :], r=pk, w=[(V, (m, c))])
        self.wload_r(wq, din['xattn_wq'], din['xattn_wq'][l])
        self.wload_r(wo, din['xattn_wo'], din['xattn_wo'][l])
        xs = self.rot("x", [128, D])
        hns = self.rot("hn", [128, D])
        hnTs = self.rot("hnT", [128, 8, 128], dt=R32)
        sts = self.rot("st", [128, 16])
        qTs = self.rot("qT", [128, 8, 128], dt=R32)
        Pms = self.rot("Pm", [128, 4 * NMEM])
        PTs = self.rot("PT", [128, 8, 128], dt=R32)
        oTs = self.rot("oT", [128, 8, 128], dt=R32)
        def xfront(i):
                j = i % 2
                x, hn, hnT, st, qT, Pm, PT, oT = xs[j], hns[j], hnTs[j], sts[j], qTs[j], Pms[j], PTs[j], oTs[j]
                self.dma(x[:, :], src[i * 128:(i + 1) * 128, :], r=[(src, i)], w=[x])
                self.rms(x, g, hn, st)
                self.transp8(hn, hnT, 0)
                pv, pk = self.psv(2, 2)
                for f in range(8):
                    for k in range(8):
                        self.mm(pv[:, f * 128:(f + 1) * 128], wq[:, k, f * 128:(f + 1) * 128], hnT[:, k, :],
                                k == 0, k == 7, r=[wq, hnT], w=pk, fast=True)
                self.copy(qT[:, :, :].rearrange("p a b -> p (a b)"), pv[:, :], r=pk, w=[qT])

        def xmid(i):
                j = i % 2
                x, hn, hnT, st, qT, Pm, PT, oT = xs[j], hns[j], hnTs[j], sts[j], qTs[j], Pms[j], PTs[j], oTs[j]
                lv, lk = self.psv(4, 2)
                for hd in range(4):
                    for c in range(2):
                        self.mm(lv[:, hd * 256:(hd + 1) * 256], qT[:, hd * 2 + c, :], KT[:, hd * 2 + c, :],
                                c == 0, c == 1, r=[qT, KT], w=lk, fast=True)
                lv3 = self.PS[:, 4:6, :].rearrange("p a (h m) -> p (a h) m", m=NMEM)
                P.op('dve', lambda e, lv3=lv3, st=st: e.tensor_reduce(out=st[:, 4:8], in_=lv3, axis=AX.X, op=ALU.max, negate=True),
                     r=lk, w=[st])
                for hd in range(4):
                    self.act(Pm[:, hd * 256:(hd + 1) * 256], lv[:, hd * 256:(hd + 1) * 256], AF.Exp, r=lk + [st], w=[Pm, st],
                             bias=st[:, 4 + hd:5 + hd], scale=1.0, accum=st[:, 8 + hd:9 + hd])
                self.recip(st[:, 12:16], st[:, 8:12], r=[st], w=[st])
                for hd in range(4):
                    self.ts(Pm[:, hd * 256:(hd + 1) * 256], Pm[:, hd * 256:(hd + 1) * 256], st[:, 12 + hd:13 + hd], None,
                            ALU.mult, None, r=[Pm, st], w=[Pm])

        def xback(i):
                j = i % 2
                x, hn, hnT, st, qT, Pm, PT, oT = xs[j], hns[j], hnTs[j], sts[j], qTs[j], Pms[j], PTs[j], oTs[j]
                self.transp8(Pm, PT, 6)
                ov, ok = self.psv(0, 2)
                for jj in range(8):
                    for c in range(2):
                        self.mm(ov[:, jj * 128:(jj + 1) * 128], V[:, c, jj * 128:(jj + 1) * 128], PT[:, (jj // 2) * 2 + c, :],
                                c == 0, c == 1, r=[V, PT], w=ok, fast=True)
                self.copy(oT[:, :, :].rearrange("p a b -> p (a b)"), ov[:, :], r=ok, w=[oT])
                yv, yk = self.psv(2, 2)
                for half in range(2):
                    for k in range(8):
                        self.mm(yv[:, half * 512:(half + 1) * 512], oT[:, k, :], wo[:, k, half * 512:(half + 1) * 512],
                                k == 0, k == 7, r=[oT, wo], w=yk, fast=True)
                self.tt(x[:, :], yv[:, :], x[:, :], ALU.add, r=yk + [x], w=[x])
                self.dma(dst[i * 128:(i + 1) * 128, :], x[:, :], r=[x], w=[(dst, i)])


        xfront(0)
        for i in range(NT):
            xmid(i)
            if i + 1 < NT:
                xfront(i + 1)
            xback(i)

    def phase_even(self, l, src, dst):
        P = self.P
        din = self.din
        jx = l // 2
        w_in = P.sb("e_win", [128, 8, 2048], R32)
        w_out = P.sb("e_wout", [128, 8, D], R32)
        pw = P.sb("e_pw", [128, 4, 128], R32)
        pw_st = P.sb("e_pwst", [128, 4, 128])
        cw = P.sb("e_cw", [128, 12])
        psc = P.sb("e_psc", [128, 4])
        g = P.sb("e_g", [128, D])
        self.gload(g, din['mix_norm'], din['mix_norm'][l:l + 1, :])
        self.wload_r(w_in, din['even_w_in'], din['even_w_in'][jx])
        self.wload_r(w_out, din['even_w_out'], din['even_w_out'][jx])
        self.dma(pw_st[:, :, :], din['even_pool_w'][jx].rearrange("g c d -> c g d"), r=[din['even_pool_w']], w=[pw_st])
        self.copy(pw[:, :, :], pw_st[:, :, :], r=[pw_st], w=[pw], eng='dve')
        self.dma(cw[:, :], din['even_conv_w'][jx], r=[din['even_conv_w']], w=[cw])
        self.dma(psc[:, :], din['even_pool_scale'][jx], r=[din['even_pool_scale']], w=[psc])
        cu_halo = P.sb("e_cuh", [128, 4, 2])
        pv_halo = P.sb("e_pvh", [128, 4, 16])
        P.op('pool', lambda e: e.memset(cu_halo[:, :, :], 0.0), w=[cu_halo])
        P.op('pool', lambda e: e.memset(pv_halo[:, :, :], 0.0), w=[pv_halo])
        hnT = P.sb("e_hnT", [128, 8, 512], R32)
        yT = P.sb("e_yT", [128, 8, 512], R32)
        xs = self.rot("e_x", [128, D])
        hns = self.rot("e_hn", [128, D])
        sts = self.rot("e_st", [128, 4])
        hts = self.rot("e_ht", [128, 8, 128])
        cus = self.rot("e_cu", [128, 514])
        pvs = self.rot("e_pv", [128, 528])
        ut = self.rot("e_ut", [128, 512])
        zt = self.rot("e_z", [128, 512])
        sA = self.rot("e_sA", [128, 528])
        sB = self.rot("e_sB", [128, 528])
        pl = self.rot("e_pl", [128, 512], dt=R32)
        invc16 = self.cst('invc16')
        tmp16 = P.sb("e_tmp16", [128, 16])
        for b in range(4):
            for t4 in range(4):
                i = b * 4 + t4
                j = i % 2
                self.dma(xs[j][:, :], src[i * 128:(i + 1) * 128, :], r=[(src, i)], w=[xs[j]])
                self.rms(xs[j], g, hns[j], sts[j])
                self.transp8(hns[j], hts[j], 6)
                self.copy(hnT[:, :, t4 * 128:(t4 + 1) * 128], hts[j][:, :, :], r=[hts[j]], w=[(hnT, t4)], eng='pool')
            for c4 in range(4):
                j = c4 % 2
                cu, pv, u_sb, z = cus[j], pvs[j], ut[j], zt[j]

                def proj(cc, bank):
                    pvw, pk = self.psv(bank, 1)
                    for k in range(8):
                        self.mm(pvw[:, :], w_in[:, k, cc * 128:(cc + 1) * 128], hnT[:, k, :], k == 0, k == 7,
                                r=[w_in, hnT], w=pk, fast=True)
                    return pvw, pk
                pu, ku = proj(c4, 0)
                self.copy(u_sb[:, :], pu[:, :], r=ku, w=[u_sb])
                pg, kg = proj(4 + c4, 1)
                self.copy(cu[:, 0:2], cu_halo[:, c4, :], r=[(cu_halo, c4)], w=[cu], eng='pool')
                self.tt(cu[:, 2:514], pg[:, :], u_sb[:, :], ALU.mult, r=kg + [u_sb, cu], w=[cu])
                self.copy(cu_halo[:, c4, :], cu[:, 512:514], r=[cu], w=[(cu_halo, c4)], eng='pool')
                self.ts(z[:, :], cu[:, 0:512], cw[:, c4 * 3:c4 * 3 + 1], None, ALU.mult, None, r=[cu, cw], w=[z])
                self.stt(z[:, :], cu[:, 1:513], cw[:, c4 * 3 + 1:c4 * 3 + 2], z[:, :], ALU.mult, ALU.add, r=[cu, cw, z], w=[z])
                self.stt(z[:, :], cu[:, 2:514], cw[:, c4 * 3 + 2:c4 * 3 + 3], z[:, :], ALU.mult, ALU.add, r=[cu, cw, z], w=[z])
                pb, kb = proj(8 + c4, 2)
                self.tt(yT[:, c4, :], pb[:, :], z[:, :], ALU.mult, r=kb + [z], w=[(yT, c4)])
                pp, kp = proj(12 + c4, 3)
                self.copy(pv[:, 0:16], pv_halo[:, c4, :], r=[(pv_halo, c4)], w=[pv], eng='pool')
                self.copy(pv[:, 16:528], pp[:, :], r=kp + [pv], w=[pv])
                self.copy(pv_halo[:, c4, :], pv[:, 512:528], r=[pv], w=[(pv_halo, c4)], eng='pool')
                a_, b_ = sA[j], sB[j]
                self.tt(a_[:, 1:528], pv[:, 1:528], pv[:, 0:527], ALU.add, r=[pv], w=[a_])
                cur = a_
                other = b_
                lo = 1
                for st_ in range(c4):
                    sh = 2 ** (st_ + 1)
                    nlo = lo + sh
                    self.tt(other[:, nlo:528], cur[:, nlo:528], cur[:, nlo - sh:528 - sh], ALU.add, r=[cur], w=[other])
                    cur, other = other, cur
                    lo = nlo
                wdw = 2 ** (c4 + 1)
                pool_t = pl[j]
                self.stt(pool_t[:, :], cur[:, 16:528], 1.0 / wdw, pv[:, 16:528], ALU.mult, ALU.subtract, r=[cur, pv], w=[pool_t])
                if b == 0:
                    self.tt(tmp16[:, :], cur[:, 16:32], invc16[:, c4 * 16:(c4 + 1) * 16], ALU.mult, r=[cur, self.C], w=[tmp16])
                    self.tt(pool_t[:, 0:16], tmp16[:, :], pv[:, 16:32], ALU.subtract, r=[tmp16, pv, pool_t], w=[pool_t])
                py, ky = self.psv(3, 1)
                self.mm(py[:, :], pw[:, c4, :], pool_t[:, :], True, True, r=[pw, pool_t], w=ky, fast=True)
                self.act(yT[:, 4 + c4, :], py[:, :], AF.Copy, r=ky + [psc], w=[(yT, 4 + c4)], scale=psc[:, c4:c4 + 1])
            for t4 in range(4):
                i = b * 4 + t4
                j = i % 2
                self.dma(xs[j][:, :], src[i * 128:(i + 1) * 128, :], r=[(src, i)], w=[xs[j]])
                ov, ok = self.psv(4, 2)
                for half in range(2):
                    for k in range(8):
                        self.mm(ov[:, half * 512:(half + 1) * 512], yT[:, k, t4 * 128:(t4 + 1) * 128],
                                w_out[:, k, half * 512:(half + 1) * 512], k == 0, k == 7, r=[yT, w_out], w=ok, fast=True)
                self.tt(xs[j][:, :], ov[:, :], xs[j][:, :], ALU.add, r=ok + [xs[j]], w=[xs[j]])
                self.dma(dst[i * 128:(i + 1) * 128, :], xs[j][:, :], r=[xs[j]], w=[(dst, i)])


    def top16(self, src_ap, work_ap, tv_ap, ti_ap, r, wk):
        P = self.P
        P.op('dve', lambda e: e.max(out=tv_ap[:, 0:8], in_=src_ap), r=r, w=wk)
        P.op('dve', lambda e: e.max_index(out=ti_ap[:, 0:8], in_max=tv_ap[:, 0:8], in_values=src_ap), r=r + wk, w=wk)
        P.op('dve', lambda e: e.match_replace(out=work_ap, in_to_replace=tv_ap[:, 0:8], in_values=src_ap, imm_value=-1e30),
             r=r + wk, w=wk)
        P.op('dve', lambda e: e.max(out=tv_ap[:, 8:16], in_=work_ap), r=wk, w=wk)
        P.op('dve', lambda e: e.max_index(out=ti_ap[:, 8:16], in_max=tv_ap[:, 8:16], in_values=work_ap), r=wk, w=wk)

    def phase_peer(self, l, src, dst):
        P = self.P
        din = self.din
        wq = P.sb("p_wq", [128, 8, 2048], BF16)
        keysT = P.sb("p_keys", [128, 16, 128])
        g = P.sb("p_g", [128, D])
        self.gload(g, din['peer_norm'], din['peer_norm'][l:l + 1, :])
        wst = self.rot("p_wst", [128, 8, 256], 2)
        for c in range(8):
            ws_ = wst[c % 2]
            self.dma(ws_[:, :, :], din['peer_wq'][l][:, c * 256:(c + 1) * 256].rearrange("(k p) c -> p k c", p=128),
                     r=[din['peer_wq']], w=[ws_])
            self.copy(wq[:, :, c * 256:(c + 1) * 256], ws_[:, :, :], r=[ws_], w=[(wq, c)], eng='act' if c % 2 == 0 else 'dve')
        self.dma(keysT[:, :, :], din['peer_keysT'][l], r=[din['peer_keysT']], w=[keysT])
        uv_l = self.uvb
        if not self.uvb_ready.get(l):
            self.convert_uv(l)
        xs = self.rot("p_x", [128, D], 2)
        xns = self.rot("p_xn", [128, D], 2)
        xnT = P.sb("p_xnT", [128, 8, 128], BF16)
        sts = self.rot("p_st", [128, 4], 2)
        qT = P.sb("p_qT", [128, 8, 128])
        s_sb = P.sb("p_s", [128, 1024])
        tv = P.sb("p_tv", [128, 16, 16])
        ti = P.sb("p_ti", [128, 16, 16], U32)
        tif = P.sb("p_tif", [128, 16, 16])
        cand = P.sb("p_cand", [128, 8, 256])
        cwk = P.sb("p_cwk", [128, 8, 256])
        cv = P.sb("p_cv", [128, 8, 16])
        cpos = P.sb("p_cpos", [128, 8, 16], U32)
        ab_u = P.sb("p_abu", [128, 2, 128], U32)
        ab_f = P.sb("p_abf", [128, 2, 128])
        isel = P.sb("p_isel", [128, 2, 128])
        eidf = P.sb("p_eidf", [128, 128])
        eids = self.rot("p_eid", [128, 128], 2, dt=I32)
        ggs = self.rot("p_gg", [128, 8, 16], 2)
        gz = P.sb("p_gz", [128, 16])
        actvs = self.rot("p_act", [128, 128], 2)
        wgts = self.rot("p_wgt", [128, 128], 2)
        GS = 4
        t1s = self.rot("p_t1", [128, GS], 4)
        t2s = self.rot("p_t2", [128, GS], 4)
        junk = P.sb("p_junk", [128, D], BF16)
        NUV = 16
        uvs = self.rot("p_uv", [128, 2 * D], NUV, dt=BF16)
        dgs = self.rot("p_dg", [128, 128], 4, dt=BF16)
        iota16 = self.cst('iota16')
        ident = self.cst('ident')
        cwkf = cwk[:, :, :].rearrange("p a b -> p (a b)")
        candf = cand[:, :, :].rearrange("p a b -> p (a b)")

        def front(i):
            x, xn, st = xs[i % 2], xns[i % 2], sts[i % 2]
            eid, gg = eids[i % 2], ggs[i % 2]
            self.dma(x[:, :], src[i * 128:(i + 1) * 128, :], r=[(src, i)], w=[x])
            self.rms(x, g, xn, st)
            yield
            self.transp8(xn, xnT, 0)
            yield
            for hf in range(2):
                qv, qk = self.psv(2, 2)
                for hh in range(8):
                    hp = hf * 8 + hh
                    for k in range(8):
                        self.mm(qv[:, hh * 128:(hh + 1) * 128], wq[:, k, hp * 128:(hp + 1) * 128], xnT[:, k, :],
                                k == 0, k == 7, r=[wq, xnT], w=qk, fast=False)
                    if hh % 2 == 1:
                        yield
                self.copy(qT[:, :, :].rearrange("p a b -> p (a b)"), qv[:, :], r=qk, w=[qT])
                sv, sk = self.psv(0, 2)
                for hh in range(8):
                    hp = hf * 8 + hh
                    self.mm(sv[:, hh * 128:(hh + 1) * 128], qT[:, hh, :], keysT[:, hp, :], True, True, r=[qT, keysT], w=sk)
                self.copy(s_sb[:, :], sv[:, :], r=sk, w=[s_sb])
                yield
                for hh in range(8):
                    hp = hf * 8 + hh
                    self.top16(s_sb[:, hh * 128:(hh + 1) * 128], cwkf[:, hh * 128:(hh + 1) * 128], tv[:, hp, :], ti[:, hp, :],
                               r=[s_sb], wk=[(tv, hp), (ti, hp), (cwk, hh)])
                    yield
            self.copy(tif[:, :, :], ti[:, :, :], r=[ti], w=[tif], eng='dve')
            tv4 = tv[:, :, :].rearrange("p (h t) a -> p h t a", t=2)
            tif4 = tif[:, :, :].rearrange("p (h t) a -> p h t a", t=2)
            cand4 = cand[:, :, :].rearrange("p h (a b) -> p h a b", b=16)
            self.tt(cand4, bcast(tv4[:, :, 0, :], 3, 16), bcast(tv4[:, :, 1, :], 2, 16), ALU.add, r=[tv], w=[cand])
            yield
            for h in range(8):
                self.top16(cand[:, h, :], cwk[:, h, :], cv[:, h, :], cpos[:, h, :],
                           r=[cand], wk=[(cv, h), (cpos, h), (cwk, h)])
                yield
            cposf = cpos[:, :, :].rearrange("p h a -> p (h a)")
            P.op('dve', lambda e: e.tensor_single_scalar(ab_u[:, 0, :], cposf, 4, ALU.logical_shift_right), r=[cpos], w=[ab_u])
            P.op('dve', lambda e: e.tensor_single_scalar(ab_u[:, 1, :], cposf, 15, ALU.bitwise_and), r=[cpos, ab_u], w=[ab_u])
            self.copy(ab_f[:, :, :], ab_u[:, :, :], r=[ab_u], w=[ab_f], eng='dve')
            yield
            eqv = candf[:, 0:1024].rearrange("p (m a) -> p m a", a=16)
            for t in range(2):
                for hh in range(2):
                    self.tt(eqv, bcast(ab_f[:, t, hh * 64:(hh + 1) * 64], 2, 16), bcast(iota16, 1, 64), ALU.is_equal,
                            r=[ab_f, self.C], w=[cand])
                    eq4 = candf[:, 0:1024].rearrange("p (h j a) -> p h j a", j=16, a=16)
                    self.tt(eq4, eq4, bcast(tif4[:, hh * 4:(hh + 1) * 4, t, :], 2, 16), ALU.mult, r=[cand, tif], w=[cand])
                    P.op('dve', lambda e, t=t, hh=hh: e.tensor_reduce(out=isel[:, t, hh * 64:(hh + 1) * 64], in_=eqv,
                                                                      axis=AX.X, op=ALU.add), r=[cand], w=[isel])
                    yield
            self.stt(eidf[:, :], isel[:, 0, :], 128.0, isel[:, 1, :], ALU.mult, ALU.add, r=[isel], w=[eidf])
            self.copy(eid[:, :], eidf[:, :], r=[eidf], w=[eid], eng='dve')
            yield
            self.tt(gg[:, :, :], cv[:, :, :], bcast(cv[:, :, 0], 2, 16), ALU.subtract, r=[cv], w=[gg])
            self.act(gg[:, :, :], gg[:, :, :], AF.Exp, r=[gg], w=[gg])
            P.op('dve', lambda e: e.tensor_reduce(out=gz[:, 0:8], in_=gg[:, :, :], axis=AX.X, op=ALU.add), r=[gg], w=[gz])
            self.recip(gz[:, 8:16], gz[:, 0:8], r=[gz], w=[gz])
            self.tt(gg[:, :, :], gg[:, :, :], bcast(gz[:, 8:16], 2, 16), ALU.mult, r=[gg, gz], w=[gg])
            yield

        def uvstage(i):
            x, xn, eid, gg = xs[i % 2], xns[i % 2], eids[i % 2], ggs[i % 2]
            actv, wgt = actvs[i % 2], wgts[i % 2]
            ggf = gg[:, :, :].rearrange("p h a -> p (h a)")
            xpv, xpk = self.psv(4, 2)
            av, ak = self.psv(6, 2)
            self.copy(xpv[:, :], xn[:, :], r=[xn], w=xpk)
            NGRP = 128 // GS

            def stA(gi):
                g0 = gi * GS
                for jj in range(g0, g0 + GS):
                    uv = uvs[jj % NUV]
                    P.op('pool', lambda e, uv=uv, jj=jj: e.indirect_dma_start(
                        out=uv[:, :], out_offset=None, in_=uv_l[:, :],
                        in_offset=bass.IndirectOffsetOnAxis(ap=eid[:, jj:jj + 1], axis=0)),
                        r=[eid, self.uvb], w=[uv], dma=True)
                    P.op('dve', lambda e, uv=uv, jj=jj: e.scalar_tensor_tensor(
                        junk[:, :], uv[:, 0:D], 1.0, xpv[:, :], ALU.mult, ALU.mult, accum_out=actv[:, jj:jj + 1]),
                        r=[uv] + xpk, w=[(actv, jj)])
                a_ = actv[:, g0:g0 + GS]
                ak_ = [(actv, jj) for jj in range(g0, g0 + GS)]
                t1, t2 = t1s[gi % 4], t2s[gi % 4]
                self.tt(t1[:, :], a_, a_, ALU.mult, r=ak_, w=[t1])
                self.ts(t1[:, :], t1[:, :], 0.044715, 1.0, ALU.mult, ALU.add, r=[t1], w=[t1])
                self.tt(t1[:, :], t1[:, :], a_, ALU.mult, r=[t1] + ak_, w=[t1])
                self.act(t2[:, :], t1[:, :], AF.Sigmoid, r=[t1], w=[t2], scale=1.5957691216057308)

            def stC(gi):
                g0 = gi * GS
                a_ = actv[:, g0:g0 + GS]
                ak_ = [(actv, jj) for jj in range(g0, g0 + GS)]
                t2 = t2s[gi % 4]
                self.tt(t2[:, :], t2[:, :], a_, ALU.mult, r=[t2] + ak_, w=[t2])
                self.tt(wgt[:, g0:g0 + GS], t2[:, :], ggf[:, g0:g0 + GS], ALU.mult, r=[t2, gg], w=[(wgt, gi)])

            def stD(gi):
                g0 = gi * GS
                for jj in range(g0, g0 + GS):
                    uv = uvs[jj % NUV]
                    dg = dgs[jj % 4]
                    self.ts(dg[:, :], ident, wgt[:, jj:jj + 1], 1.0, ALU.mult, ALU.mult, r=[self.C, (wgt, gi)], w=[dg], eng='pool')
                    for half in range(2):
                        self.mm(av[:, half * 512:(half + 1) * 512], dg[:, :], uv[:, D + half * 512:D + (half + 1) * 512],
                                jj == 0, jj == 127, r=[dg, uv], w=[ak[half]], fast=False)

            for gi in range(NGRP + 2):
                if gi < NGRP:
                    stA(gi)
                if 1 <= gi <= NGRP:
                    stC(gi - 1)
                if gi >= 2:
                    stD(gi - 2)
                yield
            self.tt(x[:, :], av[:, :], x[:, :], ALU.add, r=ak + [x], w=[x])
            self.dma(dst[i * 128:(i + 1) * 128, :], x[:, :], r=[x], w=[(dst, i)])
            yield

        for _ in front(0):
            pass
        for it in range(NT):
            active = [uvstage(it)]
            if it + 1 < NT:
                active.append(front(it + 1))
            while active:
                for gen in list(active):
                    try:
                        next(gen)
                    except StopIteration:
                        active.remove(gen)

    def phase_dsa(self, l, src, dst):
        P = self.P
        din = self.din
        jx = l // 2
        w = P.sb("d_w", [128, 8, 1736])
        w_uv = P.sb("d_wuv", [128, 8, 64])
        w_out = P.sb("d_wout", [128, 4, D])
        g = P.sb("d_g", [128, D])
        gkv = P.sb("d_gkv", [128, 128])
        b31 = P.sb("d_b31", [128, 16])
        corrT = P.sb("d_corr", [128, 8, 2, 128])
        self.gload(g, din['mix_norm'], din['mix_norm'][l:l + 1, :])
        self.gload(gkv, din['odd_kv_norm'], din['odd_kv_norm'][jx:jx + 1, :])
        self.gload(b31, din['rel_bias'], din['rel_bias'][31:32, :]) if False else self.dma(
            b31[:, 0:8], din['rel_bias'][31:32, :].broadcast_to([128, 8]), r=[din['rel_bias']], w=[b31])
        self.ts(b31[:, 8:16], b31[:, 0:8], -1.0, None, ALU.mult, None, r=[b31], w=[b31])
        self.dma(corrT[:, :, :, :], din['biasT'][:, :, :, :], r=[din['biasT']], w=[corrT])
        for h in range(8):
            self.act(corrT[:, h, :, :], corrT[:, h, :, :], AF.Exp, r=[corrT, b31], w=[corrT], bias=b31[:, 8 + h:9 + h], scale=1.0)
        self.dma(w[:, :, :], din['odd_w_in'][jx][:, 0:1736].rearrange("(k p) c -> p k c", p=128), r=[din['odd_w_in']], w=[w])
        self.dma(w_uv[:, :, :], din['odd_w_uv'][jx].rearrange("h r e -> r h e"), r=[din['odd_w_uv']], w=[w_uv])
        self.dma(w_out[:, :, :], din['odd_w_out'][jx][0:512, :].rearrange("(k p) c -> p k c", p=128), r=[din['odd_w_out']], w=[w_out])
        cT = P.sb("d_cT", [128, S])
        c_tm = P.sb("d_ctm", [128, NT, 128])
        ikT = P.sb("d_ikT", [64, S])
        xs = self.rot("d_x", [128, D])
        hn = P.sb("d_hn", [128, D])
        hnT = P.sb("d_hnT", [128, 8, 128])
        st = P.sb("d_st", [128, 8])
        craw = P.sb("d_craw", [128, 128])
        qlTs = self.rot("d_qlT", [128, 8, 128], 2)
        iqT = P.sb("d_iqT", [64, 8, 128])
        iw = P.sb("d_iw", [128, 8])
        score = P.sb("d_score", [128, S])
        work = P.sb("d_work", [128, S])
        maskT = P.sb("d_maskT", [128, NT, 128])
        rsb = self.rot("d_r", [128, 512])
        m8 = self.rot("d_m8", [128, 8])
        ETs = self.rot("d_ET", [128, NT, 128], 2)
        latT = P.sb("d_latT", [128, 8, 128])
        zz = P.sb("d_zz", [128, 16])
        yc = P.sb("d_yc", [128, 8, 64])
        ycT = P.sb("d_ycT", [128, 4, 128])
        ones = self.cst('ones')
        cmask = self.cst('cmask')
        CK, CQ, CIK, CIW = 1024, 1152, 1664, 1728
        def projpart(i):
            x = xs[i % 2]
            qlT = qlTs[i % 2]
            nk = (i + 1) * 128
            self.dma(x[:, :], src[i * 128:(i + 1) * 128, :], r=[(src, i)], w=[x])
            self.rms(x, g, hn, st)
            self.transp8(hn, hnT, 4)
            pv, pk = self.psv(6, 1)
            for k in range(8):
                self.mm(pv[:, 0:128], hnT[:, k, :], w[:, k, CK:CK + 128], k == 0, k == 7, r=[hnT, w], w=pk)
            self.copy(craw[:, :], pv[:, 0:128], r=pk, w=[craw])
            self.act(work[:, 0:128], craw[:, :], AF.Square, r=[craw], w=[work, st], accum=st[:, 2:3])
            self.act(st[:, 3:4], st[:, 2:3], AF.Sqrt, r=[st], w=[st], scale=1.0 / 128, bias=self.epsb[:, 0:1])
            self.recip(st[:, 3:4], st[:, 3:4], r=[st], w=[st])
            self.stt(c_tm[:, i, :], craw[:, :], st[:, 3:4], gkv[:, :], ALU.mult, ALU.mult, r=[craw, st, gkv], w=[(c_tm, i)])
            pv7, pk7 = self.psv(7, 1)
            self.tr(pv7[:, 0:128], c_tm[:, i, :], r=[(c_tm, i)], w=pk7)
            self.copy(cT[:, i * 128:(i + 1) * 128], pv7[:, 0:128], r=pk7, w=[(cT, i)])
            for k in range(8):
                self.mm(pv[0:64, 0:128], w[:, k, CIK:CIK + 64], hnT[:, k, :], k == 0, k == 7, r=[hnT, w], w=pk)
            self.act(ikT[:, i * 128:(i + 1) * 128], pv[0:64, 0:128], AF.Copy, r=pk, w=[(ikT, i)], scale=0.125)
            for k in range(8):
                self.mm(pv7[:, 0:8], hnT[:, k, :], w[:, k, CIW:CIW + 8], k == 0, k == 7, r=[hnT, w], w=pk7)
            self.act(iw[:, :], pv7[:, 0:8], AF.Copy, r=pk7, w=[iw], scale=8 ** -0.5)
            qv, qk = self.psv(0, 2)
            for h in range(8):
                for k in range(8):
                    self.mm(qv[:, h * 128:(h + 1) * 128], w[:, k, h * 128:(h + 1) * 128], hnT[:, k, :], k == 0, k == 7,
                            r=[hnT, w], w=qk)
            self.act(qlT[:, :, :].rearrange("p a b -> p (a b)"), qv[:, :], AF.Copy, r=qk, w=[qlT], scale=128 ** -0.5)
            iv, ik_ = self.psv(2, 2)
            for h in range(8):
                for k in range(8):
                    self.mm(iv[0:64, h * 128:(h + 1) * 128], w[:, k, CQ + h * 64:CQ + (h + 1) * 64], hnT[:, k, :], k == 0, k == 7,
                            r=[hnT, w], w=ik_)
            self.copy(iqT[:, :, :].rearrange("p a b -> p (a b)"), iv[0:64, :], r=ik_, w=[iqT])
        projpart(0)
        for i in range(NT):
            x = xs[i % 2]
            qlT = qlTs[i % 2]
            nk = (i + 1) * 128
            cnt = 0
            for c0 in range(0, nk, 512):
                cw = min(512, nk - c0)
                for h in range(8):
                    sv, sk = self.psv(4 + cnt % 2, 1)
                    r_ = rsb[cnt % 2]
                    cnt += 1
                    self.mm(sv[:, 0:cw], iqT[:, h, :], ikT[:, c0:c0 + cw], True, True, r=[iqT, ikT], w=sk)
                    self.act(r_[:, 0:cw], sv[:, 0:cw], AF.Relu, r=sk, w=[r_])
                    if h == 0:
                        self.ts(score[:, c0:c0 + cw], r_[:, 0:cw], iw[:, 0:1], None, ALU.mult, None, r=[r_, iw], w=[score])
                    else:
                        self.stt(score[:, c0:c0 + cw], r_[:, 0:cw], iw[:, h:h + 1], score[:, c0:c0 + cw], ALU.mult, ALU.add,
                                 r=[r_, iw, score], w=[score])
            if i + 1 < NT:
                projpart(i + 1)
            self.tt(score[:, i * 128:nk], score[:, i * 128:nk], cmask, ALU.add, r=[score, self.C], w=[score])
            if i >= 2:
                cur = score
                for rnd in range(32):
                    m = m8[rnd % 2]
                    P.op('dve', lambda e, m=m, cur=cur, nk=nk: e.max(out=m[:, :], in_=cur[:, 0:nk]), r=[cur], w=[m])
                    if rnd < 31:
                        P.op('dve', lambda e, m=m, cur=cur, nk=nk: e.match_replace(
                            out=work[:, 0:nk], in_to_replace=m[:, :], in_values=cur[:, 0:nk], imm_value=-1e30),
                            r=[cur, m], w=[work])
                        cur = work
                self.ts(work[:, 0:nk], score[:, 0:nk], m8[1][:, 7:8], None, ALU.is_ge, None, r=[score, m8[1]], w=[work])
            else:
                self.ts(work[:, 0:nk], score[:, 0:nk], -1e29, None, ALU.is_ge, None, r=[score], w=[work])
            for kt in range(i + 1):
                b = 6 + (kt // 4) % 2
                mv, mk = self.psv(b, 1)
                self.tr(mv[:, (kt % 4) * 128:(kt % 4 + 1) * 128], work[:, kt * 128:(kt + 1) * 128], r=[work], w=mk)
                if kt % 4 == 3 or kt == i:
                    k0 = (kt // 4) * 4
                    n = kt - k0 + 1
                    self.copy(maskT[:, k0:kt + 1, :].rearrange("p a b -> p (a b)"), mv[:, 0:n * 128], r=mk, w=[maskT], eng='pool' if False else 'act')
            lv, lk = self.psv(0, 4)
            av, ak = self.psv(6, 2)
            zv, zk = self.psv(5, 1)
            def lg(h):
                ET = ETs[h % 2]
                for kt in range(i + 1):
                    self.mm(lv[:, kt * 128:(kt + 1) * 128], cT[:, kt * 128:(kt + 1) * 128], qlT[:, h, :], True, True,
                            r=[cT, qlT], w=lk)
                ETf = ET[:, :, :].rearrange("p a b -> p (a b)")
                self.act(ETf[:, 0:nk], lv[:, 0:nk], AF.Exp, r=lk + [b31], w=[ET], bias=b31[:, h:h + 1], scale=1.0)

            def pvh(h):
                ET = ETs[h % 2]
                ETf = ET[:, :, :].rearrange("p a b -> p (a b)")
                self.tt(ETf[:, 0:nk], ETf[:, 0:nk], maskT[:, :, :].rearrange("p a b -> p (a b)")[:, 0:nk], ALU.mult,
                        r=[ET, maskT], w=[ET])
                for kt in range(max(0, i - 1), i + 1):
                    self.tt(ET[:, kt, :], ET[:, kt, :], corrT[:, h, i - kt, :], ALU.mult, r=[ET, corrT], w=[ET])
                for kt in range(i + 1):
                    self.mm(av[:, h * 128:(h + 1) * 128], c_tm[:, kt, :], ET[:, kt, :], kt == 0, kt == i, r=[c_tm, ET], w=ak)
                    self.mm(zv[:, h:h + 1], ET[:, kt, :], ones[:, 0:1], kt == 0, kt == i, r=[ET, self.C], w=zk)

            lg(0)
            for h in range(8):
                if h + 1 < 8:
                    lg(h + 1)
                pvh(h)
            self.copy(latT[:, :, :].rearrange("p a b -> p (a b)"), av[:, :], r=ak, w=[latT])
            self.copy(zz[:, 0:8], zv[:, 0:8], r=zk, w=[zz], eng='dve')
            self.recip(zz[:, 8:16], zz[:, 0:8], r=[zz], w=[zz])
            yv, yk = self.psv(4, 1)
            for h in range(8):
                self.mm(yv[:, h * 64:(h + 1) * 64], latT[:, h, :], w_uv[:, h, :], True, True, r=[latT, w_uv], w=yk)
            self.tt(yc[:, :, :], yv[:, :].rearrange("p (h e) -> p h e", e=64), bcast(zz[:, 8:16], 2, 64), ALU.mult,
                    r=yk + [zz], w=[yc])
            tv_, tk_ = self.psv(5, 1)
            ycf = yc[:, :, :].rearrange("p h e -> p (h e)")
            for k in range(4):
                self.tr(tv_[:, k * 128:(k + 1) * 128], ycf[:, k * 128:(k + 1) * 128], r=[yc], w=tk_)
            self.copy(ycT[:, :, :].rearrange("p a b -> p (a b)"), tv_[:, :], r=tk_, w=[ycT])
            ov, ok = self.psv(0, 2)
            for half in range(2):
                for k in range(4):
                    self.mm(ov[:, half * 512:(half + 1) * 512], ycT[:, k, :], w_out[:, k, half * 512:(half + 1) * 512],
                            k == 0, k == 3, r=[ycT, w_out], w=ok)
            self.tt(x[:, :], ov[:, :], x[:, :], ALU.add, r=ok + [x], w=[x])
            self.dma(dst[i * 128:(i + 1) * 128, :], x[:, :], r=[x], w=[(dst, i)])


    def phase_hgrn(self, l, src, acc):
        P = self.P
        din = self.din
        jx = l // 2
        w = P.sb("h_w", [128, 8, 2048])
        w_out = P.sb("h_wout", [128, 4, D])
        g = P.sb("h_g", [128, D])
        gn = P.sb("h_gn", [128, 512])
        gam = P.sb("h_gam", [128, 4, 512])
        lb = P.sb("h_lb", [128, 512])
        oml = P.sb("h_oml", [128, 512])
        lbT = P.sb("h_lbT", [128, 8])
        tmp = P.sb("h_tmp", [128, 512])
        self.gload(g, din['mix_norm'], din['mix_norm'][l:l + 1, :])
        self.gload(gn, din['odd_hg_norm'], din['odd_hg_norm'][jx:jx + 1, :])
        self.dma(w[:, :, :], din['odd_w_in'][jx][:, 1736:3784].rearrange("(k p) c -> p k c", p=128), r=[din['odd_w_in']], w=[w])
        self.dma(w_out[:, :, :], din['odd_w_out'][jx][512:1024, :].rearrange("(k p) c -> p k c", p=128), r=[din['odd_w_out']], w=[w_out])
        for ll in range(4):
            self.dma(gam[:, ll, :], din['hgrn_gamma'][ll:ll + 1, :].broadcast_to([128, 512]), r=[din['hgrn_gamma']], w=[(gam, ll)])
        self.act(gam[:, :, :], gam[:, :, :], AF.Exp, r=[gam], w=[gam])
        self.tt(tmp[:, :], gam[:, 0, :], gam[:, 1, :], ALU.add, r=[gam], w=[tmp])
        self.tt(tmp[:, :], tmp[:, :], gam[:, 2, :], ALU.add, r=[gam, tmp], w=[tmp])
        self.tt(tmp[:, :], tmp[:, :], gam[:, 3, :], ALU.add, r=[gam, tmp], w=[tmp])
        self.recip(tmp[:, :], tmp[:, :], r=[tmp], w=[tmp])
        P.op('dve', lambda e: e.memset(lb[:, :], 0.0), w=[lb])
        for ll in range(l):
            self.tt(lb[:, :], lb[:, :], gam[:, ll, :], ALU.add, r=[lb, gam], w=[lb])
        self.tt(lb[:, :], lb[:, :], tmp[:, :], ALU.mult, r=[lb, tmp], w=[lb])
        self.ts(oml[:, :], lb[:, :], -1.0, 1.0, ALU.mult, ALU.add, r=[lb], w=[oml])
        pv, pk = self.psv(0, 1)
        for h in range(4):
            self.tr(pv[:, h * 128:(h + 1) * 128], lb[:, h * 128:(h + 1) * 128], r=[lb], w=pk)
        for h in range(4):
            self.copy(lbT[:, h:h + 1], pv[:, h * 128:h * 128 + 1], r=pk, w=[lbT], eng='dve')
        self.ts(lbT[:, 4:8], lbT[:, 0:4], -1.0, 1.0, ALU.mult, ALU.add, r=[lbT], w=[lbT])
        Sst = [P.sb(f"h_S{j}", [128, 4, 128]) for j in range(2)]
        qt0 = P.sb("h_qt0", [128, 4, 128])
        qt1 = P.sb("h_qt1", [128, 4, 128])
        kh0 = P.sb("h_kh0", [128, 512])
        kh1 = P.sb("h_kh1", [128, 512])
        P.op('pool', lambda e: e.memset(Sst[0][:, :, :], 0.0), w=[Sst[0]])
        P.op('pool', lambda e: e.memset(qt0[:, :, :], 0.0), w=[qt0])
        P.op('pool', lambda e: e.memset(qt1[:, :, :], 0.0), w=[qt1])
        P.op('pool', lambda e: e.memset(kh0[:, :], 0.0), w=[kh0])
        P.op('pool', lambda e: e.memset(kh1[:, :], 0.0), w=[kh1])
        xs = self.rot("h_x", [128, D])
        hn = P.sb("h_hn", [128, D])
        hnT = P.sb("h_hnT", [128, 8, 128])
        st = P.sb("h_st", [128, 16])
        sg = P.sb("h_sg", [128, 512])
        f_tm = P.sb("h_f", [128, 512])
        lf = P.sb("h_lf", [128, 512])
        kk = P.sb("h_kk", [128, 512])
        i_sb = P.sb("h_i", [128, 512])
        sil = P.sb("h_sil", [128, 512])
        sgT = P.sb("h_sgT", [128, 4, 128])
        kkT = P.sb("h_kkT", [128, 4, 128])
        eAT = P.sb("h_eAT", [128, 4, 128])
        enAT = P.sb("h_enAT", [128, 4, 128])
        qtT = P.sb("h_qtT", [128, 4, 128])
        ktT = P.sb("h_ktT", [128, 4, 128])
        a_sb = P.sb("h_a", [128, 512])
        d_sb = P.sb("h_d", [128, 512])
        sc = self.rot("h_sc", [128, 128])
        y_sb = P.sb("h_y", [128, 512])
        yT = P.sb("h_yT", [128, 4, 128])
        acc_t = self.rot("h_acc", [128, D])
        U2 = self.cst('U2')
        B2 = self.cst('B2')
        HQ, HF, HI, HG = 0, 512, 1024, 1536

        def proj_tm(c0, bank):
            pvw, pkw = self.psv(bank, 1)
            for k in range(8):
                self.mm(pvw[:, :], hnT[:, k, :], w[:, k, c0:c0 + 512], k == 0, k == 7, r=[hnT, w], w=pkw)
            return pvw, pkw

        def proj_fm(c0, bank):
            pvw, pkw = self.psv(bank, 1)
            for h in range(4):
                for k in range(8):
                    self.mm(pvw[:, h * 128:(h + 1) * 128], w[:, k, c0 + h * 128:c0 + (h + 1) * 128], hnT[:, k, :],
                            k == 0, k == 7, r=[hnT, w], w=pkw)
            return pvw, pkw

        for i in range(NT):
            x = xs[i % 2]
            self.dma(x[:, :], src[i * 128:(i + 1) * 128, :], r=[(src, i)], w=[x])
            self.rms(x, g, hn, st)
            self.transp8(hn, hnT, 0)
            pf, kf = proj_tm(HF, 2)
            self.act(sg[:, :], pf[:, :], AF.Sigmoid, r=kf, w=[sg])
            self.tt(f_tm[:, :], sg[:, :], oml[:, :], ALU.mult, r=[sg, oml], w=[f_tm])
            self.tt(f_tm[:, :], f_tm[:, :], lb[:, :], ALU.add, r=[f_tm, lb], w=[f_tm])
            self.act(lf[:, :], f_tm[:, :], AF.Ln, r=[f_tm], w=[lf])
            self.ts(kk[:, :], f_tm[:, :], -1.0, 1.0, ALU.mult, ALU.add, r=[f_tm], w=[kk])
            pi_, ki_ = proj_tm(HI, 3)
            self.copy(i_sb[:, :], pi_[:, :], r=ki_, w=[i_sb])
            pg_, kg_ = proj_tm(HG, 4)
            self.act(sil[:, :], pg_[:, :], AF.Sigmoid, r=kg_, w=[sil])
            self.tt(sil[:, :], sil[:, :], pg_[:, :], ALU.mult, r=[sil] + kg_, w=[sil])
            pq, kq = proj_fm(HQ, 5)
            pfT, kfT = proj_fm(HF, 6)
            self.act(sgT[:, :, :].rearrange("p a b -> p (a b)"), pfT[:, :], AF.Sigmoid, r=kfT, w=[sgT])
            for h in range(4):
                self.ts(kkT[:, h, :], sgT[:, h, :], lbT[:, 4 + h:5 + h], lbT[:, h:h + 1], ALU.mult, ALU.add, r=[sgT, lbT], w=[kkT])
            self.ts(kkT[:, :, :], kkT[:, :, :], -1.0, 1.0, ALU.mult, ALU.add, r=[kkT], w=[kkT])
            pA, kA = self.psv(2, 1)
            self.mm(pA[:, :], U2, lf[:, :], True, True, r=[self.C, lf], w=kA)
            self.copy(a_sb[:, :], pA[:, :], r=kA, w=[a_sb])
            pE, kE = self.psv(3, 1)
            self.mm(pE[:, :], B2, lf[:, :], True, True, r=[self.C, lf], w=kE)
            pAT, kAT = self.psv(4, 1)
            for h in range(4):
                self.mm(pAT[:, h * 128:(h + 1) * 128], lf[:, h * 128:(h + 1) * 128], U2, True, True, r=[self.C, lf], w=kAT)
            self.act(eAT[:, :, :].rearrange("p a b -> p (a b)"), pAT[:, :], AF.Exp, r=kAT, w=[eAT])
            self.act(enAT[:, :, :].rearrange("p a b -> p (a b)"), pAT[:, :], AF.Exp, r=kAT, w=[enAT], scale=-1.0)
            self.tt(qtT[:, :, :].rearrange("p a b -> p (a b)"), pq[:, :], eAT[:, :, :].rearrange("p a b -> p (a b)"), ALU.mult,
                    r=kq + [eAT], w=[qtT])
            self.tt(ktT[:, :, :], kkT[:, :, :], enAT[:, :, :], ALU.mult, r=[kkT, enAT], w=[ktT])
            self.copy(qt0[:, :, 0:64], qtT[:, :, 0:64], r=[qtT], w=[qt0], eng='pool')
            self.copy(qt1[:, :, 64:128], qtT[:, :, 64:128], r=[qtT], w=[qt1], eng='pool')
            self.tt(d_sb[:, :], pE[:, :], a_sb[:, :], ALU.subtract, r=kE + [a_sb], w=[d_sb])
            self.act(d_sb[:, :], d_sb[:, :], AF.Exp, r=[d_sb], w=[d_sb])
            self.tt(kh0[0:64, :], d_sb[0:64, :], kk[0:64, :], ALU.mult, r=[d_sb, kk], w=[kh0])
            self.tt(kh1[64:128, :], d_sb[64:128, :], kk[64:128, :], ALU.mult, r=[d_sb, kk], w=[kh1])
            po, ko = self.psv(7, 1)
            for h in range(4):
                hs = slice(h * 128, (h + 1) * 128)
                S0, S1 = Sst[0], Sst[1]
                ps_, ks_ = self.psv(0, 1)
                self.mm(ps_[:, 0:128], ktT[:, h, :], qtT[:, h, :], True, True, r=[ktT, qtT], w=ks_)
                scb = sc[h % 2]
                self.tt(scb[:, :], ps_[:, 0:128], U2, ALU.mult, r=ks_ + [self.C], w=[scb])
                self.mm(po[:, hs], scb[:, :], i_sb[:, hs], True, False, r=[scb, i_sb], w=ko)
                self.mm(po[:, hs], qt0[:, h, :], S0[:, h, :], False, False, r=[qt0, (S0, h)], w=ko)
                p1, k1 = self.psv(1, 1)
                self.mm(p1[:, 0:128], kh0[:, hs], i_sb[:, hs], True, True, r=[kh0, i_sb], w=k1)
                self.stt(S1[:, h, :], S0[:, h, :], eAT[:, h, 63:64], p1[:, 0:128], ALU.mult, ALU.add, r=[(S0, h), eAT] + k1, w=[(S1, h)])
                self.mm(po[:, hs], qt1[:, h, :], S1[:, h, :], False, True, r=[qt1, (S1, h)], w=ko)
                p2, k2 = self.psv(2, 1)
                self.mm(p2[:, 0:128], kh1[:, hs], i_sb[:, hs], True, True, r=[kh1, i_sb], w=k2)
                self.stt(S0[:, h, :], S1[:, h, :], eAT[:, h, 127:128], p2[:, 0:128], ALU.mult, ALU.add, r=[(S1, h), eAT] + k2, w=[(S0, h)])
            for h in range(4):
                self.act(y_sb[:, h * 128:(h + 1) * 128], po[:, h * 128:(h + 1) * 128], AF.Square, r=ko, w=[y_sb, st], accum=st[:, 4 + h:5 + h])
            self.act(st[:, 8:12], st[:, 4:8], AF.Sqrt, r=[st], w=[st], scale=1.0 / 128, bias=self.epsb[:, 0:1])
            self.recip(st[:, 8:12], st[:, 8:12], r=[st], w=[st])
            for h in range(4):
                self.ts(y_sb[:, h * 128:(h + 1) * 128], po[:, h * 128:(h + 1) * 128], st[:, 8 + h:9 + h], None, ALU.mult, None,
                        r=ko + [st], w=[y_sb])
            self.tt(y_sb[:, :], y_sb[:, :], gn[:, :], ALU.mult, r=[y_sb, gn], w=[y_sb])
            self.tt(y_sb[:, :], y_sb[:, :], sil[:, :], ALU.mult, r=[y_sb, sil], w=[y_sb])
            tv_, tk_ = self.psv(5, 1)
            for k in range(4):
                self.tr(tv_[:, k * 128:(k + 1) * 128], y_sb[:, k * 128:(k + 1) * 128], r=[y_sb], w=tk_)
            self.copy(yT[:, :, :].rearrange("p a b -> p (a b)"), tv_[:, :], r=tk_, w=[yT])
            at = acc_t[i % 2]
            self.dma(at[:, :], acc[i * 128:(i + 1) * 128, :], r=[(acc, i)], w=[at])
            ov, ok = self.psv(2, 2)
            for half in range(2):
                for k in range(4):
                    self.mm(ov[:, half * 512:(half + 1) * 512], yT[:, k, :], w_out[:, k, half * 512:(half + 1) * 512],
                            k == 0, k == 3, r=[yT, w_out], w=ok)
            self.tt(at[:, :], ov[:, :], at[:, :], ALU.add, r=ok + [at], w=[at])
            self.dma(acc[i * 128:(i + 1) * 128, :], at[:, :], r=[at], w=[(acc, i)])

    def phase_copy(self, src, dst):
        xs = self.rot("c_x", [128, D])
        for i in range(NT):
            self.dma(xs[i % 2][:, :], src[i * 128:(i + 1) * 128, :], r=[(src, i)], w=[xs[i % 2]])
            self.dma(dst[i * 128:(i + 1) * 128, :], xs[i % 2][:, :], r=[xs[i % 2]], w=[(dst, i)])

    def phase_final(self, src):
        P = self.P
        g = P.sb("g_f", [128, D])
        self.gload(g, self.din['final_norm'], self.din['final_norm'][0:1, :])
        xs = self.rot("xf", [128, D])
        hns = self.rot("hnf", [128, D])
        sts = self.rot("stf", [128, 4])
        for i in range(NT):
            j = i % 2
            self.dma(xs[j][:, :], src[i * 128:(i + 1) * 128, :], r=[(src, i)], w=[xs[j]])
            self.rms(xs[j], g, hns[j], sts[j])
            self.dma(self.out[i * 128:(i + 1) * 128, :], hns[j][:, :], r=[hns[j]], w=[(self.out, i)])

    def run_phase(self, fn, *a):
        with ExitStack() as pes:
            self.P.pes = pes
            fn(*a)
            self.P.flush()
        self.P.pes = self.es

    def build(self, plan):
        for ph in plan:
            if ph[0] == 'peer':
                self.want_peer[ph[1]] = True
        cur = self.din['x']
        self.run_phase(self.phase_setup)
        for ph in plan:
            if ph[0] == 'xattn':
                self.run_phase(self.phase_xattn, ph[1], cur, self.hA)
                cur = self.hA
            elif ph[0] == 'even':
                self.run_phase(self.phase_even, ph[1], cur, self.hA)
                cur = self.hA
            elif ph[0] == 'peer':
                self.run_phase(self.phase_peer, ph[1], cur, self.hA)
                cur = self.hA
            elif ph[0] == 'dsa':
                other = self.hB if cur is not self.hB else self.hA
                self.run_phase(self.phase_dsa, ph[1], cur, other)
                cur = other
            elif ph[0] == 'hgrn':
                other = self.hB if cur is not self.hB else self.hA
                self.run_phase(self.phase_copy, cur, other)
                self.run_phase(self.phase_hgrn, ph[1], cur, other)
                cur = other
            elif ph[0] == 'odd':
                other = self.hB if cur is not self.hB else self.hA
                self.run_phase(self.phase_dsa, ph[1], cur, other)
                self.run_phase(self.phase_hgrn, ph[1], cur, other)
                cur = other
            elif ph[0] == 'final':
                self.run_phase(self.phase_final, cur)
        return self.nc


FULL_PLAN = []
for _l in range(DEPTH):
    FULL_PLAN.append(('even' if _l % 2 == 0 else 'odd', _l))
    FULL_PLAN.append(('xattn', _l))
    FULL_PLAN.append(('peer', _l))
FULL_PLAN.append(('final',))


def t5_bucket_np(d):
    d = np.maximum(d, 0)
    lr = np.log(np.maximum(d, 1).astype(np.float32) / np.float32(16)) / np.float32(np.log(128 / 16))
    large = 16 + (lr * np.float32(16)).astype(np.int32)
    large = np.minimum(large, 31)
    return np.where(d < 16, d, large)


def prep_inputs(inp):
    f = lambda a: np.ascontiguousarray(np.asarray(a, dtype=np.float32))
    shared = {}
    for k in IN_SHAPES:
        if k in ('x', 'mem'):
            continue
        if k == 'peer_keysT':
            a = np.asarray(inp['peer_keys'], dtype=np.float32)
            shared[k] = f(a.transpose(0, 4, 1, 2, 3).reshape(4, 128, 16, 128))
        elif k == 'even_conv_w':
            a = np.asarray(inp[k]).reshape(2, 3, 4, 128)
            shared[k] = f(a.transpose(0, 3, 2, 1).reshape(2, 128, 12))
        elif k == 'even_pool_scale':
            a = np.asarray(inp[k]).reshape(2, 4, 128)
            shared[k] = f(a.transpose(0, 2, 1))
        elif k == 'peer_uv':
            shared[k] = np.ascontiguousarray(np.concatenate(
                [np.asarray(inp['peer_u'], dtype=np.float32), np.asarray(inp['peer_v'], dtype=np.float32)], axis=-1))
        elif k == 'invc':
            shared[k] = INVC[0]
        elif k == 'biasT':
            rb = np.asarray(inp['rel_bias'], dtype=np.float32)
            ss_, tq_ = np.arange(128)[:, None], np.arange(128)[None, :]
            out = np.zeros((128, 8, 2, 128), dtype=np.float32)
            for dl in (0, 1):
                dist = np.maximum(128 * dl + tq_ - ss_, 0)
                out[:, :, dl, :] = rb[t5_bucket_np(dist)].transpose(0, 2, 1)
            shared[k] = f(out)
        elif k in ('mem_norm', 'final_norm'):
            shared[k] = f(np.asarray(inp[k]).reshape(1, D))
        else:
            shared[k] = f(inp[k])
    return shared


def kernel(plan=None, **inp):
    plan = FULL_PLAN if plan is None else plan
    m = Model()
    nc = m.build(plan)
    shared = prep_inputs(inp)
    shared['consts'] = m.carr
    shared = {k: v for k, v in shared.items() if k in m.din}
    x = np.asarray(inp['x'], dtype=np.float32)
    mem = np.asarray(inp['mem'], dtype=np.float32)
    in_maps = []
    for b in range(8):
        d = dict(shared)
        if 'x' in m.din:
            d['x'] = np.ascontiguousarray(x[b])
        if 'mem' in m.din:
            d['mem'] = np.ascontiguousarray(mem[b])
        in_maps.append(d)
    res = run_bass_kernel_spmd(nc, in_maps, core_ids=list(range(8)))
    m.es.close()
    return np.stack([np.asarray(r["out"], dtype=np.float32) for r in res.results], axis=0)
```

```python
import numpy as np
import concourse.bass as bass
import concourse.mybir as mybir
from concourse.bass_utils import run_bass_kernel_spmd
from contextlib import ExitStack

F32 = mybir.dt.float32
I32 = mybir.dt.int32
U32 = mybir.dt.uint32
AF = mybir.ActivationFunctionType
ALU = mybir.AluOpType
AX = mybir.AxisListType

ENGS = ['pe', 'dve', 'act', 'pool', 'sp']
SEM_LIMIT = 30000
DMA_K = 16


class T:
    def __init__(self, h, name):
        self.h = h
        self.name = name
        self.st = {}

    def __getitem__(self, idx):
        return self.h[idx]


def bcast(ap, axis, n):
    l = [list(x) for x in ap.ap]
    l.insert(axis, [0, n])
    return bass.AP(ap.tensor, ap.offset, l)


def rep(ap, axis, n):
    l = [list(x) for x in ap.ap]
    assert l[axis][1] == 1
    l[axis] = [0, n]
    return bass.AP(ap.tensor, ap.offset, l)


class Prog:
    def __init__(self, nc, es):
        self.nc = nc
        self.es = es
        self.pes = es
        self.ops = {e: [] for e in ENGS}
        self.base = {e: 0 for e in ENGS}
        self.waited = {e: {} for e in ENGS}
        self.waited_dma = {e: set() for e in ENGS}
        self.tiles = []
        self.csems = {e: [] for e in ENGS}
        self.ccount = {e: 0 for e in ENGS}
        self.dsems = {}
        self.dcount = {e: 0 for e in ENGS}
        self.dma_hist = {e: [] for e in ENGS}

    def sb(self, name, shape, dt=F32, glob=False):
        es = self.es if glob else self.pes
        self.uid = getattr(self, 'uid', 0) + 1
        name = f"{name}_{self.uid}"
        t = T(es.enter_context(self.nc.sbuf_tensor(name, list(shape), dt)), name)
        self.tiles.append(t)
        return t

    def ps(self, name, shape, dt=F32):
        t = T(self.es.enter_context(self.nc.psum_tensor(name, list(shape), dt)), name)
        self.tiles.append(t)
        return t

    def dram(self, name, shape, dt=F32, kind="Internal"):
        t = T(self.nc.dram_tensor(name, list(shape), dt, kind=kind).ap(), name)
        self.tiles.append(t)
        return t

    @staticmethod
    def _norm(key):
        if isinstance(key, T):
            return key, None
        return key[0], key[1]

    def _entries(self, tile, sub):
        if sub is None:
            return list(tile.st.values())
        out = []
        if sub in tile.st:
            out.append(tile.st[sub])
        if None in tile.st:
            out.append(tile.st[None])
        return out

    def op(self, eng, fn, r=(), w=(), dma=False, extra=()):
        idx = len(self.ops[eng])
        deps = set(extra)
        for key in r:
            tile, sub = self._norm(key)
            for ent in self._entries(tile, sub):
                if ent[0] is not None:
                    deps.add(ent[0])
        for key in w:
            tile, sub = self._norm(key)
            for ent in self._entries(tile, sub):
                if ent[0] is not None:
                    deps.add(ent[0])
                for e2, i2 in ent[1].items():
                    deps.add((e2, i2))
        for key in r:
            tile, sub = self._norm(key)
            ent = tile.st.setdefault(sub, [None, {}])
            ent[1][eng] = idx
        for key in w:
            tile, sub = self._norm(key)
            if sub is None:
                tile.st = {None: [(eng, idx), {}]}
            else:
                tile.st[sub] = [(eng, idx), {}]
        if dma:
            h = self.dma_hist[eng]
            if len(h) >= DMA_K:
                deps.add((eng, h[-DMA_K]))
        final = []
        for (e2, i2) in sorted(deps, reverse=True):
            if e2 == eng and i2 == idx:
                continue
            if self.ops[e2][i2]['dma']:
                if (e2, i2) in self.waited_dma[eng]:
                    continue
                self.waited_dma[eng].add((e2, i2))
                final.append((e2, i2))
            else:
                if e2 == eng and eng == 'pe':
                    continue
                if self.waited[eng].get(e2, -1) >= i2:
                    continue
                self.waited[eng][e2] = i2
                final.append((e2, i2))
        o = dict(fn=fn, deps=final, dma=dma, sig=False)
        if dma:
            o['dma_n'] = self.dcount[eng]
            self.dcount[eng] += 1
            self.dma_hist[eng].append(idx)
        self.ops[eng].append(o)
        return (eng, idx)

    def _sigof(self, e2, i2):
        o = self.ops[e2][i2]
        if o['dma']:
            n = o['dma_n']
            return self.dsems[e2][n % DMA_K], 16 * (n // DMA_K + 1)
        j, v = o['sv']
        return self.csems[e2][j], v

    def flush(self):
        nc = self.nc
        last = {}
        for e in ENGS:
            for i in range(len(self.ops[e]) - 1, self.base[e] - 1, -1):
                if not self.ops[e][i]['dma'] and self.ops[e][i]['fn'] is not None:
                    last[e] = (e, i)
                    break
        dmas = []
        for e in ENGS:
            dmas += [(e, i) for i in self.dma_hist[e][-DMA_K:] if i >= self.base[e]]
        for e in ENGS:
            extra = [v for k, v in last.items() if k != e] + dmas
            self.op(e, None, extra=extra)
        for e in ENGS:
            for o in self.ops[e][self.base[e]:]:
                for (e2, i2) in o['deps']:
                    assert i2 >= self.base[e2], "cross-phase dep"
                    self.ops[e2][i2]['sig'] = True
        for e in ENGS:
            for o in self.ops[e][self.base[e]:]:
                if o['dma']:
                    if e not in self.dsems:
                        self.dsems[e] = [self.es.enter_context(nc.semaphore(f"d_{e}_{j}")) for j in range(DMA_K)]
                    continue
                if o['sig']:
                    c = self.ccount[e]
                    j = c // SEM_LIMIT
                    while len(self.csems[e]) <= j:
                        self.csems[e].append(self.es.enter_context(nc.semaphore(f"c_{e}_{len(self.csems[e])}")))
                    o['sv'] = (j, c % SEM_LIMIT + 1)
                    self.ccount[e] = c + 1

        def run(e, eng):
            for o in self.ops[e][self.base[e]:]:
                for (e2, i2) in o['deps']:
                    s, v = self._sigof(e2, i2)
                    eng.wait_ge(s, v)
                if o['fn'] is None:
                    continue
                ins = o['fn'](eng)
                if o['dma']:
                    n = o['dma_n']
                    ins.then_inc(self.dsems[e][n % DMA_K], 16)
                elif o['sig']:
                    j, v = o['sv']
                    ins.then_inc(self.csems[e][j], 1)

        with nc.Block() as block:
            @block.tensor
            def _(eng):
                run('pe', eng)

            @block.vector
            def _(eng):
                run('dve', eng)

            @block.scalar
            def _(eng):
                run('act', eng)

            @block.gpsimd
            def _(eng):
                run('pool', eng)

            @block.sync
            def _(eng):
                run('sp', eng)
        for e in ENGS:
            self.base[e] = len(self.ops[e])
        for t in self.tiles:
            t.st = {}

D = 1024
S = 2048
NT = 16
NMEM = 256
DEPTH = 4
EPS = 1e-6
FAST_MM = True
BF16 = mybir.dt.bfloat16
F32R = mybir.dt.float32r
R32 = F32R

IN_SHAPES = {
    'x': [S, D], 'mem': [NMEM, D], 'mix_norm': [4, D],
    'even_w_in': [2, D, 2048], 'even_conv_w': [2, 128, 12], 'even_pool_w': [2, 4, 128, 128],
    'even_pool_scale': [2, 128, 4], 'even_w_out': [2, D, D],
    'odd_w_in': [2, D, 3784], 'odd_kv_norm': [2, 128], 'odd_w_uv': [2, 8, 128, 64],
    'odd_hg_norm': [2, 512], 'odd_w_out': [2, D, D], 'hgrn_gamma': [4, 512],
    'rel_bias': [32, 8], 'invc': [128, 2048], 'biasT': [128, 8, 2, 128], 'mem_norm': [1, D], 'xattn_norm': [4, D],
    'xattn_wq': [4, D, D], 'xattn_wkv': [4, D, 2 * D], 'xattn_wo': [4, D, D],
    'peer_norm': [4, D], 'peer_wq': [4, D, 2048], 'peer_keysT': [4, 128, 16, 128],
    'peer_uv': [4, 16384, 2 * D], 'final_norm': [1, D],
}


INVC = [None]


def make_consts():
    parts = {}
    parts['ident'] = np.eye(128, dtype=np.float32)
    t = np.arange(512)
    invc = np.concatenate([1.0 / np.minimum(t + 1, w) for w in (2, 4, 8, 16)]).astype(np.float32)
    INVC[0] = np.ascontiguousarray(np.tile(invc[None, :], (128, 1)).astype(np.float32))
    invc16 = np.concatenate([1.0 / np.minimum(np.arange(16) + 1, w) for w in (2, 4, 8, 16)]).astype(np.float32)
    parts['invc16'] = np.tile(invc16[None, :], (128, 1))
    parts['iota16'] = np.tile(np.arange(16, dtype=np.float32)[None, :], (128, 1))
    ii = np.arange(128)
    parts['U2'] = ((ii[:, None] // 64 == ii[None, :] // 64) & (ii[:, None] <= ii[None, :])).astype(np.float32)
    parts['B2'] = (ii[:, None] // 64 == ii[None, :] // 64).astype(np.float32)
    parts['cmask'] = np.where(ii[None, :] <= ii[:, None], 0.0, -1e30).astype(np.float32)
    parts['ones'] = np.ones((128, 8), dtype=np.float32)
    off = 0
    lay = {}
    arrs = []
    for k, v in parts.items():
        lay[k] = (off, v.shape[1])
        off += v.shape[1]
        arrs.append(v.astype(np.float32))
    return np.ascontiguousarray(np.concatenate(arrs, axis=1)), lay


class Model:
    def __init__(self):
        self.nc = bass.Bass("TRN2", target_bir_lowering=False)
        self.es = ExitStack()
        self.P = Prog(self.nc, self.es)
        P = self.P
        carr, self.clay = make_consts()
        self.carr = carr
        model = self

        class LazyIn(dict):
            def __missing__(self, k):
                shp = list(carr.shape) if k == 'consts' else IN_SHAPES[k]
                t = P.dram(k, shp, F32, kind="ExternalInput")
                self[k] = t
                return t
        self.din = LazyIn()
        self.out = P.dram("out", [S, D], F32, kind="ExternalOutput")
        self.hA = P.dram("hA", [S, D])
        self.hB = P.dram("hB", [S, D])
        self.uvb = P.dram("uvb", [16384, 2 * D], BF16)
        self.PS = P.ps("ps", [128, 8, 512])
        self.C = P.sb("consts_sb", [128, carr.shape[1]], glob=True)
        self.memT = P.sb("memT", [128, 8, NMEM], R32, glob=True)
        self.rotc = {}
        self.uvb_ready = {}
        self.want_peer = {}

    def cst(self, name):
        o, w = self.clay[name]
        return self.C[:, o:o + w]

    def psv(self, b0, nb=2):
        ap = self.PS[:, b0:b0 + nb, :].rearrange("p a b -> p (a b)")
        return ap, [(self.PS, b) for b in range(b0, b0 + nb)]

    def mm(self, out, lhsT, rhs, start, stop, r, w, fast=False):
        if FAST_MM and fast and rhs.shape[-1] % 2 == 0 and out.shape[-1] % 2 == 0:
            lhsT = lhsT.bitcast(F32R)
            rhs = rhs.bitcast(F32R)
        self.P.op('pe', lambda e: e.matmul(out, lhsT, rhs, start=start, stop=stop), r=r, w=w)

    def tr(self, out, in_, r, w):
        ident = self.cst('ident')
        self.P.op('pe', lambda e: e.transpose(out, in_, ident), r=list(r) + [self.C], w=w)

    def act(self, out, in_, func, r, w, bias=None, scale=None, accum=None):
        kw = {}
        if bias is not None:
            kw['bias'] = bias
        if scale is not None:
            kw['scale'] = scale
        if accum is not None:
            kw['accum_out'] = accum
        self.P.op('act', lambda e: e.activation(out, in_, func, **kw), r=r, w=w)

    def tt(self, out, a, b, op, r, w, eng='dve'):
        self.P.op(eng, lambda e: e.tensor_tensor(out, a, b, op), r=r, w=w)

    def ts(self, out, a, s1, s2, op0, op1, r, w, eng='dve', accum=None):
        if op1 is None:
            self.P.op(eng, lambda e: e.tensor_scalar(out, a, s1, None, op0), r=r, w=w)
        elif accum is None:
            self.P.op(eng, lambda e: e.tensor_scalar(out, a, s1, s2, op0, op1), r=r, w=w)
        else:
            self.P.op(eng, lambda e: e.tensor_scalar(out, a, s1, s2, op0, op1, accum_out=accum), r=r, w=w)

    def stt(self, out, a, scalar, b, op0, op1, r, w):
        self.P.op('dve', lambda e: e.scalar_tensor_tensor(out, a, scalar, b, op0, op1), r=r, w=w)

    def copy(self, out, in_, r, w, eng='act'):
        if eng == 'act':
            self.P.op('act', lambda e: e.copy(out, in_), r=r, w=w)
        else:
            self.P.op(eng, lambda e: e.tensor_copy(out, in_), r=r, w=w)

    def dma(self, out, in_, r, w, eng='sp'):
        self.P.op(eng, lambda e: e.dma_start(out=out, in_=in_), r=r, w=w, dma=True)

    def recip(self, out, in_, r, w):
        self.P.op('dve', lambda e: e.reciprocal(out, in_), r=r, w=w)

    def rot(self, name, shape, n=2, dt=F32):
        return [self.P.sb(f"{name}{j}", shape, dt) for j in range(n)]

    def wload(self, dst, src_t, src_ap):
        self.dma(dst[:, :, :], src_ap.rearrange("(k p) c -> p k c", p=128), r=[src_t], w=[dst])

    def wload_r(self, dst, src_t, src_ap, nk=8, cw=128):
        C = src_ap.shape[1]
        if not hasattr(self, '_wst') or self._wst_phase is not self.P.pes:
            self._wst = self.rot("wst", [128, 8, cw], 2)
            self._wst_phase = self.P.pes
            self._wst_n = 0
        for c0 in range(0, C, cw):
            c1 = min(C, c0 + cw)
            st_ = self._wst[self._wst_n % 2]
            eng = ('act', 'dve', 'pool')[self._wst_n % 3]
            self._wst_n += 1
            self.dma(st_[:, 0:nk, 0:c1 - c0], src_ap[:, c0:c1].rearrange("(k p) c -> p k c", p=128), r=[src_t], w=[st_])
            self.copy(dst[:, :, c0:c1], st_[:, 0:nk, 0:c1 - c0], r=[st_], w=[dst], eng=eng)

    def gload(self, dst, src_t, row_ap):
        n = row_ap.shape[1]
        self.dma(dst[:, :], row_ap.broadcast_to([128, n]), r=[src_t], w=[dst])

    def rms(self, x, g, hn, st, width=D):
        self.act(hn[:, :], x[:, :], AF.Square, r=[x], w=[hn, st], accum=st[:, 0:1])
        self.act(st[:, 1:2], st[:, 0:1], AF.Sqrt, r=[st], w=[st], scale=1.0 / width, bias=self.epsb[:, 0:1])
        self.recip(st[:, 1:2], st[:, 1:2], r=[st], w=[st])
        self.stt(hn[:, :], x[:, :], st[:, 1:2], g[:, :], ALU.mult, ALU.mult, r=[x, st, g], w=[hn])

    def transp8(self, src, dstT, b0, n=8):
        nb = (n * 128 + 511) // 512
        pv, pk = self.psv(b0, nb)
        for k in range(n):
            self.tr(pv[:, k * 128:(k + 1) * 128], src[:, k * 128:(k + 1) * 128], r=[src], w=pk)
        self.copy(dstT[:, :, :].rearrange("p a b -> p (a b)"), pv[:, 0:n * 128], r=pk, w=[dstT])

    def phase_setup(self):
        P = self.P
        self.dma(self.C[:, :], self.din['consts'][:, :], r=[self.din['consts']], w=[self.C])
        self.epsb = P.sb("epsb", [128, 1], glob=True)
        P.op('dve', lambda e: e.memset(self.epsb[:, :], EPS), w=[self.epsb])
        g = P.sb("g_mem", [128, D])
        self.gload(g, self.din['mem_norm'], self.din['mem_norm'][0:1, :])
        xs = self.rot("xm", [128, D])
        hn = self.rot("hnm", [128, D])
        st = self.rot("stm", [128, 4])
        mT = [P.sb(f"mT{j}", [128, 8, 128]) for j in range(2)]
        for m in range(2):
            self.dma(xs[m][:, :], self.din['mem'][m * 128:(m + 1) * 128, :], r=[self.din['mem']], w=[xs[m]])
            self.rms(xs[m], g, hn[m], st[m])
            self.transp8(hn[m], mT[m], 2 * m)
            self.copy(self.memT[:, :, m * 128:(m + 1) * 128], mT[m][:, :, :], r=[mT[m]], w=[(self.memT, m)], eng='dve')

    def convert_uv(self, l):
        P = self.P
        din = self.din
        uvb2 = self.uvb[:, :].rearrange("n (a d) -> (n a) d", a=2)
        uvf2 = din['peer_uv'][l].rearrange("n (a d) -> (n a) d", a=2)
        for c in range(16):
            P.op('pool', lambda e, c=c: e.dma_start(out=uvb2[c * 2048:(c + 1) * 2048, :],
                                                    in_=uvf2[c * 2048:(c + 1) * 2048, :]),
                 r=[din['peer_uv']], w=[self.uvb], dma=True)
        self.uvb_ready[l] = True

    def phase_xattn(self, l, src, dst):
        P = self.P
        din = self.din
        if self.want_peer.get(l):
            self.convert_uv(l)
        wq = P.sb("wq", [128, 8, D], R32)
        wo = P.sb("wo", [128, 8, D], R32)
        KT = P.sb("KT", [128, 8, NMEM], R32)
        V = P.sb("V", [128, 2, D], R32)
        g = P.sb("g_x", [128, D])
        wkv = self.rot("wkv", [128, 8, 512], dt=R32)
        self.gload(g, din['xattn_norm'], din['xattn_norm'][l:l + 1, :])
        for c in range(4):
            wc = wkv[c % 2]
            self.wload_r(wc, din['xattn_wkv'], din['xattn_wkv'][l][:, c * 512:(c + 1) * 512])
            if c < 2:
                for f in range(4):
                    fc = c * 4 + f
                    b = fc % 8
                    pv, pk = self.psv(b, 1)
                    for k in range(8):
                        self.mm(pv[:, 0:NMEM], wc[:, k, f * 128:(f + 1) * 128], self.memT[:, k, :],
                                k == 0, k == 7, r=[wc, self.memT], w=pk, fast=True)
                    self.act(KT[:, fc, :], pv[:, 0:NMEM], AF.Copy, r=pk, w=[(KT, fc)], scale=1.0 / 16.0)
            else:
                for m in range(2):
                    b = (c - 2) * 2 + m
                    pv, pk = self.psv(b, 1)
                    for k in range(8):
                        self.mm(pv[:, :], self.memT[:, k, m * 128:(m + 1) * 128], wc[:, k, :],
                                k == 0, k == 7, r=[wc, self.memT], w=pk, fast=True)
                    self.copy(V[:, m, (c - 2) * 512:(c - 1) * 512], pv[:, :], r=pk, w=[(V, (m, c))])
        self.wload_r(wq, din['xattn_wq'], din['xattn_wq'][l])
        self.wload_r(wo, din['xattn_wo'], din['xattn_wo'][l])
        xs = self.rot("x", [128, D])
        hns = self.rot("hn", [128, D])
        hnTs = self.rot("hnT", [128, 8, 128], dt=R32)
        sts = self.rot("st", [128, 16])
        qTs = self.rot("qT", [128, 8, 128], dt=R32)
        Pms = self.rot("Pm", [128, 4 * NMEM])
        PTs = self.rot("PT", [128, 8, 128], dt=R32)
        oTs = self.rot("oT", [128, 8, 128], dt=R32)
        def xfront(i):
                j = i % 2
                x, hn, hnT, st, qT, Pm, PT, oT = xs[j], hns[j], hnTs[j], sts[j], qTs[j], Pms[j], PTs[j], oTs[j]
                self.dma(x[:, :], src[i * 128:(i + 1) * 128, :], r=[(src, i)], w=[x])
                self.rms(x, g, hn, st)
                self.transp8(hn, hnT, 0)
                pv, pk = self.psv(2, 2)
                for f in range(8):
                    for k in range(8):
                        self.mm(pv[:, f * 128:(f + 1) * 128], wq[:, k, f * 128:(f + 1) * 128], hnT[:, k, :],
                                k == 0, k == 7, r=[wq, hnT], w=pk, fast=True)
                self.copy(qT[:, :, :].rearrange("p a b -> p (a b)"), pv[:, :], r=pk, w=[qT])

        def xmid(i):
                j = i % 2
                x, hn, hnT, st, qT, Pm, PT, oT = xs[j], hns[j], hnTs[j], sts[j], qTs[j], Pms[j], PTs[j], oTs[j]
                lv, lk = self.psv(4, 2)
                for hd in range(4):
                    for c in range(2):
                        self.mm(lv[:, hd * 256:(hd + 1) * 256], qT[:, hd * 2 + c, :], KT[:, hd * 2 + c, :],
                                c == 0, c == 1, r=[qT, KT], w=lk, fast=True)
                lv3 = self.PS[:, 4:6, :].rearrange("p a (h m) -> p (a h) m", m=NMEM)
                P.op('dve', lambda e, lv3=lv3, st=st: e.tensor_reduce(out=st[:, 4:8], in_=lv3, axis=AX.X, op=ALU.max, negate=True),
                     r=lk, w=[st])
                for hd in range(4):
                    self.act(Pm[:, hd * 256:(hd + 1) * 256], lv[:, hd * 256:(hd + 1) * 256], AF.Exp, r=lk + [st], w=[Pm, st],
                             bias=st[:, 4 + hd:5 + hd], scale=1.0, accum=st[:, 8 + hd:9 + hd])
                self.recip(st[:, 12:16], st[:, 8:12], r=[st], w=[st])
                for hd in range(4):
                    self.ts(Pm[:, hd * 256:(hd + 1) * 256], Pm[:, hd * 256:(hd + 1) * 256], st[:, 12 + hd:13 + hd], None,
                            ALU.mult, None, r=[Pm, st], w=[Pm])

        def xback(i):
                j = i % 2
                x, hn, hnT, st, qT, Pm, PT, oT = xs[j], hns[j], hnTs[j], sts[j], qTs[j], Pms[j], PTs[j], oTs[j]
                self.transp8(Pm, PT, 6)
                ov, ok = self.psv(0, 2)
                for jj in range(8):
                    for c in range(2):
                        self.mm(ov[:, jj * 128:(jj + 1) * 128], V[:, c, jj * 128:(jj + 1) * 128], PT[:, (jj // 2) * 2 + c, :],
                                c == 0, c == 1, r=[V, PT], w=ok, fast=True)
                self.copy(oT[:, :, :].rearrange("p a b -> p (a b)"), ov[:, :], r=ok, w=[oT])
                yv, yk = self.psv(2, 2)
                for half in range(2):
                    for k in range(8):
                        self.mm(yv[:, half * 512:(half + 1) * 512], oT[:, k, :], wo[:, k, half * 512:(half + 1) * 512],
                                k == 0, k == 7, r=[oT, wo], w=yk, fast=True)
                self.tt(x[:, :], yv[:, :], x[:, :], ALU.add, r=yk + [x], w=[x])
                self.dma(dst[i * 128:(i + 1) * 128, :], x[:, :], r=[x], w=[(dst, i)])


        xfront(0)
        for i in range(NT):
            xmid(i)
            if i + 1 < NT:
                xfront(i + 1)
            xback(i)

    def phase_even(self, l, src, dst):
        P = self.P
        din = self.din
        jx = l // 2
        w_in = P.sb("e_win", [128, 8, 2048], R32)
        w_out = P.sb("e_wout", [128, 8, D], R32)
        pw = P.sb("e_pw", [128, 4, 128], R32)
        pw_st = P.sb("e_pwst", [128, 4, 128])
        cw = P.sb("e_cw", [128, 12])
        psc = P.sb("e_psc", [128, 4])
        g = P.sb("e_g", [128, D])
        self.gload(g, din['mix_norm'], din['mix_norm'][l:l + 1, :])
        self.wload_r(w_in, din['even_w_in'], din['even_w_in'][jx])
        self.wload_r(w_out, din['even_w_out'], din['even_w_out'][jx])
        self.dma(pw_st[:, :, :], din['even_pool_w'][jx].rearrange("g c d -> c g d"), r=[din['even_pool_w']], w=[pw_st])
        self.copy(pw[:, :, :], pw_st[:, :, :], r=[pw_st], w=[pw], eng='dve')
        self.dma(cw[:, :], din['even_conv_w'][jx], r=[din['even_conv_w']], w=[cw])
        self.dma(psc[:, :], din['even_pool_scale'][jx], r=[din['even_pool_scale']], w=[psc])
        cu_halo = P.sb("e_cuh", [128, 4, 2])
        pv_halo = P.sb("e_pvh", [128, 4, 16])
        P.op('pool', lambda e: e.memset(cu_halo[:, :, :], 0.0), w=[cu_halo])
        P.op('pool', lambda e: e.memset(pv_halo[:, :, :], 0.0), w=[pv_halo])
        hnT = P.sb("e_hnT", [128, 8, 512], R32)
        yT = P.sb("e_yT", [128, 8, 512], R32)
        xs = self.rot("e_x", [128, D])
        hns = self.rot("e_hn", [128, D])
        sts = self.rot("e_st", [128, 4])
        hts = self.rot("e_ht", [128, 8, 128])
        cus = self.rot("e_cu", [128, 514])
        pvs = self.rot("e_pv", [128, 528])
        ut = self.rot("e_ut", [128, 512])
        zt = self.rot("e_z", [128, 512])
        sA = self.rot("e_sA", [128, 528])
        sB = self.rot("e_sB", [128, 528])
        pl = self.rot("e_pl", [128, 512], dt=R32)
        invc16 = self.cst('invc16')
        tmp16 = P.sb("e_tmp16", [128, 16])
        for b in range(4):
            for t4 in range(4):
                i = b * 4 + t4
                j = i % 2
                self.dma(xs[j][:, :], src[i * 128:(i + 1) * 128, :], r=[(src, i)], w=[xs[j]])
                self.rms(xs[j], g, hns[j], sts[j])
                self.transp8(hns[j], hts[j], 6)
                self.copy(hnT[:, :, t4 * 128:(t4 + 1) * 128], hts[j][:, :, :], r=[hts[j]], w=[(hnT, t4)], eng='pool')
            for c4 in range(4):
                j = c4 % 2
                cu, pv, u_sb, z = cus[j], pvs[j], ut[j], zt[j]

                def proj(cc, bank):
                    pvw, pk = self.psv(bank, 1)
                    for k in range(8):
                        self.mm(pvw[:, :], w_in[:, k, cc * 128:(cc + 1) * 128], hnT[:, k, :], k == 0, k == 7,
                                r=[w_in, hnT], w=pk, fast=True)
                    return pvw, pk
                pu, ku = proj(c4, 0)
                self.copy(u_sb[:, :], pu[:, :], r=ku, w=[u_sb])
                pg, kg = proj(4 + c4, 1)
                self.copy(cu[:, 0:2], cu_halo[:, c4, :], r=[(cu_halo, c4)], w=[cu], eng='pool')
                self.tt(cu[:, 2:514], pg[:, :], u_sb[:, :], ALU.mult, r=kg + [u_sb, cu], w=[cu])
                self.copy(cu_halo[:, c4, :], cu[:, 512:514], r=[cu], w=[(cu_halo, c4)], eng='pool')
                self.ts(z[:, :], cu[:, 0:512], cw[:, c4 * 3:c4 * 3 + 1], None, ALU.mult, None, r=[cu, cw], w=[z])
                self.stt(z[:, :], cu[:, 1:513], cw[:, c4 * 3 + 1:c4 * 3 + 2], z[:, :], ALU.mult, ALU.add, r=[cu, cw, z], w=[z])
                self.stt(z[:, :], cu[:, 2:514], cw[:, c4 * 3 + 2:c4 * 3 + 3], z[:, :], ALU.mult, ALU.add, r=[cu, cw, z], w=[z])
                pb, kb = proj(8 + c4, 2)
                self.tt(yT[:, c4, :], pb[:, :], z[:, :], ALU.mult, r=kb + [z], w=[(yT, c4)])
                pp, kp = proj(12 + c4, 3)
                self.copy(pv[:, 0:16], pv_halo[:, c4, :], r=[(pv_halo, c4)], w=[pv], eng='pool')
                self.copy(pv[:, 16:528], pp[:, :], r=kp + [pv], w=[pv])
                self.copy(pv_halo[:, c4, :], pv[:, 512:528], r=[pv], w=[(pv_halo, c4)], eng='pool')
                a_, b_ = sA[j], sB[j]
                self.tt(a_[:, 1:528], pv[:, 1:528], pv[:, 0:527], ALU.add, r=[pv], w=[a_])
                cur = a_
                other = b_
                lo = 1
                for st_ in range(c4):
                    sh = 2 ** (st_ + 1)
                    nlo = lo + sh
                    self.tt(other[:, nlo:528], cur[:, nlo:528], cur[:, nlo - sh:528 - sh], ALU.add, r=[cur], w=[other])
                    cur, other = other, cur
                    lo = nlo
                wdw = 2 ** (c4 + 1)
                pool_t = pl[j]
                self.stt(pool_t[:, :], cur[:, 16:528], 1.0 / wdw, pv[:, 16:528], ALU.mult, ALU.subtract, r=[cur, pv], w=[pool_t])
                if b == 0:
                    self.tt(tmp16[:, :], cur[:, 16:32], invc16[:, c4 * 16:(c4 + 1) * 16], ALU.mult, r=[cur, self.C], w=[tmp16])
                    self.tt(pool_t[:, 0:16], tmp16[:, :], pv[:, 16:32], ALU.subtract, r=[tmp16, pv, pool_t], w=[pool_t])
                py, ky = self.psv(3, 1)
                self.mm(py[:, :], pw[:, c4, :], pool_t[:, :], True, True, r=[pw, pool_t], w=ky, fast=True)
                self.act(yT[:, 4 + c4, :], py[:, :], AF.Copy, r=ky + [psc], w=[(yT, 4 + c4)], scale=psc[:, c4:c4 + 1])
            for t4 in range(4):
                i = b * 4 + t4
                j = i % 2
                self.dma(xs[j][:, :], src[i * 128:(i + 1) * 128, :], r=[(src, i)], w=[xs[j]])
                ov, ok = self.psv(4, 2)
                for half in range(2):
                    for k in range(8):
                        self.mm(ov[:, half * 512:(half + 1) * 512], yT[:, k, t4 * 128:(t4 + 1) * 128],
                                w_out[:, k, half * 512:(half + 1) * 512], k == 0, k == 7, r=[yT, w_out], w=ok, fast=True)
                self.tt(xs[j][:, :], ov[:, :], xs[j][:, :], ALU.add, r=ok + [xs[j]], w=[xs[j]])
                self.dma(dst[i * 128:(i + 1) * 128, :], xs[j][:, :], r=[xs[j]], w=[(dst, i)])


    def top16(self, src_ap, work_ap, tv_ap, ti_ap, r, wk):
        P = self.P
        P.op('dve', lambda e: e.max(out=tv_ap[:, 0:8], in_=src_ap), r=r, w=wk)
        P.op('dve', lambda e: e.max_index(out=ti_ap[:, 0:8], in_max=tv_ap[:, 0:8], in_values=src_ap), r=r + wk, w=wk)
        P.op('dve', lambda e: e.match_replace(out=work_ap, in_to_replace=tv_ap[:, 0:8], in_values=src_ap, imm_value=-1e30),
             r=r + wk, w=wk)
        P.op('dve', lambda e: e.max(out=tv_ap[:, 8:16], in_=work_ap), r=wk, w=wk)
        P.op('dve', lambda e: e.max_index(out=ti_ap[:, 8:16], in_max=tv_ap[:, 8:16], in_values=work_ap), r=wk, w=wk)

    def phase_peer(self, l, src, dst):
        P = self.P
        din = self.din
        wq = P.sb("p_wq", [128, 8, 2048], BF16)
        keysT = P.sb("p_keys", [128, 16, 128])
        g = P.sb("p_g", [128, D])
        self.gload(g, din['peer_norm'], din['peer_norm'][l:l + 1, :])
        wst = self.rot("p_wst", [128, 8, 256], 2)
        for c in range(8):
            ws_ = wst[c % 2]
            self.dma(ws_[:, :, :], din['peer_wq'][l][:, c * 256:(c + 1) * 256].rearrange("(k p) c -> p k c", p=128),
                     r=[din['peer_wq']], w=[ws_])
            self.copy(wq[:, :, c * 256:(c + 1) * 256], ws_[:, :, :], r=[ws_], w=[(wq, c)], eng='act' if c % 2 == 0 else 'dve')
        self.dma(keysT[:, :, :], din['peer_keysT'][l], r=[din['peer_keysT']], w=[keysT])
        uv_l = self.uvb
        if not self.uvb_ready.get(l):
            self.convert_uv(l)
        xs = self.rot("p_x", [128, D], 2)
        xns = self.rot("p_xn", [128, D], 2)
        xnT = P.sb("p_xnT", [128, 8, 128], BF16)
        sts = self.rot("p_st", [128, 4], 2)
        qT = P.sb("p_qT", [128, 8, 128])
        s_sb = P.sb("p_s", [128, 1024])
        tv = P.sb("p_tv", [128, 16, 16])
        ti = P.sb("p_ti", [128, 16, 16], U32)
        tif = P.sb("p_tif", [128, 16, 16])
        cand = P.sb("p_cand", [128, 8, 256])
        cwk = P.sb("p_cwk", [128, 8, 256])
        cv = P.sb("p_cv", [128, 8, 16])
        cpos = P.sb("p_cpos", [128, 8, 16], U32)
        ab_u = P.sb("p_abu", [128, 2, 128], U32)
        ab_f = P.sb("p_abf", [128, 2, 128])
        isel = P.sb("p_isel", [128, 2, 128])
        eidf = P.sb("p_eidf", [128, 128])
        eids = self.rot("p_eid", [128, 128], 2, dt=I32)
        ggs = self.rot("p_gg", [128, 8, 16], 2)
        gz = P.sb("p_gz", [128, 16])
        actvs = self.rot("p_act", [128, 128], 2)
        wgts = self.rot("p_wgt", [128, 128], 2)
        GS = 4
        t1s = self.rot("p_t1", [128, GS], 4)
        t2s = self.rot("p_t2", [128, GS], 4)
        junk = P.sb("p_junk", [128, D], BF16)
        NUV = 16
        uvs = self.rot("p_uv", [128, 2 * D], NUV, dt=BF16)
        dgs = self.rot("p_dg", [128, 128], 4, dt=BF16)
        iota16 = self.cst('iota16')
        ident = self.cst('ident')
        cwkf = cwk[:, :, :].rearrange("p a b -> p (a b)")
        candf = cand[:, :, :].rearrange("p a b -> p (a b)")

        def front(i):
            x, xn, st = xs[i % 2], xns[i % 2], sts[i % 2]
            eid, gg = eids[i % 2], ggs[i % 2]
            self.dma(x[:, :], src[i * 128:(i + 1) * 128, :], r=[(src, i)], w=[x])
            self.rms(x, g, xn, st)
            yield
            self.transp8(xn, xnT, 0)
            yield
            for hf in range(2):
                qv, qk = self.psv(2, 2)
                for hh in range(8):
                    hp = hf * 8 + hh
                    for k in range(8):
                        self.mm(qv[:, hh * 128:(hh + 1) * 128], wq[:, k, hp * 128:(hp + 1) * 128], xnT[:, k, :],
                                k == 0, k == 7, r=[wq, xnT], w=qk, fast=False)
                    if hh % 2 == 1:
                        yield
                self.copy(qT[:, :, :].rearrange("p a b -> p (a b)"), qv[:, :], r=qk, w=[qT])
                sv, sk = self.psv(0, 2)
                for hh in range(8):
                    hp = hf * 8 + hh
                    self.mm(sv[:, hh * 128:(hh + 1) * 128], qT[:, hh, :], keysT[:, hp, :], True, True, r=[qT, keysT], w=sk)
                self.copy(s_sb[:, :], sv[:, :], r=sk, w=[s_sb])
                yield
                for hh in range(8):
                    hp = hf * 8 + hh
                    self.top16(s_sb[:, hh * 128:(hh + 1) * 128], cwkf[:, hh * 128:(hh + 1) * 128], tv[:, hp, :], ti[:, hp, :],
                               r=[s_sb], wk=[(tv, hp), (ti, hp), (cwk, hh)])
                    yield
            self.copy(tif[:, :, :], ti[:, :, :], r=[ti], w=[tif], eng='dve')
            tv4 = tv[:, :, :].rearrange("p (h t) a -> p h t a", t=2)
            tif4 = tif[:, :, :].rearrange("p (h t) a -> p h t a", t=2)
            cand4 = cand[:, :, :].rearrange("p h (a b) -> p h a b", b=16)
            self.tt(cand4, bcast(tv4[:, :, 0, :], 3, 16), bcast(tv4[:, :, 1, :], 2, 16), ALU.add, r=[tv], w=[cand])
            yield
            for h in range(8):
                self.top16(cand[:, h, :], cwk[:, h, :], cv[:, h, :], cpos[:, h, :],
                           r=[cand], wk=[(cv, h), (cpos, h), (cwk, h)])
                yield
            cposf = cpos[:, :, :].rearrange("p h a -> p (h a)")
            P.op('dve', lambda e: e.tensor_single_scalar(ab_u[:, 0, :], cposf, 4, ALU.logical_shift_right), r=[cpos], w=[ab_u])
            P.op('dve', lambda e: e.tensor_single_scalar(ab_u[:, 1, :], cposf, 15, ALU.bitwise_and), r=[cpos, ab_u], w=[ab_u])
            self.copy(ab_f[:, :, :], ab_u[:, :, :], r=[ab_u], w=[ab_f], eng='dve')
            yield
            eqv = candf[:, 0:1024].rearrange("p (m a) -> p m a", a=16)
            for t in range(2):
                for hh in range(2):
                    self.tt(eqv, bcast(ab_f[:, t, hh * 64:(hh + 1) * 64], 2, 16), bcast(iota16, 1, 64), ALU.is_equal,
                            r=[ab_f, self.C], w=[cand])
                    eq4 = candf[:, 0:1024].rearrange("p (h j a) -> p h j a", j=16, a=16)
                    self.tt(eq4, eq4, bcast(tif4[:, hh * 4:(hh + 1) * 4, t, :], 2, 16), ALU.mult, r=[cand, tif], w=[cand])
                    P.op('dve', lambda e, t=t, hh=hh: e.tensor_reduce(out=isel[:, t, hh * 64:(hh + 1) * 64], in_=eqv,
                                                                      axis=AX.X, op=ALU.add), r=[cand], w=[isel])
                    yield
            self.stt(eidf[:, :], isel[:, 0, :], 128.0, isel[:, 1, :], ALU.mult, ALU.add, r=[isel], w=[eidf])
            self.copy(eid[:, :], eidf[:, :], r=[eidf], w=[eid], eng='dve')
            yield
            self.tt(gg[:, :, :], cv[:, :, :], bcast(cv[:, :, 0], 2, 16), ALU.subtract, r=[cv], w=[gg])
            self.act(gg[:, :, :], gg[:, :, :], AF.Exp, r=[gg], w=[gg])
            P.op('dve', lambda e: e.tensor_reduce(out=gz[:, 0:8], in_=gg[:, :, :], axis=AX.X, op=ALU.add), r=[gg], w=[gz])
            self.recip(gz[:, 8:16], gz[:, 0:8], r=[gz], w=[gz])
            self.tt(gg[:, :, :], gg[:, :, :], bcast(gz[:, 8:16], 2, 16), ALU.mult, r=[gg, gz], w=[gg])
            yield

        def uvstage(i):
            x, xn, eid, gg = xs[i % 2], xns[i % 2], eids[i % 2], ggs[i % 2]
            actv, wgt = actvs[i % 2], wgts[i % 2]
            ggf = gg[:, :, :].rearrange("p h a -> p (h a)")
            xpv, xpk = self.psv(4, 2)
            av, ak = self.psv(6, 2)
            self.copy(xpv[:, :], xn[:, :], r=[xn], w=xpk)
            NGRP = 128 // GS

            def stA(gi):
                g0 = gi * GS
                for jj in range(g0, g0 + GS):
                    uv = uvs[jj % NUV]
                    P.op('pool', lambda e, uv=uv, jj=jj: e.indirect_dma_start(
                        out=uv[:, :], out_offset=None, in_=uv_l[:, :],
                        in_offset=bass.IndirectOffsetOnAxis(ap=eid[:, jj:jj + 1], axis=0)),
                        r=[eid, self.uvb], w=[uv], dma=True)
                    P.op('dve', lambda e, uv=uv, jj=jj: e.scalar_tensor_tensor(
                        junk[:, :], uv[:, 0:D], 1.0, xpv[:, :], ALU.mult, ALU.mult, accum_out=actv[:, jj:jj + 1]),
                        r=[uv] + xpk, w=[(actv, jj)])
                a_ = actv[:, g0:g0 + GS]
                ak_ = [(actv, jj) for jj in range(g0, g0 + GS)]
                t1, t2 = t1s[gi % 4], t2s[gi % 4]
                self.act(t1[:, :], a_, AF.Square, r=ak_, w=[t1], scale=0.044715 ** 0.5)
                self.stt(t1[:, :], t1[:, :], 1.0, a_, ALU.add, ALU.mult, r=[t1] + ak_, w=[t1])
                self.act(t2[:, :], t1[:, :], AF.Sigmoid, r=[t1], w=[t2], scale=1.5957691216057308)

            def stC(gi):
                g0 = gi * GS
                a_ = actv[:, g0:g0 + GS]
                ak_ = [(actv, jj) for jj in range(g0, g0 + GS)]
                t2 = t2s[gi % 4]
                self.tt(t2[:, :], t2[:, :], a_, ALU.mult, r=[t2] + ak_, w=[t2])
                self.tt(wgt[:, g0:g0 + GS], t2[:, :], ggf[:, g0:g0 + GS], ALU.mult, r=[t2, gg], w=[(wgt, gi)])

            def stD(gi):
                g0 = gi * GS
                for jj in range(g0, g0 + GS):
                    uv = uvs[jj % NUV]
                    dg = dgs[jj % 4]
                    self.ts(dg[:, :], ident, wgt[:, jj:jj + 1], 1.0, ALU.mult, ALU.mult, r=[self.C, (wgt, gi)], w=[dg], eng='pool')
                    for half in range(2):
                        self.mm(av[:, half * 512:(half + 1) * 512], dg[:, :], uv[:, D + half * 512:D + (half + 1) * 512],
                                jj == 0, jj == 127, r=[dg, uv], w=[ak[half]], fast=False)

            for gi in range(NGRP + 2):
                if gi < NGRP:
                    stA(gi)
                if 1 <= gi <= NGRP:
                    stC(gi - 1)
                if gi >= 2:
                    stD(gi - 2)
                yield
            self.tt(x[:, :], av[:, :], x[:, :], ALU.add, r=ak + [x], w=[x])
            self.dma(dst[i * 128:(i + 1) * 128, :], x[:, :], r=[x], w=[(dst, i)])
            yield

        for _ in front(0):
            pass
        for it in range(NT):
            active = [uvstage(it)]
            if it + 1 < NT:
                active.append(front(it + 1))
            while active:
                for gen in list(active):
                    try:
                        next(gen)
                    except StopIteration:
                        active.remove(gen)

    def phase_dsa(self, l, src, dst):
        P = self.P
        din = self.din
        jx = l // 2
        w = P.sb("d_w", [128, 8, 1736])
        w_uv = P.sb("d_wuv", [128, 8, 64])
        w_out = P.sb("d_wout", [128, 4, D])
        g = P.sb("d_g", [128, D])
        gkv = P.sb("d_gkv", [128, 128])
        b31 = P.sb("d_b31", [128, 16])
        corrT = P.sb("d_corr", [128, 8, 2, 128])
        self.gload(g, din['mix_norm'], din['mix_norm'][l:l + 1, :])
        self.gload(gkv, din['odd_kv_norm'], din['odd_kv_norm'][jx:jx + 1, :])
        self.gload(b31, din['rel_bias'], din['rel_bias'][31:32, :]) if False else self.dma(
            b31[:, 0:8], din['rel_bias'][31:32, :].broadcast_to([128, 8]), r=[din['rel_bias']], w=[b31])
        self.ts(b31[:, 8:16], b31[:, 0:8], -1.0, None, ALU.mult, None, r=[b31], w=[b31])
        self.dma(corrT[:, :, :, :], din['biasT'][:, :, :, :], r=[din['biasT']], w=[corrT])
        for h in range(8):
            self.act(corrT[:, h, :, :], corrT[:, h, :, :], AF.Exp, r=[corrT, b31], w=[corrT], bias=b31[:, 8 + h:9 + h], scale=1.0)
        self.dma(w[:, :, :], din['odd_w_in'][jx][:, 0:1736].rearrange("(k p) c -> p k c", p=128), r=[din['odd_w_in']], w=[w])
        self.dma(w_uv[:, :, :], din['odd_w_uv'][jx].rearrange("h r e -> r h e"), r=[din['odd_w_uv']], w=[w_uv])
        self.dma(w_out[:, :, :], din['odd_w_out'][jx][0:512, :].rearrange("(k p) c -> p k c", p=128), r=[din['odd_w_out']], w=[w_out])
        cT = P.sb("d_cT", [128, S], R32)
        c_tm = P.sb("d_ctm", [128, NT, 128], R32)
        ikT = P.sb("d_ikT", [64, S], R32)
        xs = self.rot("d_x", [128, D])
        hn = P.sb("d_hn", [128, D])
        hnT = P.sb("d_hnT", [128, 8, 128])
        st = P.sb("d_st", [128, 8])
        craw = P.sb("d_craw", [128, 128])
        qlTs = self.rot("d_qlT", [128, 8, 128], 2, dt=R32)
        iqT = P.sb("d_iqT", [64, 8, 128], R32)
        iw = P.sb("d_iw", [128, 8])
        score = P.sb("d_score", [128, S])
        work = P.sb("d_work", [128, S])
        maskT = P.sb("d_maskT", [128, NT, 128])
        rsb = self.rot("d_r", [128, 512])
        m8 = self.rot("d_m8", [128, 8])
        ETs = self.rot("d_ET", [128, NT, 128], 2, dt=R32)
        latT = P.sb("d_latT", [128, 8, 128])
        zz = P.sb("d_zz", [128, 16])
        yc = P.sb("d_yc", [128, 8, 64])
        ycT = P.sb("d_ycT", [128, 4, 128])
        ones = self.cst('ones')
        cmask = self.cst('cmask')
        CK, CQ, CIK, CIW = 1024, 1152, 1664, 1728
        def projpart(i):
            x = xs[i % 2]
            qlT = qlTs[i % 2]
            nk = (i + 1) * 128
            self.dma(x[:, :], src[i * 128:(i + 1) * 128, :], r=[(src, i)], w=[x])
            self.rms(x, g, hn, st)
            self.transp8(hn, hnT, 4)
            pv, pk = self.psv(6, 1)
            for k in range(8):
                self.mm(pv[:, 0:128], hnT[:, k, :], w[:, k, CK:CK + 128], k == 0, k == 7, r=[hnT, w], w=pk)
            self.copy(craw[:, :], pv[:, 0:128], r=pk, w=[craw])
            self.act(work[:, 0:128], craw[:, :], AF.Square, r=[craw], w=[work, st], accum=st[:, 2:3])
            self.act(st[:, 3:4], st[:, 2:3], AF.Sqrt, r=[st], w=[st], scale=1.0 / 128, bias=self.epsb[:, 0:1])
            self.recip(st[:, 3:4], st[:, 3:4], r=[st], w=[st])
            self.stt(c_tm[:, i, :], craw[:, :], st[:, 3:4], gkv[:, :], ALU.mult, ALU.mult, r=[craw, st, gkv], w=[(c_tm, i)])
            pv7, pk7 = self.psv(7, 1)
            self.tr(pv7[:, 0:128], c_tm[:, i, :].bitcast(F32), r=[(c_tm, i)], w=pk7)
            self.copy(cT[:, i * 128:(i + 1) * 128], pv7[:, 0:128], r=pk7, w=[(cT, i)])
            for k in range(8):
                self.mm(pv[0:64, 0:128], w[:, k, CIK:CIK + 64], hnT[:, k, :], k == 0, k == 7, r=[hnT, w], w=pk)
            self.act(ikT[:, i * 128:(i + 1) * 128], pv[0:64, 0:128], AF.Copy, r=pk, w=[(ikT, i)], scale=0.125)
            for k in range(8):
                self.mm(pv7[:, 0:8], hnT[:, k, :], w[:, k, CIW:CIW + 8], k == 0, k == 7, r=[hnT, w], w=pk7)
            self.act(iw[:, :], pv7[:, 0:8], AF.Copy, r=pk7, w=[iw], scale=8 ** -0.5)
            qv, qk = self.psv(0, 2)
            for h in range(8):
                for k in range(8):
                    self.mm(qv[:, h * 128:(h + 1) * 128], w[:, k, h * 128:(h + 1) * 128], hnT[:, k, :], k == 0, k == 7,
                            r=[hnT, w], w=qk)
            self.act(qlT[:, :, :].rearrange("p a b -> p (a b)"), qv[:, :], AF.Copy, r=qk, w=[qlT], scale=128 ** -0.5)
            iv, ik_ = self.psv(2, 2)
            for h in range(8):
                for k in range(8):
                    self.mm(iv[0:64, h * 128:(h + 1) * 128], w[:, k, CQ + h * 64:CQ + (h + 1) * 64], hnT[:, k, :], k == 0, k == 7,
                            r=[hnT, w], w=ik_)
            self.copy(iqT[:, :, :].rearrange("p a b -> p (a b)"), iv[0:64, :], r=ik_, w=[iqT])
        projpart(0)
        for i in range(NT):
            x = xs[i % 2]
            qlT = qlTs[i % 2]
            nk = (i + 1) * 128
            cnt = 0
            for c0 in range(0, nk, 512):
                cw = min(512, nk - c0)
                for h in range(8):
                    sv, sk = self.psv(4 + cnt % 2, 1)
                    r_ = rsb[cnt % 2]
                    cnt += 1
                    self.mm(sv[:, 0:cw], iqT[:, h, :], ikT[:, c0:c0 + cw], True, True, r=[iqT, ikT], w=sk, fast=True)
                    self.act(r_[:, 0:cw], sv[:, 0:cw], AF.Relu, r=sk, w=[r_])
                    if h == 0:
                        self.ts(score[:, c0:c0 + cw], r_[:, 0:cw], iw[:, 0:1], None, ALU.mult, None, r=[r_, iw], w=[score])
                    else:
                        self.stt(score[:, c0:c0 + cw], r_[:, 0:cw], iw[:, h:h + 1], score[:, c0:c0 + cw], ALU.mult, ALU.add,
                                 r=[r_, iw, score], w=[score])
            if i + 1 < NT:
                projpart(i + 1)
            self.tt(score[:, i * 128:nk], score[:, i * 128:nk], cmask, ALU.add, r=[score, self.C], w=[score])
            if i >= 2:
                cur = score
                for rnd in range(32):
                    m = m8[rnd % 2]
                    P.op('dve', lambda e, m=m, cur=cur, nk=nk: e.max(out=m[:, :], in_=cur[:, 0:nk]), r=[cur], w=[m])
                    if rnd < 31:
                        P.op('dve', lambda e, m=m, cur=cur, nk=nk: e.match_replace(
                            out=work[:, 0:nk], in_to_replace=m[:, :], in_values=cur[:, 0:nk], imm_value=-1e30),
                            r=[cur, m], w=[work])
                        cur = work
                self.ts(work[:, 0:nk], score[:, 0:nk], m8[1][:, 7:8], None, ALU.is_ge, None, r=[score, m8[1]], w=[work])
            else:
                self.ts(work[:, 0:nk], score[:, 0:nk], -1e29, None, ALU.is_ge, None, r=[score], w=[work])
            for kt in range(i + 1):
                b = 6 + (kt // 4) % 2
                mv, mk = self.psv(b, 1)
                self.tr(mv[:, (kt % 4) * 128:(kt % 4 + 1) * 128], work[:, kt * 128:(kt + 1) * 128], r=[work], w=mk)
                if kt % 4 == 3 or kt == i:
                    k0 = (kt // 4) * 4
                    n = kt - k0 + 1
                    self.copy(maskT[:, k0:kt + 1, :].rearrange("p a b -> p (a b)"), mv[:, 0:n * 128], r=mk, w=[maskT], eng='pool' if False else 'act')
            lv, lk = self.psv(0, 4)
            av, ak = self.psv(6, 2)
            zv, zk = self.psv(5, 1)
            def lg(h):
                ET = ETs[h % 2]
                for kt in range(i + 1):
                    self.mm(lv[:, kt * 128:(kt + 1) * 128], cT[:, kt * 128:(kt + 1) * 128], qlT[:, h, :], True, True,
                            r=[cT, qlT], w=lk, fast=True)
                ETf = ET[:, :, :].rearrange("p a b -> p (a b)")
                self.act(ETf[:, 0:nk], lv[:, 0:nk], AF.Exp, r=lk + [b31], w=[ET], bias=b31[:, h:h + 1], scale=1.0)

            def pvh(h):
                ET = ETs[h % 2]
                ETf = ET[:, :, :].rearrange("p a b -> p (a b)")
                ETf32 = ET[:, :, :].bitcast(F32).rearrange("p a b -> p (a b)")
                self.tt(ETf[:, 0:nk], ETf32[:, 0:nk], maskT[:, :, :].rearrange("p a b -> p (a b)")[:, 0:nk], ALU.mult,
                        r=[ET, maskT], w=[ET])
                for kt in range(max(0, i - 1), i + 1):
                    self.tt(ET[:, kt, :], ET[:, kt, :].bitcast(F32), corrT[:, h, i - kt, :], ALU.mult, r=[ET, corrT], w=[ET])
                for kt in range(i + 1):
                    self.mm(av[:, h * 128:(h + 1) * 128], c_tm[:, kt, :], ET[:, kt, :], kt == 0, kt == i, r=[c_tm, ET], w=ak, fast=True)
                    self.mm(zv[:, h:h + 1], ET[:, kt, :].bitcast(F32), ones[:, 0:1], kt == 0, kt == i, r=[ET, self.C], w=zk)

            lg(0)
            for h in range(8):
                if h + 1 < 8:
                    lg(h + 1)
                pvh(h)
            self.copy(latT[:, :, :].rearrange("p a b -> p (a b)"), av[:, :], r=ak, w=[latT])
            self.copy(zz[:, 0:8], zv[:, 0:8], r=zk, w=[zz], eng='dve')
            self.recip(zz[:, 8:16], zz[:, 0:8], r=[zz], w=[zz])
            yv, yk = self.psv(4, 1)
            for h in range(8):
                self.mm(yv[:, h * 64:(h + 1) * 64], latT[:, h, :], w_uv[:, h, :], True, True, r=[latT, w_uv], w=yk)
            self.tt(yc[:, :, :], yv[:, :].rearrange("p (h e) -> p h e", e=64), bcast(zz[:, 8:16], 2, 64), ALU.mult,
                    r=yk + [zz], w=[yc])
            tv_, tk_ = self.psv(5, 1)
            ycf = yc[:, :, :].rearrange("p h e -> p (h e)")
            for k in range(4):
                self.tr(tv_[:, k * 128:(k + 1) * 128], ycf[:, k * 128:(k + 1) * 128], r=[yc], w=tk_)
            self.copy(ycT[:, :, :].rearrange("p a b -> p (a b)"), tv_[:, :], r=tk_, w=[ycT])
            ov, ok = self.psv(0, 2)
            for half in range(2):
                for k in range(4):
                    self.mm(ov[:, half * 512:(half + 1) * 512], ycT[:, k, :], w_out[:, k, half * 512:(half + 1) * 512],
                            k == 0, k == 3, r=[ycT, w_out], w=ok)
            self.tt(x[:, :], ov[:, :], x[:, :], ALU.add, r=ok + [x], w=[x])
            self.dma(dst[i * 128:(i + 1) * 128, :], x[:, :], r=[x], w=[(dst, i)])


    def phase_hgrn(self, l, src, acc):
        P = self.P
        din = self.din
        jx = l // 2
        w = P.sb("h_w", [128, 8, 2048])
        w_out = P.sb("h_wout", [128, 4, D])
        g = P.sb("h_g", [128, D])
        gn = P.sb("h_gn", [128, 512])
        gam = P.sb("h_gam", [128, 4, 512])
        lb = P.sb("h_lb", [128, 512])
        oml = P.sb("h_oml", [128, 512])
        lbT = P.sb("h_lbT", [128, 8])
        tmp = P.sb("h_tmp", [128, 512])
        self.gload(g, din['mix_norm'], din['mix_norm'][l:l + 1, :])
        self.gload(gn, din['odd_hg_norm'], din['odd_hg_norm'][jx:jx + 1, :])
        self.dma(w[:, :, :], din['odd_w_in'][jx][:, 1736:3784].rearrange("(k p) c -> p k c", p=128), r=[din['odd_w_in']], w=[w])
        self.dma(w_out[:, :, :], din['odd_w_out'][jx][512:1024, :].rearrange("(k p) c -> p k c", p=128), r=[din['odd_w_out']], w=[w_out])
        for ll in range(4):
            self.dma(gam[:, ll, :], din['hgrn_gamma'][ll:ll + 1, :].broadcast_to([128, 512]), r=[din['hgrn_gamma']], w=[(gam, ll)])
        self.act(gam[:, :, :], gam[:, :, :], AF.Exp, r=[gam], w=[gam])
        self.tt(tmp[:, :], gam[:, 0, :], gam[:, 1, :], ALU.add, r=[gam], w=[tmp])
        self.tt(tmp[:, :], tmp[:, :], gam[:, 2, :], ALU.add, r=[gam, tmp], w=[tmp])
        self.tt(tmp[:, :], tmp[:, :], gam[:, 3, :], ALU.add, r=[gam, tmp], w=[tmp])
        self.recip(tmp[:, :], tmp[:, :], r=[tmp], w=[tmp])
        P.op('dve', lambda e: e.memset(lb[:, :], 0.0), w=[lb])
        for ll in range(l):
            self.tt(lb[:, :], lb[:, :], gam[:, ll, :], ALU.add, r=[lb, gam], w=[lb])
        self.tt(lb[:, :], lb[:, :], tmp[:, :], ALU.mult, r=[lb, tmp], w=[lb])
        self.ts(oml[:, :], lb[:, :], -1.0, 1.0, ALU.mult, ALU.add, r=[lb], w=[oml])
        pv, pk = self.psv(0, 1)
        for h in range(4):
            self.tr(pv[:, h * 128:(h + 1) * 128], lb[:, h * 128:(h + 1) * 128], r=[lb], w=pk)
        for h in range(4):
            self.copy(lbT[:, h:h + 1], pv[:, h * 128:h * 128 + 1], r=pk, w=[lbT], eng='dve')
        self.ts(lbT[:, 4:8], lbT[:, 0:4], -1.0, 1.0, ALU.mult, ALU.add, r=[lbT], w=[lbT])
        Sst = [P.sb(f"h_S{j}", [128, 4, 128]) for j in range(2)]
        qt0 = P.sb("h_qt0", [128, 4, 128])
        qt1 = P.sb("h_qt1", [128, 4, 128])
        kh0 = P.sb("h_kh0", [128, 512])
        kh1 = P.sb("h_kh1", [128, 512])
        P.op('pool', lambda e: e.memset(Sst[0][:, :, :], 0.0), w=[Sst[0]])
        P.op('pool', lambda e: e.memset(qt0[:, :, :], 0.0), w=[qt0])
        P.op('pool', lambda e: e.memset(qt1[:, :, :], 0.0), w=[qt1])
        P.op('pool', lambda e: e.memset(kh0[:, :], 0.0), w=[kh0])
        P.op('pool', lambda e: e.memset(kh1[:, :], 0.0), w=[kh1])
        xs = self.rot("h_x", [128, D])
        hn = P.sb("h_hn", [128, D])
        hnT = P.sb("h_hnT", [128, 8, 128])
        st = P.sb("h_st", [128, 16])
        sg = P.sb("h_sg", [128, 512])
        f_tm = P.sb("h_f", [128, 512])
        lf = P.sb("h_lf", [128, 512])
        kk = P.sb("h_kk", [128, 512])
        i_sb = P.sb("h_i", [128, 512])
        sil = P.sb("h_sil", [128, 512])
        sgT = P.sb("h_sgT", [128, 4, 128])
        kkT = P.sb("h_kkT", [128, 4, 128])
        eAT = P.sb("h_eAT", [128, 4, 128])
        enAT = P.sb("h_enAT", [128, 4, 128])
        qtT = P.sb("h_qtT", [128, 4, 128])
        ktT = P.sb("h_ktT", [128, 4, 128])
        a_sb = P.sb("h_a", [128, 512])
        d_sb = P.sb("h_d", [128, 512])
        sc = self.rot("h_sc", [128, 128])
        y_sb = P.sb("h_y", [128, 512])
        yT = P.sb("h_yT", [128, 4, 128])
        acc_t = self.rot("h_acc", [128, D])
        U2 = self.cst('U2')
        B2 = self.cst('B2')
        HQ, HF, HI, HG = 0, 512, 1024, 1536

        def proj_tm(c0, bank):
            pvw, pkw = self.psv(bank, 1)
            for k in range(8):
                self.mm(pvw[:, :], hnT[:, k, :], w[:, k, c0:c0 + 512], k == 0, k == 7, r=[hnT, w], w=pkw)
            return pvw, pkw

        def proj_fm(c0, bank):
            pvw, pkw = self.psv(bank, 1)
            for h in range(4):
                for k in range(8):
                    self.mm(pvw[:, h * 128:(h + 1) * 128], w[:, k, c0 + h * 128:c0 + (h + 1) * 128], hnT[:, k, :],
                            k == 0, k == 7, r=[hnT, w], w=pkw)
            return pvw, pkw

        for i in range(NT):
            x = xs[i % 2]
            self.dma(x[:, :], src[i * 128:(i + 1) * 128, :], r=[(src, i)], w=[x])
            self.rms(x, g, hn, st)
            self.transp8(hn, hnT, 0)
            pf, kf = proj_tm(HF, 2)
            self.act(sg[:, :], pf[:, :], AF.Sigmoid, r=kf, w=[sg])
            self.tt(f_tm[:, :], sg[:, :], oml[:, :], ALU.mult, r=[sg, oml], w=[f_tm])
            self.tt(f_tm[:, :], f_tm[:, :], lb[:, :], ALU.add, r=[f_tm, lb], w=[f_tm])
            self.act(lf[:, :], f_tm[:, :], AF.Ln, r=[f_tm], w=[lf])
            self.ts(kk[:, :], f_tm[:, :], -1.0, 1.0, ALU.mult, ALU.add, r=[f_tm], w=[kk])
            pi_, ki_ = proj_tm(HI, 3)
            self.copy(i_sb[:, :], pi_[:, :], r=ki_, w=[i_sb])
            pg_, kg_ = proj_tm(HG, 4)
            self.act(sil[:, :], pg_[:, :], AF.Sigmoid, r=kg_, w=[sil])
            self.tt(sil[:, :], sil[:, :], pg_[:, :], ALU.mult, r=[sil] + kg_, w=[sil])
            pq, kq = proj_fm(HQ, 5)
            pfT, kfT = proj_fm(HF, 6)
            self.act(sgT[:, :, :].rearrange("p a b -> p (a b)"), pfT[:, :], AF.Sigmoid, r=kfT, w=[sgT])
            for h in range(4):
                self.ts(kkT[:, h, :], sgT[:, h, :], lbT[:, 4 + h:5 + h], lbT[:, h:h + 1], ALU.mult, ALU.add, r=[sgT, lbT], w=[kkT])
            self.ts(kkT[:, :, :], kkT[:, :, :], -1.0, 1.0, ALU.mult, ALU.add, r=[kkT], w=[kkT])
            pA, kA = self.psv(2, 1)
            self.mm(pA[:, :], U2, lf[:, :], True, True, r=[self.C, lf], w=kA)
            self.copy(a_sb[:, :], pA[:, :], r=kA, w=[a_sb])
            pE, kE = self.psv(3, 1)
            self.mm(pE[:, :], B2, lf[:, :], True, True, r=[self.C, lf], w=kE)
            pAT, kAT = self.psv(4, 1)
            for h in range(4):
                self.mm(pAT[:, h * 128:(h + 1) * 128], lf[:, h * 128:(h + 1) * 128], U2, True, True, r=[self.C, lf], w=kAT)
            self.act(eAT[:, :, :].rearrange("p a b -> p (a b)"), pAT[:, :], AF.Exp, r=kAT, w=[eAT])
            self.act(enAT[:, :, :].rearrange("p a b -> p (a b)"), pAT[:, :], AF.Exp, r=kAT, w=[enAT], scale=-1.0)
            self.tt(qtT[:, :, :].rearrange("p a b -> p (a b)"), pq[:, :], eAT[:, :, :].rearrange("p a b -> p (a b)"), ALU.mult,
                    r=kq + [eAT], w=[qtT])
            self.tt(ktT[:, :, :], kkT[:, :, :], enAT[:, :, :], ALU.mult, r=[kkT, enAT], w=[ktT])
            self.copy(qt0[:, :, 0:64], qtT[:, :, 0:64], r=[qtT], w=[qt0], eng='pool')
            self.copy(qt1[:, :, 64:128], qtT[:, :, 64:128], r=[qtT], w=[qt1], eng='pool')
            self.tt(d_sb[:, :], pE[:, :], a_sb[:, :], ALU.subtract, r=kE + [a_sb], w=[d_sb])
            self.act(d_sb[:, :], d_sb[:, :], AF.Exp, r=[d_sb], w=[d_sb])
            self.tt(kh0[0:64, :], d_sb[0:64, :], kk[0:64, :], ALU.mult, r=[d_sb, kk], w=[kh0])
            self.tt(kh1[64:128, :], d_sb[64:128, :], kk[64:128, :], ALU.mult, r=[d_sb, kk], w=[kh1])
            po, ko = self.psv(7, 1)
            for h in range(4):
                hs = slice(h * 128, (h + 1) * 128)
                S0, S1 = Sst[0], Sst[1]
                ps_, ks_ = self.psv(0, 1)
                self.mm(ps_[:, 0:128], ktT[:, h, :], qtT[:, h, :], True, True, r=[ktT, qtT], w=ks_)
                scb = sc[h % 2]
                self.tt(scb[:, :], ps_[:, 0:128], U2, ALU.mult, r=ks_ + [self.C], w=[scb])
                self.mm(po[:, hs], scb[:, :], i_sb[:, hs], True, False, r=[scb, i_sb], w=ko)
                self.mm(po[:, hs], qt0[:, h, :], S0[:, h, :], False, False, r=[qt0, (S0, h)], w=ko)
                p1, k1 = self.psv(1, 1)
                self.mm(p1[:, 0:128], kh0[:, hs], i_sb[:, hs], True, True, r=[kh0, i_sb], w=k1)
                self.stt(S1[:, h, :], S0[:, h, :], eAT[:, h, 63:64], p1[:, 0:128], ALU.mult, ALU.add, r=[(S0, h), eAT] + k1, w=[(S1, h)])
                self.mm(po[:, hs], qt1[:, h, :], S1[:, h, :], False, True, r=[qt1, (S1, h)], w=ko)
                p2, k2 = self.psv(2, 1)
                self.mm(p2[:, 0:128], kh1[:, hs], i_sb[:, hs], True, True, r=[kh1, i_sb], w=k2)
                self.stt(S0[:, h, :], S1[:, h, :], eAT[:, h, 127:128], p2[:, 0:128], ALU.mult, ALU.add, r=[(S1, h), eAT] + k2, w=[(S0, h)])
            for h in range(4):
                self.act(y_sb[:, h * 128:(h + 1) * 128], po[:, h * 128:(h + 1) * 128], AF.Square, r=ko, w=[y_sb, st], accum=st[:, 4 + h:5 + h])
            self.act(st[:, 8:12], st[:, 4:8], AF.Sqrt, r=[st], w=[st], scale=1.0 / 128, bias=self.epsb[:, 0:1])
            self.recip(st[:, 8:12], st[:, 8:12], r=[st], w=[st])
            for h in range(4):
                self.ts(y_sb[:, h * 128:(h + 1) * 128], po[:, h * 128:(h + 1) * 128], st[:, 8 + h:9 + h], None, ALU.mult, None,
                        r=ko + [st], w=[y_sb])
            self.tt(y_sb[:, :], y_sb[:, :], gn[:, :], ALU.mult, r=[y_sb, gn], w=[y_sb])
            self.tt(y_sb[:, :], y_sb[:, :], sil[:, :], ALU.mult, r=[y_sb, sil], w=[y_sb])
            tv_, tk_ = self.psv(5, 1)
            for k in range(4):
                self.tr(tv_[:, k * 128:(k + 1) * 128], y_sb[:, k * 128:(k + 1) * 128], r=[y_sb], w=tk_)
            self.copy(yT[:, :, :].rearrange("p a b -> p (a b)"), tv_[:, :], r=tk_, w=[yT])
            at = acc_t[i % 2]
            self.dma(at[:, :], acc[i * 128:(i + 1) * 128, :], r=[(acc, i)], w=[at])
            ov, ok = self.psv(2, 2)
            for half in range(2):
                for k in range(4):
                    self.mm(ov[:, half * 512:(half + 1) * 512], yT[:, k, :], w_out[:, k, half * 512:(half + 1) * 512],
                            k == 0, k == 3, r=[yT, w_out], w=ok)
            self.tt(at[:, :], ov[:, :], at[:, :], ALU.add, r=ok + [at], w=[at])
            self.dma(acc[i * 128:(i + 1) * 128, :], at[:, :], r=[at], w=[(acc, i)])

    def phase_copy(self, src, dst):
        xs = self.rot("c_x", [128, D])
        for i in range(NT):
            self.dma(xs[i % 2][:, :], src[i * 128:(i + 1) * 128, :], r=[(src, i)], w=[xs[i % 2]])
            self.dma(dst[i * 128:(i + 1) * 128, :], xs[i % 2][:, :], r=[xs[i % 2]], w=[(dst, i)])

    def phase_final(self, src):
        P = self.P
        g = P.sb("g_f", [128, D])
        self.gload(g, self.din['final_norm'], self.din['final_norm'][0:1, :])
        xs = self.rot("xf", [128, D])
        hns = self.rot("hnf", [128, D])
        sts = self.rot("stf", [128, 4])
        for i in range(NT):
            j = i % 2
            self.dma(xs[j][:, :], src[i * 128:(i + 1) * 128, :], r=[(src, i)], w=[xs[j]])
            self.rms(xs[j], g, hns[j], sts[j])
            self.dma(self.out[i * 128:(i + 1) * 128, :], hns[j][:, :], r=[hns[j]], w=[(self.out, i)])

    def run_phase(self, fn, *a):
        with ExitStack() as pes:
            self.P.pes = pes
            fn(*a)
            self.P.flush()
        self.P.pes = self.es

    def build(self, plan):
        for ph in plan:
            if ph[0] == 'peer':
                self.want_peer[ph[1]] = True
        cur = self.din['x']
        self.run_phase(self.phase_setup)
        for ph in plan:
            if ph[0] == 'xattn':
                self.run_phase(self.phase_xattn, ph[1], cur, self.hA)
                cur = self.hA
            elif ph[0] == 'even':
                self.run_phase(self.phase_even, ph[1], cur, self.hA)
                cur = self.hA
            elif ph[0] == 'peer':
                self.run_phase(self.phase_peer, ph[1], cur, self.hA)
                cur = self.hA
            elif ph[0] == 'dsa':
                other = self.hB if cur is not self.hB else self.hA
                self.run_phase(self.phase_dsa, ph[1], cur, other)
                cur = other
            elif ph[0] == 'hgrn':
                other = self.hB if cur is not self.hB else self.hA
                self.run_phase(self.phase_copy, cur, other)
                self.run_phase(self.phase_hgrn, ph[1], cur, other)
                cur = other
            elif ph[0] == 'odd':
                other = self.hB if cur is not self.hB else self.hA
                self.run_phase(self.phase_dsa, ph[1], cur, other)
                self.run_phase(self.phase_hgrn, ph[1], cur, other)
                cur = other
            elif ph[0] == 'final':
                self.run_phase(self.phase_final, cur)
        return self.nc


FULL_PLAN = []
for _l in range(DEPTH):
    FULL_PLAN.append(('even' if _l % 2 == 0 else 'odd', _l))
    FULL_PLAN.append(('xattn', _l))
    FULL_PLAN.append(('peer', _l))
FULL_PLAN.append(('final',))


def t5_bucket_np(d):
    d = np.maximum(d, 0)
    lr = np.log(np.maximum(d, 1).astype(np.float32) / np.float32(16)) / np.float32(np.log(128 / 16))
    large = 16 + (lr * np.float32(16)).astype(np.int32)
    large = np.minimum(large, 31)
    return np.where(d < 16, d, large)


def prep_inputs(inp):
    f = lambda a: np.ascontiguousarray(np.asarray(a, dtype=np.float32))
    shared = {}
    for k in IN_SHAPES:
        if k in ('x', 'mem'):
            continue
        if k == 'peer_keysT':
            a = np.asarray(inp['peer_keys'], dtype=np.float32)
            shared[k] = f(a.transpose(0, 4, 1, 2, 3).reshape(4, 128, 16, 128))
        elif k == 'even_conv_w':
            a = np.asarray(inp[k]).reshape(2, 3, 4, 128)
            shared[k] = f(a.transpose(0, 3, 2, 1).reshape(2, 128, 12))
        elif k == 'even_pool_scale':
            a = np.asarray(inp[k]).reshape(2, 4, 128)
            shared[k] = f(a.transpose(0, 2, 1))
        elif k == 'peer_uv':
            shared[k] = np.ascontiguousarray(np.concatenate(
                [np.asarray(inp['peer_u'], dtype=np.float32), np.asarray(inp['peer_v'], dtype=np.float32)], axis=-1))
        elif k == 'invc':
            shared[k] = INVC[0]
        elif k == 'biasT':
            rb = np.asarray(inp['rel_bias'], dtype=np.float32)
            ss_, tq_ = np.arange(128)[:, None], np.arange(128)[None, :]
            out = np.zeros((128, 8, 2, 128), dtype=np.float32)
            for dl in (0, 1):
                dist = np.maximum(128 * dl + tq_ - ss_, 0)
                out[:, :, dl, :] = rb[t5_bucket_np(dist)].transpose(0, 2, 1)
            shared[k] = f(out)
        elif k in ('mem_norm', 'final_norm'):
            shared[k] = f(np.asarray(inp[k]).reshape(1, D))
        else:
            shared[k] = f(inp[k])
    return shared


def kernel(plan=None, **inp):
    plan = FULL_PLAN if plan is None else plan
    m = Model()
    nc = m.build(plan)
    shared = prep_inputs(inp)
    shared['consts'] = m.carr
    shared = {k: v for k, v in shared.items() if k in m.din}
    x = np.asarray(inp['x'], dtype=np.float32)
    mem = np.asarray(inp['mem'], dtype=np.float32)
    in_maps = []
    for b in range(8):
        d = dict(shared)
        if 'x' in m.din:
            d['x'] = np.ascontiguousarray(x[b])
        if 'mem' in m.din:
            d['mem'] = np.ascontiguousarray(mem[b])
        in_maps.append(d)
    res = run_bass_kernel_spmd(nc, in_maps, core_ids=list(range(8)))
    m.es.close()
    return np.stack([np.asarray(r["out"], dtype=np.float32) for r in res.results], axis=0)
```

```python
import numpy as np
import concourse.bass as bass
import concourse.mybir as mybir
from concourse.bass_utils import run_bass_kernel_spmd
from contextlib import ExitStack

F32 = mybir.dt.float32
I32 = mybir.dt.int32
U32 = mybir.dt.uint32
AF = mybir.ActivationFunctionType
ALU = mybir.AluOpType
AX = mybir.AxisListType

ENGS = ['pe', 'dve', 'act', 'pool', 'sp']
SEM_LIMIT = 30000
DMA_K = 16


class T:
    def __init__(self, h, name):
        self.h = h
        self.name = name
        self.st = {}

    def __getitem__(self, idx):
        return self.h[idx]


def bcast(ap, axis, n):
    l = [list(x) for x in ap.ap]
    l.insert(axis, [0, n])
    return bass.AP(ap.tensor, ap.offset, l)


def rep(ap, axis, n):
    l = [list(x) for x in ap.ap]
    assert l[axis][1] == 1
    l[axis] = [0, n]
    return bass.AP(ap.tensor, ap.offset, l)


class Prog:
    def __init__(self, nc, es):
        self.nc = nc
        self.es = es
        self.pes = es
        self.ops = {e: [] for e in ENGS}
        self.base = {e: 0 for e in ENGS}
        self.waited = {e: {} for e in ENGS}
        self.waited_dma = {e: set() for e in ENGS}
        self.tiles = []
        self.csems = {e: [] for e in ENGS}
        self.ccount = {e: 0 for e in ENGS}
        self.dsems = {}
        self.dcount = {e: 0 for e in ENGS}
        self.dma_hist = {e: [] for e in ENGS}

    def sb(self, name, shape, dt=F32, glob=False):
        es = self.es if glob else self.pes
        self.uid = getattr(self, 'uid', 0) + 1
        name = f"{name}_{self.uid}"
        t = T(es.enter_context(self.nc.sbuf_tensor(name, list(shape), dt)), name)
        self.tiles.append(t)
        return t

    def ps(self, name, shape, dt=F32):
        t = T(self.es.enter_context(self.nc.psum_tensor(name, list(shape), dt)), name)
        self.tiles.append(t)
        return t

    def dram(self, name, shape, dt=F32, kind="Internal"):
        t = T(self.nc.dram_tensor(name, list(shape), dt, kind=kind).ap(), name)
        self.tiles.append(t)
        return t

    @staticmethod
    def _norm(key):
        if isinstance(key, T):
            return key, None
        return key[0], key[1]

    def _entries(self, tile, sub):
        if sub is None:
            return list(tile.st.values())
        out = []
        if sub in tile.st:
            out.append(tile.st[sub])
        if None in tile.st:
            out.append(tile.st[None])
        return out

    def op(self, eng, fn, r=(), w=(), dma=False, extra=()):
        idx = len(self.ops[eng])
        deps = set(extra)
        for key in r:
            tile, sub = self._norm(key)
            for ent in self._entries(tile, sub):
                if ent[0] is not None:
                    deps.add(ent[0])
        for key in w:
            tile, sub = self._norm(key)
            for ent in self._entries(tile, sub):
                if ent[0] is not None:
                    deps.add(ent[0])
                for e2, i2 in ent[1].items():
                    deps.add((e2, i2))
        for key in r:
            tile, sub = self._norm(key)
            ent = tile.st.setdefault(sub, [None, {}])
            ent[1][eng] = idx
        for key in w:
            tile, sub = self._norm(key)
            if sub is None:
                tile.st = {None: [(eng, idx), {}]}
            else:
                tile.st[sub] = [(eng, idx), {}]
        if dma:
            h = self.dma_hist[eng]
            if len(h) >= DMA_K:
                deps.add((eng, h[-DMA_K]))
        final = []
        for (e2, i2) in sorted(deps, reverse=True):
            if e2 == eng and i2 == idx:
                continue
            if self.ops[e2][i2]['dma']:
                if (e2, i2) in self.waited_dma[eng]:
                    continue
                self.waited_dma[eng].add((e2, i2))
                final.append((e2, i2))
            else:
                if e2 == eng and eng == 'pe':
                    continue
                if self.waited[eng].get(e2, -1) >= i2:
                    continue
                self.waited[eng][e2] = i2
                final.append((e2, i2))
        o = dict(fn=fn, deps=final, dma=dma, sig=False)
        if dma:
            o['dma_n'] = self.dcount[eng]
            self.dcount[eng] += 1
            self.dma_hist[eng].append(idx)
        self.ops[eng].append(o)
        return (eng, idx)

    def _sigof(self, e2, i2):
        o = self.ops[e2][i2]
        if o['dma']:
            n = o['dma_n']
            return self.dsems[e2][n % DMA_K], 16 * (n // DMA_K + 1)
        j, v = o['sv']
        return self.csems[e2][j], v

    def flush(self):
        nc = self.nc
        last = {}
        for e in ENGS:
            for i in range(len(self.ops[e]) - 1, self.base[e] - 1, -1):
                if not self.ops[e][i]['dma'] and self.ops[e][i]['fn'] is not None:
                    last[e] = (e, i)
                    break
        dmas = []
        for e in ENGS:
            dmas += [(e, i) for i in self.dma_hist[e][-DMA_K:] if i >= self.base[e]]
        for e in ENGS:
            extra = [v for k, v in last.items() if k != e] + dmas
            self.op(e, None, extra=extra)
        for e in ENGS:
            for o in self.ops[e][self.base[e]:]:
                for (e2, i2) in o['deps']:
                    assert i2 >= self.base[e2], "cross-phase dep"
                    self.ops[e2][i2]['sig'] = True
        for e in ENGS:
            for o in self.ops[e][self.base[e]:]:
                if o['dma']:
                    if e not in self.dsems:
                        self.dsems[e] = [self.es.enter_context(nc.semaphore(f"d_{e}_{j}")) for j in range(DMA_K)]
                    continue
                if o['sig']:
                    c = self.ccount[e]
                    j = c // SEM_LIMIT
                    while len(self.csems[e]) <= j:
                        self.csems[e].append(self.es.enter_context(nc.semaphore(f"c_{e}_{len(self.csems[e])}")))
                    o['sv'] = (j, c % SEM_LIMIT + 1)
                    self.ccount[e] = c + 1

        def run(e, eng):
            for o in self.ops[e][self.base[e]:]:
                for (e2, i2) in o['deps']:
                    s, v = self._sigof(e2, i2)
                    eng.wait_ge(s, v)
                if o['fn'] is None:
                    continue
                ins = o['fn'](eng)
                if o['dma']:
                    n = o['dma_n']
                    ins.then_inc(self.dsems[e][n % DMA_K], 16)
                elif o['sig']:
                    j, v = o['sv']
                    ins.then_inc(self.csems[e][j], 1)

        with nc.Block() as block:
            @block.tensor
            def _(eng):
                run('pe', eng)

            @block.vector
            def _(eng):
                run('dve', eng)

            @block.scalar
            def _(eng):
                run('act', eng)

            @block.gpsimd
            def _(eng):
                run('pool', eng)

            @block.sync
            def _(eng):
                run('sp', eng)
        for e in ENGS:
            self.base[e] = len(self.ops[e])
        for t in self.tiles:
            t.st = {}

D = 1024
S = 2048
NT = 16
NMEM = 256
DEPTH = 4
EPS = 1e-6
FAST_MM = True
BF16 = mybir.dt.bfloat16
F32R = mybir.dt.float32r
R32 = F32R

IN_SHAPES = {
    'x': [S, D], 'mem': [NMEM, D], 'mix_norm': [4, D],
    'even_w_in': [2, D, 2048], 'even_conv_w': [2, 128, 12], 'even_pool_w': [2, 4, 128, 128],
    'even_pool_scale': [2, 128, 4], 'even_w_out': [2, D, D],
    'odd_w_in': [2, D, 3784], 'odd_kv_norm': [2, 128], 'odd_w_uv': [2, 8, 128, 64],
    'odd_hg_norm': [2, 512], 'odd_w_out': [2, D, D], 'hgrn_gamma': [4, 512],
    'rel_bias': [32, 8], 'invc': [128, 2048], 'biasT': [128, 8, 2, 128], 'mem_norm': [1, D], 'xattn_norm': [4, D],
    'xattn_wq': [4, D, D], 'xattn_wkv': [4, D, 2 * D], 'xattn_wo': [4, D, D],
    'peer_norm': [4, D], 'peer_wq': [4, D, 2048], 'peer_keysT': [4, 128, 16, 128],
    'peer_uv': [4, 16384, 2 * D], 'final_norm': [1, D],
}


INVC = [None]


def make_consts():
    parts = {}
    parts['ident'] = np.eye(128, dtype=np.float32)
    t = np.arange(512)
    invc = np.concatenate([1.0 / np.minimum(t + 1, w) for w in (2, 4, 8, 16)]).astype(np.float32)
    INVC[0] = np.ascontiguousarray(np.tile(invc[None, :], (128, 1)).astype(np.float32))
    invc16 = np.concatenate([1.0 / np.minimum(np.arange(16) + 1, w) for w in (2, 4, 8, 16)]).astype(np.float32)
    parts['invc16'] = np.tile(invc16[None, :], (128, 1))
    parts['iota16'] = np.tile(np.arange(16, dtype=np.float32)[None, :], (128, 1))
    ii = np.arange(128)
    parts['U2'] = ((ii[:, None] // 64 == ii[None, :] // 64) & (ii[:, None] <= ii[None, :])).astype(np.float32)
    parts['B2'] = (ii[:, None] // 64 == ii[None, :] // 64).astype(np.float32)
    parts['cmask'] = np.where(ii[None, :] <= ii[:, None], 0.0, -1e30).astype(np.float32)
    parts['ones'] = np.ones((128, 8), dtype=np.float32)
    off = 0
    lay = {}
    arrs = []
    for k, v in parts.items():
        lay[k] = (off, v.shape[1])
        off += v.shape[1]
        arrs.append(v.astype(np.float32))
    return np.ascontiguousarray(np.concatenate(arrs, axis=1)), lay


class Model:
    def __init__(self):
        self.nc = bass.Bass("TRN2", target_bir_lowering=False)
        self.es = ExitStack()
        self.P = Prog(self.nc, self.es)
        P = self.P
        carr, self.clay = make_consts()
        self.carr = carr
        model = self

        class LazyIn(dict):
            def __missing__(self, k):
                shp = list(carr.shape) if k == 'consts' else IN_SHAPES[k]
                t = P.dram(k, shp, F32, kind="ExternalInput")
                self[k] = t
                return t
        self.din = LazyIn()
        self.out = P.dram("out", [S, D], F32, kind="ExternalOutput")
        self.hA = P.dram("hA", [S, D])
        self.hB = P.dram("hB", [S, D])
        self.uvb = P.dram("uvb", [16384, 2 * D], BF16)
        self.PS = P.ps("ps", [128, 8, 512])
        self.C = P.sb("consts_sb", [128, carr.shape[1]], glob=True)
        self.memT = P.sb("memT", [128, 8, NMEM], R32, glob=True)
        self.rotc = {}
        self.uvb_ready = {}
        self.want_peer = {}

    def cst(self, name):
        o, w = self.clay[name]
        return self.C[:, o:o + w]

    def psv(self, b0, nb=2):
        ap = self.PS[:, b0:b0 + nb, :].rearrange("p a b -> p (a b)")
        return ap, [(self.PS, b) for b in range(b0, b0 + nb)]

    def mm(self, out, lhsT, rhs, start, stop, r, w, fast=False):
        if FAST_MM and fast and rhs.shape[-1] % 2 == 0 and out.shape[-1] % 2 == 0:
            lhsT = lhsT.bitcast(F32R)
            rhs = rhs.bitcast(F32R)
        self.P.op('pe', lambda e: e.matmul(out, lhsT, rhs, start=start, stop=stop), r=r, w=w)

    def tr(self, out, in_, r, w):
        ident = self.cst('ident')
        self.P.op('pe', lambda e: e.transpose(out, in_, ident), r=list(r) + [self.C], w=w)

    def act(self, out, in_, func, r, w, bias=None, scale=None, accum=None):
        kw = {}
        if bias is not None:
            kw['bias'] = bias
        if scale is not None:
            kw['scale'] = scale
        if accum is not None:
            kw['accum_out'] = accum
        self.P.op('act', lambda e: e.activation(out, in_, func, **kw), r=r, w=w)

    def tt(self, out, a, b, op, r, w, eng='dve'):
        self.P.op(eng, lambda e: e.tensor_tensor(out, a, b, op), r=r, w=w)

    def ts(self, out, a, s1, s2, op0, op1, r, w, eng='dve', accum=None):
        if op1 is None:
            self.P.op(eng, lambda e: e.tensor_scalar(out, a, s1, None, op0), r=r, w=w)
        elif accum is None:
            self.P.op(eng, lambda e: e.tensor_scalar(out, a, s1, s2, op0, op1), r=r, w=w)
        else:
            self.P.op(eng, lambda e: e.tensor_scalar(out, a, s1, s2, op0, op1, accum_out=accum), r=r, w=w)

    def stt(self, out, a, scalar, b, op0, op1, r, w):
        self.P.op('dve', lambda e: e.scalar_tensor_tensor(out, a, scalar, b, op0, op1), r=r, w=w)

    def copy(self, out, in_, r, w, eng='act'):
        if eng == 'act':
            self.P.op('act', lambda e: e.copy(out, in_), r=r, w=w)
        else:
            self.P.op(eng, lambda e: e.tensor_copy(out, in_), r=r, w=w)

    def dma(self, out, in_, r, w, eng='sp'):
        self.P.op(eng, lambda e: e.dma_start(out=out, in_=in_), r=r, w=w, dma=True)

    def recip(self, out, in_, r, w):
        self.P.op('dve', lambda e: e.reciprocal(out, in_), r=r, w=w)

    def rot(self, name, shape, n=2, dt=F32):
        return [self.P.sb(f"{name}{j}", shape, dt) for j in range(n)]

    def wload(self, dst, src_t, src_ap):
        self.dma(dst[:, :, :], src_ap.rearrange("(k p) c -> p k c", p=128), r=[src_t], w=[dst])

    def wload_r(self, dst, src_t, src_ap, nk=8, cw=128):
        C = src_ap.shape[1]
        if not hasattr(self, '_wst') or self._wst_phase is not self.P.pes:
            self._wst = self.rot("wst", [128, 8, cw], 2)
            self._wst_phase = self.P.pes
            self._wst_n = 0
        for c0 in range(0, C, cw):
            c1 = min(C, c0 + cw)
            st_ = self._wst[self._wst_n % 2]
            eng = ('act', 'dve', 'pool')[self._wst_n % 3]
            self._wst_n += 1
            self.dma(st_[:, 0:nk, 0:c1 - c0], src_ap[:, c0:c1].rearrange("(k p) c -> p k c", p=128), r=[src_t], w=[st_])
            self.copy(dst[:, :, c0:c1], st_[:, 0:nk, 0:c1 - c0], r=[st_], w=[dst], eng=eng)

    def gload(self, dst, src_t, row_ap):
        n = row_ap.shape[1]
        self.dma(dst[:, :], row_ap.broadcast_to([128, n]), r=[src_t], w=[dst])

    def rms(self, x, g, hn, st, width=D):
        self.act(hn[:, :], x[:, :], AF.Square, r=[x], w=[hn, st], accum=st[:, 0:1])
        self.act(st[:, 1:2], st[:, 0:1], AF.Sqrt, r=[st], w=[st], scale=1.0 / width, bias=self.epsb[:, 0:1])
        self.recip(st[:, 1:2], st[:, 1:2], r=[st], w=[st])
        self.stt(hn[:, :], x[:, :], st[:, 1:2], g[:, :], ALU.mult, ALU.mult, r=[x, st, g], w=[hn])

    def transp8(self, src, dstT, b0, n=8):
        nb = (n * 128 + 511) // 512
        pv, pk = self.psv(b0, nb)
        for k in range(n):
            self.tr(pv[:, k * 128:(k + 1) * 128], src[:, k * 128:(k + 1) * 128], r=[src], w=pk)
        self.copy(dstT[:, :, :].rearrange("p a b -> p (a b)"), pv[:, 0:n * 128], r=pk, w=[dstT])

    def phase_setup(self):
        P = self.P
        self.dma(self.C[:, :], self.din['consts'][:, :], r=[self.din['consts']], w=[self.C])
        self.epsb = P.sb("epsb", [128, 1], glob=True)
        P.op('dve', lambda e: e.memset(self.epsb[:, :], EPS), w=[self.epsb])
        g = P.sb("g_mem", [128, D])
        self.gload(g, self.din['mem_norm'], self.din['mem_norm'][0:1, :])
        xs = self.rot("xm", [128, D])
        hn = self.rot("hnm", [128, D])
        st = self.rot("stm", [128, 4])
        mT = [P.sb(f"mT{j}", [128, 8, 128]) for j in range(2)]
        for m in range(2):
            self.dma(xs[m][:, :], self.din['mem'][m * 128:(m + 1) * 128, :], r=[self.din['mem']], w=[xs[m]])
            self.rms(xs[m], g, hn[m], st[m])
            self.transp8(hn[m], mT[m], 2 * m)
            self.copy(self.memT[:, :, m * 128:(m + 1) * 128], mT[m][:, :, :], r=[mT[m]], w=[(self.memT, m)], eng='dve')

    def convert_uv(self, l):
        P = self.P
        din = self.din
        uvb2 = self.uvb[:, :].rearrange("n (a d) -> (n a) d", a=2)
        uvf2 = din['peer_uv'][l].rearrange("n (a d) -> (n a) d", a=2)
        for c in range(16):
            P.op('pool', lambda e, c=c: e.dma_start(out=uvb2[c * 2048:(c + 1) * 2048, :],
                                                    in_=uvf2[c * 2048:(c + 1) * 2048, :]),
                 r=[din['peer_uv']], w=[self.uvb], dma=True)
        self.uvb_ready[l] = True

    def phase_xattn(self, l, src, dst):
        P = self.P
        din = self.din
        if self.want_peer.get(l):
            self.convert_uv(l)
        wq = P.sb("wq", [128, 8, D], R32)
        wo = P.sb("wo", [128, 8, D], R32)
        KT = P.sb("KT", [128, 8, NMEM], R32)
        V = P.sb("V", [128, 2, D], R32)
        g = P.sb("g_x", [128, D])
        wkv = self.rot("wkv", [128, 8, 512], dt=R32)
        self.gload(g, din['xattn_norm'], din['xattn_norm'][l:l + 1, :])
        for c in range(4):
            wc = wkv[c % 2]
            self.wload_r(wc, din['xattn_wkv'], din['xattn_wkv'][l][:, c * 512:(c + 1) * 512])
            if c < 2:
                for f in range(4):
                    fc = c * 4 + f
                    b = fc % 8
                    pv, pk = self.psv(b, 1)
                    for k in range(8):
                        self.mm(pv[:, 0:NMEM], wc[:, k, f * 128:(f + 1) * 128], self.memT[:, k, :],
                                k == 0, k == 7, r=[wc, self.memT], w=pk, fast=True)
                    self.act(KT[:, fc, :], pv[:, 0:NMEM], AF.Copy, r=pk, w=[(KT, fc)], scale=1.0 / 16.0)
            else:
                for m in range(2):
                    b = (c - 2) * 2 + m
                    pv, pk = self.psv(b, 1)
                    for k in range(8):
                        self.mm(pv[:, :], self.memT[:, k, m * 128:(m + 1) * 128], wc[:, k, :],
                                k == 0, k == 7, r=[wc, self.memT], w=pk, fast=True)
                    self.copy(V[:, m, (c - 2) * 512:(c - 1) * 512], pv[:, :], r=pk, w=[(V, (m, c))])
        self.wload_r(wq, din['xattn_wq'], din['xattn_wq'][l])
        self.wload_r(wo, din['xattn_wo'], din['xattn_wo'][l])
        xs = self.rot("x", [128, D])
        hns = self.rot("hn", [128, D])
        hnTs = self.rot("hnT", [128, 8, 128], dt=R32)
        sts = self.rot("st", [128, 16])
        qTs = self.rot("qT", [128, 8, 128], dt=R32)
        Pms = self.rot("Pm", [128, 4 * NMEM])
        PTs = self.rot("PT", [128, 8, 128], dt=R32)
        oTs = self.rot("oT", [128, 8, 128], dt=R32)
        def xfront(i):
                j = i % 2
                x, hn, hnT, st, qT, Pm, PT, oT = xs[j], hns[j], hnTs[j], sts[j], qTs[j], Pms[j], PTs[j], oTs[j]
                self.dma(x[:, :], src[i * 128:(i + 1) * 128, :], r=[(src, i)], w=[x])
                self.rms(x, g, hn, st)
                self.transp8(hn, hnT, 0)
                pv, pk = self.psv(2, 2)
                for f in range(8):
                    for k in range(8):
                        self.mm(pv[:, f * 128:(f + 1) * 128], wq[:, k, f * 128:(f + 1) * 128], hnT[:, k, :],
                                k == 0, k == 7, r=[wq, hnT], w=pk, fast=True)
                self.copy(qT[:, :, :].rearrange("p a b -> p (a b)"), pv[:, :], r=pk, w=[qT])

        def xmid(i):
                j = i % 2
                x, hn, hnT, st, qT, Pm, PT, oT = xs[j], hns[j], hnTs[j], sts[j], qTs[j], Pms[j], PTs[j], oTs[j]
                lv, lk = self.psv(4, 2)
                for hd in range(4):
                    for c in range(2):
                        self.mm(lv[:, hd * 256:(hd + 1) * 256], qT[:, hd * 2 + c, :], KT[:, hd * 2 + c, :],
                                c == 0, c == 1, r=[qT, KT], w=lk, fast=True)
                lv3 = self.PS[:, 4:6, :].rearrange("p a (h m) -> p (a h) m", m=NMEM)
                P.op('dve', lambda e, lv3=lv3, st=st: e.tensor_reduce(out=st[:, 4:8], in_=lv3, axis=AX.X, op=ALU.max, negate=True),
                     r=lk, w=[st])
                for hd in range(4):
                    self.act(Pm[:, hd * 256:(hd + 1) * 256], lv[:, hd * 256:(hd + 1) * 256], AF.Exp, r=lk + [st], w=[Pm, st],
                             bias=st[:, 4 + hd:5 + hd], scale=1.0, accum=st[:, 8 + hd:9 + hd])
                self.recip(st[:, 12:16], st[:, 8:12], r=[st], w=[st])
                for hd in range(4):
                    self.ts(Pm[:, hd * 256:(hd + 1) * 256], Pm[:, hd * 256:(hd + 1) * 256], st[:, 12 + hd:13 + hd], None,
                            ALU.mult, None, r=[Pm, st], w=[Pm])

        def xback(i):
                j = i % 2
                x, hn, hnT, st, qT, Pm, PT, oT = xs[j], hns[j], hnTs[j], sts[j], qTs[j], Pms[j], PTs[j], oTs[j]
                self.transp8(Pm, PT, 6)
                ov, ok = self.psv(0, 2)
                for jj in range(8):
                    for c in range(2):
                        self.mm(ov[:, jj * 128:(jj + 1) * 128], V[:, c, jj * 128:(jj + 1) * 128], PT[:, (jj // 2) * 2 + c, :],
                                c == 0, c == 1, r=[V, PT], w=ok, fast=True)
                self.copy(oT[:, :, :].rearrange("p a b -> p (a b)"), ov[:, :], r=ok, w=[oT])
                yv, yk = self.psv(2, 2)
                for half in range(2):
                    for k in range(8):
                        self.mm(yv[:, half * 512:(half + 1) * 512], oT[:, k, :], wo[:, k, half * 512:(half + 1) * 512],
                                k == 0, k == 7, r=[oT, wo], w=yk, fast=True)
                self.tt(x[:, :], yv[:, :], x[:, :], ALU.add, r=yk + [x], w=[x])
                self.dma(dst[i * 128:(i + 1) * 128, :], x[:, :], r=[x], w=[(dst, i)])


        xfront(0)
        for i in range(NT):
            xmid(i)
            if i + 1 < NT:
                xfront(i + 1)
            xback(i)

    def phase_even(self, l, src, dst):
        P = self.P
        din = self.din
        jx = l // 2
        w_in = P.sb("e_win", [128, 8, 2048], R32)
        w_out = P.sb("e_wout", [128, 8, D], R32)
        pw = P.sb("e_pw", [128, 4, 128], R32)
        pw_st = P.sb("e_pwst", [128, 4, 128])
        cw = P.sb("e_cw", [128, 12])
        psc = P.sb("e_psc", [128, 4])
        g = P.sb("e_g", [128, D])
        self.gload(g, din['mix_norm'], din['mix_norm'][l:l + 1, :])
        self.wload_r(w_in, din['even_w_in'], din['even_w_in'][jx])
        self.wload_r(w_out, din['even_w_out'], din['even_w_out'][jx])
        self.dma(pw_st[:, :, :], din['even_pool_w'][jx].rearrange("g c d -> c g d"), r=[din['even_pool_w']], w=[pw_st])
        self.copy(pw[:, :, :], pw_st[:, :, :], r=[pw_st], w=[pw], eng='dve')
        self.dma(cw[:, :], din['even_conv_w'][jx], r=[din['even_conv_w']], w=[cw])
        self.dma(psc[:, :], din['even_pool_scale'][jx], r=[din['even_pool_scale']], w=[psc])
        cu_halo = P.sb("e_cuh", [128, 4, 2])
        pv_halo = P.sb("e_pvh", [128, 4, 16])
        P.op('pool', lambda e: e.memset(cu_halo[:, :, :], 0.0), w=[cu_halo])
        P.op('pool', lambda e: e.memset(pv_halo[:, :, :], 0.0), w=[pv_halo])
        hnT = P.sb("e_hnT", [128, 8, 512], R32)
        yT = P.sb("e_yT", [128, 8, 512], R32)
        xs = self.rot("e_x", [128, D])
        hns = self.rot("e_hn", [128, D])
        sts = self.rot("e_st", [128, 4])
        hts = self.rot("e_ht", [128, 8, 128])
        cus = self.rot("e_cu", [128, 514])
        pvs = self.rot("e_pv", [128, 528])
        ut = self.rot("e_ut", [128, 512])
        zt = self.rot("e_z", [128, 512])
        sA = self.rot("e_sA", [128, 528])
        sB = self.rot("e_sB", [128, 528])
        pl = self.rot("e_pl", [128, 512], dt=R32)
        invc16 = self.cst('invc16')
        tmp16 = P.sb("e_tmp16", [128, 16])
        for b in range(4):
            for t4 in range(4):
                i = b * 4 + t4
                j = i % 2
                self.dma(xs[j][:, :], src[i * 128:(i + 1) * 128, :], r=[(src, i)], w=[xs[j]])
                self.rms(xs[j], g, hns[j], sts[j])
                self.transp8(hns[j], hts[j], 6)
                self.copy(hnT[:, :, t4 * 128:(t4 + 1) * 128], hts[j][:, :, :], r=[hts[j]], w=[(hnT, t4)], eng='pool')
            for c4 in range(4):
                j = c4 % 2
                cu, pv, u_sb, z = cus[j], pvs[j], ut[j], zt[j]

                def proj(cc, bank):
                    pvw, pk = self.psv(bank, 1)
                    for k in range(8):
                        self.mm(pvw[:, :], w_in[:, k, cc * 128:(cc + 1) * 128], hnT[:, k, :], k == 0, k == 7,
                                r=[w_in, hnT], w=pk, fast=True)
                    return pvw, pk
                pu, ku = proj(c4, 0)
                self.copy(u_sb[:, :], pu[:, :], r=ku, w=[u_sb])
                pg, kg = proj(4 + c4, 1)
                self.copy(cu[:, 0:2], cu_halo[:, c4, :], r=[(cu_halo, c4)], w=[cu], eng='pool')
                self.tt(cu[:, 2:514], pg[:, :], u_sb[:, :], ALU.mult, r=kg + [u_sb, cu], w=[cu])
                self.copy(cu_halo[:, c4, :], cu[:, 512:514], r=[cu], w=[(cu_halo, c4)], eng='pool')
                self.ts(z[:, :], cu[:, 0:512], cw[:, c4 * 3:c4 * 3 + 1], None, ALU.mult, None, r=[cu, cw], w=[z])
                self.stt(z[:, :], cu[:, 1:513], cw[:, c4 * 3 + 1:c4 * 3 + 2], z[:, :], ALU.mult, ALU.add, r=[cu, cw, z], w=[z])
                self.stt(z[:, :], cu[:, 2:514], cw[:, c4 * 3 + 2:c4 * 3 + 3], z[:, :], ALU.mult, ALU.add, r=[cu, cw, z], w=[z])
                pb, kb = proj(8 + c4, 2)
                self.tt(yT[:, c4, :], pb[:, :], z[:, :], ALU.mult, r=kb + [z], w=[(yT, c4)])
                pp, kp = proj(12 + c4, 3)
                self.copy(pv[:, 0:16], pv_halo[:, c4, :], r=[(pv_halo, c4)], w=[pv], eng='pool')
                self.copy(pv[:, 16:528], pp[:, :], r=kp + [pv], w=[pv])
                self.copy(pv_halo[:, c4, :], pv[:, 512:528], r=[pv], w=[(pv_halo, c4)], eng='pool')
                a_, b_ = sA[j], sB[j]
                self.tt(a_[:, 1:528], pv[:, 1:528], pv[:, 0:527], ALU.add, r=[pv], w=[a_])
                cur = a_
                other = b_
                lo = 1
                for st_ in range(c4):
                    sh = 2 ** (st_ + 1)
                    nlo = lo + sh
                    self.tt(other[:, nlo:528], cur[:, nlo:528], cur[:, nlo - sh:528 - sh], ALU.add, r=[cur], w=[other])
                    cur, other = other, cur
                    lo = nlo
                wdw = 2 ** (c4 + 1)
                pool_t = pl[j]
                self.stt(pool_t[:, :], cur[:, 16:528], 1.0 / wdw, pv[:, 16:528], ALU.mult, ALU.subtract, r=[cur, pv], w=[pool_t])
                if b == 0:
                    self.tt(tmp16[:, :], cur[:, 16:32], invc16[:, c4 * 16:(c4 + 1) * 16], ALU.mult, r=[cur, self.C], w=[tmp16])
                    self.tt(pool_t[:, 0:16], tmp16[:, :], pv[:, 16:32], ALU.subtract, r=[tmp16, pv, pool_t], w=[pool_t])
                py, ky = self.psv(3, 1)
                self.mm(py[:, :], pw[:, c4, :], pool_t[:, :], True, True, r=[pw, pool_t], w=ky, fast=True)
                self.act(yT[:, 4 + c4, :], py[:, :], AF.Copy, r=ky + [psc], w=[(yT, 4 + c4)], scale=psc[:, c4:c4 + 1])
            for t4 in range(4):
                i = b * 4 + t4
                j = i % 2
                self.dma(xs[j][:, :], src[i * 128:(i + 1) * 128, :], r=[(src, i)], w=[xs[j]])
                ov, ok = self.psv(4, 2)
                for half in range(2):
                    for k in range(8):
                        self.mm(ov[:, half * 512:(half + 1) * 512], yT[:, k, t4 * 128:(t4 + 1) * 128],
                                w_out[:, k, half * 512:(half + 1) * 512], k == 0, k == 7, r=[yT, w_out], w=ok, fast=True)
                self.tt(xs[j][:, :], ov[:, :], xs[j][:, :], ALU.add, r=ok + [xs[j]], w=[xs[j]])
                self.dma(dst[i * 128:(i + 1) * 128, :], xs[j][:, :], r=[xs[j]], w=[(dst, i)])


    def top16(self, src_ap, work_ap, tv_ap, ti_ap, r, wk):
        P = self.P
        P.op('dve', lambda e: e.max(out=tv_ap[:, 0:8], in_=src_ap), r=r, w=wk)
        P.op('dve', lambda e: e.max_index(out=ti_ap[:, 0:8], in_max=tv_ap[:, 0:8], in_values=src_ap), r=r + wk, w=wk)
        P.op('dve', lambda e: e.match_replace(out=work_ap, in_to_replace=tv_ap[:, 0:8], in_values=src_ap, imm_value=-1e30),
             r=r + wk, w=wk)
        P.op('dve', lambda e: e.max(out=tv_ap[:, 8:16], in_=work_ap), r=wk, w=wk)
        P.op('dve', lambda e: e.max_index(out=ti_ap[:, 8:16], in_max=tv_ap[:, 8:16], in_values=work_ap), r=wk, w=wk)

    def phase_peer(self, l, src, dst):
        P = self.P
        din = self.din
        wq = P.sb("p_wq", [128, 8, 2048], BF16)
        keysT = P.sb("p_keys", [128, 16, 128])
        g = P.sb("p_g", [128, D])
        self.gload(g, din['peer_norm'], din['peer_norm'][l:l + 1, :])
        wst = self.rot("p_wst", [128, 8, 256], 2)
        for c in range(8):
            ws_ = wst[c % 2]
            self.dma(ws_[:, :, :], din['peer_wq'][l][:, c * 256:(c + 1) * 256].rearrange("(k p) c -> p k c", p=128),
                     r=[din['peer_wq']], w=[ws_])
            self.copy(wq[:, :, c * 256:(c + 1) * 256], ws_[:, :, :], r=[ws_], w=[(wq, c)], eng='act' if c % 2 == 0 else 'dve')
        self.dma(keysT[:, :, :], din['peer_keysT'][l], r=[din['peer_keysT']], w=[keysT])
        uv_l = self.uvb
        if not self.uvb_ready.get(l):
            self.convert_uv(l)
        xs = self.rot("p_x", [128, D], 2)
        xns = self.rot("p_xn", [128, D], 2)
        xnT = P.sb("p_xnT", [128, 8, 128], BF16)
        sts = self.rot("p_st", [128, 4], 2)
        qT = P.sb("p_qT", [128, 8, 128])
        s_sb = P.sb("p_s", [128, 1024])
        tv = P.sb("p_tv", [128, 16, 16])
        ti = P.sb("p_ti", [128, 16, 16], U32)
        tif = P.sb("p_tif", [128, 16, 16])
        cand = P.sb("p_cand", [128, 8, 256])
        cwk = P.sb("p_cwk", [128, 8, 256])
        cv = P.sb("p_cv", [128, 8, 16])
        cpos = P.sb("p_cpos", [128, 8, 16], U32)
        ab_u = P.sb("p_abu", [128, 2, 128], U32)
        ab_f = P.sb("p_abf", [128, 2, 128])
        isel = P.sb("p_isel", [128, 2, 128])
        eidf = P.sb("p_eidf", [128, 128])
        eids = self.rot("p_eid", [128, 128], 2, dt=I32)
        ggs = self.rot("p_gg", [128, 8, 16], 2)
        gz = P.sb("p_gz", [128, 16])
        actvs = self.rot("p_act", [128, 128], 2)
        wgts = self.rot("p_wgt", [128, 128], 2)
        GS = 4
        t1s = self.rot("p_t1", [128, GS], 4)
        t2s = self.rot("p_t2", [128, GS], 4)
        junk = P.sb("p_junk", [128, D], BF16)
        NUV = 16
        uvs = self.rot("p_uv", [128, 2 * D], NUV, dt=BF16)
        dgs = self.rot("p_dg", [128, 128], 4, dt=BF16)
        iota16 = self.cst('iota16')
        ident = self.cst('ident')
        cwkf = cwk[:, :, :].rearrange("p a b -> p (a b)")
        candf = cand[:, :, :].rearrange("p a b -> p (a b)")

        def front(i):
            x, xn, st = xs[i % 2], xns[i % 2], sts[i % 2]
            eid, gg = eids[i % 2], ggs[i % 2]
            self.dma(x[:, :], src[i * 128:(i + 1) * 128, :], r=[(src, i)], w=[x])
            self.rms(x, g, xn, st)
            yield
            self.transp8(xn, xnT, 0)
            yield
            for hf in range(2):
                qv, qk = self.psv(2, 2)
                for hh in range(8):
                    hp = hf * 8 + hh
                    for k in range(8):
                        self.mm(qv[:, hh * 128:(hh + 1) * 128], wq[:, k, hp * 128:(hp + 1) * 128], xnT[:, k, :],
                                k == 0, k == 7, r=[wq, xnT], w=qk, fast=False)
                    if hh % 2 == 1:
                        yield
                self.copy(qT[:, :, :].rearrange("p a b -> p (a b)"), qv[:, :], r=qk, w=[qT])
                sv, sk = self.psv(0, 2)
                for hh in range(8):
                    hp = hf * 8 + hh
                    self.mm(sv[:, hh * 128:(hh + 1) * 128], qT[:, hh, :], keysT[:, hp, :], True, True, r=[qT, keysT], w=sk)
                self.copy(s_sb[:, :], sv[:, :], r=sk, w=[s_sb])
                yield
                for hh in range(8):
                    hp = hf * 8 + hh
                    self.top16(s_sb[:, hh * 128:(hh + 1) * 128], cwkf[:, hh * 128:(hh + 1) * 128], tv[:, hp, :], ti[:, hp, :],
                               r=[s_sb], wk=[(tv, hp), (ti, hp), (cwk, hh)])
                    yield
            self.copy(tif[:, :, :], ti[:, :, :], r=[ti], w=[tif], eng='dve')
            tv4 = tv[:, :, :].rearrange("p (h t) a -> p h t a", t=2)
            tif4 = tif[:, :, :].rearrange("p (h t) a -> p h t a", t=2)
            cand4 = cand[:, :, :].rearrange("p h (a b) -> p h a b", b=16)
            self.tt(cand4, bcast(tv4[:, :, 0, :], 3, 16), bcast(tv4[:, :, 1, :], 2, 16), ALU.add, r=[tv], w=[cand])
            yield
            for h in range(8):
                self.top16(cand[:, h, :], cwk[:, h, :], cv[:, h, :], cpos[:, h, :],
                           r=[cand], wk=[(cv, h), (cpos, h), (cwk, h)])
                yield
            cposf = cpos[:, :, :].rearrange("p h a -> p (h a)")
            P.op('dve', lambda e: e.tensor_single_scalar(ab_u[:, 0, :], cposf, 4, ALU.logical_shift_right), r=[cpos], w=[ab_u])
            P.op('dve', lambda e: e.tensor_single_scalar(ab_u[:, 1, :], cposf, 15, ALU.bitwise_and), r=[cpos, ab_u], w=[ab_u])
            self.copy(ab_f[:, :, :], ab_u[:, :, :], r=[ab_u], w=[ab_f], eng='dve')
            yield
            eqv = candf[:, 0:1024].rearrange("p (m a) -> p m a", a=16)
            for t in range(2):
                for hh in range(2):
                    self.tt(eqv, bcast(ab_f[:, t, hh * 64:(hh + 1) * 64], 2, 16), bcast(iota16, 1, 64), ALU.is_equal,
                            r=[ab_f, self.C], w=[cand])
                    eq4 = candf[:, 0:1024].rearrange("p (h j a) -> p h j a", j=16, a=16)
                    self.tt(eq4, eq4, bcast(tif4[:, hh * 4:(hh + 1) * 4, t, :], 2, 16), ALU.mult, r=[cand, tif], w=[cand])
                    P.op('dve', lambda e, t=t, hh=hh: e.tensor_reduce(out=isel[:, t, hh * 64:(hh + 1) * 64], in_=eqv,
                                                                      axis=AX.X, op=ALU.add), r=[cand], w=[isel])
                    yield
            self.stt(eidf[:, :], isel[:, 0, :], 128.0, isel[:, 1, :], ALU.mult, ALU.add, r=[isel], w=[eidf])
            self.copy(eid[:, :], eidf[:, :], r=[eidf], w=[eid], eng='dve')
            yield
            self.tt(gg[:, :, :], cv[:, :, :], bcast(cv[:, :, 0], 2, 16), ALU.subtract, r=[cv], w=[gg])
            self.act(gg[:, :, :], gg[:, :, :], AF.Exp, r=[gg], w=[gg])
            P.op('dve', lambda e: e.tensor_reduce(out=gz[:, 0:8], in_=gg[:, :, :], axis=AX.X, op=ALU.add), r=[gg], w=[gz])
            self.recip(gz[:, 8:16], gz[:, 0:8], r=[gz], w=[gz])
            self.tt(gg[:, :, :], gg[:, :, :], bcast(gz[:, 8:16], 2, 16), ALU.mult, r=[gg, gz], w=[gg])
            yield

        def uvstage(i):
            x, xn, eid, gg = xs[i % 2], xns[i % 2], eids[i % 2], ggs[i % 2]
            actv, wgt = actvs[i % 2], wgts[i % 2]
            ggf = gg[:, :, :].rearrange("p h a -> p (h a)")
            xpv, xpk = self.psv(4, 2)
            av, ak = self.psv(6, 2)
            self.copy(xpv[:, :], xn[:, :], r=[xn], w=xpk)
            NGRP = 128 // GS

            def stA(gi):
                g0 = gi * GS
                for jj in range(g0, g0 + GS):
                    uv = uvs[jj % NUV]
                    P.op('pool', lambda e, uv=uv, jj=jj: e.indirect_dma_start(
                        out=uv[:, :], out_offset=None, in_=uv_l[:, :],
                        in_offset=bass.IndirectOffsetOnAxis(ap=eid[:, jj:jj + 1], axis=0)),
                        r=[eid, self.uvb], w=[uv], dma=True)
                    P.op('dve', lambda e, uv=uv, jj=jj: e.scalar_tensor_tensor(
                        junk[:, :], uv[:, 0:D], 1.0, xpv[:, :], ALU.mult, ALU.mult, accum_out=actv[:, jj:jj + 1]),
                        r=[uv] + xpk, w=[(actv, jj)])
                a_ = actv[:, g0:g0 + GS]
                ak_ = [(actv, jj) for jj in range(g0, g0 + GS)]
                t1, t2 = t1s[gi % 4], t2s[gi % 4]
                self.act(t1[:, :], a_, AF.Square, r=ak_, w=[t1], scale=0.044715 ** 0.5)
                self.stt(t1[:, :], t1[:, :], 1.0, a_, ALU.add, ALU.mult, r=[t1] + ak_, w=[t1])
                self.act(t2[:, :], t1[:, :], AF.Sigmoid, r=[t1], w=[t2], scale=1.5957691216057308)

            def stC(gi):
                g0 = gi * GS
                a_ = actv[:, g0:g0 + GS]
                ak_ = [(actv, jj) for jj in range(g0, g0 + GS)]
                t2 = t2s[gi % 4]
                self.tt(t2[:, :], t2[:, :], a_, ALU.mult, r=[t2] + ak_, w=[t2])
                self.tt(wgt[:, g0:g0 + GS], t2[:, :], ggf[:, g0:g0 + GS], ALU.mult, r=[t2, gg], w=[(wgt, gi)])

            def stD(gi):
                g0 = gi * GS
                for jj in range(g0, g0 + GS):
                    uv = uvs[jj % NUV]
                    dg = dgs[jj % 4]
                    self.ts(dg[:, :], ident, wgt[:, jj:jj + 1], 1.0, ALU.mult, ALU.mult, r=[self.C, (wgt, gi)], w=[dg], eng='pool')
                    for half in range(2):
                        self.mm(av[:, half * 512:(half + 1) * 512], dg[:, :], uv[:, D + half * 512:D + (half + 1) * 512],
                                jj == 0, jj == 127, r=[dg, uv], w=[ak[half]], fast=False)

            for gi in range(NGRP + 2):
                if gi < NGRP:
                    stA(gi)
                if 1 <= gi <= NGRP:
                    stC(gi - 1)
                if gi >= 2:
                    stD(gi - 2)
                yield
            self.tt(x[:, :], av[:, :], x[:, :], ALU.add, r=ak + [x], w=[x])
            self.dma(dst[i * 128:(i + 1) * 128, :], x[:, :], r=[x], w=[(dst, i)])
            yield

        for _ in front(0):
            pass
        for it in range(NT):
            active = [uvstage(it)]
            if it + 1 < NT:
                active.append(front(it + 1))
            while active:
                for gen in list(active):
                    try:
                        next(gen)
                    except StopIteration:
                        active.remove(gen)

    def phase_dsa(self, l, src, dst):
        P = self.P
        din = self.din
        jx = l // 2
        w = P.sb("d_w", [128, 8, 1736])
        w_uv = P.sb("d_wuv", [128, 8, 64])
        w_out = P.sb("d_wout", [128, 4, D])
        g = P.sb("d_g", [128, D])
        gkv = P.sb("d_gkv", [128, 128])
        b31 = P.sb("d_b31", [128, 16])
        corrT = P.sb("d_corr", [128, 8, 2, 128])
        self.gload(g, din['mix_norm'], din['mix_norm'][l:l + 1, :])
        self.gload(gkv, din['odd_kv_norm'], din['odd_kv_norm'][jx:jx + 1, :])
        self.gload(b31, din['rel_bias'], din['rel_bias'][31:32, :]) if False else self.dma(
            b31[:, 0:8], din['rel_bias'][31:32, :].broadcast_to([128, 8]), r=[din['rel_bias']], w=[b31])
        self.ts(b31[:, 8:16], b31[:, 0:8], -1.0, None, ALU.mult, None, r=[b31], w=[b31])
        self.dma(corrT[:, :, :, :], din['biasT'][:, :, :, :], r=[din['biasT']], w=[corrT])
        for h in range(8):
            self.act(corrT[:, h, :, :], corrT[:, h, :, :], AF.Exp, r=[corrT, b31], w=[corrT], bias=b31[:, 8 + h:9 + h], scale=1.0)
        self.dma(w[:, :, :], din['odd_w_in'][jx][:, 0:1736].rearrange("(k p) c -> p k c", p=128), r=[din['odd_w_in']], w=[w])
        self.dma(w_uv[:, :, :], din['odd_w_uv'][jx].rearrange("h r e -> r h e"), r=[din['odd_w_uv']], w=[w_uv])
        self.dma(w_out[:, :, :], din['odd_w_out'][jx][0:512, :].rearrange("(k p) c -> p k c", p=128), r=[din['odd_w_out']], w=[w_out])
        cT = P.sb("d_cT", [128, S], R32)
        c_tm = P.sb("d_ctm", [128, NT, 128], R32)
        ikT = P.sb("d_ikT", [64, S], R32)
        xs = self.rot("d_x", [128, D])
        hn = P.sb("d_hn", [128, D])
        hnT = P.sb("d_hnT", [128, 8, 128])
        st = P.sb("d_st", [128, 8])
        craw = P.sb("d_craw", [128, 128])
        qlTs = self.rot("d_qlT", [128, 8, 128], 2, dt=R32)
        iqT = P.sb("d_iqT", [64, 8, 128], R32)
        iw = P.sb("d_iw", [128, 8])
        score = P.sb("d_score", [128, S])
        work = P.sb("d_work", [128, S])
        maskT = P.sb("d_maskT", [128, NT, 128])
        rsb = self.rot("d_r", [128, 512])
        m8 = self.rot("d_m8", [128, 8])
        ETs = self.rot("d_ET", [128, NT, 128], 2, dt=R32)
        latT = P.sb("d_latT", [128, 8, 128])
        zz = P.sb("d_zz", [128, 16])
        yc = P.sb("d_yc", [128, 8, 64])
        ycT = P.sb("d_ycT", [128, 4, 128])
        ones = self.cst('ones')
        cmask = self.cst('cmask')
        CK, CQ, CIK, CIW = 1024, 1152, 1664, 1728
        def projpart(i):
            x = xs[i % 2]
            qlT = qlTs[i % 2]
            nk = (i + 1) * 128
            self.dma(x[:, :], src[i * 128:(i + 1) * 128, :], r=[(src, i)], w=[x])
            self.rms(x, g, hn, st)
            self.transp8(hn, hnT, 4)
            pv, pk = self.psv(6, 1)
            for k in range(8):
                self.mm(pv[:, 0:128], hnT[:, k, :], w[:, k, CK:CK + 128], k == 0, k == 7, r=[hnT, w], w=pk)
            self.copy(craw[:, :], pv[:, 0:128], r=pk, w=[craw])
            self.act(work[:, 0:128], craw[:, :], AF.Square, r=[craw], w=[work, st], accum=st[:, 2:3])
            self.act(st[:, 3:4], st[:, 2:3], AF.Sqrt, r=[st], w=[st], scale=1.0 / 128, bias=self.epsb[:, 0:1])
            self.recip(st[:, 3:4], st[:, 3:4], r=[st], w=[st])
            self.stt(c_tm[:, i, :], craw[:, :], st[:, 3:4], gkv[:, :], ALU.mult, ALU.mult, r=[craw, st, gkv], w=[(c_tm, i)])
            pv7, pk7 = self.psv(7, 1)
            self.tr(pv7[:, 0:128], c_tm[:, i, :].bitcast(F32), r=[(c_tm, i)], w=pk7)
            self.copy(cT[:, i * 128:(i + 1) * 128], pv7[:, 0:128], r=pk7, w=[(cT, i)])
            for k in range(8):
                self.mm(pv[0:64, 0:128], w[:, k, CIK:CIK + 64], hnT[:, k, :], k == 0, k == 7, r=[hnT, w], w=pk)
            self.act(ikT[:, i * 128:(i + 1) * 128], pv[0:64, 0:128], AF.Copy, r=pk, w=[(ikT, i)], scale=0.125)
            for k in range(8):
                self.mm(pv7[:, 0:8], hnT[:, k, :], w[:, k, CIW:CIW + 8], k == 0, k == 7, r=[hnT, w], w=pk7)
            self.act(iw[:, :], pv7[:, 0:8], AF.Copy, r=pk7, w=[iw], scale=8 ** -0.5)
            qv, qk = self.psv(0, 2)
            for h in range(8):
                for k in range(8):
                    self.mm(qv[:, h * 128:(h + 1) * 128], w[:, k, h * 128:(h + 1) * 128], hnT[:, k, :], k == 0, k == 7,
                            r=[hnT, w], w=qk)
            self.act(qlT[:, :, :].rearrange("p a b -> p (a b)"), qv[:, :], AF.Copy, r=qk, w=[qlT], scale=128 ** -0.5)
            iv, ik_ = self.psv(2, 2)
            for h in range(8):
                for k in range(8):
                    self.mm(iv[0:64, h * 128:(h + 1) * 128], w[:, k, CQ + h * 64:CQ + (h + 1) * 64], hnT[:, k, :], k == 0, k == 7,
                            r=[hnT, w], w=ik_)
            self.copy(iqT[:, :, :].rearrange("p a b -> p (a b)"), iv[0:64, :], r=ik_, w=[iqT])
        projpart(0)
        for i in range(NT):
            x = xs[i % 2]
            qlT = qlTs[i % 2]
            nk = (i + 1) * 128
            cnt = 0
            for c0 in range(0, nk, 512):
                cw = min(512, nk - c0)
                for h in range(8):
                    sv, sk = self.psv(4 + cnt % 2, 1)
                    r_ = rsb[cnt % 2]
                    cnt += 1
                    self.mm(sv[:, 0:cw], iqT[:, h, :], ikT[:, c0:c0 + cw], True, True, r=[iqT, ikT], w=sk, fast=True)
                    self.act(r_[:, 0:cw], sv[:, 0:cw], AF.Relu, r=sk, w=[r_])
                    if h == 0:
                        self.ts(score[:, c0:c0 + cw], r_[:, 0:cw], iw[:, 0:1], None, ALU.mult, None, r=[r_, iw], w=[score])
                    else:
                        self.stt(score[:, c0:c0 + cw], r_[:, 0:cw], iw[:, h:h + 1], score[:, c0:c0 + cw], ALU.mult, ALU.add,
                                 r=[r_, iw, score], w=[score])
            if i + 1 < NT:
                projpart(i + 1)
            self.tt(score[:, i * 128:nk], score[:, i * 128:nk], cmask, ALU.add, r=[score, self.C], w=[score])
            if i >= 2:
                cur = score
                for rnd in range(32):
                    m = m8[rnd % 2]
                    P.op('dve', lambda e, m=m, cur=cur, nk=nk: e.max(out=m[:, :], in_=cur[:, 0:nk]), r=[cur], w=[m])
                    if rnd < 31:
                        P.op('dve', lambda e, m=m, cur=cur, nk=nk: e.match_replace(
                            out=work[:, 0:nk], in_to_replace=m[:, :], in_values=cur[:, 0:nk], imm_value=-1e30),
                            r=[cur, m], w=[work])
                        cur = work
                self.ts(work[:, 0:nk], score[:, 0:nk], m8[1][:, 7:8], None, ALU.is_ge, None, r=[score, m8[1]], w=[work])
            else:
                self.ts(work[:, 0:nk], score[:, 0:nk], -1e29, None, ALU.is_ge, None, r=[score], w=[work])
            for kt in range(i + 1):
                b = 6 + (kt // 4) % 2
                mv, mk = self.psv(b, 1)
                self.tr(mv[:, (kt % 4) * 128:(kt % 4 + 1) * 128], work[:, kt * 128:(kt + 1) * 128], r=[work], w=mk)
                if kt % 4 == 3 or kt == i:
                    k0 = (kt // 4) * 4
                    n = kt - k0 + 1
                    self.copy(maskT[:, k0:kt + 1, :].rearrange("p a b -> p (a b)"), mv[:, 0:n * 128], r=mk, w=[maskT], eng='pool' if False else 'act')
            lv, lk = self.psv(0, 4)
            av, ak = self.psv(6, 2)
            zv, zk = self.psv(5, 1)
            def lg(h):
                ET = ETs[h % 2]
                for kt in range(i + 1):
                    self.mm(lv[:, kt * 128:(kt + 1) * 128], cT[:, kt * 128:(kt + 1) * 128], qlT[:, h, :], True, True,
                            r=[cT, qlT], w=lk, fast=True)
                ETf = ET[:, :, :].rearrange("p a b -> p (a b)")
                self.act(ETf[:, 0:nk], lv[:, 0:nk], AF.Exp, r=lk + [b31], w=[ET], bias=b31[:, h:h + 1], scale=1.0)

            def pvh(h):
                ET = ETs[h % 2]
                ETf = ET[:, :, :].rearrange("p a b -> p (a b)")
                ETf32 = ET[:, :, :].bitcast(F32).rearrange("p a b -> p (a b)")
                self.tt(ETf[:, 0:nk], ETf32[:, 0:nk], maskT[:, :, :].rearrange("p a b -> p (a b)")[:, 0:nk], ALU.mult,
                        r=[ET, maskT], w=[ET])
                for kt in range(max(0, i - 1), i + 1):
                    self.tt(ET[:, kt, :], ET[:, kt, :].bitcast(F32), corrT[:, h, i - kt, :], ALU.mult, r=[ET, corrT], w=[ET])
                for kt in range(i + 1):
                    self.mm(av[:, h * 128:(h + 1) * 128], c_tm[:, kt, :], ET[:, kt, :], kt == 0, kt == i, r=[c_tm, ET], w=ak, fast=True)
                    self.mm(zv[:, h:h + 1], ET[:, kt, :].bitcast(F32), ones[:, 0:1], kt == 0, kt == i, r=[ET, self.C], w=zk)

            lg(0)
            for h in range(8):
                if h + 1 < 8:
                    lg(h + 1)
                pvh(h)
            self.copy(latT[:, :, :].rearrange("p a b -> p (a b)"), av[:, :], r=ak, w=[latT])
            self.copy(zz[:, 0:8], zv[:, 0:8], r=zk, w=[zz], eng='dve')
            self.recip(zz[:, 8:16], zz[:, 0:8], r=[zz], w=[zz])
            yv, yk = self.psv(4, 1)
            for h in range(8):
                self.mm(yv[:, h * 64:(h + 1) * 64], latT[:, h, :], w_uv[:, h, :], True, True, r=[latT, w_uv], w=yk)
            self.tt(yc[:, :, :], yv[:, :].rearrange("p (h e) -> p h e", e=64), bcast(zz[:, 8:16], 2, 64), ALU.mult,
                    r=yk + [zz], w=[yc])
            tv_, tk_ = self.psv(5, 1)
            ycf = yc[:, :, :].rearrange("p h e -> p (h e)")
            for k in range(4):
                self.tr(tv_[:, k * 128:(k + 1) * 128], ycf[:, k * 128:(k + 1) * 128], r=[yc], w=tk_)
            self.copy(ycT[:, :, :].rearrange("p a b -> p (a b)"), tv_[:, :], r=tk_, w=[ycT])
            ov, ok = self.psv(0, 2)
            for half in range(2):
                for k in range(4):
                    self.mm(ov[:, half * 512:(half + 1) * 512], ycT[:, k, :], w_out[:, k, half * 512:(half + 1) * 512],
                            k == 0, k == 3, r=[ycT, w_out], w=ok)
            self.tt(x[:, :], ov[:, :], x[:, :], ALU.add, r=ok + [x], w=[x])
            self.dma(dst[i * 128:(i + 1) * 128, :], x[:, :], r=[x], w=[(dst, i)])


    def phase_hgrn(self, l, src, acc):
        P = self.P
        din = self.din
        jx = l // 2
        w = P.sb("h_w", [128, 8, 2048], R32)
        w_out = P.sb("h_wout", [128, 4, D])
        g = P.sb("h_g", [128, D])
        gn = P.sb("h_gn", [128, 512])
        gam = P.sb("h_gam", [128, 4, 512])
        lb = P.sb("h_lb", [128, 512])
        oml = P.sb("h_oml", [128, 512])
        lbT = P.sb("h_lbT", [128, 8])
        tmp = P.sb("h_tmp", [128, 512])
        self.gload(g, din['mix_norm'], din['mix_norm'][l:l + 1, :])
        self.gload(gn, din['odd_hg_norm'], din['odd_hg_norm'][jx:jx + 1, :])
        self.wload_r(w, din['odd_w_in'], din['odd_w_in'][jx][:, 1736:3784])
        self.dma(w_out[:, :, :], din['odd_w_out'][jx][512:1024, :].rearrange("(k p) c -> p k c", p=128), r=[din['odd_w_out']], w=[w_out])
        for ll in range(4):
            self.dma(gam[:, ll, :], din['hgrn_gamma'][ll:ll + 1, :].broadcast_to([128, 512]), r=[din['hgrn_gamma']], w=[(gam, ll)])
        self.act(gam[:, :, :], gam[:, :, :], AF.Exp, r=[gam], w=[gam])
        self.tt(tmp[:, :], gam[:, 0, :], gam[:, 1, :], ALU.add, r=[gam], w=[tmp])
        self.tt(tmp[:, :], tmp[:, :], gam[:, 2, :], ALU.add, r=[gam, tmp], w=[tmp])
        self.tt(tmp[:, :], tmp[:, :], gam[:, 3, :], ALU.add, r=[gam, tmp], w=[tmp])
        self.recip(tmp[:, :], tmp[:, :], r=[tmp], w=[tmp])
        P.op('dve', lambda e: e.memset(lb[:, :], 0.0), w=[lb])
        for ll in range(l):
            self.tt(lb[:, :], lb[:, :], gam[:, ll, :], ALU.add, r=[lb, gam], w=[lb])
        self.tt(lb[:, :], lb[:, :], tmp[:, :], ALU.mult, r=[lb, tmp], w=[lb])
        self.ts(oml[:, :], lb[:, :], -1.0, 1.0, ALU.mult, ALU.add, r=[lb], w=[oml])
        pv, pk = self.psv(0, 1)
        for h in range(4):
            self.tr(pv[:, h * 128:(h + 1) * 128], lb[:, h * 128:(h + 1) * 128], r=[lb], w=pk)
        for h in range(4):
            self.copy(lbT[:, h:h + 1], pv[:, h * 128:h * 128 + 1], r=pk, w=[lbT], eng='dve')
        self.ts(lbT[:, 4:8], lbT[:, 0:4], -1.0, 1.0, ALU.mult, ALU.add, r=[lbT], w=[lbT])
        Sst = [P.sb(f"h_S{j}", [128, 4, 128]) for j in range(2)]
        qt0 = P.sb("h_qt0", [128, 4, 128])
        qt1 = P.sb("h_qt1", [128, 4, 128])
        kh0 = P.sb("h_kh0", [128, 512])
        kh1 = P.sb("h_kh1", [128, 512])
        P.op('pool', lambda e: e.memset(Sst[0][:, :, :], 0.0), w=[Sst[0]])
        P.op('pool', lambda e: e.memset(qt0[:, :, :], 0.0), w=[qt0])
        P.op('pool', lambda e: e.memset(qt1[:, :, :], 0.0), w=[qt1])
        P.op('pool', lambda e: e.memset(kh0[:, :], 0.0), w=[kh0])
        P.op('pool', lambda e: e.memset(kh1[:, :], 0.0), w=[kh1])
        xs = self.rot("h_x", [128, D])
        hn = P.sb("h_hn", [128, D])
        hnT = P.sb("h_hnT", [128, 8, 128], R32)
        st = P.sb("h_st", [128, 16])
        sg = P.sb("h_sg", [128, 512])
        f_tm = P.sb("h_f", [128, 512])
        lf = P.sb("h_lf", [128, 512])
        kk = P.sb("h_kk", [128, 512])
        i_sb = P.sb("h_i", [128, 512])
        sil = P.sb("h_sil", [128, 512])
        sgT = P.sb("h_sgT", [128, 4, 128])
        kkT = P.sb("h_kkT", [128, 4, 128])
        eAT = P.sb("h_eAT", [128, 4, 128])
        enAT = P.sb("h_enAT", [128, 4, 128])
        qtT = P.sb("h_qtT", [128, 4, 128])
        ktT = P.sb("h_ktT", [128, 4, 128])
        a_sb = P.sb("h_a", [128, 512])
        d_sb = P.sb("h_d", [128, 512])
        sc = self.rot("h_sc", [128, 128])
        y_sb = P.sb("h_y", [128, 512])
        yT = P.sb("h_yT", [128, 4, 128])
        acc_t = self.rot("h_acc", [128, D])
        U2 = self.cst('U2')
        B2 = self.cst('B2')
        HQ, HF, HI, HG = 0, 512, 1024, 1536

        def proj_tm(c0, bank):
            pvw, pkw = self.psv(bank, 1)
            for k in range(8):
                self.mm(pvw[:, :], hnT[:, k, :], w[:, k, c0:c0 + 512], k == 0, k == 7, r=[hnT, w], w=pkw, fast=True)
            return pvw, pkw

        def proj_fm(c0, bank):
            pvw, pkw = self.psv(bank, 1)
            for h in range(4):
                for k in range(8):
                    self.mm(pvw[:, h * 128:(h + 1) * 128], w[:, k, c0 + h * 128:c0 + (h + 1) * 128], hnT[:, k, :],
                            k == 0, k == 7, r=[hnT, w], w=pkw, fast=True)
            return pvw, pkw

        for i in range(NT):
            x = xs[i % 2]
            self.dma(x[:, :], src[i * 128:(i + 1) * 128, :], r=[(src, i)], w=[x])
            self.rms(x, g, hn, st)
            self.transp8(hn, hnT, 0)
            pf, kf = proj_tm(HF, 2)
            self.act(sg[:, :], pf[:, :], AF.Sigmoid, r=kf, w=[sg])
            self.tt(f_tm[:, :], sg[:, :], oml[:, :], ALU.mult, r=[sg, oml], w=[f_tm])
            self.tt(f_tm[:, :], f_tm[:, :], lb[:, :], ALU.add, r=[f_tm, lb], w=[f_tm])
            self.act(lf[:, :], f_tm[:, :], AF.Ln, r=[f_tm], w=[lf])
            self.ts(kk[:, :], f_tm[:, :], -1.0, 1.0, ALU.mult, ALU.add, r=[f_tm], w=[kk])
            pi_, ki_ = proj_tm(HI, 3)
            self.copy(i_sb[:, :], pi_[:, :], r=ki_, w=[i_sb])
            pg_, kg_ = proj_tm(HG, 4)
            self.act(sil[:, :], pg_[:, :], AF.Sigmoid, r=kg_, w=[sil])
            self.tt(sil[:, :], sil[:, :], pg_[:, :], ALU.mult, r=[sil] + kg_, w=[sil])
            pq, kq = proj_fm(HQ, 5)
            pfT, kfT = proj_fm(HF, 6)
            self.act(sgT[:, :, :].rearrange("p a b -> p (a b)"), pfT[:, :], AF.Sigmoid, r=kfT, w=[sgT])
            for h in range(4):
                self.ts(kkT[:, h, :], sgT[:, h, :], lbT[:, 4 + h:5 + h], lbT[:, h:h + 1], ALU.mult, ALU.add, r=[sgT, lbT], w=[kkT])
            self.ts(kkT[:, :, :], kkT[:, :, :], -1.0, 1.0, ALU.mult, ALU.add, r=[kkT], w=[kkT])
            pA, kA = self.psv(2, 1)
            self.mm(pA[:, :], U2, lf[:, :], True, True, r=[self.C, lf], w=kA)
            self.copy(a_sb[:, :], pA[:, :], r=kA, w=[a_sb])
            pE, kE = self.psv(3, 1)
            self.mm(pE[:, :], B2, lf[:, :], True, True, r=[self.C, lf], w=kE)
            pAT, kAT = self.psv(4, 1)
            for h in range(4):
                self.mm(pAT[:, h * 128:(h + 1) * 128], lf[:, h * 128:(h + 1) * 128], U2, True, True, r=[self.C, lf], w=kAT)
            self.act(eAT[:, :, :].rearrange("p a b -> p (a b)"), pAT[:, :], AF.Exp, r=kAT, w=[eAT])
            self.act(enAT[:, :, :].rearrange("p a b -> p (a b)"), pAT[:, :], AF.Exp, r=kAT, w=[enAT], scale=-1.0)
            self.tt(qtT[:, :, :].rearrange("p a b -> p (a b)"), pq[:, :], eAT[:, :, :].rearrange("p a b -> p (a b)"), ALU.mult,
                    r=kq + [eAT], w=[qtT])
            self.tt(ktT[:, :, :], kkT[:, :, :], enAT[:, :, :], ALU.mult, r=[kkT, enAT], w=[ktT])
            self.copy(qt0[:, :, 0:64], qtT[:, :, 0:64], r=[qtT], w=[qt0], eng='pool')
            self.copy(qt1[:, :, 64:128], qtT[:, :, 64:128], r=[qtT], w=[qt1], eng='pool')
            self.tt(d_sb[:, :], pE[:, :], a_sb[:, :], ALU.subtract, r=kE + [a_sb], w=[d_sb])
            self.act(d_sb[:, :], d_sb[:, :], AF.Exp, r=[d_sb], w=[d_sb])
            self.tt(kh0[0:64, :], d_sb[0:64, :], kk[0:64, :], ALU.mult, r=[d_sb, kk], w=[kh0])
            self.tt(kh1[64:128, :], d_sb[64:128, :], kk[64:128, :], ALU.mult, r=[d_sb, kk], w=[kh1])
            po, ko = self.psv(7, 1)
            for h in range(4):
                hs = slice(h * 128, (h + 1) * 128)
                S0, S1 = Sst[0], Sst[1]
                ps_, ks_ = self.psv(0, 1)
                self.mm(ps_[:, 0:128], ktT[:, h, :], qtT[:, h, :], True, True, r=[ktT, qtT], w=ks_)
                scb = sc[h % 2]
                self.tt(scb[:, :], ps_[:, 0:128], U2, ALU.mult, r=ks_ + [self.C], w=[scb])
                self.mm(po[:, hs], scb[:, :], i_sb[:, hs], True, False, r=[scb, i_sb], w=ko)
                self.mm(po[:, hs], qt0[:, h, :], S0[:, h, :], False, False, r=[qt0, (S0, h)], w=ko)
                p1, k1 = self.psv(1, 1)
                self.mm(p1[:, 0:128], kh0[:, hs], i_sb[:, hs], True, True, r=[kh0, i_sb], w=k1)
                self.stt(S1[:, h, :], S0[:, h, :], eAT[:, h, 63:64], p1[:, 0:128], ALU.mult, ALU.add, r=[(S0, h), eAT] + k1, w=[(S1, h)])
                self.mm(po[:, hs], qt1[:, h, :], S1[:, h, :], False, True, r=[qt1, (S1, h)], w=ko)
                p2, k2 = self.psv(2, 1)
                self.mm(p2[:, 0:128], kh1[:, hs], i_sb[:, hs], True, True, r=[kh1, i_sb], w=k2)
                self.stt(S0[:, h, :], S1[:, h, :], eAT[:, h, 127:128], p2[:, 0:128], ALU.mult, ALU.add, r=[(S1, h), eAT] + k2, w=[(S0, h)])
            for h in range(4):
                self.act(y_sb[:, h * 128:(h + 1) * 128], po[:, h * 128:(h + 1) * 128], AF.Square, r=ko, w=[y_sb, st], accum=st[:, 4 + h:5 + h])
            self.act(st[:, 8:12], st[:, 4:8], AF.Sqrt, r=[st], w=[st], scale=1.0 / 128, bias=self.epsb[:, 0:1])
            self.recip(st[:, 8:12], st[:, 8:12], r=[st], w=[st])
            for h in range(4):
                self.ts(y_sb[:, h * 128:(h + 1) * 128], po[:, h * 128:(h + 1) * 128], st[:, 8 + h:9 + h], None, ALU.mult, None,
                        r=ko + [st], w=[y_sb])
            self.tt(y_sb[:, :], y_sb[:, :], gn[:, :], ALU.mult, r=[y_sb, gn], w=[y_sb])
            self.tt(y_sb[:, :], y_sb[:, :], sil[:, :], ALU.mult, r=[y_sb, sil], w=[y_sb])
            tv_, tk_ = self.psv(5, 1)
            for k in range(4):
                self.tr(tv_[:, k * 128:(k + 1) * 128], y_sb[:, k * 128:(k + 1) * 128], r=[y_sb], w=tk_)
            self.copy(yT[:, :, :].rearrange("p a b -> p (a b)"), tv_[:, :], r=tk_, w=[yT])
            at = acc_t[i % 2]
            self.dma(at[:, :], acc[i * 128:(i + 1) * 128, :], r=[(acc, i)], w=[at])
            ov, ok = self.psv(2, 2)
            for half in range(2):
                for k in range(4):
                    self.mm(ov[:, half * 512:(half + 1) * 512], yT[:, k, :], w_out[:, k, half * 512:(half + 1) * 512],
                            k == 0, k == 3, r=[yT, w_out], w=ok)
            self.tt(at[:, :], ov[:, :], at[:, :], ALU.add, r=ok + [at], w=[at])
            self.dma(acc[i * 128:(i + 1) * 128, :], at[:, :], r=[at], w=[(acc, i)])

    def phase_copy(self, src, dst):
        xs = self.rot("c_x", [128, D])
        for i in range(NT):
            self.dma(xs[i % 2][:, :], src[i * 128:(i + 1) * 128, :], r=[(src, i)], w=[xs[i % 2]])
            self.dma(dst[i * 128:(i + 1) * 128, :], xs[i % 2][:, :], r=[xs[i % 2]], w=[(dst, i)])

    def phase_final(self, src):
        P = self.P
        g = P.sb("g_f", [128, D])
        self.gload(g, self.din['final_norm'], self.din['final_norm'][0:1, :])
        xs = self.rot("xf", [128, D])
        hns = self.rot("hnf", [128, D])
        sts = self.rot("stf", [128, 4])
        for i in range(NT):
            j = i % 2
            self.dma(xs[j][:, :], src[i * 128:(i + 1) * 128, :], r=[(src, i)], w=[xs[j]])
            self.rms(xs[j], g, hns[j], sts[j])
            self.dma(self.out[i * 128:(i + 1) * 128, :], hns[j][:, :], r=[hns[j]], w=[(self.out, i)])

    def run_phase(self, fn, *a):
        with ExitStack() as pes:
            self.P.pes = pes
            fn(*a)
            self.P.flush()
        self.P.pes = self.es

    def build(self, plan):
        for ph in plan:
            if ph[0] == 'peer':
                self.want_peer[ph[1]] = True
        cur = self.din['x']
        self.run_phase(self.phase_setup)
        for ph in plan:
            if ph[0] == 'xattn':
                self.run_phase(self.phase_xattn, ph[1], cur, self.hA)
                cur = self.hA
            elif ph[0] == 'even':
                self.run_phase(self.phase_even, ph[1], cur, self.hA)
                cur = self.hA
            elif ph[0] == 'peer':
                self.run_phase(self.phase_peer, ph[1], cur, self.hA)
                cur = self.hA
            elif ph[0] == 'dsa':
                other = self.hB if cur is not self.hB else self.hA
                self.run_phase(self.phase_dsa, ph[1], cur, other)
                cur = other
            elif ph[0] == 'hgrn':
                other = self.hB if cur is not self.hB else self.hA
                self.run_phase(self.phase_copy, cur, other)
                self.run_phase(self.phase_hgrn, ph[1], cur, other)
                cur = other
            elif ph[0] == 'odd':
                other = self.hB if cur is not self.hB else self.hA
                self.run_phase(self.phase_dsa, ph[1], cur, other)
                self.run_phase(self.phase_hgrn, ph[1], cur, other)
                cur = other
            elif ph[0] == 'final':
                self.run_phase(self.phase_final, cur)
        return self.nc


FULL_PLAN = []
for _l in range(DEPTH):
    FULL_PLAN.append(('even' if _l % 2 == 0 else 'odd', _l))
    FULL_PLAN.append(('xattn', _l))
    FULL_PLAN.append(('peer', _l))
FULL_PLAN.append(('final',))


def t5_bucket_np(d):
    d = np.maximum(d, 0)
    lr = np.log(np.maximum(d, 1).astype(np.float32) / np.float32(16)) / np.float32(np.log(128 / 16))
    large = 16 + (lr * np.float32(16)).astype(np.int32)
    large = np.minimum(large, 31)
    return np.where(d < 16, d, large)


def prep_inputs(inp):
    f = lambda a: np.ascontiguousarray(np.asarray(a, dtype=np.float32))
    shared = {}
    for k in IN_SHAPES:
        if k in ('x', 'mem'):
            continue
        if k == 'peer_keysT':
            a = np.asarray(inp['peer_keys'], dtype=np.float32)
            shared[k] = f(a.transpose(0, 4, 1, 2, 3).reshape(4, 128, 16, 128))
        elif k == 'even_conv_w':
            a = np.asarray(inp[k]).reshape(2, 3, 4, 128)
            shared[k] = f(a.transpose(0, 3, 2, 1).reshape(2, 128, 12))
        elif k == 'even_pool_scale':
            a = np.asarray(inp[k]).reshape(2, 4, 128)
            shared[k] = f(a.transpose(0, 2, 1))
        elif k == 'peer_uv':
            shared[k] = np.ascontiguousarray(np.concatenate(
                [np.asarray(inp['peer_u'], dtype=np.float32), np.asarray(inp['peer_v'], dtype=np.float32)], axis=-1))
        elif k == 'invc':
            shared[k] = INVC[0]
        elif k == 'biasT':
            rb = np.asarray(inp['rel_bias'], dtype=np.float32)
            ss_, tq_ = np.arange(128)[:, None], np.arange(128)[None, :]
            out = np.zeros((128, 8, 2, 128), dtype=np.float32)
            for dl in (0, 1):
                dist = np.maximum(128 * dl + tq_ - ss_, 0)
                out[:, :, dl, :] = rb[t5_bucket_np(dist)].transpose(0, 2, 1)
            shared[k] = f(out)
        elif k in ('mem_norm', 'final_norm'):
            shared[k] = f(np.asarray(inp[k]).reshape(1, D))
        else:
            shared[k] = f(inp[k])
    return shared


def kernel(plan=None, **inp):
    plan = FULL_PLAN if plan is None else plan
    m = Model()
    nc = m.build(plan)
    shared = prep_inputs(inp)
    shared['consts'] = m.carr
    shared = {k: v for k, v in shared.items() if k in m.din}
    x = np.asarray(inp['x'], dtype=np.float32)
    mem = np.asarray(inp['mem'], dtype=np.float32)
    in_maps = []
    for b in range(8):
        d = dict(shared)
        if 'x' in m.din:
            d['x'] = np.ascontiguousarray(x[b])
        if 'mem' in m.din:
            d['mem'] = np.ascontiguousarray(mem[b])
        in_maps.append(d)
    res = run_bass_kernel_spmd(nc, in_maps, core_ids=list(range(8)))
    m.es.close()
    return np.stack([np.asarray(r["out"], dtype=np.float32) for r in res.results], axis=0)
```
